# Optimizing a Trainium2 kernel written in Bass

```python
import math
import jax, jax.numpy as jnp
from jax import lax
import numpy as np

D_MODEL = 1024
BATCH = 2
SEQ = 8192
DEPTH = 1

CHUNK = 64
N_META = 16
ATT_HEADS = 8
HEAD_DIM = 64
ATT_WIDTH = ATT_HEADS * HEAD_DIM
Q_BLOCK = 128
SSM_WIDTH = D_MODEL // 2
SSM_GROUP = 16
SSM_GROUPS = SSM_WIDTH // SSM_GROUP
SSM_STATE = 64
N_EXPERTS = 32
TOP_K = 4
D_EXPERT = D_MODEL
SWIGLU_LIMIT = 7.0
SWIGLU_ALPHA = 1.702
LN_EPS = 1e-5
DEEPNORM_ALPHA = (2.0 * DEPTH) ** 0.25
DEEPNORM_BETA = (8.0 * DEPTH) ** -0.25

Q_OFF = 0
K_OFF = Q_OFF + ATT_WIDTH
V_OFF = K_OFF + ATT_WIDTH
F_OFF = V_OFF + ATT_WIDTH
U_OFF = F_OFF + ATT_HEADS
GA_OFF = U_OFF + SSM_WIDTH
GB_OFF = GA_OFF + D_MODEL
IN_COLS = GB_OFF + D_MODEL

kernel_name = "hybrid_fox_s5_moe_deepnorm"


def layer_norm(x, g, b):
    xf = x.astype(jnp.float32)
    mu = jnp.mean(xf, axis=-1, keepdims=True)
    var = jnp.mean(jnp.square(xf - mu), axis=-1, keepdims=True)
    return ((xf - mu) * lax.rsqrt(var + LN_EPS) * g + b).astype(x.dtype)


def forgetting_attention(q, k, v, log_f):
    bsz, length, n_h, dh = q.shape
    n_blk = -(-length // Q_BLOCK)
    pad = n_blk * Q_BLOCK - length
    to_bhld = lambda a: jnp.pad(a.transpose(0, 2, 1, 3), ((0, 0), (0, 0), (0, pad), (0, 0)))
    q, k, v = to_bhld(q), to_bhld(k), to_bhld(v)
    cum_f = jnp.cumsum(log_f, axis=1).transpose(0, 2, 1)
    cum_f = jnp.pad(cum_f, ((0, 0), (0, 0), (0, pad)))
    k_pos = jnp.arange(n_blk * Q_BLOCK)
    scale = 1.0 / math.sqrt(dh)

    def one_block(i):
        start = i * Q_BLOCK
        q_blk = lax.dynamic_slice_in_dim(q, start, Q_BLOCK, axis=2)
        f_q = lax.dynamic_slice_in_dim(cum_f, start, Q_BLOCK, axis=2)
        s = jnp.einsum('bhqd,bhkd->bhqk', q_blk, k).astype(jnp.float32) * scale
        s = s + f_q[..., None] - cum_f[:, :, None, :]
        q_pos = start + jnp.arange(Q_BLOCK)
        s = jnp.where(k_pos[None, :] <= q_pos[:, None], s, -jnp.inf)
        p = jax.nn.softmax(s, axis=-1).astype(v.dtype)
        return jnp.einsum('bhqk,bhkd->bhqd', p, v)

    out = lax.map(one_block, jnp.arange(n_blk))
    out = out.transpose(1, 0, 3, 2, 4).reshape(bsz, n_blk * Q_BLOCK, n_h * dh)
    return out[:, :length]


def s5_ssm(u, a_re, a_im, log_dt, b_re, b_im, c_re, c_im, d_skip):
    bsz, length, n_g, n_c = u.shape
    f32 = jnp.float32
    a_re = a_re.astype(f32); a_im = a_im.astype(f32)
    dt = jnp.exp(log_dt.astype(f32))[:, None]
    mag = jnp.exp(a_re * dt)
    ang = a_im * dt
    lb_re, lb_im = mag * jnp.cos(ang), mag * jnp.sin(ang)
    den = a_re * a_re + a_im * a_im
    z_re, z_im = lb_re - 1.0, lb_im
    coef_re = (z_re * a_re + z_im * a_im) / den
    coef_im = (z_im * a_re - z_re * a_im) / den
    b_re = b_re.astype(f32); b_im = b_im.astype(f32)
    bb_re = coef_re[..., None] * b_re - coef_im[..., None] * b_im
    bb_im = coef_re[..., None] * b_im + coef_im[..., None] * b_re
    uf = u.astype(f32)
    bu_re = jnp.einsum('blgc,gpc->lbgp', uf, bb_re)
    bu_im = jnp.einsum('blgc,gpc->lbgp', uf, bb_im)
    a_r = jnp.broadcast_to(lb_re, (length, 1, n_g, lb_re.shape[-1]))
    a_i = jnp.broadcast_to(lb_im, (length, 1, n_g, lb_im.shape[-1]))

    def combine(e1, e2):
        ar1, ai1, br1, bi1 = e1
        ar2, ai2, br2, bi2 = e2
        return (ar1 * ar2 - ai1 * ai2,
                ar1 * ai2 + ai1 * ar2,
                ar2 * br1 - ai2 * bi1 + br2,
                ar2 * bi1 + ai2 * br1 + bi2)

    _, _, x_re, x_im = lax.associative_scan(combine, (a_r, a_i, bu_re, bu_im), axis=0)
    y = (jnp.einsum('lbgp,gcp->blgc', x_re, c_re.astype(f32))
         - jnp.einsum('lbgp,gcp->blgc', x_im, c_im.astype(f32))
         + d_skip.astype(f32) * uf)
    return y.reshape(bsz, length, n_g * n_c)


def hybrid_mixer(h, w_in, b_f, w_up_a, w_up_b, w_o, a_re, a_im, log_dt, b_re, b_im,
                 c_re, c_im, d_skip, w_glu, b_glu):
    bsz, length, _ = h.shape
    proj = h @ w_in
    q = proj[..., Q_OFF:K_OFF].reshape(bsz, length, ATT_HEADS, HEAD_DIM)
    k = proj[..., K_OFF:V_OFF].reshape(bsz, length, ATT_HEADS, HEAD_DIM)
    v = proj[..., V_OFF:F_OFF].reshape(bsz, length, ATT_HEADS, HEAD_DIM)
    log_f = jax.nn.log_sigmoid((proj[..., F_OFF:U_OFF] + b_f).astype(jnp.float32))
    y_a = forgetting_attention(q, k, v, log_f)

    u = proj[..., U_OFF:GA_OFF].reshape(bsz, length, SSM_GROUPS, SSM_GROUP)
    y_b = jax.nn.gelu(s5_ssm(u, a_re, a_im, log_dt, b_re, b_im, c_re, c_im, d_skip)).astype(h.dtype)
    y_b = y_b * jax.nn.sigmoid(y_b @ w_glu + b_glu)

    g_a = jax.nn.sigmoid(proj[..., GA_OFF:GB_OFF])
    g_b = jax.nn.sigmoid(proj[..., GB_OFF:IN_COLS])
    merged = g_a * (y_a @ w_up_a) + g_b * (y_b @ w_up_b)
    return merged @ w_o


def moe_ffn(h, w_router, b_router, w_gate, b_gate, w_up, b_up, w_down, b_down):
    bsz, length, dm = h.shape
    t = h.reshape(-1, dm)
    logits = (t @ w_router + b_router).astype(jnp.float32)
    top_v, top_i = lax.top_k(logits, TOP_K)
    top_w = jax.nn.softmax(top_v, axis=-1)
    comb = jnp.einsum('tk,tke->te', top_w, jax.nn.one_hot(top_i, N_EXPERTS, dtype=jnp.float32))
    out = jnp.zeros(t.shape, jnp.float32)
    for e in range(N_EXPERTS):
        gate = jnp.minimum(t @ w_gate[e] + b_gate[e], SWIGLU_LIMIT)
        up = jnp.clip(t @ w_up[e] + b_up[e], -SWIGLU_LIMIT, SWIGLU_LIMIT)
        act = (up + 1.0) * gate * jax.nn.sigmoid(SWIGLU_ALPHA * gate)
        out = out + comb[:, e:e + 1] * (act @ w_down[e] + b_down[e])
    return out.reshape(bsz, length, dm).astype(h.dtype)


def setup_inputs(seed: int = 0) -> dict:
    key = jax.random.key(seed)
    ks = iter(jax.random.split(key, 40))
    nrm = lambda shape, s: jax.random.normal(next(ks), shape, jnp.float32) * s
    dm, L_ = D_MODEL, DEPTH
    x = nrm((BATCH, SEQ, dm), 1.0)
    meta = nrm((N_META, dm), 1.0)
    ln_in_g = 1.0 + nrm((dm,), 0.02)
    ln_in_b = nrm((dm,), 0.02)
    w_in = nrm((L_, dm, IN_COLS), dm ** -0.5)
    w_in = w_in.at[:, :, V_OFF:F_OFF].multiply(DEEPNORM_BETA)
    b_f = jax.random.uniform(next(ks), (L_, ATT_HEADS), jnp.float32, 1.0, 4.0)
    w_up_a = nrm((L_, ATT_WIDTH, dm), DEEPNORM_BETA * ATT_WIDTH ** -0.5)
    w_up_b = nrm((L_, SSM_WIDTH, dm), DEEPNORM_BETA * SSM_WIDTH ** -0.5)
    w_o = nrm((L_, dm, dm), DEEPNORM_BETA * dm ** -0.5)
    ssm_a_re = -0.5 + nrm((L_, SSM_GROUPS, SSM_STATE), 0.01)
    ssm_a_im = jnp.pi * jnp.arange(SSM_STATE, dtype=jnp.float32) + nrm((L_, SSM_GROUPS, SSM_STATE), 0.01)
    ssm_log_dt = jax.random.uniform(next(ks), (L_, SSM_GROUPS), jnp.float32,
                                    math.log(0.001), math.log(0.1))
    ssm_b_re = nrm((L_, SSM_GROUPS, SSM_STATE, SSM_GROUP), (2 * SSM_GROUP) ** -0.5)
    ssm_b_im = nrm((L_, SSM_GROUPS, SSM_STATE, SSM_GROUP), (2 * SSM_GROUP) ** -0.5)
    ssm_c_re = nrm((L_, SSM_GROUPS, SSM_GROUP, SSM_STATE), (2 * SSM_STATE) ** -0.5)
    ssm_c_im = nrm((L_, SSM_GROUPS, SSM_GROUP, SSM_STATE), (2 * SSM_STATE) ** -0.5)
    ssm_d = nrm((L_, SSM_GROUPS, SSM_GROUP), 1.0)
    w_glu = nrm((L_, SSM_WIDTH, SSM_WIDTH), SSM_WIDTH ** -0.5)
    b_glu = nrm((L_, SSM_WIDTH), 0.02)
    ln1_g = 1.0 + nrm((L_, dm), 0.02)
    ln1_b = nrm((L_, dm), 0.02)
    w_router = nrm((L_, dm, N_EXPERTS), dm ** -0.5)
    b_router = nrm((L_, N_EXPERTS), 0.01)
    w_gate = nrm((L_, N_EXPERTS, dm, D_EXPERT), dm ** -0.5)
    b_gate = nrm((L_, N_EXPERTS, D_EXPERT), 0.02)
    w_up = nrm((L_, N_EXPERTS, dm, D_EXPERT), dm ** -0.5)
    b_up = nrm((L_, N_EXPERTS, D_EXPERT), 0.02)
    w_down = nrm((L_, N_EXPERTS, D_EXPERT, dm), DEEPNORM_BETA * D_EXPERT ** -0.5)
    b_down = nrm((L_, N_EXPERTS, dm), 0.02)
    ln2_g = 1.0 + nrm((L_, dm), 0.02)
    ln2_b = nrm((L_, dm), 0.02)
    return {"x": x, "meta": meta, "ln_in_g": ln_in_g, "ln_in_b": ln_in_b,
            "w_in": w_in, "b_f": b_f, "w_up_a": w_up_a, "w_up_b": w_up_b, "w_o": w_o,
            "ssm_a_re": ssm_a_re, "ssm_a_im": ssm_a_im, "ssm_log_dt": ssm_log_dt,
            "ssm_b_re": ssm_b_re, "ssm_b_im": ssm_b_im, "ssm_c_re": ssm_c_re, "ssm_c_im": ssm_c_im,
            "ssm_d": ssm_d, "w_glu": w_glu, "b_glu": b_glu, "ln1_g": ln1_g, "ln1_b": ln1_b,
            "w_router": w_router, "b_router": b_router, "w_gate": w_gate, "b_gate": b_gate,
            "w_up": w_up, "b_up": b_up, "w_down": w_down, "b_down": b_down,
            "ln2_g": ln2_g, "ln2_b": ln2_b}


def reference(x, meta, ln_in_g, ln_in_b, w_in, b_f, w_up_a, w_up_b, w_o,
              ssm_a_re, ssm_a_im, ssm_log_dt, ssm_b_re, ssm_b_im, ssm_c_re, ssm_c_im,
              ssm_d, w_glu, b_glu, ln1_g, ln1_b, w_router, b_router, w_gate, b_gate,
              w_up, b_up, w_down, b_down, ln2_g, ln2_b):
    bsz = x.shape[0]
    meta_b = jnp.broadcast_to(meta[None].astype(x.dtype), (bsz, N_META, D_MODEL))
    h = jnp.concatenate([meta_b, x], axis=1)
    h = layer_norm(h, ln_in_g, ln_in_b)
    for l in range(DEPTH):
        mix = hybrid_mixer(h, w_in[l], b_f[l], w_up_a[l], w_up_b[l], w_o[l],
                           ssm_a_re[l], ssm_a_im[l], ssm_log_dt[l], ssm_b_re[l], ssm_b_im[l],
                           ssm_c_re[l], ssm_c_im[l], ssm_d[l], w_glu[l], b_glu[l])
        h = layer_norm(DEEPNORM_ALPHA * h + mix, ln1_g[l], ln1_b[l])
        ffn = moe_ffn(h, w_router[l], b_router[l], w_gate[l], b_gate[l],
                      w_up[l], b_up[l], w_down[l], b_down[l])
        h = layer_norm(DEEPNORM_ALPHA * h + ffn, ln2_g[l], ln2_b[l])
    return h[:, N_META:]
```

```python
import math
from contextlib import ExitStack

import numpy as np
import concourse.bass as bass
import concourse.mybir as mybir
from concourse.bass_utils import run_bass_kernel_spmd

F32 = mybir.dt.float32
BF16 = mybir.dt.bfloat16
AF = mybir.ActivationFunctionType
ALU = mybir.AluOpType

D_MODEL = 1024
SEQ = 8192
N_META = 16
N_EXPERTS = 32
LN_EPS = 1e-5
DN_ALPHA = 2.0 ** 0.25
SW_LIMIT = 7.0
SW_ALPHA = 1.702
Q_OFF, K_OFF, V_OFF, F_OFF, U_OFF, GA_OFF, GB_OFF = 0, 512, 1024, 1536, 1544, 2056, 3080


class Sched:
    ENGS = ("pe", "act", "dve", "pool", "sp")
    SEM_ROLL = 20000

    def __init__(self, nc, stack):
        self.nc = nc
        self.stack = stack
        self.eng_obj = {"pe": nc.tensor, "act": nc.scalar, "dve": nc.vector,
                        "pool": nc.gpsimd, "sp": nc.sync}
        self.eng_sems = {e: [stack.enter_context(nc.semaphore(f"s_{e}0"))] for e in self.ENGS}
        self.eng_cnt = {e: 0 for e in self.ENGS}
        self.streams = {}
        self.nsem = 0
        self.fz = {e: stack.enter_context(nc.sbuf_tensor(f"fz_{e}", [128, 2], F32)) for e in ("act", "dve", "pool")}
        self.reset()

    def reset(self):
        self.ops = []
        self.lastw = {}
        self.readers = {}

    def stream(self, name, depth):
        if name not in self.streams:
            sems = [self.stack.enter_context(self.nc.semaphore(f"d_{name}{i}")) for i in range(depth)]
            self.streams[name] = dict(sems=sems, cnt=[0] * depth, k=0)
        return name

    def add(self, eng, fn, r=(), w=(), dma=None, fence=False):
        idx = len(self.ops)
        deps = set()
        for k in r:
            if k in self.lastw:
                deps.add(self.lastw[k])
        for k in w:
            if k in self.lastw:
                deps.add(self.lastw[k])
            for x in self.readers.get(k, ()):
                deps.add(x)
        deps.discard(idx)
        for k in r:
            self.readers.setdefault(k, []).append(idx)
        for k in w:
            self.lastw[k] = idx
            self.readers[k] = []
        self.ops.append(dict(eng=eng, fn=fn, deps=deps, dma=dma, used=False, sig=None, fence=fence))
        for d in deps:
            self.ops[d]["used"] = True
        return idx

    def emit(self, final_wait_dma=True):
        nc = self.nc
        ops = self.ops
        for op in ops:
            if op["dma"] is not None and len(self.streams[op["dma"]]["sems"]) == 0:
                self.nsem += 1
                sem = self.stack.enter_context(nc.semaphore(f"d_one{self.nsem}"))
                op["sig"] = (sem, 16, 16)
            elif op["dma"] is not None:
                st = self.streams[op["dma"]]
                k = st["k"] % len(st["sems"])
                st["k"] += 1
                st["cnt"][k] += 16
                op["sig"] = (st["sems"][k], st["cnt"][k], 16)
            elif op["used"]:
                e = op["eng"]
                if self.eng_cnt[e] >= self.SEM_ROLL:
                    self.eng_sems[e].append(self.stack.enter_context(nc.semaphore(f"s_{e}{len(self.eng_sems[e])}")))
                    self.eng_cnt[e] = 0
                self.eng_cnt[e] += 1
                op["sig"] = (self.eng_sems[e][-1], self.eng_cnt[e], 1)
        per_eng = {e: [] for e in self.ENGS}
        for i, op in enumerate(ops):
            waits = []
            for d in sorted(op["deps"]):
                dop = ops[d]
                if dop["dma"] is None and dop["eng"] == "pe" and op["eng"] == "pe":
                    continue
                waits.append(dop["sig"])
            per_eng[op["eng"]].append((waits, op))
        pending = [op["sig"] for op in ops if op["dma"] is not None]
        with nc.Block() as blk:
            def make(ename):
                lst = per_eng[ename]

                def body(eng):
                    known = {}
                    for waits, op in lst:
                        for (sem, val, _inc) in waits:
                            key = id(sem)
                            if known.get(key, (None, 0))[1] >= val:
                                continue
                            eng.wait_ge(sem, val)
                            known[key] = (sem, val)
                        ins = op["fn"](eng)
                        if op["sig"] is not None:
                            if op["fence"] and ename in self.fz:
                                fz = self.fz[ename]
                                if ename == "act":
                                    ins = eng.copy(out=fz[:, 0:1], in_=fz[:, 1:2])
                                else:
                                    ins = eng.memset(fz[:, 0:1], 0.0)
                            ins.then_inc(op["sig"][0], op["sig"][2])
                    if ename == "sp" and final_wait_dma:
                        best = {}
                        for (sem, val, _inc) in pending:
                            if best.get(id(sem), (None, 0))[1] < val:
                                best[id(sem)] = (sem, val)
                        for sem, val in best.values():
                            if known.get(id(sem), (None, 0))[1] < val:
                                eng.wait_ge(sem, val)
                return body
            blk.tensor(make("pe"))
            blk.scalar(make("act"))
            blk.vector(make("dve"))
            blk.gpsimd(make("pool"))
            blk.sync(make("sp"))
        self.reset()


def _bc(ap, shape):
    return ap.to_broadcast(shape)


def _ln_tile(S, tile_ap, st6, mv, rstd, gB, bB, key, tag):
    S.add("dve", lambda e: e.bn_stats(out=st6[:, 0, :], in_=tile_ap[:, 0:512]), r=[key], w=[tag + "st0"])
    S.add("dve", lambda e: e.bn_stats(out=st6[:, 1, :], in_=tile_ap[:, 512:1024]), r=[key], w=[tag + "st1"])
    S.add("dve", lambda e: e.bn_aggr(out=mv[:], in_=st6[:]), r=[tag + "st0", tag + "st1"], w=[tag + "mv"], fence=True)
    S.add("dve", lambda e: e.tensor_scalar(out=rstd[:], in0=mv[:, 1:2], scalar1=LN_EPS, scalar2=None,
                                           op0=ALU.add), r=[tag + "mv"], w=[tag + "rs"], fence=True)
    S.add("act", lambda e: e.sqrt(out=rstd[:], in_=rstd[:]), r=[tag + "rs"], w=[tag + "rs"], fence=True)
    S.add("dve", lambda e: e.reciprocal(out=rstd[:], in_=rstd[:]), r=[tag + "rs"], w=[tag + "rs"], fence=True)
    S.add("dve", lambda e: e.tensor_scalar(out=tile_ap, in0=tile_ap, scalar1=mv[:, 0:1], scalar2=rstd[:, 0:1],
                                           op0=ALU.subtract, op1=ALU.mult), r=[key, tag + "mv", tag + "rs"], w=[key])
    S.add("dve", lambda e: e.tensor_tensor(out=tile_ap, in0=tile_ap, in1=gB, op=ALU.mult), r=[key, "lnB"], w=[key])
    S.add("dve", lambda e: e.tensor_tensor(out=tile_ap, in0=tile_ap, in1=bB, op=ALU.add), r=[key, "lnB"], w=[key])


def build_p2(NTOK=2048, NE=32):
    NT = NTOK // 128
    NG = NTOK // 512
    nc = bass.Bass("TRN2", target_bir_lowering=False)

    def din(name, shape, dtype=F32):
        return nc.dram_tensor(name, shape, dtype, kind="ExternalInput").ap()
    x = din("x", [NTOK, 1024])
    yaT = din("yaT", [512, NTOK])
    ysT = din("ysT", [512, NTOK])
    lnp = din("lnp", [6, 1024])
    wg = din("wg", [1024, 2048])
    wo = din("wo", [1024, 1024])
    wua = din("wua", [512, 1024])
    wub = din("wub", [512, 1024])
    wglu = din("wglu", [512, 512])
    bglu = din("bglu", [128, 4])
    wr = din("wr", [1024, 32])
    br = din("br", [1, 32])
    wgate = din("wgate", [NE, 1024, 1024])
    wup = din("wup", [NE, 1024, 1024])
    wdown = din("wdown", [NE, 1024, 1024])
    bgu = din("bgu", [128, 2 * NE * 8])
    bdown = din("bdown", [N_EXPERTS, 1024])
    ident = din("ident", [128, 128])
    out = nc.dram_tensor("out", [NTOK, 1024], F32, kind="ExternalOutput").ap()

    with ExitStack() as stack:
        S = Sched(nc, stack)

        def sb(name, shape, dtype):
            return stack.enter_context(nc.sbuf_tensor(name, shape, dtype))

        def psum(name, shape, dtype):
            return stack.enter_context(nc.psum_tensor(name, shape, dtype))
        hres = sb("hres", [128, NT, 1024], F32)
        identb = sb("identb", [128, 128], BF16)
        ident32 = sb("ident32", [128, 128], F32)
        lnB = sb("lnB", [128, 4, 1024], F32)
        comb = sb("comb", [128, NT, 32], F32)
        st6 = sb("st6", [128, 2, 6], F32)
        mv = sb("mv", [128, 2], F32)
        rstd = sb("rstd", [128, 1], F32)
        pss = [psum(f"ps{i}", [128, 512], F32) for i in range(6)]
        pT = [psum(f"pT{i}", [128, 1024], BF16) for i in range(2)]
        S.stream("x", 2)
        S.stream("misc", 0)
        S.stream("gw", 3)
        S.stream("yin", 0)
        S.stream("wr", 8)
        S.stream("out", 2)

        with ExitStack() as st2:
            def sb2(name, shape, dtype):
                return st2.enter_context(nc.sbuf_tensor(name, shape, dtype))
            xb = [sb2(f"xb{i}", [128, 1024], BF16) for i in range(2)]
            hT = sb2("hT", [128, 8, 512], BF16)
            ya = sb2("ya", [128, 4, 512], BF16)
            ys32 = sb2("ys32", [128, 4, 512], F32)
            gtmp = sb2("gtmp", [128, 512], F32)
            sgm = sb2("sgm", [128, 512], F32)
            yb = sb2("yb", [128, 4, 512], BF16)
            yb2 = sb2("yb2", [128, 4, 512], BF16)
            merged = sb2("merged", [128, 8, 512], BF16)
            gw = [sb2(f"gw{i}", [128, 8, 512], BF16) for i in range(3)]
            wuat = sb2("wuat", [128, 4, 1024], BF16)
            wubt = sb2("wubt", [128, 4, 1024], BF16)
            wglut = sb2("wglut", [128, 4, 512], BF16)
            bglut = sb2("bglut", [128, 4], F32)
            gat = [sb2(f"gat{i}", [128, 512], F32) for i in range(2)]
            gbt = [sb2(f"gbt{i}", [128, 512], F32) for i in range(2)]

            S.add("sp", lambda e: e.dma_start(out=ident32[:], in_=ident[:, :]), w=["ident32"], dma="misc")
            S.add("dve", lambda e: e.tensor_copy(out=identb[:], in_=ident32[:]), r=["ident32"], w=["identb"])
            for i in range(4):
                S.add("sp", lambda e, i=i: e.dma_start(out=lnB[:, i, :], in_=lnp[i:i + 1, :].partition_broadcast(128)),
                      w=["lnB"], dma="misc")
            S.add("sp", lambda e: e.dma_start(out=bglut[:], in_=bglu[:, :]), w=["bglut"], dma="misc")
            S.add("pool", lambda e: e.dma_start(out=wuat[:], in_=wua.rearrange("(c p) n -> p c n", p=128)), w=["wuat"], dma="misc")
            S.add("pool", lambda e: e.dma_start(out=wubt[:], in_=wub.rearrange("(c p) n -> p c n", p=128)), w=["wubt"], dma="misc")
            S.add("pool", lambda e: e.dma_start(out=wglut[:], in_=wglu.rearrange("(c p) n -> p c n", p=128)), w=["wglut"], dma="misc")
            gwk = 0
            for tg in range(NG):
                c0 = tg * 512
                for ti in range(4):
                    tt = tg * 4 + ti
                    hk = ("hres", tt)
                    S.add("sp", lambda e, tt=tt: e.dma_start(out=hres[:, tt, :], in_=x[tt * 128:(tt + 1) * 128, :]),
                          w=[hk], dma="x")
                    _ln_tile(S, hres[:, tt, :], st6, mv, rstd, lnB[:, 0, :], lnB[:, 1, :], hk, "ln")
                    b = tt % 2
                    S.add("act", lambda e, tt=tt, b=b: e.copy(out=xb[b][:], in_=hres[:, tt, :]), r=[hk], w=[("xb", b)])

                    def tr(e, b=b):
                        ins = None
                        for kc in range(8):
                            ins = e.transpose(out=pT[b][:, kc * 128:(kc + 1) * 128], in_=xb[b][:, kc * 128:(kc + 1) * 128],
                                              identity=identb[:])
                        return ins
                    S.add("pe", tr, r=[("xb", b), "identb"], w=[("pT", b)])
                    S.add("act", lambda e, b=b, ti=ti: e.copy(out=hT[:, :, ti * 128:(ti + 1) * 128],
                                                              in_=pT[b][:, :].rearrange("p (k t) -> p k t", k=8)),
                          r=[("pT", b)], w=["hT"])
                S.add("pool", lambda e, c0=c0: e.dma_start(out=ya[:], in_=yaT.rearrange("(c p) t -> p c t", p=128)[:, :, c0:c0 + 512]),
                      w=["ya"], dma="yin")
                S.add("sp", lambda e, c0=c0: e.dma_start(out=ys32[:], in_=ysT.rearrange("(c p) t -> p c t", p=128)[:, :, c0:c0 + 512]),
                      w=["ys32"], dma="yin")
                for c in range(4):
                    S.add("dve", lambda e, c=c: e.tensor_tensor(out=gtmp[:], in0=ys32[:, c, :], in1=ys32[:, c, :], op=ALU.mult), r=["ys32"], w=["gtmp"])
                    S.add("dve", lambda e: e.tensor_scalar(out=gtmp[:], in0=gtmp[:], scalar1=0.044715, scalar2=1.0,
                                                           op0=ALU.mult, op1=ALU.add), r=["gtmp"], w=["gtmp"])
                    S.add("dve", lambda e, c=c: e.tensor_tensor(out=gtmp[:], in0=gtmp[:], in1=ys32[:, c, :], op=ALU.mult), r=["gtmp", "ys32"], w=["gtmp"])
                    S.add("act", lambda e: e.activation(out=sgm[:], in_=gtmp[:], func=AF.Sigmoid, scale=1.5957691216), r=["gtmp"], w=["sgm"])
                    S.add("dve", lambda e, c=c: e.tensor_tensor(out=yb[:, c, :], in0=ys32[:, c, :], in1=sgm[:], op=ALU.mult), r=["ys32", "sgm"], w=["yb"])
                for cc in range(4):
                    pk = cc % 2

                    def mm(e, cc=cc, pk=pk):
                        ins = None
                        for c in range(4):
                            ins = e.matmul(pss[pk][:, :], wglut[:, c, cc * 128:(cc + 1) * 128], yb[:, c, :],
                                           start=(c == 0), stop=(c == 3))
                        return ins
                    S.add("pe", mm, r=["wglut", "yb"], w=[("ps", pk)])
                    S.add("act", lambda e, cc=cc, pk=pk: e.activation(out=sgm[:], in_=pss[pk][:, :], func=AF.Sigmoid,
                                                                      bias=bglut[:, cc:cc + 1], scale=1.0),
                          r=[("ps", pk), "bglut"], w=["sgm"])
                    S.add("dve", lambda e, cc=cc: e.tensor_tensor(out=yb2[:, cc, :], in0=yb[:, cc, :], in1=sgm[:], op=ALU.mult),
                          r=["yb", "sgm"], w=["yb2"])
                for j in range(4):
                    slot = gwk % 3
                    gwk += 1
                    S.add("pool", lambda e, j=j, slot=slot: e.dma_start(
                        out=gw[slot][:], in_=wg.rearrange("(k p) n -> p k n", p=128)[:, :, j * 512:(j + 1) * 512]),
                        w=[("gw", slot)], dma="gw")
                    for dcl in range(2):
                        dc = 2 * j + dcl
                        tb = dc % 2

                        def mmg(e, slot=slot, dcl=dcl, off=0, pk=0):
                            ins = None
                            for kc in range(8):
                                ins = e.matmul(pss[pk][:, :], gw[slot][:, kc, dcl * 256 + off:dcl * 256 + off + 128], hT[:, kc, :],
                                               start=(kc == 0), stop=(kc == 7))
                            return ins
                        S.add("pe", lambda e, f=mmg: f(e, off=0, pk=0), r=[("gw", slot), "hT"], w=[("ps", 0)])
                        S.add("pe", lambda e, f=mmg: f(e, off=128, pk=1), r=[("gw", slot), "hT"], w=[("ps", 1)])

                        def mmu(e, dc=dc, wt=None, src=None, pk=2):
                            ins = None
                            for c in range(4):
                                ins = e.matmul(pss[pk][:, :], wt[:, c, dc * 128:(dc + 1) * 128], src[:, c, :],
                                               start=(c == 0), stop=(c == 3))
                            return ins
                        S.add("pe", lambda e, f=mmu: f(e, wt=wuat, src=ya, pk=2), r=["wuat", "ya"], w=[("ps", 2)])
                        S.add("pe", lambda e, f=mmu: f(e, wt=wubt, src=yb2, pk=3), r=["wubt", "yb2"], w=[("ps", 3)])
                        S.add("act", lambda e, tb=tb: e.activation(out=gat[tb][:], in_=pss[0][:, :], func=AF.Sigmoid),
                              r=[("ps", 0)], w=[("gat", tb)])
                        S.add("act", lambda e, tb=tb: e.activation(out=gbt[tb][:], in_=pss[1][:, :], func=AF.Sigmoid),
                              r=[("ps", 1)], w=[("gbt", tb)])
                        S.add("dve", lambda e, tb=tb: e.tensor_tensor(out=gat[tb][:], in0=gat[tb][:], in1=pss[2][:, :], op=ALU.mult),
                              r=[("gat", tb), ("ps", 2)], w=[("gat", tb)])
                        S.add("dve", lambda e, tb=tb: e.tensor_tensor(out=gbt[tb][:], in0=gbt[tb][:], in1=pss[3][:, :], op=ALU.mult),
                              r=[("gbt", tb), ("ps", 3)], w=[("gbt", tb)])
                        S.add("dve", lambda e, tb=tb, dc=dc: e.tensor_tensor(out=merged[:, dc, :], in0=gat[tb][:], in1=gbt[tb][:], op=ALU.add),
                              r=[("gat", tb), ("gbt", tb)], w=["merged"])
                for half in range(2):
                    slot = gwk % 3
                    gwk += 1
                    S.add("pool", lambda e, half=half, slot=slot: e.dma_start(
                        out=gw[slot][:], in_=wo.rearrange("(k p) n -> p k n", p=128)[:, :, half * 512:(half + 1) * 512]),
                        w=[("gw", slot)], dma="gw")
                    for ti in range(4):
                        tt = tg * 4 + ti
                        pk = 4 + (ti % 2)

                        def mmo(e, slot=slot, ti=ti, pk=pk):
                            ins = None
                            for kc in range(8):
                                ins = e.matmul(pss[pk][:, :], merged[:, kc, ti * 128:(ti + 1) * 128], gw[slot][:, kc, :],
                                               start=(kc == 0), stop=(kc == 7))
                            return ins
                        S.add("pe", mmo, r=["merged", ("gw", slot)], w=[("ps", pk)])
                        S.add("dve", lambda e, tt=tt, half=half, pk=pk: e.scalar_tensor_tensor(
                            out=hres[:, tt, half * 512:(half + 1) * 512], in0=hres[:, tt, half * 512:(half + 1) * 512],
                            scalar=DN_ALPHA, in1=pss[pk][:, :], op0=ALU.mult, op1=ALU.add),
                            r=[("hres", tt), ("ps", pk)], w=[("hres", tt)])
                for ti in range(4):
                    tt = tg * 4 + ti
                    _ln_tile(S, hres[:, tt, :], st6, mv, rstd, lnB[:, 2, :], lnB[:, 3, :], ("hres", tt), "ln")
            S.emit()

        x1T = sb("x1T", [128, 8, NTOK], BF16)
        with ExitStack() as st2:
            def sb2(name, shape, dtype):
                return st2.enter_context(nc.sbuf_tensor(name, shape, dtype))
            xb = [sb2(f"xc{i}", [128, 1024], BF16) for i in range(2)]
            wrt = sb2("wrt", [128, 8, 32], BF16)
            brB = sb2("brB", [128, 32], F32)
            bdn = sb2("bdn", [N_EXPERTS, 1024], BF16)
            lg = sb2("lg", [128, 32], F32)
            ex = sb2("ex", [128, 32], F32)
            msk = sb2("msk", [128, 32], F32)
            m8 = sb2("m8", [128, 8], F32)
            negm = sb2("negm", [128, 1], F32)
            ssum = sb2("ssum", [128, 1], F32)
            combTb = sb2("combTb", [32, 128], BF16)
            S.add("pool", lambda e: e.dma_start(out=wrt[:], in_=wr.rearrange("(k p) n -> p k n", p=128)), w=["wrt"], dma="misc")
            S.add("pool", lambda e: e.dma_start(out=bdn[:], in_=bdown[:, :]), w=["bdn"], dma="misc")
            S.add("sp", lambda e: e.dma_start(out=brB[:], in_=br[0:1, :].partition_broadcast(128)), w=["brB"], dma="misc")
            for tt in range(NT):
                b = tt % 2
                hk = ("hres", tt)
                S.add("act", lambda e, tt=tt, b=b: e.copy(out=xb[b][:], in_=hres[:, tt, :]), r=[hk], w=[("xb", b)])

                def tr(e, b=b):
                    ins = None
                    for kc in range(8):
                        ins = e.transpose(out=pT[b][:, kc * 128:(kc + 1) * 128], in_=xb[b][:, kc * 128:(kc + 1) * 128],
                                          identity=identb[:])
                    return ins
                S.add("pe", tr, r=[("xb", b)], w=[("pT", b)])
                S.add("act", lambda e, b=b, tt=tt: e.copy(out=x1T[:, :, tt * 128:(tt + 1) * 128],
                                                          in_=pT[b][:, :].rearrange("p (k t) -> p k t", k=8)),
                      r=[("pT", b)], w=[("x1T", tt)])

                def mmr(e, tt=tt):
                    ins = None
                    for kc in range(8):
                        ins = e.matmul(pss[0][:, 0:32], x1T[:, kc, tt * 128:(tt + 1) * 128], wrt[:, kc, :],
                                       start=(kc == 0), stop=(kc == 7))
                    return ins
                S.add("pe", mmr, r=[("x1T", tt), "wrt"], w=[("ps", 0)])
                S.add("dve", lambda e: e.tensor_tensor(out=lg[:], in0=pss[0][:, 0:32], in1=brB[:], op=ALU.add),
                      r=[("ps", 0), "brB"], w=["lg"])
                S.add("dve", lambda e: e.max(out=m8[:], in_=lg[:]), r=["lg"], w=["m8"])
                S.add("dve", lambda e: e.tensor_scalar(out=msk[:], in0=lg[:], scalar1=m8[:, 3:4], scalar2=None, op0=ALU.is_ge),
                      r=["lg", "m8"], w=["msk"])
                S.add("dve", lambda e: e.tensor_scalar(out=negm[:], in0=m8[:, 0:1], scalar1=-1.0, scalar2=None, op0=ALU.mult),
                      r=["m8"], w=["negm"])
                S.add("act", lambda e: e.activation(out=ex[:], in_=lg[:], func=AF.Exp, bias=negm[:, 0:1], scale=1.0),
                      r=["lg", "negm"], w=["ex"])
                S.add("dve", lambda e: e.tensor_tensor(out=ex[:], in0=ex[:], in1=msk[:], op=ALU.mult), r=["ex", "msk"], w=["ex"])
                S.add("dve", lambda e: e.reduce_sum(out=ssum[:], in_=ex[:], axis=mybir.AxisListType.X), r=["ex"], w=["ssum"])
                S.add("dve", lambda e: e.reciprocal(out=ssum[:], in_=ssum[:]), r=["ssum"], w=["ssum"])
                S.add("dve", lambda e, tt=tt: e.tensor_scalar(out=comb[:, tt, :], in0=ex[:], scalar1=ssum[:, 0:1], scalar2=None, op0=ALU.mult),
                      r=["ex", "ssum"], w=[("comb", tt)])
                S.add("pe", lambda e, tt=tt: e.transpose(out=pss[1][0:32, 0:128], in_=comb[:, tt, :], identity=ident32[:]),
                      r=[("comb", tt)], w=[("ps", 1)])
                S.add("act", lambda e: e.copy(out=combTb[:], in_=pss[1][0:32, 0:128]), r=[("ps", 1)], w=["combTb"])
                for half in range(2):
                    pk = 2 + half
                    S.add("pe", lambda e, half=half, pk=pk: e.matmul(pss[pk][:, :], combTb[:, :], bdn[:, half * 512:(half + 1) * 512],
                                                                     start=True, stop=True),
                          r=["combTb", "bdn"], w=[("ps", pk)])
                    S.add("dve", lambda e, tt=tt, half=half, pk=pk: e.scalar_tensor_tensor(
                        out=hres[:, tt, half * 512:(half + 1) * 512], in0=hres[:, tt, half * 512:(half + 1) * 512],
                        scalar=DN_ALPHA, in1=pss[pk][:, :], op0=ALU.mult, op1=ALU.add),
                        r=[hk, ("ps", pk)], w=[hk])
            S.emit()

        with ExitStack() as st2:
            def sb2(name, shape, dtype):
                return st2.enter_context(nc.sbuf_tensor(name, shape, dtype))
            R = 8
            ring = [sb2(f"ring{i}", [128, 8, 256], BF16) for i in range(R)]
            actT = sb2("actT", [128, 8, NTOK], BF16)
            bgut = sb2("bgut", [128, 2 * NE * 8], F32)
            gs = [sb2(f"gs{i}", [128, 512], F32) for i in range(2)]
            sg = [sb2(f"sg{i}", [128, 512], F32) for i in range(2)]
            uu = [sb2(f"uu{i}", [128, 512], F32) for i in range(2)]
            S.add("sp", lambda e: e.dma_start(out=bgut[:], in_=bgu[:, :]), w=["bgut"], dma="misc")
            rk = 0

            def load(wsrc, e_, q):
                nonlocal rk
                slot = rk % R
                rk += 1
                S.add("pool", lambda e, slot=slot: e.dma_start(
                    out=ring[slot][:], in_=wsrc[e_].rearrange("(k p) n -> p k n", p=128)[:, :, q * 256:(q + 1) * 256]),
                    w=[("ring", slot)], dma="wr")
                return slot
            tcount = 0
            for ex_ in range(NE):
                for q in range(4):
                    sg_ = load(wgate, ex_, q)
                    su_ = load(wup, ex_, q)
                    for fcl in range(2):
                        fc = 2 * q + fcl
                        bgcol = (0 * NE + ex_) * 8 + fc
                        bucol = (1 * NE + ex_) * 8 + fc
                        for tg in range(NG):
                            tb = tcount % 2
                            tcount += 1
                            pg, pu = tb, 2 + tb

                            def mmgu(e, slot=None, pk=None, fcl=fcl, tg=tg):
                                ins = None
                                for kc in range(8):
                                    ins = e.matmul(pss[pk][:, :], ring[slot][:, kc, fcl * 128:(fcl + 1) * 128],
                                                   x1T[:, kc, tg * 512:(tg + 1) * 512], start=(kc == 0), stop=(kc == 7))
                                return ins
                            S.add("pe", lambda e, f=mmgu, s=sg_, pk=pg: f(e, slot=s, pk=pk), r=[("ring", sg_)], w=[("ps", pg)])
                            S.add("pe", lambda e, f=mmgu, s=su_, pk=pu: f(e, slot=s, pk=pk), r=[("ring", su_)], w=[("ps", pu)])
                            S.add("dve", lambda e, tb=tb, pg=pg, c=bgcol: e.tensor_scalar(
                                out=gs[tb][:], in0=pss[pg][:, :], scalar1=bgut[:, c:c + 1], scalar2=SW_LIMIT, op0=ALU.add, op1=ALU.min),
                                r=[("ps", pg), "bgut"], w=[("gs", tb)])
                            S.add("act", lambda e, tb=tb: e.activation(out=sg[tb][:], in_=gs[tb][:], func=AF.Sigmoid, scale=SW_ALPHA),
                                  r=[("gs", tb)], w=[("sg", tb)])
                            S.add("dve", lambda e, tb=tb, pu=pu, c=bucol: e.tensor_scalar(
                                out=uu[tb][:], in0=pss[pu][:, :], scalar1=bgut[:, c:c + 1], scalar2=SW_LIMIT, op0=ALU.add, op1=ALU.min),
                                r=[("ps", pu), "bgut"], w=[("uu", tb)])
                            S.add("dve", lambda e, tb=tb: e.tensor_scalar(
                                out=uu[tb][:], in0=uu[tb][:], scalar1=-SW_LIMIT, scalar2=1.0, op0=ALU.max, op1=ALU.add),
                                r=[("uu", tb)], w=[("uu", tb)])
                            S.add("dve", lambda e, tb=tb: e.tensor_tensor(out=gs[tb][:], in0=gs[tb][:], in1=sg[tb][:], op=ALU.mult),
                                  r=[("gs", tb), ("sg", tb)], w=[("gs", tb)])
                            S.add("dve", lambda e, tb=tb, fc=fc, tg=tg: e.tensor_tensor(
                                out=actT[:, fc, tg * 512:(tg + 1) * 512], in0=gs[tb][:], in1=uu[tb][:], op=ALU.mult),
                                r=[("gs", tb), ("uu", tb)], w=[("actT", tg)])
                for q in range(4):
                    sd_ = load(wdown, ex_, q)
                    for tt in range(NT):
                        pk = 4 + (tt % 2)

                        def mmd(e, slot=sd_, tt=tt, pk=pk):
                            ins = None
                            for fc in range(8):
                                ins = e.matmul(pss[pk][:, 0:256], actT[:, fc, tt * 128:(tt + 1) * 128], ring[slot][:, fc, :],
                                               start=(fc == 0), stop=(fc == 7))
                            return ins
                        S.add("pe", mmd, r=[("ring", sd_), ("actT", tt // 4)], w=[("ps", pk)])
                        S.add("dve", lambda e, tt=tt, q=q, pk=pk, ex_=ex_: e.scalar_tensor_tensor(
                            out=hres[:, tt, q * 256:(q + 1) * 256], in0=pss[pk][:, 0:256], scalar=comb[:, tt, ex_:ex_ + 1],
                            in1=hres[:, tt, q * 256:(q + 1) * 256], op0=ALU.mult, op1=ALU.add),
                            r=[("ps", pk), ("hres", tt)], w=[("hres", tt)])
            S.emit()

        for i in range(2):
            S.add("sp", lambda e, i=i: e.dma_start(out=lnB[:, i, :], in_=lnp[4 + i:5 + i, :].partition_broadcast(128)),
                  w=["lnB"], dma="misc")
        for tt in range(NT):
            _ln_tile(S, hres[:, tt, :], st6, mv, rstd, lnB[:, 0, :], lnB[:, 1, :], ("hres", tt), "ln")
            S.add("sp", lambda e, tt=tt: e.dma_start(out=out[tt * 128:(tt + 1) * 128, :], in_=hres[:, tt, :]),
                  r=[("hres", tt)], dma="out")
        S.emit()
    return nc


def build_p1(NX=8192, do_attn=True, do_ssm=True, dbg=()):
    NXT = NX // 128
    NQG = NX // 512
    NTOKS = N_META + NX
    NCH = NTOKS // 8
    NKT = NXT + 1
    nc = bass.Bass("TRN2", target_bir_lowering=False)
    dbgs = set(dbg) if isinstance(dbg, (tuple, list, set)) else {dbg}

    def din(name, shape, dtype=F32):
        return nc.dram_tensor(name, shape, dtype, kind="ExternalInput").ap()
    x = din("x", [NX, 1024])
    meta = din("meta", [N_META, 1024])
    lnp = din("lnp", [128, 16])
    lnrow = din("lnrow", [2, 1024])
    w1 = din("w1", [1024, 514])
    bfn = din("bf", [2, 1])
    lamp = din("lamp", [128, 16])
    logdt = din("logdt", [1, 8])
    bstk_d = din("bstk", [128, 128])
    bstks_d = din("bstks", [128, 128])
    cstk_d = din("cstk", [128, 128])
    dvec_d = din("dvec", [128, 1])
    cst = din("cst", [128, 128 * 4 + 8 * 128 + 4])
    yatt = nc.dram_tensor("yatt", [128, NX], F32, kind="ExternalOutput").ap()
    yssm = nc.dram_tensor("yssm", [128, NX], F32, kind="ExternalOutput").ap()
    NC_ = 128 * 4 + 8 * 128 + 4

    with ExitStack() as stack:
        S = Sched(nc, stack)

        def sb(name, shape, dtype):
            return stack.enter_context(nc.sbuf_tensor(name, shape, dtype))

        def psum(name, shape, dtype):
            return stack.enter_context(nc.psum_tensor(name, shape, dtype))
        QT = [sb(f"QT{h}", [65, NX], BF16) for h in range(2)]
        KT = [sb(f"KT{h}", [65, NTOKS], BF16) for h in range(2)]
        VV = [sb(f"VV{h}", [128, NKT, 128], BF16) for h in range(2)]
        uT = sb("uT", [128, NTOKS], BF16)
        NFcol = sb("NFcol", [128, NKT, 2], F32)
        cst32 = sb("cst32", [128, NC_], F32)
        identb = sb("identb", [128, 128], BF16)
        trib = sb("trib", [128, 128], BF16)
        ident32 = cst32[:, 0:128]
        Jm = cst32[:, 128:256]
        tri32 = cst32[:, 256:384]
        bmask = cst32[:, 512:512 + 1024]
        sgnR = cst32[:, 1536:1537]
        pss = [psum(f"ps{i}", [128, 512], F32) for i in range(6)]
        pT = [psum(f"pT{i}", [128, 1024], BF16) for i in range(2)]
        S.stream("x", 2)
        S.stream("misc", 0)
        S.stream("row", 4)
        S.stream("out", 2)
        S.stream("rl", 2)

        with ExitStack() as st2:
            def sb2(name, shape, dtype):
                return st2.enter_context(nc.sbuf_tensor(name, shape, dtype))
            w1t = sb2("w1t", [128, 8, 576], BF16)
            lnpt = sb2("lnpt", [128, 16], F32)
            lnB = sb2("lnB1", [128, 2, 1024], F32)
            xt = [sb2(f"xt{i}", [128, 1024], F32) for i in range(2)]
            xn = [sb2(f"xn{i}", [128, 1024], BF16) for i in range(2)]
            hT = sb2("hT", [128, 8, 512], BF16)
            st6 = sb2("st6", [128, 2, 6], F32)
            mv = sb2("mv", [128, 2], F32)
            rstd = sb2("rstd", [128, 1], F32)
            negbf = sb2("negbf", [2, 1], F32)
            e1 = sb2("e1", [2, 512], F32)
            nfg = sb2("nfg", [2, 512], F32)
            carry = sb2("carry", [2, 1], F32)
            ones2 = sb2("ones2", [2, 512], F32)
            fb = sb2("fb", [2, 512], BF16)

            S.add("sp", lambda e: e.dma_start(out=cst32[:], in_=cst[:, :]), w=["cst"], dma="misc")
            S.add("sp", lambda e: e.dma_start(out=lnpt[:], in_=lnp[:, :]), w=["lnpt"], dma="misc")
            for i in range(2):
                S.add("sp", lambda e, i=i: e.dma_start(out=lnB[:, i, :], in_=lnrow[i:i + 1, :].partition_broadcast(128)), w=["lnB"], dma="misc")
            S.add("sp", lambda e: e.dma_start(out=negbf[:], in_=bfn[:, :]), w=["negbf"], dma="misc")
            S.add("dve", lambda e: e.memset(w1t[:, :, 512:576], 0.0), w=["w1t"])
            S.add("pool", lambda e: e.dma_start(out=w1t[:, :, 0:514], in_=w1.rearrange("(k p) n -> p k n", p=128)), w=["w1t"], dma="misc")
            S.add("dve", lambda e: e.tensor_copy(out=identb[:], in_=ident32), r=["cst"], w=["identb"])
            S.add("dve", lambda e: e.tensor_copy(out=trib[:], in_=tri32), r=["cst"], w=["trib"])
            S.add("dve", lambda e: e.tensor_scalar(out=negbf[:], in0=negbf[:], scalar1=-1.0, scalar2=None, op0=ALU.mult),
                  r=["negbf"], w=["negbf"])
            S.add("dve", lambda e: e.memset(ones2[:], 1.0), w=["ones2"])
            S.add("dve", lambda e: e.memset(carry[:], 0.0), w=["carry"])
            for h in range(2):
                S.add("dve", lambda e, h=h: e.memset(KT[h][64:65, :], 1.0), w=[("KTa", h)])
                S.add("pool", lambda e, h=h: e.memset(VV[h][:, :, 64:128], 1.0), w=[("VVo", h)])

            for g in range(NQG + 1):
                n = N_META if g == 0 else 512
                ntile = 1 if g == 0 else 4
                kc0 = 0 if g == 0 else N_META + (g - 1) * 512
                qc0 = (g - 1) * 512
                for ti in range(ntile):
                    np_ = N_META if g == 0 else 128
                    b = (g * 4 + ti) % 2
                    if g == 0:
                        S.add("sp", lambda e, b=b: e.dma_start(out=xt[b][0:N_META, :], in_=meta[:, :]), w=[("xt", b)], dma="x")
                    else:
                        r0 = (g - 1) * 512 + ti * 128
                        S.add("sp", lambda e, b=b, r0=r0: e.dma_start(out=xt[b][:, :], in_=x[r0:r0 + 128, :]), w=[("xt", b)], dma="x")
                    xa = xt[b][0:np_, :]
                    S.add("dve", lambda e, xa=xa, np_=np_: e.bn_stats(out=st6[0:np_, 0, :], in_=xa[:, 0:512]), r=[("xt", b)], w=["st0"])
                    S.add("dve", lambda e, xa=xa, np_=np_: e.bn_stats(out=st6[0:np_, 1, :], in_=xa[:, 512:1024]), r=[("xt", b)], w=["st1"])
                    S.add("dve", lambda e, np_=np_: e.bn_aggr(out=mv[0:np_, :], in_=st6[0:np_, :, :]), r=["st0", "st1"], w=["mv"], fence=True)
                    S.add("dve", lambda e, np_=np_: e.tensor_scalar(out=rstd[0:np_, :], in0=mv[0:np_, 1:2], scalar1=LN_EPS, scalar2=None,
                                                                    op0=ALU.add), r=["mv"], w=["rs"], fence=True)
                    S.add("act", lambda e, np_=np_: e.sqrt(out=rstd[0:np_, :], in_=rstd[0:np_, :]), r=["rs"], w=["rs"], fence=True)
                    S.add("dve", lambda e, np_=np_: e.reciprocal(out=rstd[0:np_, :], in_=rstd[0:np_, :]), r=["rs"], w=["rs"], fence=True)
                    S.add("dve", lambda e, xa=xa, b=b, np_=np_: e.tensor_scalar(
                        out=xa, in0=xa, scalar1=mv[0:np_, 0:1], scalar2=rstd[0:np_, 0:1],
                        op0=ALU.subtract, op1=ALU.mult), r=[("xt", b), "mv", "rs"], w=[("xt", b)])
                    S.add("dve", lambda e, xa=xa, np_=np_: e.tensor_tensor(out=xa, in0=xa, in1=lnB[0:np_, 0, :], op=ALU.mult),
                          r=[("xt", b), "lnB"], w=[("xt", b)])
                    S.add("dve", lambda e, xa=xa, np_=np_: e.tensor_tensor(out=xa, in0=xa, in1=lnB[0:np_, 1, :], op=ALU.add),
                          r=[("xt", b), "lnB"], w=[("xt", b)])
                    S.add("act", lambda e, xa=xa, b=b, np_=np_: e.copy(out=xn[b][0:np_, :], in_=xa), r=[("xt", b)], w=[("xn", b)])

                    def tr(e, b=b, np_=np_):
                        ins = None
                        for kc in range(8):
                            ins = e.transpose(out=pT[b][:, kc * 128:kc * 128 + np_], in_=xn[b][0:np_, kc * 128:(kc + 1) * 128],
                                              identity=identb[0:np_, 0:np_])
                        return ins
                    S.add("pe", tr, r=[("xn", b), "identb"], w=[("pT", b)])
                    S.add("act", lambda e, b=b, ti=ti, np_=np_: e.copy(
                        out=hT[:, :, ti * 128:ti * 128 + np_], in_=pT[b][:, :].rearrange("p (k t) -> p k t", k=8)[:, :, 0:np_]),
                        r=[("pT", b)], w=["hT"])

                if 8 in dbgs:
                    S.emit()
                def proj(e, c_lo, c_hi, pk, n=n):
                    ins = None
                    for kc in range(8):
                        ins = e.matmul(pss[pk][0:c_hi - c_lo, 0:n], w1t[:, kc, c_lo:c_hi], hT[:, kc, 0:n],
                                       start=(kc == 0), stop=(kc == 7))
                    return ins
                for h in range(2):
                    if g > 0:
                        S.add("pe", lambda e, f=proj, h=h: f(e, h * 64, h * 64 + 64, h), r=["w1t", "hT"], w=[("ps", h)])
                        S.add("act", lambda e, h=h, qc0=qc0: e.mul(out=QT[h][0:64, qc0:qc0 + 512], in_=pss[h][0:64, :], mul=0.125),
                              r=[("ps", h)], w=[("QT", h)])
                    S.add("pe", lambda e, f=proj, h=h: f(e, 128 + h * 64, 128 + h * 64 + 64, 2 + h), r=["w1t", "hT"], w=[("ps", 2 + h)])
                    S.add("dve", lambda e, h=h, kc0=kc0, n=n: e.tensor_copy(out=KT[h][0:64, kc0:kc0 + n], in_=pss[2 + h][0:64, 0:n]),
                          r=[("ps", 2 + h)], w=[("KT", h)])
                S.add("pe", lambda e, f=proj: f(e, 384, 512, 4), r=["w1t", "hT"], w=[("ps", 4)])
                S.add("act", lambda e, kc0=kc0, n=n: e.copy(out=uT[:, kc0:kc0 + n], in_=pss[4][:, 0:n]), r=[("ps", 4)], w=["uT"])
                if 1 in dbgs:
                    continue
                S.add("pe", lambda e, f=proj: f(e, 512, 576, 5), r=["w1t", "hT"], w=[("ps", 5)])
                S.add("act", lambda e, n=n: e.activation(out=e1[:, 0:n], in_=pss[5][0:2, 0:n], func=AF.Exp, bias=negbf[:, 0:1], scale=-1.0),
                      r=[("ps", 5), "negbf"], w=["e1"])
                if 11 in dbgs:
                    continue
                S.add("dve", lambda e, n=n: e.tensor_scalar(out=e1[:, 0:n], in0=e1[:, 0:n], scalar1=1.0, scalar2=None, op0=ALU.add),
                      r=["e1"], w=["e1"])
                S.add("act", lambda e, n=n: e.activation(out=e1[:, 0:n], in_=e1[:, 0:n], func=AF.Ln), r=["e1"], w=["e1"])
                if 12 in dbgs:
                    continue
                S.add("dve", lambda e, n=n: e.tensor_tensor_scan(out=nfg[:, 0:n], data0=ones2[:, 0:n], data1=e1[:, 0:n], initial=carry[:, 0:1],
                                                                 op0=ALU.mult, op1=ALU.add), r=["e1", "carry", "ones2"], w=["nfg"])
                if 13 in dbgs:
                    continue
                S.add("dve", lambda e, n=n: e.tensor_copy(out=carry[:], in_=nfg[:, n - 1:n]), r=["nfg"], w=["carry"])
                if 2 in dbgs:
                    continue
                if g > 0:
                    S.add("dve", lambda e: e.tensor_scalar(out=fb[:], in0=nfg[:], scalar1=-1.0, scalar2=None, op0=ALU.mult),
                          r=["nfg"], w=["fb"])
                    for h in range(2):
                        S.add("sp", lambda e, h=h, qc0=qc0: e.dma_start(out=QT[h][64:65, qc0:qc0 + 512], in_=fb[h:h + 1, :]),
                              r=["fb"], w=[("QTa", h)], dma="row")
                if 3 in dbgs:
                    continue
                for ti in range(ntile):
                    np_ = N_META if g == 0 else 128
                    kt = 0 if g == 0 else 1 + (g - 1) * 4 + ti
                    S.add("pe", lambda e, ti=ti, np_=np_: e.transpose(out=pss[5][0:np_, 256 + 2 * ti:258 + 2 * ti],
                                                                      in_=nfg[0:2, ti * 128:ti * 128 + np_], identity=ident32[0:2, 0:2]),
                          r=["nfg"], w=[("ps", 5)])
                    S.add("dve", lambda e, ti=ti, np_=np_, kt=kt: e.tensor_copy(out=NFcol[0:np_, kt, :], in_=pss[5][0:np_, 256 + 2 * ti:258 + 2 * ti]),
                          r=[("ps", 5)], w=["NFcol"])
                if 4 in dbgs:
                    continue
                for ti in range(ntile):
                    np_ = N_META if g == 0 else 128
                    kt = 0 if g == 0 else 1 + (g - 1) * 4 + ti
                    pk = ti % 2

                    def mmv(e, ti=ti, np_=np_, pk=pk):
                        ins = None
                        for kc in range(8):
                            ins = e.matmul(pss[pk][0:np_, 0:128], hT[:, kc, ti * 128:ti * 128 + np_], w1t[:, kc, 256:384],
                                           start=(kc == 0), stop=(kc == 7))
                        return ins
                    S.add("pe", mmv, r=["w1t", "hT"], w=[("ps", pk)])
                    if 5 in dbgs:
                        continue
                    S.add("act", lambda e, np_=np_, kt=kt, pk=pk: e.copy(out=VV[0][0:np_, kt, 0:64], in_=pss[pk][0:np_, 0:64]),
                          r=[("ps", pk), ("VVo", 0)], w=[("VV", 0)])
                    if 6 in dbgs:
                        continue
                    S.add("act", lambda e, np_=np_, kt=kt, pk=pk: e.copy(out=VV[1][0:np_, kt, 0:64], in_=pss[pk][0:np_, 64:128]),
                          r=[("ps", pk), ("VVo", 1)], w=[("VV", 1)])
            if 9 in dbgs:
                d1 = nc.dram_tensor("d_nf", [128, NKT * 2], F32, kind="ExternalOutput").ap()
                d2 = nc.dram_tensor("d_qt", [65, NX], BF16, kind="ExternalOutput").ap()
                d3 = nc.dram_tensor("d_kt", [65, NTOKS], BF16, kind="ExternalOutput").ap()
                d4 = nc.dram_tensor("d_vv", [128, NKT * 128], BF16, kind="ExternalOutput").ap()
                d5 = nc.dram_tensor("d_ut", [128, NTOKS], BF16, kind="ExternalOutput").ap()
                S.add("sp", lambda e: e.dma_start(out=d1[:, :], in_=NFcol[:, :, :].rearrange("p a b -> p (a b)")), r=["NFcol"], dma="out")
                S.add("sp", lambda e: e.dma_start(out=d2[:, :], in_=QT[0][:, :]), r=[("QT", 0), ("QTa", 0)], dma="out")
                S.add("sp", lambda e: e.dma_start(out=d3[:, :], in_=KT[0][:, :]), r=[("KT", 0), ("KTa", 0)], dma="out")
                S.add("sp", lambda e: e.dma_start(out=d4[:, :], in_=VV[0][:, :, :].rearrange("p a b -> p (a b)")), r=[("VV", 0), ("VVo", 0)], dma="out")
                S.add("sp", lambda e: e.dma_start(out=d5[:, :], in_=uT[:, :]), r=["uT"], dma="out")
                d6 = nc.dram_tensor("d_xt", [128, 1024], F32, kind="ExternalOutput").ap()
                d7 = nc.dram_tensor("d_xn", [128, 1024], BF16, kind="ExternalOutput").ap()
                d8 = nc.dram_tensor("d_ht", [128, 8 * 512], BF16, kind="ExternalOutput").ap()
                d9 = nc.dram_tensor("d_mv", [128, 3], F32, kind="ExternalOutput").ap()
                S.add("sp", lambda e: e.dma_start(out=d6[:, :], in_=xt[1][:, :]), r=[("xt", 1)], dma="out")
                S.add("sp", lambda e: e.dma_start(out=d7[:, :], in_=xn[1][:, :]), r=[("xn", 1)], dma="out")
                S.add("sp", lambda e: e.dma_start(out=d8[:, :], in_=hT[:, :, :].rearrange("p a b -> p (a b)")), r=["hT"], dma="out")
                S.add("sp", lambda e: e.dma_start(out=d9[:, 0:2], in_=mv[:, :], allow_slow_non_contiguous=True), r=["mv"], dma="out")
                S.add("sp", lambda e: e.dma_start(out=d9[:, 2:3], in_=rstd[:, :], allow_slow_non_contiguous=True), r=["rs"], dma="out")
            S.emit()

        if do_ssm:
            _ssm_block(nc, S, stack, locals())
        if do_attn:
            with ExitStack() as st2:
                def sb2(name, shape, dtype):
                    return st2.enter_context(nc.sbuf_tensor(name, shape, dtype))
                NPT = 3
                pt = [sb2(f"pt{i}", [128, 512], BF16) for i in range(NPT)]
                rl = [sb2(f"rl{i}", [128, 512], F32) for i in range(2)]
                rl2 = [sb2(f"rlb{i}", [64, 512], F32) for i in range(2)]
                ot = [sb2(f"ot{i}", [64, 512], F32) for i in range(2)]
                it = 0
                gi = 0
                for h in range(2):
                    for i in range(NQG):
                        ab = 4 + (gi % 2)
                        ob = gi % 2
                        gi += 1
                        nblk = 4 * i + 5
                        for jb in range(nblk):
                            nk = N_META if jb == 0 else 128
                            kcol = 0 if jb == 0 else N_META + (jb - 1) * 128
                            jt = jb - 1
                            qs = (jt - 4 * i) * 128 if (jb > 0 and jt >= 4 * i) else 0
                            sbk = it % 2
                            pb = it % NPT
                            it += 1
                            S.add("pe", lambda e, h=h, nk=nk, kcol=kcol, qs=qs, sbk=sbk, i=i: e.matmul(
                                pss[sbk][0:nk, qs:512], KT[h][0:65, kcol:kcol + nk], QT[h][0:65, i * 512 + qs:(i + 1) * 512],
                                start=True, stop=True), w=[("ps", sbk)])
                            S.add("act", lambda e, h=h, nk=nk, qs=qs, sbk=sbk, pb=pb, jb=jb: e.activation(
                                out=pt[pb][0:nk, qs:512], in_=pss[sbk][0:nk, qs:512], func=AF.Exp,
                                bias=NFcol[0:nk, jb, h:h + 1], scale=1.0), r=[("ps", sbk)], w=[("pt", pb)])
                            if jb > 0 and jt >= 4 * i:
                                S.add("dve", lambda e, pb=pb, qs=qs: e.tensor_tensor(
                                    out=pt[pb][:, qs:qs + 128], in0=pt[pb][:, qs:qs + 128], in1=trib[:], op=ALU.mult),
                                    r=[("pt", pb)], w=[("pt", pb)])
                            S.add("pe", lambda e, h=h, nk=nk, qs=qs, pb=pb, jb=jb, ab=ab, nblk=nblk: e.matmul(
                                pss[ab][:, qs:512], VV[h][0:nk, jb, :], pt[pb][0:nk, qs:512],
                                start=(jb == 0), stop=(jb == nblk - 1)), r=[("pt", pb)], w=[("ps", ab)])
                        S.add("dve", lambda e, ab=ab, ob=ob: e.reciprocal(out=rl[ob][64:128, :], in_=pss[ab][64:128, :]),
                              r=[("ps", ab)], w=[("rl", ob)])
                        S.add("sp", lambda e, ob=ob: e.dma_start(out=rl2[ob][:, :], in_=rl[ob][64:128, :]),
                              r=[("rl", ob)], w=[("rl2", ob)], dma="rl")
                        S.add("dve", lambda e, ab=ab, ob=ob: e.tensor_tensor(out=ot[ob][:, :], in0=pss[ab][0:64, :], in1=rl2[ob][:, :], op=ALU.mult),
                              r=[("ps", ab), ("rl2", ob)], w=[("ot", ob)])
                        S.add("sp", lambda e, h=h, i=i, ob=ob: e.dma_start(out=yatt[h * 64:(h + 1) * 64, i * 512:(i + 1) * 512], in_=ot[ob][:, :]),
                              r=[("ot", ob)], dma="out")
                S.emit()
    return nc


def _ssm_block(nc, S, stack, L):
    uT, pss, cst32 = L["uT"], L["pss"], L["cst32"]
    lamp, logdt, bstk_d, cstk_d, dvec_d, yssm = L["lamp"], L["logdt"], L["bstk_d"], L["cstk_d"], L["dvec_d"], L["yssm"]
    NX, NCH, NTOKS = L["NX"], L["NCH"], L["NTOKS"]
    ident32 = cst32[:, 0:128]
    Jm = cst32[:, 128:256]
    bmask = cst32[:, 512:512 + 1024]
    sgnR = cst32[:, 1536:1537]
    NLEV = max(1, (NCH - 1).bit_length())
    POW_SEQ = list(range(9))
    NPW = 9 + NLEV
    PI = math.pi
    with ExitStack() as st2:
        def sb2(name, shape, dtype):
            return st2.enter_context(nc.sbuf_tensor(name, shape, dtype))
        lam = sb2("lam", [128, 16], F32)
        dtb = sb2("dtb", [128, 8], F32)
        t0 = sb2("t0", [128, 8], F32)
        t1 = sb2("t1", [128, 8], F32)
        t2 = sb2("t2", [128, 8], F32)
        t3 = sb2("t3", [128, 8], F32)
        pw_re = sb2("pw_re", [128, NPW + 1, 8], F32)
        pw_im = sb2("pw_im", [128, NPW + 1, 8], F32)
        pw_ims = sb2("pw_ims", [128, NPW + 1, 8], F32)
        bstk = sb2("bstk_sb", [128, 128], F32)
        cstk = sb2("cstk_sb", [128, 128], F32)
        dvec = sb2("dvec_sb", [128, 1], F32)
        bpad = sb2("bpad", [128, 128], BF16)
        cpad = sb2("cpad", [128, 128], BF16)
        Rb = sb2("Rb", [128, 10, 128], BF16)
        Rh = sb2("Rh", [128, NLEV, 128], F32)
        bbar = sb2("bbar", [128, 128], BF16)
        lhsA = sb2("lhsA", [128, 8, 128], BF16)
        CL0 = sb2("CL0", [128, 128], BF16)
        CLall = sb2("CLall", [128, 8, 8, 128], BF16)
        Wb = sb2("Wb", [128, 8, 128], BF16)
        Ea = sb2("Ea", [128, NCH], F32)
        Eb = sb2("Eb", [128, NCH], F32)
        Sin = sb2("Sin", [128, 8, NCH], BF16)
        yo = [sb2("yo0", [128, 4096], F32)]
        S.stream("ssm", 0)
        S.add("sp", lambda e: e.dma_start(out=lam[:], in_=lamp[:, :]), w=["lam"], dma="ssm")
        S.add("sp", lambda e: e.dma_start(out=dtb[:], in_=logdt[0:1, :].partition_broadcast(128)), w=["dtb"], dma="ssm")
        S.add("sp", lambda e: e.dma_start(out=bstk[:], in_=bstk_d[:, :]), w=["bstk"], dma="ssm")
        S.add("sp", lambda e: e.dma_start(out=cstk[:], in_=cstk_d[:, :]), w=["cstk"], dma="ssm")
        S.add("sp", lambda e: e.dma_start(out=dvec[:], in_=dvec_d[:, :]), w=["dvec"], dma="ssm")
        are, aim = lam[:, 0:8], lam[:, 8:16]

        def V(fn, r, w):
            S.add("dve", fn, r=r, w=w)

        def tt(out, a, b, op, r, w):
            V(lambda e: e.tensor_tensor(out=out, in0=a, in1=b, op=op), r, w)

        def ts(out, a, s1, s2, op0, op1, r, w):
            if op1 is None:
                V(lambda e: e.tensor_scalar(out=out, in0=a, scalar1=s1, scalar2=None, op0=op0), r, w)
            else:
                V(lambda e: e.tensor_scalar(out=out, in0=a, scalar1=s1, scalar2=s2, op0=op0, op1=op1), r, w)
        S.add("act", lambda e: e.activation(out=dtb[:], in_=dtb[:], func=AF.Exp), r=["dtb"], w=["dtb"])
        tt(t0[:], are, dtb[:], ALU.mult, ["lam", "dtb"], ["t0"])
        S.add("act", lambda e: e.activation(out=t0[:], in_=t0[:], func=AF.Exp), r=["t0"], w=["t0"])
        tt(t1[:], aim, dtb[:], ALU.mult, ["lam", "dtb"], ["t1"])
        MAGIC = 12582912.0
        ts(t1[:], t1[:], 1.0 / (2.0 * PI), None, ALU.mult, None, ["t1"], ["t1"])
        ts(t3[:], t1[:], MAGIC, None, ALU.add, None, ["t1"], ["t3"])
        ts(t3[:], t3[:], -MAGIC, None, ALU.add, None, ["t3"], ["t3"])
        tt(t2[:], t1[:], t3[:], ALU.subtract, ["t1", "t3"], ["t2"])
        S.add("act", lambda e: e.activation(out=t2[:], in_=t2[:], func=AF.Sin, scale=2.0 * PI), r=["t2"], w=["t2"])
        ts(t1[:], t1[:], 0.25, None, ALU.add, None, ["t1"], ["t1"])
        ts(t3[:], t1[:], MAGIC, None, ALU.add, None, ["t1"], ["t3"])
        ts(t3[:], t3[:], -MAGIC, None, ALU.add, None, ["t3"], ["t3"])
        tt(t3[:], t1[:], t3[:], ALU.subtract, ["t1", "t3"], ["t3"])
        S.add("act", lambda e: e.activation(out=t3[:], in_=t3[:], func=AF.Sin, scale=2.0 * PI), r=["t3"], w=["t3"])
        tt(pw_re[:, 1, :], t0[:], t3[:], ALU.mult, ["t0", "t3"], ["pw"])
        tt(pw_im[:, 1, :], t0[:], t2[:], ALU.mult, ["t0", "t2"], ["pw"])
        V(lambda e: e.memset(pw_re[:, 0, :], 1.0), [], ["pw"])
        V(lambda e: e.memset(pw_im[:, 0, :], 0.0), [], ["pw"])

        def cmul(o, a, b):
            tt(t0[:], pw_re[:, a, :], pw_re[:, b, :], ALU.mult, ["pw"], ["t0"])
            tt(t1[:], pw_im[:, a, :], pw_im[:, b, :], ALU.mult, ["pw"], ["t1"])
            tt(t2[:], pw_re[:, a, :], pw_im[:, b, :], ALU.mult, ["pw"], ["t2"])
            tt(t3[:], pw_im[:, a, :], pw_re[:, b, :], ALU.mult, ["pw"], ["t3"])
            tt(pw_re[:, o, :], t0[:], t1[:], ALU.subtract, ["t0", "t1"], ["pw"])
            tt(pw_im[:, o, :], t2[:], t3[:], ALU.add, ["t2", "t3"], ["pw"])
        for m in range(2, 9):
            cmul(m, m - 1, 1)
        for l in range(1, NLEV):
            cmul(8 + l, 8 + l - 1, 8 + l - 1)
        CO = NPW
        tt(t0[:], are, are, ALU.mult, ["lam"], ["t0"])
        tt(t1[:], aim, aim, ALU.mult, ["lam"], ["t1"])
        tt(t0[:], t0[:], t1[:], ALU.add, ["t0", "t1"], ["t0"])
        V(lambda e: e.reciprocal(out=t0[:], in_=t0[:]), ["t0"], ["t0"])
        ts(t1[:], pw_re[:, 1, :], -1.0, None, ALU.add, None, ["pw"], ["t1"])
        tt(t2[:], t1[:], are, ALU.mult, ["t1", "lam"], ["t2"])
        tt(t3[:], pw_im[:, 1, :], aim, ALU.mult, ["pw", "lam"], ["t3"])
        tt(t2[:], t2[:], t3[:], ALU.add, ["t2", "t3"], ["t2"])
        tt(pw_re[:, CO, :], t2[:], t0[:], ALU.mult, ["t2", "t0"], ["pw"])
        tt(t2[:], pw_im[:, 1, :], are, ALU.mult, ["pw", "lam"], ["t2"])
        tt(t3[:], t1[:], aim, ALU.mult, ["t1", "lam"], ["t3"])
        tt(t2[:], t2[:], t3[:], ALU.subtract, ["t2", "t3"], ["t2"])
        tt(pw_im[:, CO, :], t2[:], t0[:], ALU.mult, ["t2", "t0"], ["pw"])
        V(lambda e: e.tensor_scalar(out=pw_ims[:], in0=pw_im[:], scalar1=sgnR, scalar2=None, op0=ALU.mult), ["pw", "cst"], ["pws"])

        def build_R(out_ap, idx, g, key):
            V(lambda e: e.tensor_scalar(out=out_ap, in0=ident32, scalar1=pw_re[:, idx, g:g + 1], scalar2=None, op0=ALU.mult),
              ["pw", "cst"], [key])
            V(lambda e: e.scalar_tensor_tensor(out=out_ap, in0=Jm, scalar=pw_ims[:, idx, g:g + 1], in1=out_ap,
                                               op0=ALU.mult, op1=ALU.add), ["pws", "cst", key], [key])

        NT3 = [(c, min(342, NCH - c)) for c in range(0, NCH, 342)]
        evk = 0
        for g in range(8):
            for m in range(9):
                build_R(Rb[:, m, :], m, g, ("Rb", m))
            build_R(Rb[:, 9, :], CO, g, ("Rb", 9))
            for l in range(NLEV):
                build_R(Rh[:, l, :], 8 + l, g, ("Rh", l))
            tt(bpad[:], bstk[:], bmask[:, g * 128:(g + 1) * 128], ALU.mult, ["bstk", "cst"], ["bpad"])
            tt(cpad[:], cstk[:], bmask[:, g * 128:(g + 1) * 128], ALU.mult, ["cstk", "cst"], ["cpad"])
            S.add("pe", lambda e: e.matmul(pss[0][:, 0:128], Rb[:, 9, :], bpad[:], start=True, stop=True),
                  r=[("Rb", 9), "bpad"], w=[("ps", 0)])
            S.add("act", lambda e: e.copy(out=bbar[:], in_=pss[0][:, 0:128]), r=[("ps", 0)], w=["bbar"])
            for i in range(8):
                pk = i % 2
                S.add("pe", lambda e, i=i, pk=pk: e.matmul(pss[pk][:, 128:256], bbar[:], Rb[:, 7 - i, :], start=True, stop=True),
                      r=["bbar", ("Rb", 7 - i)], w=[("ps", pk)])
                if i % 2 == 0:
                    S.add("act", lambda e, i=i, pk=pk: e.copy(out=lhsA[:, i, :], in_=pss[pk][:, 128:256]), r=[("ps", pk)], w=[("lhsA", i)])
                else:
                    S.add("dve", lambda e, i=i, pk=pk: e.tensor_copy(out=lhsA[:, i, :], in_=pss[pk][:, 128:256]), r=[("ps", pk)], w=[("lhsA", i)])
            for d in range(9):
                pk = d % 2
                dst = CL0[:] if d == 0 else CLall[:, g, d - 1, :]
                S.add("pe", lambda e, d=d, pk=pk: e.matmul(pss[pk][:, 256:384], Rb[:, d, :], cpad[:], start=True, stop=True),
                      r=[("Rb", d), "cpad"], w=[("ps", pk)])
                S.add("dve", lambda e, dst=dst, pk=pk: e.tensor_scalar(out=dst, in0=pss[pk][:, 256:384], scalar1=sgnR, scalar2=None, op0=ALU.mult),
                      r=[("ps", pk), "cst"], w=[("CL", g, d)])
            for d in range(8):
                bank = 2 + d // 4
                col = (d % 4) * 128
                S.add("pe", lambda e, d=d, bank=bank, col=col, g=g: e.matmul(
                    pss[bank][:, col:col + 128], bbar[:], (CL0[:] if d == 0 else CLall[:, g, d - 1, :]),
                    start=(g == 0), stop=(g == 7)), r=["bbar", ("CL", g, d)], w=[("ps", bank)])
            for (c0, n) in NT3:
                pk = 4 + (evk % 2)
                evk += 1

                def mmA(e, c0=c0, n=n, pk=pk):
                    ins = None
                    for i in range(8):
                        ins = e.matmul(pss[pk][:, 0:n], lhsA[:, i, :], uT[:, c0 * 8 + i:(c0 + n) * 8:8], start=(i == 0), stop=(i == 7))
                    return ins
                S.add("pe", mmA, r=[("lhsA", i) for i in range(8)], w=[("ps", pk)])
                S.add("act", lambda e, c0=c0, n=n, pk=pk: e.copy(out=Ea[:, c0:c0 + n], in_=pss[pk][:, 0:n]), r=[("ps", pk)], w=["Ea"])
            src, dst_ = Ea, Eb
            sk, dk = "Ea", "Eb"
            for l in range(NLEV):
                s = 1 << l
                S.add("act", lambda e, src=src, dst_=dst_, s=s: e.copy(out=dst_[:, 0:s], in_=src[:, 0:s]), r=[sk], w=[dk])
                c = s
                while c < NCH:
                    n = min(512, NCH - c)
                    pk = 4 + (evk % 2)
                    evk += 1
                    S.add("pe", lambda e, l=l, c=c, n=n, s=s, pk=pk, src=src: e.matmul(
                        pss[pk][:, 0:n], Rh[:, l, :], src[:, c - s:c - s + n], start=True, stop=True), r=[("Rh", l), sk], w=[("ps", pk)])
                    S.add("dve", lambda e, c=c, n=n, pk=pk, src=src, dst_=dst_: e.tensor_tensor(
                        out=dst_[:, c:c + n], in0=pss[pk][:, 0:n], in1=src[:, c:c + n], op=ALU.add), r=[("ps", pk), sk], w=[dk])
                    c += n
                src, dst_ = dst_, src
                sk, dk = dk, sk
            S.add("dve", lambda e, g=g: e.memset(Sin[:, g, 0:1], 0.0), w=[("Sin", g)])
            S.add("act", lambda e, g=g, src=src: e.copy(out=Sin[:, g, 1:NCH], in_=src[:, 0:NCH - 1]), r=[sk], w=[("Sin", g)])
        for d in range(8):
            bank = 2 + d // 4
            col = (d % 4) * 128
            if d == 0:
                S.add("dve", lambda e, bank=bank, col=col: e.scalar_tensor_tensor(
                    out=Wb[:, 0, :], in0=ident32, scalar=dvec[:, 0:1], in1=pss[bank][:, col:col + 128], op0=ALU.mult, op1=ALU.add),
                    r=[("ps", bank), "dvec", "cst"], w=["Wb"])
            else:
                S.add("act", lambda e, d=d, bank=bank, col=col: e.copy(out=Wb[:, d, :], in_=pss[bank][:, col:col + 128]),
                      r=[("ps", bank)], w=["Wb"])
        kch = 2
        oi = 0
        while kch < NCH:
            n = min(512, NCH - kch)
            ob = 0
            for j in range(8):
                pk = 4 + (evk % 2)
                evk += 1

                def mmo(e, j=j, kch=kch, n=n, pk=pk):
                    ins = None
                    tot = (j + 1) + 8
                    q = 0
                    for i in range(j + 1):
                        ins = e.matmul(pss[pk][:, 0:n], Wb[:, j - i, :], uT[:, kch * 8 + i:(kch + n) * 8:8], start=(q == 0), stop=(q == tot - 1))
                        q += 1
                    for g in range(8):
                        ins = e.matmul(pss[pk][:, 0:n], CLall[:, g, j, :], Sin[:, g, kch:kch + n], start=(q == 0), stop=(q == tot - 1))
                        q += 1
                    return ins
                S.add("pe", mmo, r=["Wb"] + [("Sin", g) for g in range(8)] + [("CL", g, d) for g in range(8) for d in range(1, 9)],
                      w=[("ps", pk)])
                eng = "act" if j % 2 == 0 else "dve"
                if eng == "act":
                    S.add("act", lambda e, j=j, n=n, pk=pk, ob=ob: e.copy(out=yo[ob][:, j:n * 8:8], in_=pss[pk][:, 0:n]),
                          r=[("ps", pk)], w=[("yo", ob)])
                else:
                    S.add("dve", lambda e, j=j, n=n, pk=pk, ob=ob: e.tensor_copy(out=yo[ob][:, j:n * 8:8], in_=pss[pk][:, 0:n]),
                          r=[("ps", pk)], w=[("yo", ob)])
            t0_ = (kch - 2) * 8
            S.add("sp", lambda e, ob=ob, t0_=t0_, n=n: e.dma_start(out=yssm[:, t0_:t0_ + n * 8], in_=yo[ob][:, 0:n * 8]),
                  r=[("yo", ob)], dma="out")
            kch += n
            oi += 1
        S.emit()


def _consts():
    c = np.zeros((128, 128 * 4 + 8 * 128 + 4), np.float32)
    c[:, 0:128] = np.eye(128, dtype=np.float32)
    J = np.zeros((128, 128), np.float32)
    for p in range(64):
        J[p, 64 + p] = 1.0
        J[64 + p, p] = 1.0
    c[:, 128:256] = J
    s = np.arange(128)[:, None]
    t = np.arange(128)[None, :]
    c[:, 256:384] = (s <= t).astype(np.float32)
    for g in range(8):
        c[:, 512 + g * 128 + g * 16: 512 + g * 128 + (g + 1) * 16] = 1.0
    c[0:64, 1536] = 1.0
    c[64:128, 1536] = -1.0
    return c


def prep_p1(inp, b, r, NX=SEQ):
    hs = [2 * r, 2 * r + 1]
    w_in = inp["w_in"][0]
    cols = []
    for h in hs:
        cols.append(w_in[:, Q_OFF + h * 64:Q_OFF + (h + 1) * 64])
    for h in hs:
        cols.append(w_in[:, K_OFF + h * 64:K_OFF + (h + 1) * 64])
    for h in hs:
        cols.append(w_in[:, V_OFF + h * 64:V_OFF + (h + 1) * 64])
    cols.append(w_in[:, U_OFF + r * 128:U_OFF + (r + 1) * 128])
    cols.append(w_in[:, F_OFF + 2 * r:F_OFF + 2 * r + 2])
    w1 = np.ascontiguousarray(np.concatenate(cols, axis=1), dtype=np.float32)
    lnp = np.concatenate([inp["ln_in_g"].reshape(8, 128).T, inp["ln_in_b"].reshape(8, 128).T], axis=1)
    gs = slice(8 * r, 8 * r + 8)
    a_re = inp["ssm_a_re"][0][gs]
    a_im = inp["ssm_a_im"][0][gs]
    lamp = np.concatenate([np.concatenate([a_re.T, a_re.T], axis=0), np.concatenate([a_im.T, a_im.T], axis=0)], axis=1)
    b_re = inp["ssm_b_re"][0][gs]
    b_im = inp["ssm_b_im"][0][gs]
    bstk = np.concatenate([b_re.transpose(1, 0, 2).reshape(64, 128), b_im.transpose(1, 0, 2).reshape(64, 128)], axis=0)
    c_re = inp["ssm_c_re"][0][gs]
    c_im = inp["ssm_c_im"][0][gs]
    cstk = np.concatenate([c_re.transpose(2, 0, 1).reshape(64, 128), c_im.transpose(2, 0, 1).reshape(64, 128)], axis=0)
    f32 = lambda a: np.ascontiguousarray(a, dtype=np.float32)
    return dict(x=f32(inp["x"][b][:NX]), meta=f32(inp["meta"]), lnp=f32(lnp), w1=w1,
                lnrow=f32(np.stack([inp["ln_in_g"], inp["ln_in_b"]])),
                bf=f32(inp["b_f"][0][2 * r:2 * r + 2].reshape(2, 1)), lamp=f32(lamp),
                logdt=f32(inp["ssm_log_dt"][0][gs].reshape(1, 8)), bstk=f32(bstk), bstks=f32(bstk), cstk=f32(cstk),
                dvec=f32(inp["ssm_d"][0][gs].reshape(128, 1)), cst=_consts())


def prep_p2_shared(inp):
    f32 = lambda a: np.ascontiguousarray(a, dtype=np.float32)
    w_in = inp["w_in"][0]
    w_ga = w_in[:, GA_OFF:GB_OFF]
    w_gb = w_in[:, GB_OFF:GB_OFF + 1024]
    wg = np.zeros((1024, 2048), np.float32)
    for j in range(4):
        for dcl in range(2):
            dc = 2 * j + dcl
            o = j * 512 + dcl * 256
            wg[:, o:o + 128] = w_ga[:, dc * 128:(dc + 1) * 128]
            wg[:, o + 128:o + 256] = w_gb[:, dc * 128:(dc + 1) * 128]
    lnp = np.stack([inp["ln_in_g"], inp["ln_in_b"], inp["ln1_g"][0], inp["ln1_b"][0], inp["ln2_g"][0], inp["ln2_b"][0]])
    bgu = np.zeros((128, 2, N_EXPERTS, 8), np.float32)
    bgu[:, 0] = inp["b_gate"][0].reshape(N_EXPERTS, 8, 128).transpose(2, 0, 1)
    bgu[:, 1] = inp["b_up"][0].reshape(N_EXPERTS, 8, 128).transpose(2, 0, 1)
    return dict(lnp=f32(lnp), wg=wg, wo=f32(inp["w_o"][0]), wua=f32(inp["w_up_a"][0]), wub=f32(inp["w_up_b"][0]),
                wglu=f32(inp["w_glu"][0]), bglu=f32(inp["b_glu"][0].reshape(4, 128).T), wr=f32(inp["w_router"][0]),
                br=f32(inp["b_router"][0].reshape(1, 32)), wgate=f32(inp["w_gate"][0]), wup=f32(inp["w_up"][0]),
                wdown=f32(inp["w_down"][0]), bgu=f32(bgu.reshape(128, -1)), bdown=f32(inp["b_down"][0]),
                ident=np.eye(128, dtype=np.float32))


_NC_CACHE = {}


def kernel(**inputs):
    inp = {k: np.asarray(v) for k, v in inputs.items()}
    B = inp["x"].shape[0]
    if "p1" not in _NC_CACHE:
        _NC_CACHE["p1"] = build_p1(SEQ)
        _NC_CACHE["p2"] = build_p2(2048, N_EXPERTS)
    maps1 = [prep_p1(inp, c // 4, c % 4) for c in range(8)]
    res1 = run_bass_kernel_spmd(_NC_CACHE["p1"], maps1, core_ids=list(range(8)))
    yaT = np.zeros((B, 512, SEQ), np.float32)
    ysT = np.zeros((B, 512, SEQ), np.float32)
    for c in range(8):
        b, r = c // 4, c % 4
        yaT[b, r * 128:(r + 1) * 128] = res1.results[c]["yatt"]
        ysT[b, r * 128:(r + 1) * 128] = res1.results[c]["yssm"]
    shared = prep_p2_shared(inp)
    maps2 = []
    for c in range(8):
        b, q = c // 4, c % 4
        sl = slice(q * 2048, (q + 1) * 2048)
        m = dict(shared)
        m["x"] = np.ascontiguousarray(inp["x"][b, sl], dtype=np.float32)
        m["yaT"] = np.ascontiguousarray(yaT[b][:, sl])
        m["ysT"] = np.ascontiguousarray(ysT[b][:, sl])
        maps2.append(m)
    res2 = run_bass_kernel_spmd(_NC_CACHE["p2"], maps2, core_ids=list(range(8)))
    out = np.zeros((B, SEQ, D_MODEL), np.float32)
    for c in range(8):
        b, q = c // 4, c % 4
        out[b, q * 2048:(q + 1) * 2048] = res2.results[c]["out"]
    return out
```

```python
import math
from contextlib import ExitStack

import numpy as np
import concourse.bass as bass
import concourse.mybir as mybir
from concourse.bass_utils import run_bass_kernel_spmd

F32 = mybir.dt.float32
BF16 = mybir.dt.bfloat16
AF = mybir.ActivationFunctionType
ALU = mybir.AluOpType

D_MODEL = 1024
SEQ = 8192
N_META = 16
N_EXPERTS = 32
LN_EPS = 1e-5
DN_ALPHA = 2.0 ** 0.25
SW_LIMIT = 7.0
SW_ALPHA = 1.702
Q_OFF, K_OFF, V_OFF, F_OFF, U_OFF, GA_OFF, GB_OFF = 0, 512, 1024, 1536, 1544, 2056, 3080


class Sched:
    ENGS = ("pe", "act", "dve", "pool", "sp")
    SEM_ROLL = 1 << 30

    def __init__(self, nc, stack):
        self.nc = nc
        self.stack = stack
        self.eng_obj = {"pe": nc.tensor, "act": nc.scalar, "dve": nc.vector,
                        "pool": nc.gpsimd, "sp": nc.sync}
        self.eng_sems = {e: [stack.enter_context(nc.semaphore(f"s_{e}0"))] for e in self.ENGS}
        self.eng_cnt = {e: 0 for e in self.ENGS}
        self.streams = {}
        self.nsem = 0
        self.fz = {e: stack.enter_context(nc.sbuf_tensor(f"fz_{e}", [128, 2], F32)) for e in ("act", "dve", "pool")}
        self.reset()

    def reset(self):
        self.ops = []
        self.lastw = {}
        self.readers = {}

    def stream(self, name, depth):
        if name not in self.streams:
            sems = [self.stack.enter_context(self.nc.semaphore(f"d_{name}{i}")) for i in range(depth)]
            self.streams[name] = dict(sems=sems, cnt=[0] * depth, k=0)
        return name

    def add(self, eng, fn, r=(), w=(), dma=None, fence=False):
        idx = len(self.ops)
        deps = set()
        for k in r:
            if k in self.lastw:
                deps.add(self.lastw[k])
        for k in w:
            if k in self.lastw:
                deps.add(self.lastw[k])
            for x in self.readers.get(k, ()):
                deps.add(x)
        deps.discard(idx)
        for k in r:
            self.readers.setdefault(k, []).append(idx)
        for k in w:
            self.lastw[k] = idx
            self.readers[k] = []
        self.ops.append(dict(eng=eng, fn=fn, deps=deps, dma=dma, used=False, sig=None, fence=fence))
        for d in deps:
            self.ops[d]["used"] = True
        return idx

    def emit(self, final_wait_dma=True):
        nc = self.nc
        ops = self.ops
        for op in ops:
            if op["dma"] is not None and len(self.streams[op["dma"]]["sems"]) == 0:
                self.nsem += 1
                sem = self.stack.enter_context(nc.semaphore(f"d_one{self.nsem}"))
                op["sig"] = (sem, 16, 16)
            elif op["dma"] is not None:
                st = self.streams[op["dma"]]
                k = st["k"] % len(st["sems"])
                st["k"] += 1
                st["cnt"][k] += 16
                op["sig"] = (st["sems"][k], st["cnt"][k], 16)
            elif op["used"]:
                e = op["eng"]
                if self.eng_cnt[e] >= self.SEM_ROLL:
                    self.eng_sems[e].append(self.stack.enter_context(nc.semaphore(f"s_{e}{len(self.eng_sems[e])}")))
                    self.eng_cnt[e] = 0
                self.eng_cnt[e] += 1
                op["sig"] = (self.eng_sems[e][-1], self.eng_cnt[e], 1)
        per_eng = {e: [] for e in self.ENGS}
        for i, op in enumerate(ops):
            waits = []
            for d in sorted(op["deps"]):
                dop = ops[d]
                if dop["dma"] is None and dop["eng"] == "pe" and op["eng"] == "pe":
                    continue
                waits.append(dop["sig"])
            per_eng[op["eng"]].append((waits, op))
        pending = [op["sig"] for op in ops if op["dma"] is not None]
        with nc.Block() as blk:
            def make(ename):
                lst = per_eng[ename]

                def body(eng):
                    known = {}
                    for waits, op in lst:
                        for (sem, val, _inc) in waits:
                            key = id(sem)
                            if known.get(key, (None, 0))[1] >= val:
                                continue
                            eng.wait_ge(sem, val)
                            known[key] = (sem, val)
                        ins = op["fn"](eng)
                        if op["sig"] is not None:
                            if op["fence"] and ename in self.fz:
                                fz = self.fz[ename]
                                if ename == "act":
                                    ins = eng.copy(out=fz[:, 0:1], in_=fz[:, 1:2])
                                else:
                                    ins = eng.memset(fz[:, 0:1], 0.0)
                            ins.then_inc(op["sig"][0], op["sig"][2])
                    if ename == "sp" and final_wait_dma:
                        best = {}
                        for (sem, val, _inc) in pending:
                            if best.get(id(sem), (None, 0))[1] < val:
                                best[id(sem)] = (sem, val)
                        for sem, val in best.values():
                            if known.get(id(sem), (None, 0))[1] < val:
                                eng.wait_ge(sem, val)
                return body
            blk.tensor(make("pe"))
            blk.scalar(make("act"))
            blk.vector(make("dve"))
            blk.gpsimd(make("pool"))
            blk.sync(make("sp"))
        self.reset()


def _bc(ap, shape):
    return ap.to_broadcast(shape)


def _ln_tile(S, tile_ap, st6, mv, rstd, gB, bB, key, tag):
    S.add("dve", lambda e: e.bn_stats(out=st6[:, 0, :], in_=tile_ap[:, 0:512]), r=[key], w=[tag + "st0"])
    S.add("dve", lambda e: e.bn_stats(out=st6[:, 1, :], in_=tile_ap[:, 512:1024]), r=[key], w=[tag + "st1"])
    S.add("dve", lambda e: e.bn_aggr(out=mv[:], in_=st6[:]), r=[tag + "st0", tag + "st1"], w=[tag + "mv"], fence=True)
    S.add("dve", lambda e: e.tensor_scalar(out=rstd[:], in0=mv[:, 1:2], scalar1=LN_EPS, scalar2=None,
                                           op0=ALU.add), r=[tag + "mv"], w=[tag + "rs"], fence=True)
    S.add("act", lambda e: e.sqrt(out=rstd[:], in_=rstd[:]), r=[tag + "rs"], w=[tag + "rs"], fence=True)
    S.add("dve", lambda e: e.reciprocal(out=rstd[:], in_=rstd[:]), r=[tag + "rs"], w=[tag + "rs"], fence=True)
    S.add("dve", lambda e: e.tensor_scalar(out=tile_ap, in0=tile_ap, scalar1=mv[:, 0:1], scalar2=rstd[:, 0:1],
                                           op0=ALU.subtract, op1=ALU.mult), r=[key, tag + "mv", tag + "rs"], w=[key])
    S.add("dve", lambda e: e.tensor_tensor(out=tile_ap, in0=tile_ap, in1=gB, op=ALU.mult), r=[key, "lnB"], w=[key])
    S.add("dve", lambda e: e.tensor_tensor(out=tile_ap, in0=tile_ap, in1=bB, op=ALU.add), r=[key, "lnB"], w=[key])


def build_p2(NTOK=2048, NE=32, ctx=None):
    NT = NTOK // 128
    NG = NTOK // 512
    fused = ctx is not None
    nc = ctx["nc"] if fused else bass.Bass("TRN2", target_bir_lowering=False)

    def din(name, shape, dtype=F32):
        return nc.dram_tensor(name, shape, dtype, kind="ExternalInput").ap()
    x = din("x2", [NTOK, 1024])
    if fused:
        ygat = ctx["ygat"]
        onehot = din("onehot", [128, 4])
    else:
        yaT = din("yaT", [512, NTOK])
        ysT = din("ysT", [512, NTOK])
    lnp = din("lnp2", [6, 1024])
    wg = din("wg", [1024, 2048])
    wo = din("wo", [1024, 1024])
    wua = din("wua", [512, 1024])
    wub = din("wub", [512, 1024])
    wglu = din("wglu", [512, 512])
    bglu = din("bglu", [128, 4])
    wr = din("wr", [1024, 32])
    br = din("br", [1, 32])
    wgate = din("wgate", [NE, 1024, 1024])
    wup = din("wup", [NE, 1024, 1024])
    wdown = din("wdown", [NE, 1024, 1024])
    bgu = din("bgu", [128, 2 * NE * 8])
    bdown = din("bdown", [N_EXPERTS, 1024])
    ident = din("ident", [128, 128])
    out = nc.dram_tensor("out", [NTOK, 1024], F32, kind="ExternalOutput").ap()

    with ExitStack() as stack:
        S = ctx["S"] if fused else Sched(nc, stack)

        def sb(name, shape, dtype):
            return stack.enter_context(nc.sbuf_tensor("b_" + name, shape, dtype))

        def psum(name, shape, dtype):
            return stack.enter_context(nc.psum_tensor("b_" + name, shape, dtype))
        hres = sb("hres", [128, NT, 1024], F32)
        identb = sb("identb", [128, 128], BF16)
        ident32 = sb("ident32", [128, 128], F32)
        lnB = sb("lnB", [128, 4, 1024], F32)
        comb = sb("comb", [128, NT, 32], F32)
        st6 = sb("st6", [128, 2, 6], F32)
        mv = sb("mv", [128, 2], F32)
        rstd = sb("rstd", [128, 1], F32)
        pss = [psum(f"ps{i}", [128, 512], F32) for i in range(6)]
        pT = [psum(f"pT{i}", [128, 1024], BF16) for i in range(2)]
        S.stream("x", 2)
        S.stream("misc", 0)
        S.stream("gw", 3)
        S.stream("yin", 0)
        S.stream("wr", 8)
        S.stream("out", 2)

        with ExitStack() as st2:
            def sb2(name, shape, dtype):
                return st2.enter_context(nc.sbuf_tensor("b_" + name, shape, dtype))
            xb = [sb2(f"xb{i}", [128, 1024], BF16) for i in range(2)]
            hT = sb2("hT", [128, 8, 512], BF16)
            ya = sb2("ya", [128, 4, 512], BF16)
            ys32 = sb2("ys32", [128, 4, 512], F32)
            gtmp = sb2("gtmp", [128, 512], F32)
            sgm = sb2("sgm", [128, 512], F32)
            yb = sb2("yb", [128, 4, 512], BF16)
            yb2 = sb2("yb2", [128, 4, 512], BF16)
            merged = sb2("merged", [128, 8, 512], BF16)
            gw = [sb2(f"gw{i}", [128, 8, 512], BF16) for i in range(3)]
            wuat = sb2("wuat", [128, 4, 1024], BF16)
            wubt = sb2("wubt", [128, 4, 1024], BF16)
            wglut = sb2("wglut", [128, 4, 512], BF16)
            bglut = sb2("bglut", [128, 4], F32)
            if fused:
                cand = [sb2(f"cand{i}", [128, 4, 512], BF16) for i in range(2)]
                oht = sb2("oht", [128, 4], F32)
                S.stream("cand", 2)
                S.add("sp", lambda e: e.dma_start(out=oht[:], in_=onehot[:, :]), w=["oht"], dma="misc")
            gat = [sb2(f"gat{i}", [128, 512], F32) for i in range(2)]
            gbt = [sb2(f"gbt{i}", [128, 512], F32) for i in range(2)]

            S.add("sp", lambda e: e.dma_start(out=ident32[:], in_=ident[:, :]), w=["ident32"], dma="misc")
            S.add("dve", lambda e: e.tensor_copy(out=identb[:], in_=ident32[:]), r=["ident32"], w=["identb"])
            for i in range(4):
                S.add("sp", lambda e, i=i: e.dma_start(out=lnB[:, i, :], in_=lnp[i:i + 1, :].partition_broadcast(128)),
                      w=["lnB"], dma="misc")
            S.add("sp", lambda e: e.dma_start(out=bglut[:], in_=bglu[:, :]), w=["bglut"], dma="misc")
            S.add("pool", lambda e: e.dma_start(out=wuat[:], in_=wua.rearrange("(c p) n -> p c n", p=128)), w=["wuat"], dma="misc")
            S.add("pool", lambda e: e.dma_start(out=wubt[:], in_=wub.rearrange("(c p) n -> p c n", p=128)), w=["wubt"], dma="misc")
            S.add("pool", lambda e: e.dma_start(out=wglut[:], in_=wglu.rearrange("(c p) n -> p c n", p=128)), w=["wglut"], dma="misc")
            gwk = 0
            for tg in range(NG):
                c0 = tg * 512
                for ti in range(4):
                    tt = tg * 4 + ti
                    hk = ("hres", tt)
                    S.add("sp", lambda e, tt=tt: e.dma_start(out=hres[:, tt, :], in_=x[tt * 128:(tt + 1) * 128, :]),
                          w=[hk], dma="x")
                    _ln_tile(S, hres[:, tt, :], st6, mv, rstd, lnB[:, 0, :], lnB[:, 1, :], hk, "ln")
                    b = tt % 2
                    S.add("act", lambda e, tt=tt, b=b: e.copy(out=xb[b][:], in_=hres[:, tt, :]), r=[hk], w=[("xb", b)])

                    def tr(e, b=b):
                        ins = None
                        for kc in range(8):
                            ins = e.transpose(out=pT[b][:, kc * 128:(kc + 1) * 128], in_=xb[b][:, kc * 128:(kc + 1) * 128],
                                              identity=identb[:])
                        return ins
                    S.add("pe", tr, r=[("xb", b), "identb"], w=[("pT", b)])
                    S.add("act", lambda e, b=b, ti=ti: e.copy(out=hT[:, :, ti * 128:(ti + 1) * 128],
                                                              in_=pT[b][:, :].rearrange("p (k t) -> p k t", k=8)),
                          r=[("pT", b)], w=["hT"])
                if fused:
                    for which, dst, dkey in ((0, ya, "ya"), (1, ys32, "ys32")):
                        for q in range(4):
                            cb = (2 * tg + which + q) % 2
                            src = ygat[q].rearrange("(r w p) t -> p w r t", r=4, w=2, p=128)[:, which, :, c0:c0 + 512]
                            S.add("sp", lambda e, cb=cb, src=src: e.dma_start(out=cand[cb][:], in_=src), w=[("cand", cb)], dma="cand")
                            if q == 0:
                                S.add("dve", lambda e, cb=cb, dst=dst: e.tensor_scalar(out=dst[:], in0=cand[cb][:], scalar1=oht[:, 0:1], scalar2=None,
                                                                                       op0=ALU.mult), r=[("cand", cb), "oht"], w=[dkey])
                            else:
                                S.add("dve", lambda e, cb=cb, dst=dst, q=q: e.scalar_tensor_tensor(
                                    out=dst[:], in0=cand[cb][:], scalar=oht[:, q:q + 1], in1=dst[:], op0=ALU.mult, op1=ALU.add),
                                    r=[("cand", cb), "oht", dkey], w=[dkey])
                else:
                    S.add("pool", lambda e, c0=c0: e.dma_start(out=ya[:], in_=yaT.rearrange("(c p) t -> p c t", p=128)[:, :, c0:c0 + 512]),
                          w=["ya"], dma="yin")
                    S.add("sp", lambda e, c0=c0: e.dma_start(out=ys32[:], in_=ysT.rearrange("(c p) t -> p c t", p=128)[:, :, c0:c0 + 512]),
                          w=["ys32"], dma="yin")
                for c in range(4):
                    S.add("dve", lambda e, c=c: e.tensor_tensor(out=gtmp[:], in0=ys32[:, c, :], in1=ys32[:, c, :], op=ALU.mult), r=["ys32"], w=["gtmp"])
                    S.add("dve", lambda e: e.tensor_scalar(out=gtmp[:], in0=gtmp[:], scalar1=0.044715, scalar2=1.0,
                                                           op0=ALU.mult, op1=ALU.add), r=["gtmp"], w=["gtmp"])
                    S.add("dve", lambda e, c=c: e.tensor_tensor(out=gtmp[:], in0=gtmp[:], in1=ys32[:, c, :], op=ALU.mult), r=["gtmp", "ys32"], w=["gtmp"])
                    S.add("act", lambda e: e.activation(out=sgm[:], in_=gtmp[:], func=AF.Sigmoid, scale=1.5957691216), r=["gtmp"], w=["sgm"])
                    S.add("dve", lambda e, c=c: e.tensor_tensor(out=yb[:, c, :], in0=ys32[:, c, :], in1=sgm[:], op=ALU.mult), r=["ys32", "sgm"], w=["yb"])
                for cc in range(4):
                    pk = cc % 2

                    def mm(e, cc=cc, pk=pk):
                        ins = None
                        for c in range(4):
                            ins = e.matmul(pss[pk][:, :], wglut[:, c, cc * 128:(cc + 1) * 128], yb[:, c, :],
                                           start=(c == 0), stop=(c == 3))
                        return ins
                    S.add("pe", mm, r=["wglut", "yb"], w=[("ps", pk)])
                    S.add("act", lambda e, cc=cc, pk=pk: e.activation(out=sgm[:], in_=pss[pk][:, :], func=AF.Sigmoid,
                                                                      bias=bglut[:, cc:cc + 1], scale=1.0),
                          r=[("ps", pk), "bglut"], w=["sgm"])
                    S.add("dve", lambda e, cc=cc: e.tensor_tensor(out=yb2[:, cc, :], in0=yb[:, cc, :], in1=sgm[:], op=ALU.mult),
                          r=["yb", "sgm"], w=["yb2"])
                for j in range(4):
                    slot = gwk % 3
                    gwk += 1
                    S.add("pool", lambda e, j=j, slot=slot: e.dma_start(
                        out=gw[slot][:], in_=wg.rearrange("(k p) n -> p k n", p=128)[:, :, j * 512:(j + 1) * 512]),
                        w=[("gw", slot)], dma="gw")
                    for dcl in range(2):
                        dc = 2 * j + dcl
                        tb = dc % 2

                        def mmg(e, slot=slot, dcl=dcl, off=0, pk=0):
                            ins = None
                            for kc in range(8):
                                ins = e.matmul(pss[pk][:, :], gw[slot][:, kc, dcl * 256 + off:dcl * 256 + off + 128], hT[:, kc, :],
                                               start=(kc == 0), stop=(kc == 7))
                            return ins
                        S.add("pe", lambda e, f=mmg: f(e, off=0, pk=0), r=[("gw", slot), "hT"], w=[("ps", 0)])
                        S.add("pe", lambda e, f=mmg: f(e, off=128, pk=1), r=[("gw", slot), "hT"], w=[("ps", 1)])

                        def mmu(e, dc=dc, wt=None, src=None, pk=2):
                            ins = None
                            for c in range(4):
                                ins = e.matmul(pss[pk][:, :], wt[:, c, dc * 128:(dc + 1) * 128], src[:, c, :],
                                               start=(c == 0), stop=(c == 3))
                            return ins
                        S.add("pe", lambda e, f=mmu: f(e, wt=wuat, src=ya, pk=2), r=["wuat", "ya"], w=[("ps", 2)])
                        S.add("pe", lambda e, f=mmu: f(e, wt=wubt, src=yb2, pk=3), r=["wubt", "yb2"], w=[("ps", 3)])
                        S.add("act", lambda e, tb=tb: e.activation(out=gat[tb][:], in_=pss[0][:, :], func=AF.Sigmoid),
                              r=[("ps", 0)], w=[("gat", tb)])
                        S.add("act", lambda e, tb=tb: e.activation(out=gbt[tb][:], in_=pss[1][:, :], func=AF.Sigmoid),
                              r=[("ps", 1)], w=[("gbt", tb)])
                        S.add("dve", lambda e, tb=tb: e.tensor_tensor(out=gat[tb][:], in0=gat[tb][:], in1=pss[2][:, :], op=ALU.mult),
                              r=[("gat", tb), ("ps", 2)], w=[("gat", tb)])
                        S.add("dve", lambda e, tb=tb: e.tensor_tensor(out=gbt[tb][:], in0=gbt[tb][:], in1=pss[3][:, :], op=ALU.mult),
                              r=[("gbt", tb), ("ps", 3)], w=[("gbt", tb)])
                        S.add("dve", lambda e, tb=tb, dc=dc: e.tensor_tensor(out=merged[:, dc, :], in0=gat[tb][:], in1=gbt[tb][:], op=ALU.add),
                              r=[("gat", tb), ("gbt", tb)], w=["merged"])
                for half in range(2):
                    slot = gwk % 3
                    gwk += 1
                    S.add("pool", lambda e, half=half, slot=slot: e.dma_start(
                        out=gw[slot][:], in_=wo.rearrange("(k p) n -> p k n", p=128)[:, :, half * 512:(half + 1) * 512]),
                        w=[("gw", slot)], dma="gw")
                    for ti in range(4):
                        tt = tg * 4 + ti
                        pk = 4 + (ti % 2)

                        def mmo(e, slot=slot, ti=ti, pk=pk):
                            ins = None
                            for kc in range(8):
                                ins = e.matmul(pss[pk][:, :], merged[:, kc, ti * 128:(ti + 1) * 128], gw[slot][:, kc, :],
                                               start=(kc == 0), stop=(kc == 7))
                            return ins
                        S.add("pe", mmo, r=["merged", ("gw", slot)], w=[("ps", pk)])
                        S.add("dve", lambda e, tt=tt, half=half, pk=pk: e.scalar_tensor_tensor(
                            out=hres[:, tt, half * 512:(half + 1) * 512], in0=hres[:, tt, half * 512:(half + 1) * 512],
                            scalar=DN_ALPHA, in1=pss[pk][:, :], op0=ALU.mult, op1=ALU.add),
                            r=[("hres", tt), ("ps", pk)], w=[("hres", tt)])
                for ti in range(4):
                    tt = tg * 4 + ti
                    _ln_tile(S, hres[:, tt, :], st6, mv, rstd, lnB[:, 2, :], lnB[:, 3, :], ("hres", tt), "ln")
            S.emit()

        x1T = sb("x1T", [128, 8, NTOK], BF16)
        with ExitStack() as st2:
            def sb2(name, shape, dtype):
                return st2.enter_context(nc.sbuf_tensor("b_" + name, shape, dtype))
            xb = [sb2(f"xc{i}", [128, 1024], BF16) for i in range(2)]
            wrt = sb2("wrt", [128, 8, 32], BF16)
            brB = sb2("brB", [128, 32], F32)
            bdn = sb2("bdn", [N_EXPERTS, 1024], BF16)
            lg = sb2("lg", [128, 32], F32)
            ex = sb2("ex", [128, 32], F32)
            msk = sb2("msk", [128, 32], F32)
            m8 = sb2("m8", [128, 8], F32)
            negm = sb2("negm", [128, 1], F32)
            ssum = sb2("ssum", [128, 1], F32)
            combTb = sb2("combTb", [32, 128], BF16)
            S.add("pool", lambda e: e.dma_start(out=wrt[:], in_=wr.rearrange("(k p) n -> p k n", p=128)), w=["wrt"], dma="misc")
            S.add("pool", lambda e: e.dma_start(out=bdn[:], in_=bdown[:, :]), w=["bdn"], dma="misc")
            S.add("sp", lambda e: e.dma_start(out=brB[:], in_=br[0:1, :].partition_broadcast(128)), w=["brB"], dma="misc")
            for tt in range(NT):
                b = tt % 2
                hk = ("hres", tt)
                S.add("act", lambda e, tt=tt, b=b: e.copy(out=xb[b][:], in_=hres[:, tt, :]), r=[hk], w=[("xb", b)])

                def tr(e, b=b):
                    ins = None
                    for kc in range(8):
                        ins = e.transpose(out=pT[b][:, kc * 128:(kc + 1) * 128], in_=xb[b][:, kc * 128:(kc + 1) * 128],
                                          identity=identb[:])
                    return ins
                S.add("pe", tr, r=[("xb", b)], w=[("pT", b)])
                S.add("act", lambda e, b=b, tt=tt: e.copy(out=x1T[:, :, tt * 128:(tt + 1) * 128],
                                                          in_=pT[b][:, :].rearrange("p (k t) -> p k t", k=8)),
                      r=[("pT", b)], w=[("x1T", tt)])

                def mmr(e, tt=tt):
                    ins = None
                    for kc in range(8):
                        ins = e.matmul(pss[0][:, 0:32], x1T[:, kc, tt * 128:(tt + 1) * 128], wrt[:, kc, :],
                                       start=(kc == 0), stop=(kc == 7))
                    return ins
                S.add("pe", mmr, r=[("x1T", tt), "wrt"], w=[("ps", 0)])
                S.add("dve", lambda e: e.tensor_tensor(out=lg[:], in0=pss[0][:, 0:32], in1=brB[:], op=ALU.add),
                      r=[("ps", 0), "brB"], w=["lg"])
                S.add("dve", lambda e: e.max(out=m8[:], in_=lg[:]), r=["lg"], w=["m8"])
                S.add("dve", lambda e: e.tensor_scalar(out=msk[:], in0=lg[:], scalar1=m8[:, 3:4], scalar2=None, op0=ALU.is_ge),
                      r=["lg", "m8"], w=["msk"])
                S.add("dve", lambda e: e.tensor_scalar(out=negm[:], in0=m8[:, 0:1], scalar1=-1.0, scalar2=None, op0=ALU.mult),
                      r=["m8"], w=["negm"])
                S.add("act", lambda e: e.activation(out=ex[:], in_=lg[:], func=AF.Exp, bias=negm[:, 0:1], scale=1.0),
                      r=["lg", "negm"], w=["ex"])
                S.add("dve", lambda e: e.tensor_tensor(out=ex[:], in0=ex[:], in1=msk[:], op=ALU.mult), r=["ex", "msk"], w=["ex"])
                S.add("dve", lambda e: e.reduce_sum(out=ssum[:], in_=ex[:], axis=mybir.AxisListType.X), r=["ex"], w=["ssum"])
                S.add("dve", lambda e: e.reciprocal(out=ssum[:], in_=ssum[:]), r=["ssum"], w=["ssum"])
                S.add("dve", lambda e, tt=tt: e.tensor_scalar(out=comb[:, tt, :], in0=ex[:], scalar1=ssum[:, 0:1], scalar2=None, op0=ALU.mult),
                      r=["ex", "ssum"], w=[("comb", tt)])
                S.add("pe", lambda e, tt=tt: e.transpose(out=pss[1][0:32, 0:128], in_=comb[:, tt, :], identity=ident32[:]),
                      r=[("comb", tt)], w=[("ps", 1)])
                S.add("act", lambda e: e.copy(out=combTb[:], in_=pss[1][0:32, 0:128]), r=[("ps", 1)], w=["combTb"])
                for half in range(2):
                    pk = 2 + half
                    S.add("pe", lambda e, half=half, pk=pk: e.matmul(pss[pk][:, :], combTb[:, :], bdn[:, half * 512:(half + 1) * 512],
                                                                     start=True, stop=True),
                          r=["combTb", "bdn"], w=[("ps", pk)])
                    S.add("dve", lambda e, tt=tt, half=half, pk=pk: e.scalar_tensor_tensor(
                        out=hres[:, tt, half * 512:(half + 1) * 512], in0=hres[:, tt, half * 512:(half + 1) * 512],
                        scalar=DN_ALPHA, in1=pss[pk][:, :], op0=ALU.mult, op1=ALU.add),
                        r=[hk, ("ps", pk)], w=[hk])
            S.emit()

        with ExitStack() as st2:
            def sb2(name, shape, dtype):
                return st2.enter_context(nc.sbuf_tensor("b_" + name, shape, dtype))
            R = 8
            ring = [sb2(f"ring{i}", [128, 8, 256], BF16) for i in range(R)]
            actT = sb2("actT", [128, 8, NTOK], BF16)
            bgut = sb2("bgut", [128, 2 * NE * 8], F32)
            gs = [sb2(f"gs{i}", [128, 512], F32) for i in range(2)]
            sg = [sb2(f"sg{i}", [128, 512], F32) for i in range(2)]
            uu = [sb2(f"uu{i}", [128, 512], F32) for i in range(2)]
            S.add("sp", lambda e: e.dma_start(out=bgut[:], in_=bgu[:, :]), w=["bgut"], dma="misc")
            rk = 0

            def load(wsrc, e_, q):
                nonlocal rk
                slot = rk % R
                rk += 1
                S.add("pool", lambda e, slot=slot: e.dma_start(
                    out=ring[slot][:], in_=wsrc[e_].rearrange("(k p) n -> p k n", p=128)[:, :, q * 256:(q + 1) * 256]),
                    w=[("ring", slot)], dma="wr")
                return slot
            tcount = 0
            for ex_ in range(NE):
                for q in range(4):
                    sg_ = load(wgate, ex_, q)
                    su_ = load(wup, ex_, q)
                    for fcl in range(2):
                        fc = 2 * q + fcl
                        bgcol = (0 * NE + ex_) * 8 + fc
                        bucol = (1 * NE + ex_) * 8 + fc
                        for tg in range(NG):
                            tb = tcount % 2
                            tcount += 1
                            pg, pu = tb, 2 + tb

                            def mmgu(e, slot=None, pk=None, fcl=fcl, tg=tg):
                                ins = None
                                for kc in range(8):
                                    ins = e.matmul(pss[pk][:, :], ring[slot][:, kc, fcl * 128:(fcl + 1) * 128],
                                                   x1T[:, kc, tg * 512:(tg + 1) * 512], start=(kc == 0), stop=(kc == 7))
                                return ins
                            S.add("pe", lambda e, f=mmgu, s=sg_, pk=pg: f(e, slot=s, pk=pk), r=[("ring", sg_)], w=[("ps", pg)])
                            S.add("pe", lambda e, f=mmgu, s=su_, pk=pu: f(e, slot=s, pk=pk), r=[("ring", su_)], w=[("ps", pu)])
                            S.add("dve", lambda e, tb=tb, pg=pg, c=bgcol: e.tensor_scalar(
                                out=gs[tb][:], in0=pss[pg][:, :], scalar1=bgut[:, c:c + 1], scalar2=SW_LIMIT, op0=ALU.add, op1=ALU.min),
                                r=[("ps", pg), "bgut"], w=[("gs", tb)])
                            S.add("act", lambda e, tb=tb: e.activation(out=sg[tb][:], in_=gs[tb][:], func=AF.Sigmoid, scale=SW_ALPHA),
                                  r=[("gs", tb)], w=[("sg", tb)])
                            S.add("dve", lambda e, tb=tb, pu=pu, c=bucol: e.tensor_scalar(
                                out=uu[tb][:], in0=pss[pu][:, :], scalar1=bgut[:, c:c + 1], scalar2=SW_LIMIT, op0=ALU.add, op1=ALU.min),
                                r=[("ps", pu), "bgut"], w=[("uu", tb)])
                            S.add("dve", lambda e, tb=tb: e.tensor_scalar(
                                out=uu[tb][:], in0=uu[tb][:], scalar1=-SW_LIMIT, scalar2=1.0, op0=ALU.max, op1=ALU.add),
                                r=[("uu", tb)], w=[("uu", tb)])
                            S.add("dve", lambda e, tb=tb: e.tensor_tensor(out=gs[tb][:], in0=gs[tb][:], in1=sg[tb][:], op=ALU.mult),
                                  r=[("gs", tb), ("sg", tb)], w=[("gs", tb)])
                            S.add("dve", lambda e, tb=tb, fc=fc, tg=tg: e.tensor_tensor(
                                out=actT[:, fc, tg * 512:(tg + 1) * 512], in0=gs[tb][:], in1=uu[tb][:], op=ALU.mult),
                                r=[("gs", tb), ("uu", tb)], w=[("actT", tg)])
                for q in range(4):
                    sd_ = load(wdown, ex_, q)
                    for tt in range(NT):
                        pk = 4 + (tt % 2)

                        def mmd(e, slot=sd_, tt=tt, pk=pk):
                            ins = None
                            for fc in range(8):
                                ins = e.matmul(pss[pk][:, 0:256], actT[:, fc, tt * 128:(tt + 1) * 128], ring[slot][:, fc, :],
                                               start=(fc == 0), stop=(fc == 7))
                            return ins
                        S.add("pe", mmd, r=[("ring", sd_), ("actT", tt // 4)], w=[("ps", pk)])
                        S.add("dve", lambda e, tt=tt, q=q, pk=pk, ex_=ex_: e.scalar_tensor_tensor(
                            out=hres[:, tt, q * 256:(q + 1) * 256], in0=pss[pk][:, 0:256], scalar=comb[:, tt, ex_:ex_ + 1],
                            in1=hres[:, tt, q * 256:(q + 1) * 256], op0=ALU.mult, op1=ALU.add),
                            r=[("ps", pk), ("hres", tt)], w=[("hres", tt)])
            S.emit()

        for i in range(2):
            S.add("sp", lambda e, i=i: e.dma_start(out=lnB[:, i, :], in_=lnp[4 + i:5 + i, :].partition_broadcast(128)),
                  w=["lnB"], dma="misc")
        for tt in range(NT):
            _ln_tile(S, hres[:, tt, :], st6, mv, rstd, lnB[:, 0, :], lnB[:, 1, :], ("hres", tt), "ln")
            S.add("sp", lambda e, tt=tt: e.dma_start(out=out[tt * 128:(tt + 1) * 128, :], in_=hres[:, tt, :]),
                  r=[("hres", tt)], dma="out")
        S.emit()
    return nc


def build_p1(NX=8192, do_attn=True, do_ssm=True, dbg=(), ctx=None):
    NXT = NX // 128
    NQG = NX // 512
    NTOKS = N_META + NX
    NCH = NTOKS // 8
    NKT = NXT + 1
    fused = ctx is not None
    nc = ctx["nc"] if fused else bass.Bass("TRN2", target_bir_lowering=False)
    ODT = BF16 if fused else F32
    dbgs = set(dbg) if isinstance(dbg, (tuple, list, set)) else {dbg}

    def din(name, shape, dtype=F32):
        return nc.dram_tensor(name, shape, dtype, kind="ExternalInput").ap()
    x = din("x", [NX, 1024])
    meta = din("meta", [N_META, 1024])
    lnp = din("lnp", [128, 16])
    lnrow = din("lnrow", [2, 1024])
    w1 = din("w1", [1024, 514])
    bfn = din("bf", [2, 1])
    lamp = din("lamp", [128, 16])
    logdt = din("logdt", [1, 8])
    bstk_d = din("bstk", [128, 128])
    cstk_d = din("cstk", [128, 128])
    dvec_d = din("dvec", [128, 1])
    cst = din("cst", [128, 128 * 4 + 8 * 128 + 4])
    if fused:
        yatt = yssm = None
    else:
        yatt = nc.dram_tensor("yatt", [128, NX], F32, kind="ExternalOutput").ap()
        yssm = nc.dram_tensor("yssm", [128, NX], F32, kind="ExternalOutput").ap()
    NC_ = 128 * 4 + 8 * 128 + 4
    CH = NX // 4

    def ydst(which, r0, r1, t0, n):
        if fused:
            k = t0 // CH
            assert (t0 + n - 1) // CH == k
            return ctx["ybuf"][k][which * 128 + r0:which * 128 + r1, t0 - k * CH:t0 - k * CH + n]
        return (yatt if which == 0 else yssm)[r0:r1, t0:t0 + n]

    with ExitStack() as stack:
        S = ctx["S"] if fused else Sched(nc, stack)

        def sb(name, shape, dtype):
            return stack.enter_context(nc.sbuf_tensor(name, shape, dtype))

        def psum(name, shape, dtype):
            return stack.enter_context(nc.psum_tensor(name, shape, dtype))
        QT = [sb(f"QT{h}", [65, NX], BF16) for h in range(2)]
        KT = [sb(f"KT{h}", [65, NTOKS], BF16) for h in range(2)]
        VV = [sb(f"VV{h}", [128, NKT, 128], BF16) for h in range(2)]
        uT = sb("uT", [128, NTOKS], BF16)
        NFcol = sb("NFcol", [128, NKT, 2], F32)
        cst32 = sb("cst32", [128, NC_], F32)
        identb = sb("identb", [128, 128], BF16)
        trib = sb("trib", [128, 128], BF16)
        ident32 = cst32[:, 0:128]
        Jm = cst32[:, 128:256]
        tri32 = cst32[:, 256:384]
        bmask = cst32[:, 512:512 + 1024]
        sgnR = cst32[:, 1536:1537]
        pss = [psum(f"ps{i}", [128, 512], F32) for i in range(6)]
        pT = [psum(f"pT{i}", [128, 1024], BF16) for i in range(2)]
        S.stream("x", 2)
        S.stream("misc", 0)
        S.stream("row", 4)
        S.stream("out", 2)
        S.stream("rl", 2)

        with ExitStack() as st2:
            def sb2(name, shape, dtype):
                return st2.enter_context(nc.sbuf_tensor(name, shape, dtype))
            w1t = sb2("w1t", [128, 8, 576], BF16)
            lnpt = sb2("lnpt", [128, 16], F32)
            lnB = sb2("lnB1", [128, 2, 1024], F32)
            xt = [sb2(f"xt{i}", [128, 1024], F32) for i in range(2)]
            xn = [sb2(f"xn{i}", [128, 1024], BF16) for i in range(2)]
            hT = sb2("hT", [128, 8, 512], BF16)
            st6 = sb2("st6", [128, 2, 6], F32)
            mv = sb2("mv", [128, 2], F32)
            rstd = sb2("rstd", [128, 1], F32)
            negbf = sb2("negbf", [2, 1], F32)
            e1 = sb2("e1", [2, 512], F32)
            nfg = sb2("nfg", [2, 512], F32)
            carry = sb2("carry", [2, 1], F32)
            ones2 = sb2("ones2", [2, 512], F32)
            fb = sb2("fb", [2, 512], BF16)

            S.add("sp", lambda e: e.dma_start(out=cst32[:], in_=cst[:, :]), w=["cst"], dma="misc")
            S.add("sp", lambda e: e.dma_start(out=lnpt[:], in_=lnp[:, :]), w=["lnpt"], dma="misc")
            for i in range(2):
                S.add("sp", lambda e, i=i: e.dma_start(out=lnB[:, i, :], in_=lnrow[i:i + 1, :].partition_broadcast(128)), w=["lnB"], dma="misc")
            S.add("sp", lambda e: e.dma_start(out=negbf[:], in_=bfn[:, :]), w=["negbf"], dma="misc")
            S.add("dve", lambda e: e.memset(w1t[:, :, 512:576], 0.0), w=["w1t"])
            S.add("pool", lambda e: e.dma_start(out=w1t[:, :, 0:514], in_=w1.rearrange("(k p) n -> p k n", p=128)), w=["w1t"], dma="misc")
            S.add("dve", lambda e: e.tensor_copy(out=identb[:], in_=ident32), r=["cst"], w=["identb"])
            S.add("dve", lambda e: e.tensor_copy(out=trib[:], in_=tri32), r=["cst"], w=["trib"])
            S.add("dve", lambda e: e.tensor_scalar(out=negbf[:], in0=negbf[:], scalar1=-1.0, scalar2=None, op0=ALU.mult),
                  r=["negbf"], w=["negbf"])
            S.add("dve", lambda e: e.memset(ones2[:], 1.0), w=["ones2"])
            S.add("dve", lambda e: e.memset(carry[:], 0.0), w=["carry"])
            for h in range(2):
                S.add("dve", lambda e, h=h: e.memset(KT[h][64:65, :], 1.0), w=[("KTa", h)])
                S.add("pool", lambda e, h=h: e.memset(VV[h][:, :, 64:128], 1.0), w=[("VVo", h)])

            for g in range(NQG + 1):
                n = N_META if g == 0 else 512
                ntile = 1 if g == 0 else 4
                kc0 = 0 if g == 0 else N_META + (g - 1) * 512
                qc0 = (g - 1) * 512
                for ti in range(ntile):
                    np_ = N_META if g == 0 else 128
                    b = (g * 4 + ti) % 2
                    if g == 0:
                        S.add("sp", lambda e, b=b: e.dma_start(out=xt[b][0:N_META, :], in_=meta[:, :]), w=[("xt", b)], dma="x")
                    else:
                        r0 = (g - 1) * 512 + ti * 128
                        S.add("sp", lambda e, b=b, r0=r0: e.dma_start(out=xt[b][:, :], in_=x[r0:r0 + 128, :]), w=[("xt", b)], dma="x")
                    xa = xt[b][0:np_, :]
                    S.add("dve", lambda e, xa=xa, np_=np_: e.bn_stats(out=st6[0:np_, 0, :], in_=xa[:, 0:512]), r=[("xt", b)], w=["st0"])
                    S.add("dve", lambda e, xa=xa, np_=np_: e.bn_stats(out=st6[0:np_, 1, :], in_=xa[:, 512:1024]), r=[("xt", b)], w=["st1"])
                    S.add("dve", lambda e, np_=np_: e.bn_aggr(out=mv[0:np_, :], in_=st6[0:np_, :, :]), r=["st0", "st1"], w=["mv"], fence=True)
                    S.add("dve", lambda e, np_=np_: e.tensor_scalar(out=rstd[0:np_, :], in0=mv[0:np_, 1:2], scalar1=LN_EPS, scalar2=None,
                                                                    op0=ALU.add), r=["mv"], w=["rs"], fence=True)
                    S.add("act", lambda e, np_=np_: e.sqrt(out=rstd[0:np_, :], in_=rstd[0:np_, :]), r=["rs"], w=["rs"], fence=True)
                    S.add("dve", lambda e, np_=np_: e.reciprocal(out=rstd[0:np_, :], in_=rstd[0:np_, :]), r=["rs"], w=["rs"], fence=True)
                    S.add("dve", lambda e, xa=xa, b=b, np_=np_: e.tensor_scalar(
                        out=xa, in0=xa, scalar1=mv[0:np_, 0:1], scalar2=rstd[0:np_, 0:1],
                        op0=ALU.subtract, op1=ALU.mult), r=[("xt", b), "mv", "rs"], w=[("xt", b)])
                    S.add("dve", lambda e, xa=xa, np_=np_: e.tensor_tensor(out=xa, in0=xa, in1=lnB[0:np_, 0, :], op=ALU.mult),
                          r=[("xt", b), "lnB"], w=[("xt", b)])
                    S.add("dve", lambda e, xa=xa, np_=np_: e.tensor_tensor(out=xa, in0=xa, in1=lnB[0:np_, 1, :], op=ALU.add),
                          r=[("xt", b), "lnB"], w=[("xt", b)])
                    S.add("act", lambda e, xa=xa, b=b, np_=np_: e.copy(out=xn[b][0:np_, :], in_=xa), r=[("xt", b)], w=[("xn", b)])

                    def tr(e, b=b, np_=np_):
                        ins = None
                        for kc in range(8):
                            ins = e.transpose(out=pT[b][:, kc * 128:kc * 128 + np_], in_=xn[b][0:np_, kc * 128:(kc + 1) * 128],
                                              identity=identb[0:np_, 0:np_])
                        return ins
                    S.add("pe", tr, r=[("xn", b), "identb"], w=[("pT", b)])
                    S.add("act", lambda e, b=b, ti=ti, np_=np_: e.copy(
                        out=hT[:, :, ti * 128:ti * 128 + np_], in_=pT[b][:, :].rearrange("p (k t) -> p k t", k=8)[:, :, 0:np_]),
                        r=[("pT", b)], w=["hT"])

                if 8 in dbgs:
                    S.emit()
                def proj(e, c_lo, c_hi, pk, n=n):
                    ins = None
                    for kc in range(8):
                        ins = e.matmul(pss[pk][0:c_hi - c_lo, 0:n], w1t[:, kc, c_lo:c_hi], hT[:, kc, 0:n],
                                       start=(kc == 0), stop=(kc == 7))
                    return ins
                for h in range(2):
                    if g > 0:
                        S.add("pe", lambda e, f=proj, h=h: f(e, h * 64, h * 64 + 64, h), r=["w1t", "hT"], w=[("ps", h)])
                        S.add("act", lambda e, h=h, qc0=qc0: e.mul(out=QT[h][0:64, qc0:qc0 + 512], in_=pss[h][0:64, :], mul=0.125),
                              r=[("ps", h)], w=[("QT", h)])
                    S.add("pe", lambda e, f=proj, h=h: f(e, 128 + h * 64, 128 + h * 64 + 64, 2 + h), r=["w1t", "hT"], w=[("ps", 2 + h)])
                    S.add("dve", lambda e, h=h, kc0=kc0, n=n: e.tensor_copy(out=KT[h][0:64, kc0:kc0 + n], in_=pss[2 + h][0:64, 0:n]),
                          r=[("ps", 2 + h)], w=[("KT", h)])
                S.add("pe", lambda e, f=proj: f(e, 384, 512, 4), r=["w1t", "hT"], w=[("ps", 4)])
                S.add("act", lambda e, kc0=kc0, n=n: e.copy(out=uT[:, kc0:kc0 + n], in_=pss[4][:, 0:n]), r=[("ps", 4)], w=["uT"])
                if 1 in dbgs:
                    continue
                S.add("pe", lambda e, f=proj: f(e, 512, 576, 5), r=["w1t", "hT"], w=[("ps", 5)])
                S.add("act", lambda e, n=n: e.activation(out=e1[:, 0:n], in_=pss[5][0:2, 0:n], func=AF.Exp, bias=negbf[:, 0:1], scale=-1.0),
                      r=[("ps", 5), "negbf"], w=["e1"])
                if 11 in dbgs:
                    continue
                S.add("dve", lambda e, n=n: e.tensor_scalar(out=e1[:, 0:n], in0=e1[:, 0:n], scalar1=1.0, scalar2=None, op0=ALU.add),
                      r=["e1"], w=["e1"])
                S.add("act", lambda e, n=n: e.activation(out=e1[:, 0:n], in_=e1[:, 0:n], func=AF.Ln), r=["e1"], w=["e1"])
                if 12 in dbgs:
                    continue
                S.add("dve", lambda e, n=n: e.tensor_tensor_scan(out=nfg[:, 0:n], data0=ones2[:, 0:n], data1=e1[:, 0:n], initial=carry[:, 0:1],
                                                                 op0=ALU.mult, op1=ALU.add), r=["e1", "carry", "ones2"], w=["nfg"])
                if 13 in dbgs:
                    continue
                S.add("dve", lambda e, n=n: e.tensor_copy(out=carry[:], in_=nfg[:, n - 1:n]), r=["nfg"], w=["carry"])
                if 2 in dbgs:
                    continue
                if g > 0:
                    S.add("dve", lambda e: e.tensor_scalar(out=fb[:], in0=nfg[:], scalar1=-1.0, scalar2=None, op0=ALU.mult),
                          r=["nfg"], w=["fb"])
                    for h in range(2):
                        S.add("sp", lambda e, h=h, qc0=qc0: e.dma_start(out=QT[h][64:65, qc0:qc0 + 512], in_=fb[h:h + 1, :]),
                              r=["fb"], w=[("QTa", h)], dma="row")
                if 3 in dbgs:
                    continue
                for ti in range(ntile):
                    np_ = N_META if g == 0 else 128
                    kt = 0 if g == 0 else 1 + (g - 1) * 4 + ti
                    S.add("pe", lambda e, ti=ti, np_=np_: e.transpose(out=pss[5][0:np_, 256 + 2 * ti:258 + 2 * ti],
                                                                      in_=nfg[0:2, ti * 128:ti * 128 + np_], identity=ident32[0:2, 0:2]),
                          r=["nfg"], w=[("ps", 5)])
                    S.add("dve", lambda e, ti=ti, np_=np_, kt=kt: e.tensor_copy(out=NFcol[0:np_, kt, :], in_=pss[5][0:np_, 256 + 2 * ti:258 + 2 * ti]),
                          r=[("ps", 5)], w=["NFcol"])
                if 4 in dbgs:
                    continue
                for ti in range(ntile):
                    np_ = N_META if g == 0 else 128
                    kt = 0 if g == 0 else 1 + (g - 1) * 4 + ti
                    pk = ti % 2

                    def mmv(e, ti=ti, np_=np_, pk=pk):
                        ins = None
                        for kc in range(8):
                            ins = e.matmul(pss[pk][0:np_, 0:128], hT[:, kc, ti * 128:ti * 128 + np_], w1t[:, kc, 256:384],
                                           start=(kc == 0), stop=(kc == 7))
                        return ins
                    S.add("pe", mmv, r=["w1t", "hT"], w=[("ps", pk)])
                    if 5 in dbgs:
                        continue
                    S.add("act", lambda e, np_=np_, kt=kt, pk=pk: e.copy(out=VV[0][0:np_, kt, 0:64], in_=pss[pk][0:np_, 0:64]),
                          r=[("ps", pk), ("VVo", 0)], w=[("VV", 0)])
                    if 6 in dbgs:
                        continue
                    S.add("act", lambda e, np_=np_, kt=kt, pk=pk: e.copy(out=VV[1][0:np_, kt, 0:64], in_=pss[pk][0:np_, 64:128]),
                          r=[("ps", pk), ("VVo", 1)], w=[("VV", 1)])
            if 9 in dbgs:
                d1 = nc.dram_tensor("d_nf", [128, NKT * 2], F32, kind="ExternalOutput").ap()
                d2 = nc.dram_tensor("d_qt", [65, NX], BF16, kind="ExternalOutput").ap()
                d3 = nc.dram_tensor("d_kt", [65, NTOKS], BF16, kind="ExternalOutput").ap()
                d4 = nc.dram_tensor("d_vv", [128, NKT * 128], BF16, kind="ExternalOutput").ap()
                d5 = nc.dram_tensor("d_ut", [128, NTOKS], BF16, kind="ExternalOutput").ap()
                S.add("sp", lambda e: e.dma_start(out=d1[:, :], in_=NFcol[:, :, :].rearrange("p a b -> p (a b)")), r=["NFcol"], dma="out")
                S.add("sp", lambda e: e.dma_start(out=d2[:, :], in_=QT[0][:, :]), r=[("QT", 0), ("QTa", 0)], dma="out")
                S.add("sp", lambda e: e.dma_start(out=d3[:, :], in_=KT[0][:, :]), r=[("KT", 0), ("KTa", 0)], dma="out")
                S.add("sp", lambda e: e.dma_start(out=d4[:, :], in_=VV[0][:, :, :].rearrange("p a b -> p (a b)")), r=[("VV", 0), ("VVo", 0)], dma="out")
                S.add("sp", lambda e: e.dma_start(out=d5[:, :], in_=uT[:, :]), r=["uT"], dma="out")
                d6 = nc.dram_tensor("d_xt", [128, 1024], F32, kind="ExternalOutput").ap()
                d7 = nc.dram_tensor("d_xn", [128, 1024], BF16, kind="ExternalOutput").ap()
                d8 = nc.dram_tensor("d_ht", [128, 8 * 512], BF16, kind="ExternalOutput").ap()
                d9 = nc.dram_tensor("d_mv", [128, 3], F32, kind="ExternalOutput").ap()
                S.add("sp", lambda e: e.dma_start(out=d6[:, :], in_=xt[1][:, :]), r=[("xt", 1)], dma="out")
                S.add("sp", lambda e: e.dma_start(out=d7[:, :], in_=xn[1][:, :]), r=[("xn", 1)], dma="out")
                S.add("sp", lambda e: e.dma_start(out=d8[:, :], in_=hT[:, :, :].rearrange("p a b -> p (a b)")), r=["hT"], dma="out")
                S.add("sp", lambda e: e.dma_start(out=d9[:, 0:2], in_=mv[:, :], allow_slow_non_contiguous=True), r=["mv"], dma="out")
                S.add("sp", lambda e: e.dma_start(out=d9[:, 2:3], in_=rstd[:, :], allow_slow_non_contiguous=True), r=["rs"], dma="out")
            S.emit()

        if do_ssm:
            _ssm_block(nc, S, stack, locals())
        if do_attn:
            with ExitStack() as st2:
                def sb2(name, shape, dtype):
                    return st2.enter_context(nc.sbuf_tensor(name, shape, dtype))
                NPT = 3
                pt = [sb2(f"pt{i}", [128, 512], BF16) for i in range(NPT)]
                rl = [sb2(f"rl{i}", [128, 512], F32) for i in range(2)]
                rl2 = [sb2(f"rlb{i}", [64, 512], F32) for i in range(2)]
                ot = [sb2(f"ot{i}", [64, 512], ODT) for i in range(2)]
                it = 0
                gi = 0
                for h in range(2):
                    for i in range(NQG):
                        ab = 4 + (gi % 2)
                        ob = gi % 2
                        gi += 1
                        nblk = 4 * i + 5
                        blks = []
                        for jb in range(nblk):
                            nk = N_META if jb == 0 else 128
                            kcol = 0 if jb == 0 else N_META + (jb - 1) * 128
                            jt = jb - 1
                            diag = jb > 0 and jt >= 4 * i
                            qs = (jt - 4 * i) * 128 if diag else 0
                            blks.append(dict(jb=jb, nk=nk, kcol=kcol, qs=qs, diag=diag, sbk=it % 2, pb=it % NPT))
                            it += 1

                        def emit_s(bk, h=h, i=i):
                            nk, kcol, qs, sbk, pb, jb = bk["nk"], bk["kcol"], bk["qs"], bk["sbk"], bk["pb"], bk["jb"]
                            S.add("pe", lambda e: e.matmul(
                                pss[sbk][0:nk, qs:512], KT[h][0:65, kcol:kcol + nk], QT[h][0:65, i * 512 + qs:(i + 1) * 512],
                                start=True, stop=True), w=[("ps", sbk)])
                            S.add("act", lambda e: e.activation(
                                out=pt[pb][0:nk, qs:512], in_=pss[sbk][0:nk, qs:512], func=AF.Exp,
                                bias=NFcol[0:nk, jb, h:h + 1], scale=1.0), r=[("ps", sbk)], w=[("pt", pb)])
                            if bk["diag"]:
                                S.add("dve", lambda e: e.tensor_tensor(
                                    out=pt[pb][:, qs:qs + 128], in0=pt[pb][:, qs:qs + 128], in1=trib[:], op=ALU.mult),
                                    r=[("pt", pb)], w=[("pt", pb)])

                        def emit_pv(bk, h=h, ab=ab, nblk=nblk):
                            nk, qs, pb, jb = bk["nk"], bk["qs"], bk["pb"], bk["jb"]
                            S.add("pe", lambda e: e.matmul(
                                pss[ab][:, qs:512], VV[h][0:nk, jb, :], pt[pb][0:nk, qs:512],
                                start=(jb == 0), stop=(jb == nblk - 1)), r=[("pt", pb)], w=[("ps", ab)])
                        emit_s(blks[0])
                        for jb in range(nblk):
                            if jb + 1 < nblk:
                                emit_s(blks[jb + 1])
                            emit_pv(blks[jb])
                        S.add("dve", lambda e, ab=ab, ob=ob: e.reciprocal(out=rl[ob][64:128, :], in_=pss[ab][64:128, :]),
                              r=[("ps", ab)], w=[("rl", ob)])
                        S.add("sp", lambda e, ob=ob: e.dma_start(out=rl2[ob][:, :], in_=rl[ob][64:128, :]),
                              r=[("rl", ob)], w=[("rl2", ob)], dma="rl")
                        S.add("dve", lambda e, ab=ab, ob=ob: e.tensor_tensor(out=ot[ob][:, :], in0=pss[ab][0:64, :], in1=rl2[ob][:, :], op=ALU.mult),
                              r=[("ps", ab), ("rl2", ob)], w=[("ot", ob)])
                        S.add("sp", lambda e, h=h, i=i, ob=ob: e.dma_start(out=ydst(0, h * 64, (h + 1) * 64, i * 512, 512), in_=ot[ob][:, :]),
                              r=[("ot", ob)], dma="out")
                S.emit()
    return nc


def _ssm_block(nc, S, stack, L):
    uT, pss, cst32 = L["uT"], L["pss"], L["cst32"]
    lamp, logdt, bstk_d, cstk_d, dvec_d, yssm = L["lamp"], L["logdt"], L["bstk_d"], L["cstk_d"], L["dvec_d"], L["yssm"]
    NX, NCH, NTOKS = L["NX"], L["NCH"], L["NTOKS"]
    ident32 = cst32[:, 0:128]
    Jm = cst32[:, 128:256]
    bmask = cst32[:, 512:512 + 1024]
    sgnR = cst32[:, 1536:1537]
    NLEV = max(1, (NCH - 1).bit_length())
    POW_SEQ = list(range(9))
    NPW = 9 + NLEV
    PI = math.pi
    with ExitStack() as st2:
        def sb2(name, shape, dtype):
            return st2.enter_context(nc.sbuf_tensor(name, shape, dtype))
        lam = sb2("lam", [128, 16], F32)
        dtb = sb2("dtb", [128, 8], F32)
        t0 = sb2("t0", [128, 8], F32)
        t1 = sb2("t1", [128, 8], F32)
        t2 = sb2("t2", [128, 8], F32)
        t3 = sb2("t3", [128, 8], F32)
        pw_re = sb2("pw_re", [128, NPW + 1, 8], F32)
        pw_im = sb2("pw_im", [128, NPW + 1, 8], F32)
        pw_ims = sb2("pw_ims", [128, NPW + 1, 8], F32)
        bstk = sb2("bstk_sb", [128, 128], F32)
        cstk = sb2("cstk_sb", [128, 128], F32)
        dvec = sb2("dvec_sb", [128, 1], F32)
        bpad = sb2("bpad", [128, 128], BF16)
        cpad = sb2("cpad", [128, 128], BF16)
        Rb = sb2("Rb", [128, 10, 128], BF16)
        Rh = sb2("Rh", [128, NLEV, 128], F32)
        bbar = sb2("bbar", [128, 128], BF16)
        lhsA = sb2("lhsA", [128, 8, 128], BF16)
        CL0 = sb2("CL0", [128, 128], BF16)
        CLall = sb2("CLall", [128, 8, 8, 128], BF16)
        Wb = sb2("Wb", [128, 8, 128], BF16)
        Ea = sb2("Ea", [128, NCH], F32)
        Eb = sb2("Eb", [128, NCH], F32)
        Sin = sb2("Sin", [128, 8, NCH], BF16)
        yo = [sb2("yo0", [128, 4096], L["ODT"])]
        S.stream("ssm", 0)
        S.add("sp", lambda e: e.dma_start(out=lam[:], in_=lamp[:, :]), w=["lam"], dma="ssm")
        S.add("sp", lambda e: e.dma_start(out=dtb[:], in_=logdt[0:1, :].partition_broadcast(128)), w=["dtb"], dma="ssm")
        S.add("sp", lambda e: e.dma_start(out=bstk[:], in_=bstk_d[:, :]), w=["bstk"], dma="ssm")
        S.add("sp", lambda e: e.dma_start(out=cstk[:], in_=cstk_d[:, :]), w=["cstk"], dma="ssm")
        S.add("sp", lambda e: e.dma_start(out=dvec[:], in_=dvec_d[:, :]), w=["dvec"], dma="ssm")
        are, aim = lam[:, 0:8], lam[:, 8:16]

        def V(fn, r, w):
            S.add("dve", fn, r=r, w=w)

        def tt(out, a, b, op, r, w):
            V(lambda e: e.tensor_tensor(out=out, in0=a, in1=b, op=op), r, w)

        def ts(out, a, s1, s2, op0, op1, r, w):
            if op1 is None:
                V(lambda e: e.tensor_scalar(out=out, in0=a, scalar1=s1, scalar2=None, op0=op0), r, w)
            else:
                V(lambda e: e.tensor_scalar(out=out, in0=a, scalar1=s1, scalar2=s2, op0=op0, op1=op1), r, w)
        S.add("act", lambda e: e.activation(out=dtb[:], in_=dtb[:], func=AF.Exp), r=["dtb"], w=["dtb"])
        tt(t0[:], are, dtb[:], ALU.mult, ["lam", "dtb"], ["t0"])
        S.add("act", lambda e: e.activation(out=t0[:], in_=t0[:], func=AF.Exp), r=["t0"], w=["t0"])
        tt(t1[:], aim, dtb[:], ALU.mult, ["lam", "dtb"], ["t1"])
        MAGIC = 12582912.0
        ts(t1[:], t1[:], 1.0 / (2.0 * PI), None, ALU.mult, None, ["t1"], ["t1"])
        ts(t3[:], t1[:], MAGIC, None, ALU.add, None, ["t1"], ["t3"])
        ts(t3[:], t3[:], -MAGIC, None, ALU.add, None, ["t3"], ["t3"])
        tt(t2[:], t1[:], t3[:], ALU.subtract, ["t1", "t3"], ["t2"])
        S.add("act", lambda e: e.activation(out=t2[:], in_=t2[:], func=AF.Sin, scale=2.0 * PI), r=["t2"], w=["t2"])
        ts(t1[:], t1[:], 0.25, None, ALU.add, None, ["t1"], ["t1"])
        ts(t3[:], t1[:], MAGIC, None, ALU.add, None, ["t1"], ["t3"])
        ts(t3[:], t3[:], -MAGIC, None, ALU.add, None, ["t3"], ["t3"])
        tt(t3[:], t1[:], t3[:], ALU.subtract, ["t1", "t3"], ["t3"])
        S.add("act", lambda e: e.activation(out=t3[:], in_=t3[:], func=AF.Sin, scale=2.0 * PI), r=["t3"], w=["t3"])
        tt(pw_re[:, 1, :], t0[:], t3[:], ALU.mult, ["t0", "t3"], ["pw"])
        tt(pw_im[:, 1, :], t0[:], t2[:], ALU.mult, ["t0", "t2"], ["pw"])
        V(lambda e: e.memset(pw_re[:, 0, :], 1.0), [], ["pw"])
        V(lambda e: e.memset(pw_im[:, 0, :], 0.0), [], ["pw"])

        def cmul(o, a, b):
            tt(t0[:], pw_re[:, a, :], pw_re[:, b, :], ALU.mult, ["pw"], ["t0"])
            tt(t1[:], pw_im[:, a, :], pw_im[:, b, :], ALU.mult, ["pw"], ["t1"])
            tt(t2[:], pw_re[:, a, :], pw_im[:, b, :], ALU.mult, ["pw"], ["t2"])
            tt(t3[:], pw_im[:, a, :], pw_re[:, b, :], ALU.mult, ["pw"], ["t3"])
            tt(pw_re[:, o, :], t0[:], t1[:], ALU.subtract, ["t0", "t1"], ["pw"])
            tt(pw_im[:, o, :], t2[:], t3[:], ALU.add, ["t2", "t3"], ["pw"])
        for m in range(2, 9):
            cmul(m, m - 1, 1)
        for l in range(1, NLEV):
            cmul(8 + l, 8 + l - 1, 8 + l - 1)
        CO = NPW
        tt(t0[:], are, are, ALU.mult, ["lam"], ["t0"])
        tt(t1[:], aim, aim, ALU.mult, ["lam"], ["t1"])
        tt(t0[:], t0[:], t1[:], ALU.add, ["t0", "t1"], ["t0"])
        V(lambda e: e.reciprocal(out=t0[:], in_=t0[:]), ["t0"], ["t0"])
        ts(t1[:], pw_re[:, 1, :], -1.0, None, ALU.add, None, ["pw"], ["t1"])
        tt(t2[:], t1[:], are, ALU.mult, ["t1", "lam"], ["t2"])
        tt(t3[:], pw_im[:, 1, :], aim, ALU.mult, ["pw", "lam"], ["t3"])
        tt(t2[:], t2[:], t3[:], ALU.add, ["t2", "t3"], ["t2"])
        tt(pw_re[:, CO, :], t2[:], t0[:], ALU.mult, ["t2", "t0"], ["pw"])
        tt(t2[:], pw_im[:, 1, :], are, ALU.mult, ["pw", "lam"], ["t2"])
        tt(t3[:], t1[:], aim, ALU.mult, ["t1", "lam"], ["t3"])
        tt(t2[:], t2[:], t3[:], ALU.subtract, ["t2", "t3"], ["t2"])
        tt(pw_im[:, CO, :], t2[:], t0[:], ALU.mult, ["t2", "t0"], ["pw"])
        V(lambda e: e.tensor_scalar(out=pw_ims[:], in0=pw_im[:], scalar1=sgnR, scalar2=None, op0=ALU.mult), ["pw", "cst"], ["pws"])

        def build_R(out_ap, idx, g, key):
            V(lambda e: e.tensor_scalar(out=out_ap, in0=ident32, scalar1=pw_re[:, idx, g:g + 1], scalar2=None, op0=ALU.mult),
              ["pw", "cst"], [key])
            V(lambda e: e.scalar_tensor_tensor(out=out_ap, in0=Jm, scalar=pw_ims[:, idx, g:g + 1], in1=out_ap,
                                               op0=ALU.mult, op1=ALU.add), ["pws", "cst", key], [key])

        NT3 = [(c, min(342, NCH - c)) for c in range(0, NCH, 342)]
        evk = 0
        for g in range(8):
            for m in range(9):
                build_R(Rb[:, m, :], m, g, ("Rb", m))
            build_R(Rb[:, 9, :], CO, g, ("Rb", 9))
            for l in range(NLEV):
                build_R(Rh[:, l, :], 8 + l, g, ("Rh", l))
            tt(bpad[:], bstk[:], bmask[:, g * 128:(g + 1) * 128], ALU.mult, ["bstk", "cst"], ["bpad"])
            tt(cpad[:], cstk[:], bmask[:, g * 128:(g + 1) * 128], ALU.mult, ["cstk", "cst"], ["cpad"])
            S.add("pe", lambda e: e.matmul(pss[0][:, 0:128], Rb[:, 9, :], bpad[:], start=True, stop=True),
                  r=[("Rb", 9), "bpad"], w=[("ps", 0)])
            S.add("act", lambda e: e.copy(out=bbar[:], in_=pss[0][:, 0:128]), r=[("ps", 0)], w=["bbar"])
            for i in range(8):
                pk = i % 2
                S.add("pe", lambda e, i=i, pk=pk: e.matmul(pss[pk][:, 128:256], bbar[:], Rb[:, 7 - i, :], start=True, stop=True),
                      r=["bbar", ("Rb", 7 - i)], w=[("ps", pk)])
                if i % 2 == 0:
                    S.add("act", lambda e, i=i, pk=pk: e.copy(out=lhsA[:, i, :], in_=pss[pk][:, 128:256]), r=[("ps", pk)], w=[("lhsA", i)])
                else:
                    S.add("dve", lambda e, i=i, pk=pk: e.tensor_copy(out=lhsA[:, i, :], in_=pss[pk][:, 128:256]), r=[("ps", pk)], w=[("lhsA", i)])
            for d in range(9):
                pk = d % 2
                dst = CL0[:] if d == 0 else CLall[:, g, d - 1, :]
                S.add("pe", lambda e, d=d, pk=pk: e.matmul(pss[pk][:, 256:384], Rb[:, d, :], cpad[:], start=True, stop=True),
                      r=[("Rb", d), "cpad"], w=[("ps", pk)])
                S.add("dve", lambda e, dst=dst, pk=pk: e.tensor_scalar(out=dst, in0=pss[pk][:, 256:384], scalar1=sgnR, scalar2=None, op0=ALU.mult),
                      r=[("ps", pk), "cst"], w=[("CL", g, d)])
            for d in range(8):
                bank = 2 + d // 4
                col = (d % 4) * 128
                S.add("pe", lambda e, d=d, bank=bank, col=col, g=g: e.matmul(
                    pss[bank][:, col:col + 128], bbar[:], (CL0[:] if d == 0 else CLall[:, g, d - 1, :]),
                    start=(g == 0), stop=(g == 7)), r=["bbar", ("CL", g, d)], w=[("ps", bank)])
            for (c0, n) in NT3:
                pk = 4 + (evk % 2)
                evk += 1

                def mmA(e, c0=c0, n=n, pk=pk):
                    ins = None
                    for i in range(8):
                        ins = e.matmul(pss[pk][:, 0:n], lhsA[:, i, :], uT[:, c0 * 8 + i:(c0 + n) * 8:8], start=(i == 0), stop=(i == 7))
                    return ins
                S.add("pe", mmA, r=[("lhsA", i) for i in range(8)], w=[("ps", pk)])
                S.add("act", lambda e, c0=c0, n=n, pk=pk: e.copy(out=Ea[:, c0:c0 + n], in_=pss[pk][:, 0:n]), r=[("ps", pk)], w=["Ea"])
            src, dst_ = Ea, Eb
            sk, dk = "Ea", "Eb"
            for l in range(NLEV):
                s = 1 << l
                S.add("act", lambda e, src=src, dst_=dst_, s=s: e.copy(out=dst_[:, 0:s], in_=src[:, 0:s]), r=[sk], w=[dk])
                c = s
                while c < NCH:
                    n = min(512, NCH - c)
                    pk = 4 + (evk % 2)
                    evk += 1
                    S.add("pe", lambda e, l=l, c=c, n=n, s=s, pk=pk, src=src: e.matmul(
                        pss[pk][:, 0:n], Rh[:, l, :], src[:, c - s:c - s + n], start=True, stop=True), r=[("Rh", l), sk], w=[("ps", pk)])
                    S.add("dve", lambda e, c=c, n=n, pk=pk, src=src, dst_=dst_: e.tensor_tensor(
                        out=dst_[:, c:c + n], in0=pss[pk][:, 0:n], in1=src[:, c:c + n], op=ALU.add), r=[("ps", pk), sk], w=[dk])
                    c += n
                src, dst_ = dst_, src
                sk, dk = dk, sk
            S.add("dve", lambda e, g=g: e.memset(Sin[:, g, 0:1], 0.0), w=[("Sin", g)])
            S.add("act", lambda e, g=g, src=src: e.copy(out=Sin[:, g, 1:NCH], in_=src[:, 0:NCH - 1]), r=[sk], w=[("Sin", g)])
        for d in range(8):
            bank = 2 + d // 4
            col = (d % 4) * 128
            if d == 0:
                S.add("dve", lambda e, bank=bank, col=col: e.scalar_tensor_tensor(
                    out=Wb[:, 0, :], in0=ident32, scalar=dvec[:, 0:1], in1=pss[bank][:, col:col + 128], op0=ALU.mult, op1=ALU.add),
                    r=[("ps", bank), "dvec", "cst"], w=["Wb"])
            else:
                S.add("act", lambda e, d=d, bank=bank, col=col: e.copy(out=Wb[:, d, :], in_=pss[bank][:, col:col + 128]),
                      r=[("ps", bank)], w=["Wb"])
        kch = 2
        oi = 0
        while kch < NCH:
            n = min(512, NCH - kch)
            ob = 0
            for j in range(8):
                pk = 4 + (evk % 2)
                evk += 1

                def mmo(e, j=j, kch=kch, n=n, pk=pk):
                    ins = None
                    tot = (j + 1) + 8
                    q = 0
                    for i in range(j + 1):
                        ins = e.matmul(pss[pk][:, 0:n], Wb[:, j - i, :], uT[:, kch * 8 + i:(kch + n) * 8:8], start=(q == 0), stop=(q == tot - 1))
                        q += 1
                    for g in range(8):
                        ins = e.matmul(pss[pk][:, 0:n], CLall[:, g, j, :], Sin[:, g, kch:kch + n], start=(q == 0), stop=(q == tot - 1))
                        q += 1
                    return ins
                S.add("pe", mmo, r=["Wb"] + [("Sin", g) for g in range(8)] + [("CL", g, d) for g in range(8) for d in range(1, 9)],
                      w=[("ps", pk)])
                eng = "act" if j % 2 == 0 else "dve"
                if eng == "act":
                    S.add("act", lambda e, j=j, n=n, pk=pk, ob=ob: e.copy(out=yo[ob][:, j:n * 8:8], in_=pss[pk][:, 0:n]),
                          r=[("ps", pk)], w=[("yo", ob)])
                else:
                    S.add("dve", lambda e, j=j, n=n, pk=pk, ob=ob: e.tensor_copy(out=yo[ob][:, j:n * 8:8], in_=pss[pk][:, 0:n]),
                          r=[("ps", pk)], w=[("yo", ob)])
            t0_ = (kch - 2) * 8
            CH = NX // 4
            for cs in range(0, n * 8, min(CH, n * 8)):
                cn = min(CH, n * 8)
                S.add("sp", lambda e, ob=ob, t0_=t0_, cs=cs, cn=cn: e.dma_start(out=L["ydst"](1, 0, 128, t0_ + cs, cn), in_=yo[ob][:, cs:cs + cn]),
                      r=[("yo", ob)], dma="out")
            kch += n
            oi += 1
        S.emit()


def _consts():
    c = np.zeros((128, 128 * 4 + 8 * 128 + 4), np.float32)
    c[:, 0:128] = np.eye(128, dtype=np.float32)
    J = np.zeros((128, 128), np.float32)
    for p in range(64):
        J[p, 64 + p] = 1.0
        J[64 + p, p] = 1.0
    c[:, 128:256] = J
    s = np.arange(128)[:, None]
    t = np.arange(128)[None, :]
    c[:, 256:384] = (s <= t).astype(np.float32)
    for g in range(8):
        c[:, 512 + g * 128 + g * 16: 512 + g * 128 + (g + 1) * 16] = 1.0
    c[0:64, 1536] = 1.0
    c[64:128, 1536] = -1.0
    return c


def prep_p1(inp, b, r, NX=SEQ):
    hs = [2 * r, 2 * r + 1]
    w_in = inp["w_in"][0]
    cols = []
    for h in hs:
        cols.append(w_in[:, Q_OFF + h * 64:Q_OFF + (h + 1) * 64])
    for h in hs:
        cols.append(w_in[:, K_OFF + h * 64:K_OFF + (h + 1) * 64])
    for h in hs:
        cols.append(w_in[:, V_OFF + h * 64:V_OFF + (h + 1) * 64])
    cols.append(w_in[:, U_OFF + r * 128:U_OFF + (r + 1) * 128])
    cols.append(w_in[:, F_OFF + 2 * r:F_OFF + 2 * r + 2])
    w1 = np.ascontiguousarray(np.concatenate(cols, axis=1), dtype=np.float32)
    lnp = np.concatenate([inp["ln_in_g"].reshape(8, 128).T, inp["ln_in_b"].reshape(8, 128).T], axis=1)
    gs = slice(8 * r, 8 * r + 8)
    a_re = inp["ssm_a_re"][0][gs]
    a_im = inp["ssm_a_im"][0][gs]
    lamp = np.concatenate([np.concatenate([a_re.T, a_re.T], axis=0), np.concatenate([a_im.T, a_im.T], axis=0)], axis=1)
    b_re = inp["ssm_b_re"][0][gs]
    b_im = inp["ssm_b_im"][0][gs]
    bstk = np.concatenate([b_re.transpose(1, 0, 2).reshape(64, 128), b_im.transpose(1, 0, 2).reshape(64, 128)], axis=0)
    c_re = inp["ssm_c_re"][0][gs]
    c_im = inp["ssm_c_im"][0][gs]
    cstk = np.concatenate([c_re.transpose(2, 0, 1).reshape(64, 128), c_im.transpose(2, 0, 1).reshape(64, 128)], axis=0)
    f32 = lambda a: np.ascontiguousarray(a, dtype=np.float32)
    return dict(x=f32(inp["x"][b][:NX]), meta=f32(inp["meta"]), lnp=f32(lnp), w1=w1,
                lnrow=f32(np.stack([inp["ln_in_g"], inp["ln_in_b"]])),
                bf=f32(inp["b_f"][0][2 * r:2 * r + 2].reshape(2, 1)), lamp=f32(lamp),
                logdt=f32(inp["ssm_log_dt"][0][gs].reshape(1, 8)), bstk=f32(bstk), bstks=f32(bstk), cstk=f32(cstk),
                dvec=f32(inp["ssm_d"][0][gs].reshape(128, 1)), cst=_consts())


def prep_p2_shared(inp):
    f32 = lambda a: np.ascontiguousarray(a, dtype=np.float32)
    w_in = inp["w_in"][0]
    w_ga = w_in[:, GA_OFF:GB_OFF]
    w_gb = w_in[:, GB_OFF:GB_OFF + 1024]
    wg = np.zeros((1024, 2048), np.float32)
    for j in range(4):
        for dcl in range(2):
            dc = 2 * j + dcl
            o = j * 512 + dcl * 256
            wg[:, o:o + 128] = w_ga[:, dc * 128:(dc + 1) * 128]
            wg[:, o + 128:o + 256] = w_gb[:, dc * 128:(dc + 1) * 128]
    lnp = np.stack([inp["ln_in_g"], inp["ln_in_b"], inp["ln1_g"][0], inp["ln1_b"][0], inp["ln2_g"][0], inp["ln2_b"][0]])
    bgu = np.zeros((128, 2, N_EXPERTS, 8), np.float32)
    bgu[:, 0] = inp["b_gate"][0].reshape(N_EXPERTS, 8, 128).transpose(2, 0, 1)
    bgu[:, 1] = inp["b_up"][0].reshape(N_EXPERTS, 8, 128).transpose(2, 0, 1)
    return dict(lnp=f32(lnp), wg=wg, wo=f32(inp["w_o"][0]), wua=f32(inp["w_up_a"][0]), wub=f32(inp["w_up_b"][0]),
                wglu=f32(inp["w_glu"][0]), bglu=f32(inp["b_glu"][0].reshape(4, 128).T), wr=f32(inp["w_router"][0]),
                br=f32(inp["b_router"][0].reshape(1, 32)), wgate=f32(inp["w_gate"][0]), wup=f32(inp["w_up"][0]),
                wdown=f32(inp["w_down"][0]), bgu=f32(bgu.reshape(128, -1)), bdown=f32(inp["b_down"][0]),
                ident=np.eye(128, dtype=np.float32))


_NC_CACHE = {}


def build_fused(NX=SEQ, NE=N_EXPERTS):
    nc = bass.Bass("TRN2", target_bir_lowering=False)
    with ExitStack() as top:
        S = Sched(nc, top)
        CH = NX // 4
        ybuf = [nc.dram_tensor(f"ybuf_in{k}", [256, CH], BF16) for k in range(4)]
        ygat = [nc.dram_tensor(f"ybuf_out{k}", [4 * 256, CH], BF16) for k in range(4)]
        build_p1(NX, ctx=dict(nc=nc, S=S, ybuf=[t.ap() for t in ybuf]))
        for k in range(4):
            S.add("pool", lambda e, k=k: e.collective_compute("AllGather", ALU.bypass, replica_groups=[[0, 1, 2, 3], [4, 5, 6, 7]],
                                                              ins=[ybuf[k].ap().opt()], outs=[ygat[k].ap().opt()]), w=[("ygat", k)])
            S.add("pool", lambda e: e.memset(S.fz["pool"][:, 1:2], 0.0), r=[("ygat", k)], w=["fzp"])
        S.emit()
        build_p2(NX // 4, NE, ctx=dict(nc=nc, S=S, ygat=[t.ap() for t in ygat]))
    return nc


def make_maps(inp, NX=SEQ, NE=N_EXPERTS):
    shared = prep_p2_shared(inp)
    shared["lnp2"] = shared.pop("lnp")
    for k in ("wgate", "wup", "wdown"):
        shared[k] = shared[k][:NE]
    shared["bgu"] = np.ascontiguousarray(shared["bgu"].reshape(128, 2, N_EXPERTS, 8)[:, :, :NE].reshape(128, -1))
    NTOK = NX // 4
    maps = []
    for c in range(8):
        b, r = c // 4, c % 4
        m = dict(shared)
        m.update(prep_p1(inp, b, r, NX))
        m.pop("bstks")
        m["x2"] = np.ascontiguousarray(inp["x"][b, r * NTOK:(r + 1) * NTOK], dtype=np.float32)
        oh = np.zeros((128, 4), np.float32)
        oh[:, r] = 1.0
        m["onehot"] = oh
        maps.append(m)
    return maps


def kernel(**inputs):
    inp = {k: np.asarray(v) for k, v in inputs.items()}
    B = inp["x"].shape[0]
    if "f" not in _NC_CACHE:
        _NC_CACHE["f"] = build_fused()
    maps = make_maps(inp)
    if False:
        m = None
    res = run_bass_kernel_spmd(_NC_CACHE["f"], maps, core_ids=list(range(8)))
    out = np.zeros((B, SEQ, D_MODEL), np.float32)
    for c in range(8):
        b, r = c // 4, c % 4
        out[b, r * 2048:(r + 1) * 2048] = res.results[c]["out"]
    return out
```

```python
import math
from contextlib import ExitStack

import numpy as np
import concourse.bass as bass
import concourse.mybir as mybir
from concourse.bass_utils import run_bass_kernel_spmd

F32 = mybir.dt.float32
BF16 = mybir.dt.bfloat16
AF = mybir.ActivationFunctionType
ALU = mybir.AluOpType

D_MODEL = 1024
SEQ = 8192
N_META = 16
N_EXPERTS = 32
LN_EPS = 1e-5
DN_ALPHA = 2.0 ** 0.25
SW_LIMIT = 7.0
SW_ALPHA = 1.702
Q_OFF, K_OFF, V_OFF, F_OFF, U_OFF, GA_OFF, GB_OFF = 0, 512, 1024, 1536, 1544, 2056, 3080


class Sched:
    ENGS = ("pe", "act", "dve", "pool", "sp")
    SEM_ROLL = 1 << 30

    def __init__(self, nc, stack):
        self.nc = nc
        self.stack = stack
        self.eng_obj = {"pe": nc.tensor, "act": nc.scalar, "dve": nc.vector,
                        "pool": nc.gpsimd, "sp": nc.sync}
        self.eng_sems = {e: [stack.enter_context(nc.semaphore(f"s_{e}0"))] for e in self.ENGS}
        self.eng_cnt = {e: 0 for e in self.ENGS}
        self.streams = {}
        self.nsem = 0
        self.fz = {e: stack.enter_context(nc.sbuf_tensor(f"fz_{e}", [128, 2], F32)) for e in ("act", "dve", "pool")}
        self.reset()

    def reset(self):
        self.ops = []
        self.lastw = {}
        self.readers = {}

    def stream(self, name, depth):
        if name not in self.streams:
            sems = [self.stack.enter_context(self.nc.semaphore(f"d_{name}{i}")) for i in range(depth)]
            self.streams[name] = dict(sems=sems, cnt=[0] * depth, k=0)
        return name

    def add(self, eng, fn, r=(), w=(), dma=None, fence=False):
        idx = len(self.ops)
        deps = set()
        for k in r:
            if k in self.lastw:
                deps.add(self.lastw[k])
        for k in w:
            if k in self.lastw:
                deps.add(self.lastw[k])
            for x in self.readers.get(k, ()):
                deps.add(x)
        deps.discard(idx)
        for k in r:
            self.readers.setdefault(k, []).append(idx)
        for k in w:
            self.lastw[k] = idx
            self.readers[k] = []
        self.ops.append(dict(eng=eng, fn=fn, deps=deps, dma=dma, used=False, sig=None, fence=fence))
        for d in deps:
            self.ops[d]["used"] = True
        return idx

    def emit(self, final_wait_dma=True):
        nc = self.nc
        ops = self.ops
        for op in ops:
            if op["dma"] is not None and len(self.streams[op["dma"]]["sems"]) == 0:
                self.nsem += 1
                sem = self.stack.enter_context(nc.semaphore(f"d_one{self.nsem}"))
                op["sig"] = (sem, 16, 16)
            elif op["dma"] is not None:
                st = self.streams[op["dma"]]
                k = st["k"] % len(st["sems"])
                st["k"] += 1
                st["cnt"][k] += 16
                op["sig"] = (st["sems"][k], st["cnt"][k], 16)
            elif op["used"]:
                e = op["eng"]
                if self.eng_cnt[e] >= self.SEM_ROLL:
                    self.eng_sems[e].append(self.stack.enter_context(nc.semaphore(f"s_{e}{len(self.eng_sems[e])}")))
                    self.eng_cnt[e] = 0
                self.eng_cnt[e] += 1
                op["sig"] = (self.eng_sems[e][-1], self.eng_cnt[e], 1)
        per_eng = {e: [] for e in self.ENGS}
        for i, op in enumerate(ops):
            waits = []
            for d in sorted(op["deps"]):
                dop = ops[d]
                if dop["dma"] is None and dop["eng"] == "pe" and op["eng"] == "pe":
                    continue
                waits.append(dop["sig"])
            per_eng[op["eng"]].append((waits, op))
        pending = [op["sig"] for op in ops if op["dma"] is not None]
        with nc.Block() as blk:
            def make(ename):
                lst = per_eng[ename]

                def body(eng):
                    known = {}
                    for waits, op in lst:
                        for (sem, val, _inc) in waits:
                            key = id(sem)
                            if known.get(key, (None, 0))[1] >= val:
                                continue
                            eng.wait_ge(sem, val)
                            known[key] = (sem, val)
                        ins = op["fn"](eng)
                        if op["sig"] is not None:
                            if op["fence"] and ename in self.fz:
                                fz = self.fz[ename]
                                if ename == "act":
                                    ins = eng.memzero(fz[:, 0:1])
                                else:
                                    ins = eng.memset(fz[:, 0:1], 0.0)
                            ins.then_inc(op["sig"][0], op["sig"][2])
                    if ename == "sp" and final_wait_dma:
                        best = {}
                        for (sem, val, _inc) in pending:
                            if best.get(id(sem), (None, 0))[1] < val:
                                best[id(sem)] = (sem, val)
                        for sem, val in best.values():
                            if known.get(id(sem), (None, 0))[1] < val:
                                eng.wait_ge(sem, val)
                return body
            blk.tensor(make("pe"))
            blk.scalar(make("act"))
            blk.vector(make("dve"))
            blk.gpsimd(make("pool"))
            blk.sync(make("sp"))
        self.reset()


def _bc(ap, shape):
    return ap.to_broadcast(shape)


def _ln_tile(S, tile_ap, st6, mv, rstd, gB, bB, key, tag):
    S.add("dve", lambda e: e.bn_stats(out=st6[:, 0, :], in_=tile_ap[:, 0:512]), r=[key], w=[tag + "st0"])
    S.add("dve", lambda e: e.bn_stats(out=st6[:, 1, :], in_=tile_ap[:, 512:1024]), r=[key], w=[tag + "st1"])
    S.add("dve", lambda e: e.bn_aggr(out=mv[:], in_=st6[:]), r=[tag + "st0", tag + "st1"], w=[tag + "mv"], fence=True)
    S.add("dve", lambda e: e.tensor_scalar(out=rstd[:], in0=mv[:, 1:2], scalar1=LN_EPS, scalar2=None,
                                           op0=ALU.add), r=[tag + "mv"], w=[tag + "rs"], fence=True)
    S.add("act", lambda e: e.sqrt(out=rstd[:], in_=rstd[:]), r=[tag + "rs"], w=[tag + "rs"], fence=True)
    S.add("dve", lambda e: e.reciprocal(out=rstd[:], in_=rstd[:]), r=[tag + "rs"], w=[tag + "rs"], fence=True)
    S.add("dve", lambda e: e.tensor_scalar(out=tile_ap, in0=tile_ap, scalar1=mv[:, 0:1], scalar2=rstd[:, 0:1],
                                           op0=ALU.subtract, op1=ALU.mult), r=[key, tag + "mv", tag + "rs"], w=[key])
    S.add("dve", lambda e: e.tensor_tensor(out=tile_ap, in0=tile_ap, in1=gB, op=ALU.mult), r=[key, "lnB"], w=[key])
    S.add("dve", lambda e: e.tensor_tensor(out=tile_ap, in0=tile_ap, in1=bB, op=ALU.add), r=[key, "lnB"], w=[key])


def build_p2(NTOK=2048, NE=32, ctx=None):
    NT = NTOK // 128
    NG = NTOK // 512
    fused = ctx is not None
    nc = ctx["nc"] if fused else bass.Bass("TRN2", target_bir_lowering=False)

    def din(name, shape, dtype=F32):
        return nc.dram_tensor(name, shape, dtype, kind="ExternalInput").ap()
    x = din("x2", [NTOK, 1024])
    if fused:
        ygat = ctx["ygat"]
        onehot = din("onehot", [128, 4])
    else:
        yaT = din("yaT", [512, NTOK])
        ysT = din("ysT", [512, NTOK])
    lnp = din("lnp2", [6, 1024])
    wg = din("wg", [1024, 2048])
    wo = din("wo", [1024, 1024])
    wua = din("wua", [512, 1024])
    wub = din("wub", [512, 1024])
    wglu = din("wglu", [512, 512])
    bglu = din("bglu", [128, 4])
    wr = din("wr", [1024, 32])
    br = din("br", [1, 32])
    wgate = din("wgate", [NE, 1024, 1024])
    wup = din("wup", [NE, 1024, 1024])
    wdown = din("wdown", [NE, 1024, 1024])
    bgu = din("bgu", [128, 2 * NE * 8])
    bdown = din("bdown", [N_EXPERTS, 1024])
    ident = din("ident", [128, 128])
    out = nc.dram_tensor("out", [NTOK, 1024], F32, kind="ExternalOutput").ap()

    with ExitStack() as stack:
        S = ctx["S"] if fused else Sched(nc, stack)

        def sb(name, shape, dtype):
            return stack.enter_context(nc.sbuf_tensor("b_" + name, shape, dtype))

        def psum(name, shape, dtype):
            return stack.enter_context(nc.psum_tensor("b_" + name, shape, dtype))
        hres = sb("hres", [128, NT, 1024], F32)
        identb = sb("identb", [128, 128], BF16)
        ident32 = sb("ident32", [128, 128], F32)
        lnB = sb("lnB", [128, 4, 1024], F32)
        comb = sb("comb", [128, NT, 32], F32)
        st6 = sb("st6", [128, 2, 6], F32)
        mv = sb("mv", [128, 2], F32)
        rstd = sb("rstd", [128, 1], F32)
        pss = [psum(f"ps{i}", [128, 512], F32) for i in range(6)]
        pT = [psum(f"pT{i}", [128, 1024], BF16) for i in range(2)]
        S.stream("x", 2)
        S.stream("misc", 0)
        S.stream("gw", 3)
        S.stream("yin", 0)
        S.stream("wr", 8)
        S.stream("out", 2)

        with ExitStack() as st2:
            def sb2(name, shape, dtype):
                return st2.enter_context(nc.sbuf_tensor("b_" + name, shape, dtype))
            xb = [sb2(f"xb{i}", [128, 1024], BF16) for i in range(2)]
            hT = sb2("hT", [128, 8, 512], BF16)
            ya = sb2("ya", [128, 4, 512], BF16)
            ys32 = sb2("ys32", [128, 4, 512], F32)
            gtmp = sb2("gtmp", [128, 512], F32)
            sgm = sb2("sgm", [128, 512], F32)
            yb = sb2("yb", [128, 4, 512], BF16)
            yb2 = sb2("yb2", [128, 4, 512], BF16)
            merged = sb2("merged", [128, 8, 512], BF16)
            gw = [sb2(f"gw{i}", [128, 8, 512], BF16) for i in range(3)]
            wuat = sb2("wuat", [128, 4, 1024], BF16)
            wubt = sb2("wubt", [128, 4, 1024], BF16)
            wglut = sb2("wglut", [128, 4, 512], BF16)
            bglut = sb2("bglut", [128, 4], F32)
            if fused:
                cand = [sb2(f"cand{i}", [128, 4, 512], BF16) for i in range(2)]
                oht = sb2("oht", [128, 4], F32)
                S.stream("cand", 2)
                S.add("sp", lambda e: e.dma_start(out=oht[:], in_=onehot[:, :]), w=["oht"], dma="misc")
            gat = [sb2(f"gat{i}", [128, 512], F32) for i in range(2)]
            gbt = [sb2(f"gbt{i}", [128, 512], F32) for i in range(2)]

            S.add("sp", lambda e: e.dma_start(out=ident32[:], in_=ident[:, :]), w=["ident32"], dma="misc")
            S.add("dve", lambda e: e.tensor_copy(out=identb[:], in_=ident32[:]), r=["ident32"], w=["identb"])
            for i in range(4):
                S.add("sp", lambda e, i=i: e.dma_start(out=lnB[:, i, :], in_=lnp[i:i + 1, :].partition_broadcast(128)),
                      w=["lnB"], dma="misc")
            S.add("sp", lambda e: e.dma_start(out=bglut[:], in_=bglu[:, :]), w=["bglut"], dma="misc")
            S.add("pool", lambda e: e.dma_start(out=wuat[:], in_=wua.rearrange("(c p) n -> p c n", p=128)), w=["wuat"], dma="misc")
            S.add("pool", lambda e: e.dma_start(out=wubt[:], in_=wub.rearrange("(c p) n -> p c n", p=128)), w=["wubt"], dma="misc")
            S.add("pool", lambda e: e.dma_start(out=wglut[:], in_=wglu.rearrange("(c p) n -> p c n", p=128)), w=["wglut"], dma="misc")
            gwk = 0
            for tg in range(NG):
                c0 = tg * 512
                for ti in range(4):
                    tt = tg * 4 + ti
                    hk = ("hres", tt)
                    S.add("sp", lambda e, tt=tt: e.dma_start(out=hres[:, tt, :], in_=x[tt * 128:(tt + 1) * 128, :]),
                          w=[hk], dma="x")
                    _ln_tile(S, hres[:, tt, :], st6, mv, rstd, lnB[:, 0, :], lnB[:, 1, :], hk, "ln")
                    b = tt % 2
                    S.add("act", lambda e, tt=tt, b=b: e.copy(out=xb[b][:], in_=hres[:, tt, :]), r=[hk], w=[("xb", b)])

                    def tr(e, b=b):
                        ins = None
                        for kc in range(8):
                            ins = e.transpose(out=pT[b][:, kc * 128:(kc + 1) * 128], in_=xb[b][:, kc * 128:(kc + 1) * 128],
                                              identity=identb[:])
                        return ins
                    S.add("pe", tr, r=[("xb", b), "identb"], w=[("pT", b)])
                    S.add("act", lambda e, b=b, ti=ti: e.copy(out=hT[:, :, ti * 128:(ti + 1) * 128],
                                                              in_=pT[b][:, :].rearrange("p (k t) -> p k t", k=8)),
                          r=[("pT", b)], w=["hT"])
                if fused:
                    for which, dst, dkey in ((0, ya, "ya"), (1, ys32, "ys32")):
                        for q in range(4):
                            cb = (2 * tg + which + q) % 2
                            src = ygat[q].rearrange("(r w p) t -> p w r t", r=4, w=2, p=128)[:, which, :, c0:c0 + 512]
                            S.add("sp", lambda e, cb=cb, src=src: e.dma_start(out=cand[cb][:], in_=src), w=[("cand", cb)], dma="cand")
                            if q == 0:
                                S.add("dve", lambda e, cb=cb, dst=dst: e.tensor_scalar(out=dst[:], in0=cand[cb][:], scalar1=oht[:, 0:1], scalar2=None,
                                                                                       op0=ALU.mult), r=[("cand", cb), "oht"], w=[dkey])
                            else:
                                S.add("dve", lambda e, cb=cb, dst=dst, q=q: e.scalar_tensor_tensor(
                                    out=dst[:], in0=cand[cb][:], scalar=oht[:, q:q + 1], in1=dst[:], op0=ALU.mult, op1=ALU.add),
                                    r=[("cand", cb), "oht", dkey], w=[dkey])
                else:
                    S.add("pool", lambda e, c0=c0: e.dma_start(out=ya[:], in_=yaT.rearrange("(c p) t -> p c t", p=128)[:, :, c0:c0 + 512]),
                          w=["ya"], dma="yin")
                    S.add("sp", lambda e, c0=c0: e.dma_start(out=ys32[:], in_=ysT.rearrange("(c p) t -> p c t", p=128)[:, :, c0:c0 + 512]),
                          w=["ys32"], dma="yin")
                for c in range(4):
                    S.add("dve", lambda e, c=c: e.tensor_tensor(out=gtmp[:], in0=ys32[:, c, :], in1=ys32[:, c, :], op=ALU.mult), r=["ys32"], w=["gtmp"])
                    S.add("dve", lambda e: e.tensor_scalar(out=gtmp[:], in0=gtmp[:], scalar1=0.044715, scalar2=1.0,
                                                           op0=ALU.mult, op1=ALU.add), r=["gtmp"], w=["gtmp"])
                    S.add("dve", lambda e, c=c: e.tensor_tensor(out=gtmp[:], in0=gtmp[:], in1=ys32[:, c, :], op=ALU.mult), r=["gtmp", "ys32"], w=["gtmp"])
                    S.add("act", lambda e: e.activation(out=sgm[:], in_=gtmp[:], func=AF.Sigmoid, scale=1.5957691216), r=["gtmp"], w=["sgm"])
                    S.add("dve", lambda e, c=c: e.tensor_tensor(out=yb[:, c, :], in0=ys32[:, c, :], in1=sgm[:], op=ALU.mult), r=["ys32", "sgm"], w=["yb"])
                for cc in range(4):
                    pk = cc % 2

                    def mm(e, cc=cc, pk=pk):
                        ins = None
                        for c in range(4):
                            ins = e.matmul(pss[pk][:, :], wglut[:, c, cc * 128:(cc + 1) * 128], yb[:, c, :],
                                           start=(c == 0), stop=(c == 3))
                        return ins
                    S.add("pe", mm, r=["wglut", "yb"], w=[("ps", pk)])
                    S.add("act", lambda e, cc=cc, pk=pk: e.activation(out=sgm[:], in_=pss[pk][:, :], func=AF.Sigmoid,
                                                                      bias=bglut[:, cc:cc + 1], scale=1.0),
                          r=[("ps", pk), "bglut"], w=["sgm"])
                    S.add("dve", lambda e, cc=cc: e.tensor_tensor(out=yb2[:, cc, :], in0=yb[:, cc, :], in1=sgm[:], op=ALU.mult),
                          r=["yb", "sgm"], w=["yb2"])
                for j in range(4):
                    slot = gwk % 3
                    gwk += 1
                    S.add("pool", lambda e, j=j, slot=slot: e.dma_start(
                        out=gw[slot][:], in_=wg.rearrange("(k p) n -> p k n", p=128)[:, :, j * 512:(j + 1) * 512]),
                        w=[("gw", slot)], dma="gw")
                    for dcl in range(2):
                        dc = 2 * j + dcl
                        tb = dc % 2

                        def mmg(e, slot=slot, dcl=dcl, off=0, pk=0):
                            ins = None
                            for kc in range(8):
                                ins = e.matmul(pss[pk][:, :], gw[slot][:, kc, dcl * 256 + off:dcl * 256 + off + 128], hT[:, kc, :],
                                               start=(kc == 0), stop=(kc == 7))
                            return ins
                        S.add("pe", lambda e, f=mmg: f(e, off=0, pk=0), r=[("gw", slot), "hT"], w=[("ps", 0)])
                        S.add("pe", lambda e, f=mmg: f(e, off=128, pk=1), r=[("gw", slot), "hT"], w=[("ps", 1)])

                        def mmu(e, dc=dc, wt=None, src=None, pk=2):
                            ins = None
                            for c in range(4):
                                ins = e.matmul(pss[pk][:, :], wt[:, c, dc * 128:(dc + 1) * 128], src[:, c, :],
                                               start=(c == 0), stop=(c == 3))
                            return ins
                        S.add("pe", lambda e, f=mmu: f(e, wt=wuat, src=ya, pk=2), r=["wuat", "ya"], w=[("ps", 2)])
                        S.add("pe", lambda e, f=mmu: f(e, wt=wubt, src=yb2, pk=3), r=["wubt", "yb2"], w=[("ps", 3)])
                        S.add("act", lambda e, tb=tb: e.activation(out=gat[tb][:], in_=pss[0][:, :], func=AF.Sigmoid),
                              r=[("ps", 0)], w=[("gat", tb)])
                        S.add("act", lambda e, tb=tb: e.activation(out=gbt[tb][:], in_=pss[1][:, :], func=AF.Sigmoid),
                              r=[("ps", 1)], w=[("gbt", tb)])
                        S.add("dve", lambda e, tb=tb: e.tensor_tensor(out=gat[tb][:], in0=gat[tb][:], in1=pss[2][:, :], op=ALU.mult),
                              r=[("gat", tb), ("ps", 2)], w=[("gat", tb)])
                        S.add("dve", lambda e, tb=tb: e.tensor_tensor(out=gbt[tb][:], in0=gbt[tb][:], in1=pss[3][:, :], op=ALU.mult),
                              r=[("gbt", tb), ("ps", 3)], w=[("gbt", tb)])
                        S.add("dve", lambda e, tb=tb, dc=dc: e.tensor_tensor(out=merged[:, dc, :], in0=gat[tb][:], in1=gbt[tb][:], op=ALU.add),
                              r=[("gat", tb), ("gbt", tb)], w=["merged"])
                for half in range(2):
                    slot = gwk % 3
                    gwk += 1
                    S.add("pool", lambda e, half=half, slot=slot: e.dma_start(
                        out=gw[slot][:], in_=wo.rearrange("(k p) n -> p k n", p=128)[:, :, half * 512:(half + 1) * 512]),
                        w=[("gw", slot)], dma="gw")
                    for ti in range(4):
                        tt = tg * 4 + ti
                        pk = 4 + (ti % 2)

                        def mmo(e, slot=slot, ti=ti, pk=pk):
                            ins = None
                            for kc in range(8):
                                ins = e.matmul(pss[pk][:, :], merged[:, kc, ti * 128:(ti + 1) * 128], gw[slot][:, kc, :],
                                               start=(kc == 0), stop=(kc == 7))
                            return ins
                        S.add("pe", mmo, r=["merged", ("gw", slot)], w=[("ps", pk)])
                        S.add("dve", lambda e, tt=tt, half=half, pk=pk: e.scalar_tensor_tensor(
                            out=hres[:, tt, half * 512:(half + 1) * 512], in0=hres[:, tt, half * 512:(half + 1) * 512],
                            scalar=DN_ALPHA, in1=pss[pk][:, :], op0=ALU.mult, op1=ALU.add),
                            r=[("hres", tt), ("ps", pk)], w=[("hres", tt)])
                for ti in range(4):
                    tt = tg * 4 + ti
                    _ln_tile(S, hres[:, tt, :], st6, mv, rstd, lnB[:, 2, :], lnB[:, 3, :], ("hres", tt), "ln")
            S.emit()

        x1T = sb("x1T", [128, 8, NTOK], BF16)
        with ExitStack() as st2:
            def sb2(name, shape, dtype):
                return st2.enter_context(nc.sbuf_tensor("b_" + name, shape, dtype))
            xb = [sb2(f"xc{i}", [128, 1024], BF16) for i in range(2)]
            wrt = sb2("wrt", [128, 8, 32], BF16)
            brB = sb2("brB", [128, 32], F32)
            bdn = sb2("bdn", [N_EXPERTS, 1024], BF16)
            lg = sb2("lg", [128, 32], F32)
            ex = sb2("ex", [128, 32], F32)
            msk = sb2("msk", [128, 32], F32)
            m8 = sb2("m8", [128, 8], F32)
            negm = sb2("negm", [128, 1], F32)
            ssum = sb2("ssum", [128, 1], F32)
            combTb = sb2("combTb", [32, 128], BF16)
            S.add("pool", lambda e: e.dma_start(out=wrt[:], in_=wr.rearrange("(k p) n -> p k n", p=128)), w=["wrt"], dma="misc")
            S.add("pool", lambda e: e.dma_start(out=bdn[:], in_=bdown[:, :]), w=["bdn"], dma="misc")
            S.add("sp", lambda e: e.dma_start(out=brB[:], in_=br[0:1, :].partition_broadcast(128)), w=["brB"], dma="misc")
            for tt in range(NT):
                b = tt % 2
                hk = ("hres", tt)
                S.add("act", lambda e, tt=tt, b=b: e.copy(out=xb[b][:], in_=hres[:, tt, :]), r=[hk], w=[("xb", b)])

                def tr(e, b=b):
                    ins = None
                    for kc in range(8):
                        ins = e.transpose(out=pT[b][:, kc * 128:(kc + 1) * 128], in_=xb[b][:, kc * 128:(kc + 1) * 128],
                                          identity=identb[:])
                    return ins
                S.add("pe", tr, r=[("xb", b)], w=[("pT", b)])
                S.add("act", lambda e, b=b, tt=tt: e.copy(out=x1T[:, :, tt * 128:(tt + 1) * 128],
                                                          in_=pT[b][:, :].rearrange("p (k t) -> p k t", k=8)),
                      r=[("pT", b)], w=[("x1T", tt)])

                def mmr(e, tt=tt):
                    ins = None
                    for kc in range(8):
                        ins = e.matmul(pss[0][:, 0:32], x1T[:, kc, tt * 128:(tt + 1) * 128], wrt[:, kc, :],
                                       start=(kc == 0), stop=(kc == 7))
                    return ins
                S.add("pe", mmr, r=[("x1T", tt), "wrt"], w=[("ps", 0)])
                S.add("dve", lambda e: e.tensor_tensor(out=lg[:], in0=pss[0][:, 0:32], in1=brB[:], op=ALU.add),
                      r=[("ps", 0), "brB"], w=["lg"])
                S.add("dve", lambda e: e.max(out=m8[:], in_=lg[:]), r=["lg"], w=["m8"])
                S.add("dve", lambda e: e.tensor_scalar(out=msk[:], in0=lg[:], scalar1=m8[:, 3:4], scalar2=None, op0=ALU.is_ge),
                      r=["lg", "m8"], w=["msk"])
                S.add("dve", lambda e: e.tensor_scalar(out=negm[:], in0=m8[:, 0:1], scalar1=-1.0, scalar2=None, op0=ALU.mult),
                      r=["m8"], w=["negm"])
                S.add("act", lambda e: e.activation(out=ex[:], in_=lg[:], func=AF.Exp, bias=negm[:, 0:1], scale=1.0),
                      r=["lg", "negm"], w=["ex"])
                S.add("dve", lambda e: e.tensor_tensor(out=ex[:], in0=ex[:], in1=msk[:], op=ALU.mult), r=["ex", "msk"], w=["ex"])
                S.add("dve", lambda e: e.reduce_sum(out=ssum[:], in_=ex[:], axis=mybir.AxisListType.X), r=["ex"], w=["ssum"])
                S.add("dve", lambda e: e.reciprocal(out=ssum[:], in_=ssum[:]), r=["ssum"], w=["ssum"])
                S.add("dve", lambda e, tt=tt: e.tensor_scalar(out=comb[:, tt, :], in0=ex[:], scalar1=ssum[:, 0:1], scalar2=None, op0=ALU.mult),
                      r=["ex", "ssum"], w=[("comb", tt)])
                S.add("pe", lambda e, tt=tt: e.transpose(out=pss[1][0:32, 0:128], in_=comb[:, tt, :], identity=ident32[:]),
                      r=[("comb", tt)], w=[("ps", 1)])
                S.add("act", lambda e: e.copy(out=combTb[:], in_=pss[1][0:32, 0:128]), r=[("ps", 1)], w=["combTb"])
                for half in range(2):
                    pk = 2 + half
                    S.add("pe", lambda e, half=half, pk=pk: e.matmul(pss[pk][:, :], combTb[:, :], bdn[:, half * 512:(half + 1) * 512],
                                                                     start=True, stop=True),
                          r=["combTb", "bdn"], w=[("ps", pk)])
                    S.add("dve", lambda e, tt=tt, half=half, pk=pk: e.scalar_tensor_tensor(
                        out=hres[:, tt, half * 512:(half + 1) * 512], in0=hres[:, tt, half * 512:(half + 1) * 512],
                        scalar=DN_ALPHA, in1=pss[pk][:, :], op0=ALU.mult, op1=ALU.add),
                        r=[hk, ("ps", pk)], w=[hk])
            S.emit()

        with ExitStack() as st2:
            def sb2(name, shape, dtype):
                return st2.enter_context(nc.sbuf_tensor("b_" + name, shape, dtype))
            R = 8
            ring = [sb2(f"ring{i}", [128, 8, 256], BF16) for i in range(R)]
            actT = sb2("actT", [128, 8, NTOK], BF16)
            bgut = sb2("bgut", [128, 2 * NE * 8], F32)
            gs = [sb2(f"gs{i}", [128, 512], F32) for i in range(2)]
            sg = [sb2(f"sg{i}", [128, 512], F32) for i in range(2)]
            uu = [sb2(f"uu{i}", [128, 512], F32) for i in range(2)]
            S.add("sp", lambda e: e.dma_start(out=bgut[:], in_=bgu[:, :]), w=["bgut"], dma="misc")
            rk = 0

            def load(wsrc, e_, q):
                nonlocal rk
                slot = rk % R
                rk += 1
                S.add("pool", lambda e, slot=slot: e.dma_start(
                    out=ring[slot][:], in_=wsrc[e_].rearrange("(k p) n -> p k n", p=128)[:, :, q * 256:(q + 1) * 256]),
                    w=[("ring", slot)], dma="wr")
                return slot
            tcount = 0
            for ex_ in range(NE):
                for q in range(4):
                    sg_ = load(wgate, ex_, q)
                    su_ = load(wup, ex_, q)
                    for fcl in range(2):
                        fc = 2 * q + fcl
                        bgcol = (0 * NE + ex_) * 8 + fc
                        bucol = (1 * NE + ex_) * 8 + fc
                        for tg in range(NG):
                            tb = tcount % 2
                            tcount += 1
                            pg, pu = tb, 2 + tb

                            def mmgu(e, slot=None, pk=None, fcl=fcl, tg=tg):
                                ins = None
                                for kc in range(8):
                                    ins = e.matmul(pss[pk][:, :], ring[slot][:, kc, fcl * 128:(fcl + 1) * 128],
                                                   x1T[:, kc, tg * 512:(tg + 1) * 512], start=(kc == 0), stop=(kc == 7))
                                return ins
                            S.add("pe", lambda e, f=mmgu, s=sg_, pk=pg: f(e, slot=s, pk=pk), r=[("ring", sg_)], w=[("ps", pg)])
                            S.add("pe", lambda e, f=mmgu, s=su_, pk=pu: f(e, slot=s, pk=pk), r=[("ring", su_)], w=[("ps", pu)])
                            S.add("dve", lambda e, tb=tb, pg=pg, c=bgcol: e.tensor_scalar(
                                out=gs[tb][:], in0=pss[pg][:, :], scalar1=bgut[:, c:c + 1], scalar2=SW_LIMIT, op0=ALU.add, op1=ALU.min),
                                r=[("ps", pg), "bgut"], w=[("gs", tb)])
                            S.add("act", lambda e, tb=tb: e.activation(out=sg[tb][:], in_=gs[tb][:], func=AF.Sigmoid, scale=SW_ALPHA),
                                  r=[("gs", tb)], w=[("sg", tb)])
                            S.add("dve", lambda e, tb=tb, pu=pu, c=bucol: e.tensor_scalar(
                                out=uu[tb][:], in0=pss[pu][:, :], scalar1=bgut[:, c:c + 1], scalar2=SW_LIMIT, op0=ALU.add, op1=ALU.min),
                                r=[("ps", pu), "bgut"], w=[("uu", tb)])
                            S.add("dve", lambda e, tb=tb: e.tensor_scalar(
                                out=uu[tb][:], in0=uu[tb][:], scalar1=-SW_LIMIT, scalar2=1.0, op0=ALU.max, op1=ALU.add),
                                r=[("uu", tb)], w=[("uu", tb)])
                            S.add("dve", lambda e, tb=tb: e.tensor_tensor(out=gs[tb][:], in0=gs[tb][:], in1=sg[tb][:], op=ALU.mult),
                                  r=[("gs", tb), ("sg", tb)], w=[("gs", tb)])
                            S.add("dve", lambda e, tb=tb, fc=fc, tg=tg: e.tensor_tensor(
                                out=actT[:, fc, tg * 512:(tg + 1) * 512], in0=gs[tb][:], in1=uu[tb][:], op=ALU.mult),
                                r=[("gs", tb), ("uu", tb)], w=[("actT", tg)])
                for q in range(4):
                    sd_ = load(wdown, ex_, q)
                    for tt in range(NT):
                        pk = 4 + (tt % 2)

                        def mmd(e, slot=sd_, tt=tt, pk=pk):
                            ins = None
                            for fc in range(8):
                                ins = e.matmul(pss[pk][:, 0:256], actT[:, fc, tt * 128:(tt + 1) * 128], ring[slot][:, fc, :],
                                               start=(fc == 0), stop=(fc == 7))
                            return ins
                        S.add("pe", mmd, r=[("ring", sd_), ("actT", tt // 4)], w=[("ps", pk)])
                        S.add("dve", lambda e, tt=tt, q=q, pk=pk, ex_=ex_: e.scalar_tensor_tensor(
                            out=hres[:, tt, q * 256:(q + 1) * 256], in0=pss[pk][:, 0:256], scalar=comb[:, tt, ex_:ex_ + 1],
                            in1=hres[:, tt, q * 256:(q + 1) * 256], op0=ALU.mult, op1=ALU.add),
                            r=[("ps", pk), ("hres", tt)], w=[("hres", tt)])
            S.emit()

        for i in range(2):
            S.add("sp", lambda e, i=i: e.dma_start(out=lnB[:, i, :], in_=lnp[4 + i:5 + i, :].partition_broadcast(128)),
                  w=["lnB"], dma="misc")
        for tt in range(NT):
            _ln_tile(S, hres[:, tt, :], st6, mv, rstd, lnB[:, 0, :], lnB[:, 1, :], ("hres", tt), "ln")
            S.add("sp", lambda e, tt=tt: e.dma_start(out=out[tt * 128:(tt + 1) * 128, :], in_=hres[:, tt, :]),
                  r=[("hres", tt)], dma="out")
        S.emit()
    return nc


def build_p1(NX=8192, do_attn=True, do_ssm=True, dbg=(), ctx=None):
    NXT = NX // 128
    NQG = NX // 512
    NTOKS = N_META + NX
    NCH = NTOKS // 8
    NKT = NXT + 1
    fused = ctx is not None
    nc = ctx["nc"] if fused else bass.Bass("TRN2", target_bir_lowering=False)
    ODT = BF16 if fused else F32
    dbgs = set(dbg) if isinstance(dbg, (tuple, list, set)) else {dbg}

    def din(name, shape, dtype=F32):
        return nc.dram_tensor(name, shape, dtype, kind="ExternalInput").ap()
    x = din("x", [NX, 1024])
    meta = din("meta", [N_META, 1024])
    lnp = din("lnp", [128, 16])
    lnrow = din("lnrow", [2, 1024])
    w1 = din("w1", [1024, 514])
    bfn = din("bf", [2, 1])
    lamp = din("lamp", [128, 16])
    logdt = din("logdt", [1, 8])
    bstk_d = din("bstk", [128, 128])
    cstk_d = din("cstk", [128, 128])
    dvec_d = din("dvec", [128, 1])
    cst = din("cst", [128, 128 * 4 + 8 * 128 + 4])
    if fused:
        yatt = yssm = None
    else:
        yatt = nc.dram_tensor("yatt", [128, NX], F32, kind="ExternalOutput").ap()
        yssm = nc.dram_tensor("yssm", [128, NX], F32, kind="ExternalOutput").ap()
    NC_ = 128 * 4 + 8 * 128 + 4
    CH = NX // 4

    def ydst(which, r0, r1, t0, n):
        if fused:
            k = t0 // CH
            assert (t0 + n - 1) // CH == k
            return ctx["ybuf"][k][which * 128 + r0:which * 128 + r1, t0 - k * CH:t0 - k * CH + n]
        return (yatt if which == 0 else yssm)[r0:r1, t0:t0 + n]

    with ExitStack() as stack:
        S = ctx["S"] if fused else Sched(nc, stack)

        def sb(name, shape, dtype):
            return stack.enter_context(nc.sbuf_tensor(name, shape, dtype))

        def psum(name, shape, dtype):
            return stack.enter_context(nc.psum_tensor(name, shape, dtype))
        QT = [sb(f"QT{h}", [65, NX], BF16) for h in range(2)]
        KT = [sb(f"KT{h}", [65, NTOKS], BF16) for h in range(2)]
        VV = [sb(f"VV{h}", [128, NKT, 128], BF16) for h in range(2)]
        uT = sb("uT", [128, NTOKS], BF16)
        NFcol = sb("NFcol", [128, NKT, 2], F32)
        cst32 = sb("cst32", [128, NC_], F32)
        identb = sb("identb", [128, 128], BF16)
        trib = sb("trib", [128, 128], BF16)
        ident32 = cst32[:, 0:128]
        Jm = cst32[:, 128:256]
        tri32 = cst32[:, 256:384]
        bmask = cst32[:, 512:512 + 1024]
        sgnR = cst32[:, 1536:1537]
        pss = [psum(f"ps{i}", [128, 512], F32) for i in range(6)]
        pT = [psum(f"pT{i}", [128, 1024], BF16) for i in range(2)]
        S.stream("x", 2)
        S.stream("misc", 0)
        S.stream("row", 4)
        S.stream("out", 2)
        S.stream("rl", 2)

        with ExitStack() as st2:
            def sb2(name, shape, dtype):
                return st2.enter_context(nc.sbuf_tensor(name, shape, dtype))
            w1t = sb2("w1t", [128, 8, 576], BF16)
            lnpt = sb2("lnpt", [128, 16], F32)
            lnB = sb2("lnB1", [128, 2, 1024], F32)
            xt = [sb2(f"xt{i}", [128, 1024], F32) for i in range(2)]
            xn = [sb2(f"xn{i}", [128, 1024], BF16) for i in range(2)]
            hT = sb2("hT", [128, 8, 512], BF16)
            st6 = sb2("st6", [128, 2, 6], F32)
            mv = sb2("mv", [128, 2], F32)
            rstd = sb2("rstd", [128, 1], F32)
            negbf = sb2("negbf", [2, 1], F32)
            e1 = sb2("e1", [2, 512], F32)
            nfg = sb2("nfg", [2, 512], F32)
            carry = sb2("carry", [2, 1], F32)
            ones2 = sb2("ones2", [2, 512], F32)
            fb = sb2("fb", [2, 512], BF16)

            S.add("sp", lambda e: e.dma_start(out=cst32[:], in_=cst[:, :]), w=["cst"], dma="misc")
            S.add("sp", lambda e: e.dma_start(out=lnpt[:], in_=lnp[:, :]), w=["lnpt"], dma="misc")
            for i in range(2):
                S.add("sp", lambda e, i=i: e.dma_start(out=lnB[:, i, :], in_=lnrow[i:i + 1, :].partition_broadcast(128)), w=["lnB"], dma="misc")
            S.add("sp", lambda e: e.dma_start(out=negbf[:], in_=bfn[:, :]), w=["negbf"], dma="misc")
            S.add("dve", lambda e: e.memset(w1t[:, :, 512:576], 0.0), w=["w1t"])
            S.add("pool", lambda e: e.dma_start(out=w1t[:, :, 0:514], in_=w1.rearrange("(k p) n -> p k n", p=128)), w=["w1t"], dma="misc")
            S.add("dve", lambda e: e.tensor_copy(out=identb[:], in_=ident32), r=["cst"], w=["identb"])
            S.add("dve", lambda e: e.tensor_copy(out=trib[:], in_=tri32), r=["cst"], w=["trib"])
            S.add("dve", lambda e: e.tensor_scalar(out=negbf[:], in0=negbf[:], scalar1=-1.0, scalar2=None, op0=ALU.mult),
                  r=["negbf"], w=["negbf"])
            S.add("dve", lambda e: e.memset(ones2[:], 1.0), w=["ones2"])
            S.add("dve", lambda e: e.memset(carry[:], 0.0), w=["carry"])
            for h in range(2):
                S.add("dve", lambda e, h=h: e.memset(KT[h][64:65, :], 1.0), w=[("KTa", h)])
                S.add("pool", lambda e, h=h: e.memset(VV[h][:, :, 64:128], 1.0), w=[("VVo", h)])

            for g in range(NQG + 1):
                n = N_META if g == 0 else 512
                ntile = 1 if g == 0 else 4
                kc0 = 0 if g == 0 else N_META + (g - 1) * 512
                qc0 = (g - 1) * 512
                for ti in range(ntile):
                    np_ = N_META if g == 0 else 128
                    b = (g * 4 + ti) % 2
                    if g == 0:
                        S.add("sp", lambda e, b=b: e.dma_start(out=xt[b][0:N_META, :], in_=meta[:, :]), w=[("xt", b)], dma="x")
                    else:
                        r0 = (g - 1) * 512 + ti * 128
                        S.add("sp", lambda e, b=b, r0=r0: e.dma_start(out=xt[b][:, :], in_=x[r0:r0 + 128, :]), w=[("xt", b)], dma="x")
                    xa = xt[b][0:np_, :]
                    S.add("dve", lambda e, xa=xa, np_=np_: e.bn_stats(out=st6[0:np_, 0, :], in_=xa[:, 0:512]), r=[("xt", b)], w=["st0"])
                    S.add("dve", lambda e, xa=xa, np_=np_: e.bn_stats(out=st6[0:np_, 1, :], in_=xa[:, 512:1024]), r=[("xt", b)], w=["st1"])
                    S.add("dve", lambda e, np_=np_: e.bn_aggr(out=mv[0:np_, :], in_=st6[0:np_, :, :]), r=["st0", "st1"], w=["mv"], fence=True)
                    S.add("dve", lambda e, np_=np_: e.tensor_scalar(out=rstd[0:np_, :], in0=mv[0:np_, 1:2], scalar1=LN_EPS, scalar2=None,
                                                                    op0=ALU.add), r=["mv"], w=["rs"], fence=True)
                    S.add("act", lambda e, np_=np_: e.sqrt(out=rstd[0:np_, :], in_=rstd[0:np_, :]), r=["rs"], w=["rs"], fence=True)
                    S.add("dve", lambda e, np_=np_: e.reciprocal(out=rstd[0:np_, :], in_=rstd[0:np_, :]), r=["rs"], w=["rs"], fence=True)
                    S.add("dve", lambda e, xa=xa, b=b, np_=np_: e.tensor_scalar(
                        out=xa, in0=xa, scalar1=mv[0:np_, 0:1], scalar2=rstd[0:np_, 0:1],
                        op0=ALU.subtract, op1=ALU.mult), r=[("xt", b), "mv", "rs"], w=[("xt", b)])
                    S.add("dve", lambda e, xa=xa, np_=np_: e.tensor_tensor(out=xa, in0=xa, in1=lnB[0:np_, 0, :], op=ALU.mult),
                          r=[("xt", b), "lnB"], w=[("xt", b)])
                    S.add("dve", lambda e, xa=xa, np_=np_: e.tensor_tensor(out=xa, in0=xa, in1=lnB[0:np_, 1, :], op=ALU.add),
                          r=[("xt", b), "lnB"], w=[("xt", b)])
                    S.add("act", lambda e, xa=xa, b=b, np_=np_: e.copy(out=xn[b][0:np_, :], in_=xa), r=[("xt", b)], w=[("xn", b)])

                    def tr(e, b=b, np_=np_):
                        ins = None
                        for kc in range(8):
                            ins = e.transpose(out=pT[b][:, kc * 128:kc * 128 + np_], in_=xn[b][0:np_, kc * 128:(kc + 1) * 128],
                                              identity=identb[0:np_, 0:np_])
                        return ins
                    S.add("pe", tr, r=[("xn", b), "identb"], w=[("pT", b)])
                    S.add("act", lambda e, b=b, ti=ti, np_=np_: e.copy(
                        out=hT[:, :, ti * 128:ti * 128 + np_], in_=pT[b][:, :].rearrange("p (k t) -> p k t", k=8)[:, :, 0:np_]),
                        r=[("pT", b)], w=["hT"])

                if 8 in dbgs:
                    S.emit()
                def proj(e, c_lo, c_hi, pk, n=n):
                    ins = None
                    for kc in range(8):
                        ins = e.matmul(pss[pk][0:c_hi - c_lo, 0:n], w1t[:, kc, c_lo:c_hi], hT[:, kc, 0:n],
                                       start=(kc == 0), stop=(kc == 7))
                    return ins
                for h in range(2):
                    if g > 0:
                        S.add("pe", lambda e, f=proj, h=h: f(e, h * 64, h * 64 + 64, h), r=["w1t", "hT"], w=[("ps", h)])
                        S.add("act", lambda e, h=h, qc0=qc0: e.mul(out=QT[h][0:64, qc0:qc0 + 512], in_=pss[h][0:64, :], mul=0.125),
                              r=[("ps", h)], w=[("QT", h)])
                    S.add("pe", lambda e, f=proj, h=h: f(e, 128 + h * 64, 128 + h * 64 + 64, 2 + h), r=["w1t", "hT"], w=[("ps", 2 + h)])
                    S.add("dve", lambda e, h=h, kc0=kc0, n=n: e.tensor_copy(out=KT[h][0:64, kc0:kc0 + n], in_=pss[2 + h][0:64, 0:n]),
                          r=[("ps", 2 + h)], w=[("KT", h)])
                S.add("pe", lambda e, f=proj: f(e, 384, 512, 4), r=["w1t", "hT"], w=[("ps", 4)])
                S.add("act", lambda e, kc0=kc0, n=n: e.copy(out=uT[:, kc0:kc0 + n], in_=pss[4][:, 0:n]), r=[("ps", 4)], w=["uT"])
                if 1 in dbgs:
                    continue
                S.add("pe", lambda e, f=proj: f(e, 512, 576, 5), r=["w1t", "hT"], w=[("ps", 5)])
                S.add("act", lambda e, n=n: e.activation(out=e1[:, 0:n], in_=pss[5][0:2, 0:n], func=AF.Exp, bias=negbf[:, 0:1], scale=-1.0),
                      r=[("ps", 5), "negbf"], w=["e1"])
                if 11 in dbgs:
                    continue
                S.add("dve", lambda e, n=n: e.tensor_scalar(out=e1[:, 0:n], in0=e1[:, 0:n], scalar1=1.0, scalar2=None, op0=ALU.add),
                      r=["e1"], w=["e1"])
                S.add("act", lambda e, n=n: e.activation(out=e1[:, 0:n], in_=e1[:, 0:n], func=AF.Ln), r=["e1"], w=["e1"])
                if 12 in dbgs:
                    continue
                S.add("dve", lambda e, n=n: e.tensor_tensor_scan(out=nfg[:, 0:n], data0=ones2[:, 0:n], data1=e1[:, 0:n], initial=carry[:, 0:1],
                                                                 op0=ALU.mult, op1=ALU.add), r=["e1", "carry", "ones2"], w=["nfg"])
                if 13 in dbgs:
                    continue
                S.add("dve", lambda e, n=n: e.tensor_copy(out=carry[:], in_=nfg[:, n - 1:n]), r=["nfg"], w=["carry"])
                if 2 in dbgs:
                    continue
                if g > 0:
                    S.add("dve", lambda e: e.tensor_scalar(out=fb[:], in0=nfg[:], scalar1=-1.0, scalar2=None, op0=ALU.mult),
                          r=["nfg"], w=["fb"])
                    for h in range(2):
                        S.add("sp", lambda e, h=h, qc0=qc0: e.dma_start(out=QT[h][64:65, qc0:qc0 + 512], in_=fb[h:h + 1, :]),
                              r=["fb"], w=[("QTa", h)], dma="row")
                if 3 in dbgs:
                    continue
                for ti in range(ntile):
                    np_ = N_META if g == 0 else 128
                    kt = 0 if g == 0 else 1 + (g - 1) * 4 + ti
                    S.add("pe", lambda e, ti=ti, np_=np_: e.transpose(out=pss[5][0:np_, 256 + 2 * ti:258 + 2 * ti],
                                                                      in_=nfg[0:2, ti * 128:ti * 128 + np_], identity=ident32[0:2, 0:2]),
                          r=["nfg"], w=[("ps", 5)])
                    S.add("dve", lambda e, ti=ti, np_=np_, kt=kt: e.tensor_copy(out=NFcol[0:np_, kt, :], in_=pss[5][0:np_, 256 + 2 * ti:258 + 2 * ti]),
                          r=[("ps", 5)], w=["NFcol"])
                if 4 in dbgs:
                    continue
                for ti in range(ntile):
                    np_ = N_META if g == 0 else 128
                    kt = 0 if g == 0 else 1 + (g - 1) * 4 + ti
                    pk = ti % 2

                    def mmv(e, ti=ti, np_=np_, pk=pk):
                        ins = None
                        for kc in range(8):
                            ins = e.matmul(pss[pk][0:np_, 0:128], hT[:, kc, ti * 128:ti * 128 + np_], w1t[:, kc, 256:384],
                                           start=(kc == 0), stop=(kc == 7))
                        return ins
                    S.add("pe", mmv, r=["w1t", "hT"], w=[("ps", pk)])
                    if 5 in dbgs:
                        continue
                    S.add("act", lambda e, np_=np_, kt=kt, pk=pk: e.copy(out=VV[0][0:np_, kt, 0:64], in_=pss[pk][0:np_, 0:64]),
                          r=[("ps", pk), ("VVo", 0)], w=[("VV", 0)])
                    if 6 in dbgs:
                        continue
                    S.add("act", lambda e, np_=np_, kt=kt, pk=pk: e.copy(out=VV[1][0:np_, kt, 0:64], in_=pss[pk][0:np_, 64:128]),
                          r=[("ps", pk), ("VVo", 1)], w=[("VV", 1)])
            if 9 in dbgs:
                d1 = nc.dram_tensor("d_nf", [128, NKT * 2], F32, kind="ExternalOutput").ap()
                d2 = nc.dram_tensor("d_qt", [65, NX], BF16, kind="ExternalOutput").ap()
                d3 = nc.dram_tensor("d_kt", [65, NTOKS], BF16, kind="ExternalOutput").ap()
                d4 = nc.dram_tensor("d_vv", [128, NKT * 128], BF16, kind="ExternalOutput").ap()
                d5 = nc.dram_tensor("d_ut", [128, NTOKS], BF16, kind="ExternalOutput").ap()
                S.add("sp", lambda e: e.dma_start(out=d1[:, :], in_=NFcol[:, :, :].rearrange("p a b -> p (a b)")), r=["NFcol"], dma="out")
                S.add("sp", lambda e: e.dma_start(out=d2[:, :], in_=QT[0][:, :]), r=[("QT", 0), ("QTa", 0)], dma="out")
                S.add("sp", lambda e: e.dma_start(out=d3[:, :], in_=KT[0][:, :]), r=[("KT", 0), ("KTa", 0)], dma="out")
                S.add("sp", lambda e: e.dma_start(out=d4[:, :], in_=VV[0][:, :, :].rearrange("p a b -> p (a b)")), r=[("VV", 0), ("VVo", 0)], dma="out")
                S.add("sp", lambda e: e.dma_start(out=d5[:, :], in_=uT[:, :]), r=["uT"], dma="out")
                d6 = nc.dram_tensor("d_xt", [128, 1024], F32, kind="ExternalOutput").ap()
                d7 = nc.dram_tensor("d_xn", [128, 1024], BF16, kind="ExternalOutput").ap()
                d8 = nc.dram_tensor("d_ht", [128, 8 * 512], BF16, kind="ExternalOutput").ap()
                d9 = nc.dram_tensor("d_mv", [128, 3], F32, kind="ExternalOutput").ap()
                S.add("sp", lambda e: e.dma_start(out=d6[:, :], in_=xt[1][:, :]), r=[("xt", 1)], dma="out")
                S.add("sp", lambda e: e.dma_start(out=d7[:, :], in_=xn[1][:, :]), r=[("xn", 1)], dma="out")
                S.add("sp", lambda e: e.dma_start(out=d8[:, :], in_=hT[:, :, :].rearrange("p a b -> p (a b)")), r=["hT"], dma="out")
                S.add("sp", lambda e: e.dma_start(out=d9[:, 0:2], in_=mv[:, :], allow_slow_non_contiguous=True), r=["mv"], dma="out")
                S.add("sp", lambda e: e.dma_start(out=d9[:, 2:3], in_=rstd[:, :], allow_slow_non_contiguous=True), r=["rs"], dma="out")
            S.emit()

        if do_ssm:
            _ssm_block(nc, S, stack, locals())
        if do_attn:
            with ExitStack() as st2:
                def sb2(name, shape, dtype):
                    return st2.enter_context(nc.sbuf_tensor(name, shape, dtype))
                NPT = 3
                pt = [sb2(f"pt{i}", [128, 512], BF16) for i in range(NPT)]
                rl = [sb2(f"rl{i}", [128, 512], F32) for i in range(2)]
                rl2 = [sb2(f"rlb{i}", [64, 512], F32) for i in range(2)]
                ot = [sb2(f"ot{i}", [64, 512], ODT) for i in range(2)]
                it = 0
                gi = 0
                for h in range(2):
                    for i in range(NQG):
                        ab = 4 + (gi % 2)
                        ob = gi % 2
                        gi += 1
                        nblk = 4 * i + 5
                        blks = []
                        for jb in range(nblk):
                            nk = N_META if jb == 0 else 128
                            kcol = 0 if jb == 0 else N_META + (jb - 1) * 128
                            jt = jb - 1
                            diag = jb > 0 and jt >= 4 * i
                            qs = (jt - 4 * i) * 128 if diag else 0
                            blks.append(dict(jb=jb, nk=nk, kcol=kcol, qs=qs, diag=diag, sbk=it % 2, pb=it % NPT))
                            it += 1

                        def emit_s(bk, h=h, i=i):
                            nk, kcol, qs, sbk, pb, jb = bk["nk"], bk["kcol"], bk["qs"], bk["sbk"], bk["pb"], bk["jb"]
                            S.add("pe", lambda e: e.matmul(
                                pss[sbk][0:nk, qs:512], KT[h][0:65, kcol:kcol + nk], QT[h][0:65, i * 512 + qs:(i + 1) * 512],
                                start=True, stop=True), w=[("ps", sbk)])
                            S.add("act", lambda e: e.activation(
                                out=pt[pb][0:nk, qs:512], in_=pss[sbk][0:nk, qs:512], func=AF.Exp,
                                bias=NFcol[0:nk, jb, h:h + 1], scale=1.0), r=[("ps", sbk)], w=[("pt", pb)])
                            if bk["diag"]:
                                S.add("dve", lambda e: e.tensor_tensor(
                                    out=pt[pb][:, qs:qs + 128], in0=pt[pb][:, qs:qs + 128], in1=trib[:], op=ALU.mult),
                                    r=[("pt", pb)], w=[("pt", pb)])

                        def emit_pv(bk, h=h, ab=ab, nblk=nblk):
                            nk, qs, pb, jb = bk["nk"], bk["qs"], bk["pb"], bk["jb"]
                            S.add("pe", lambda e: e.matmul(
                                pss[ab][:, qs:512], VV[h][0:nk, jb, :], pt[pb][0:nk, qs:512],
                                start=(jb == 0), stop=(jb == nblk - 1)), r=[("pt", pb)], w=[("ps", ab)])
                        emit_s(blks[0])
                        for jb in range(nblk):
                            if jb + 1 < nblk:
                                emit_s(blks[jb + 1])
                            emit_pv(blks[jb])
                        S.add("dve", lambda e, ab=ab, ob=ob: e.reciprocal(out=rl[ob][64:128, :], in_=pss[ab][64:128, :]),
                              r=[("ps", ab)], w=[("rl", ob)])
                        S.add("sp", lambda e, ob=ob: e.dma_start(out=rl2[ob][:, :], in_=rl[ob][64:128, :]),
                              r=[("rl", ob)], w=[("rl2", ob)], dma="rl")
                        S.add("dve", lambda e, ab=ab, ob=ob: e.tensor_tensor(out=ot[ob][:, :], in0=pss[ab][0:64, :], in1=rl2[ob][:, :], op=ALU.mult),
                              r=[("ps", ab), ("rl2", ob)], w=[("ot", ob)])
                        S.add("sp", lambda e, h=h, i=i, ob=ob: e.dma_start(out=ydst(0, h * 64, (h + 1) * 64, i * 512, 512), in_=ot[ob][:, :]),
                              r=[("ot", ob)], dma="out")
                S.emit()
    return nc


def _ssm_block(nc, S, stack, L):
    uT, pss, cst32 = L["uT"], L["pss"], L["cst32"]
    lamp, logdt, bstk_d, cstk_d, dvec_d, yssm = L["lamp"], L["logdt"], L["bstk_d"], L["cstk_d"], L["dvec_d"], L["yssm"]
    NX, NCH, NTOKS = L["NX"], L["NCH"], L["NTOKS"]
    ident32 = cst32[:, 0:128]
    Jm = cst32[:, 128:256]
    bmask = cst32[:, 512:512 + 1024]
    sgnR = cst32[:, 1536:1537]
    NLEV = max(1, (NCH - 1).bit_length())
    POW_SEQ = list(range(9))
    NPW = 9 + NLEV
    PI = math.pi
    with ExitStack() as st2:
        def sb2(name, shape, dtype):
            return st2.enter_context(nc.sbuf_tensor(name, shape, dtype))
        lam = sb2("lam", [128, 16], F32)
        dtb = sb2("dtb", [128, 8], F32)
        t0 = sb2("t0", [128, 8], F32)
        t1 = sb2("t1", [128, 8], F32)
        t2 = sb2("t2", [128, 8], F32)
        t3 = sb2("t3", [128, 8], F32)
        pw_re = sb2("pw_re", [128, NPW + 1, 8], F32)
        pw_im = sb2("pw_im", [128, NPW + 1, 8], F32)
        pw_ims = sb2("pw_ims", [128, NPW + 1, 8], F32)
        bstk = sb2("bstk_sb", [128, 128], F32)
        cstk = sb2("cstk_sb", [128, 128], F32)
        dvec = sb2("dvec_sb", [128, 1], F32)
        bpad = sb2("bpad", [128, 128], BF16)
        cpad = sb2("cpad", [128, 128], BF16)
        Rb = sb2("Rb", [128, 10, 128], BF16)
        Rh = sb2("Rh", [128, NLEV, 128], F32)
        bbar = sb2("bbar", [128, 128], BF16)
        lhsA = sb2("lhsA", [128, 8, 128], BF16)
        CL0 = sb2("CL0", [128, 128], BF16)
        CLall = sb2("CLall", [128, 8, 8, 128], BF16)
        Wb = sb2("Wb", [128, 8, 128], BF16)
        Ea = sb2("Ea", [128, NCH], F32)
        Eb = sb2("Eb", [128, NCH], F32)
        Sin = sb2("Sin", [128, 8, NCH], BF16)
        yo = [sb2("yo0", [128, 4096], L["ODT"])]
        S.stream("ssm", 0)
        S.add("sp", lambda e: e.dma_start(out=lam[:], in_=lamp[:, :]), w=["lam"], dma="ssm")
        S.add("sp", lambda e: e.dma_start(out=dtb[:], in_=logdt[0:1, :].partition_broadcast(128)), w=["dtb"], dma="ssm")
        S.add("sp", lambda e: e.dma_start(out=bstk[:], in_=bstk_d[:, :]), w=["bstk"], dma="ssm")
        S.add("sp", lambda e: e.dma_start(out=cstk[:], in_=cstk_d[:, :]), w=["cstk"], dma="ssm")
        S.add("sp", lambda e: e.dma_start(out=dvec[:], in_=dvec_d[:, :]), w=["dvec"], dma="ssm")
        are, aim = lam[:, 0:8], lam[:, 8:16]

        def V(fn, r, w):
            S.add("dve", fn, r=r, w=w)

        def tt(out, a, b, op, r, w):
            V(lambda e: e.tensor_tensor(out=out, in0=a, in1=b, op=op), r, w)

        def ts(out, a, s1, s2, op0, op1, r, w):
            if op1 is None:
                V(lambda e: e.tensor_scalar(out=out, in0=a, scalar1=s1, scalar2=None, op0=op0), r, w)
            else:
                V(lambda e: e.tensor_scalar(out=out, in0=a, scalar1=s1, scalar2=s2, op0=op0, op1=op1), r, w)
        S.add("act", lambda e: e.activation(out=dtb[:], in_=dtb[:], func=AF.Exp), r=["dtb"], w=["dtb"])
        tt(t0[:], are, dtb[:], ALU.mult, ["lam", "dtb"], ["t0"])
        S.add("act", lambda e: e.activation(out=t0[:], in_=t0[:], func=AF.Exp), r=["t0"], w=["t0"])
        tt(t1[:], aim, dtb[:], ALU.mult, ["lam", "dtb"], ["t1"])
        MAGIC = 12582912.0
        ts(t1[:], t1[:], 1.0 / (2.0 * PI), None, ALU.mult, None, ["t1"], ["t1"])
        ts(t3[:], t1[:], MAGIC, None, ALU.add, None, ["t1"], ["t3"])
        ts(t3[:], t3[:], -MAGIC, None, ALU.add, None, ["t3"], ["t3"])
        tt(t2[:], t1[:], t3[:], ALU.subtract, ["t1", "t3"], ["t2"])
        S.add("act", lambda e: e.activation(out=t2[:], in_=t2[:], func=AF.Sin, scale=2.0 * PI), r=["t2"], w=["t2"])
        ts(t1[:], t1[:], 0.25, None, ALU.add, None, ["t1"], ["t1"])
        ts(t3[:], t1[:], MAGIC, None, ALU.add, None, ["t1"], ["t3"])
        ts(t3[:], t3[:], -MAGIC, None, ALU.add, None, ["t3"], ["t3"])
        tt(t3[:], t1[:], t3[:], ALU.subtract, ["t1", "t3"], ["t3"])
        S.add("act", lambda e: e.activation(out=t3[:], in_=t3[:], func=AF.Sin, scale=2.0 * PI), r=["t3"], w=["t3"])
        tt(pw_re[:, 1, :], t0[:], t3[:], ALU.mult, ["t0", "t3"], ["pw"])
        tt(pw_im[:, 1, :], t0[:], t2[:], ALU.mult, ["t0", "t2"], ["pw"])
        V(lambda e: e.memset(pw_re[:, 0, :], 1.0), [], ["pw"])
        V(lambda e: e.memset(pw_im[:, 0, :], 0.0), [], ["pw"])

        def cmul(o, a, b):
            tt(t0[:], pw_re[:, a, :], pw_re[:, b, :], ALU.mult, ["pw"], ["t0"])
            tt(t1[:], pw_im[:, a, :], pw_im[:, b, :], ALU.mult, ["pw"], ["t1"])
            tt(t2[:], pw_re[:, a, :], pw_im[:, b, :], ALU.mult, ["pw"], ["t2"])
            tt(t3[:], pw_im[:, a, :], pw_re[:, b, :], ALU.mult, ["pw"], ["t3"])
            tt(pw_re[:, o, :], t0[:], t1[:], ALU.subtract, ["t0", "t1"], ["pw"])
            tt(pw_im[:, o, :], t2[:], t3[:], ALU.add, ["t2", "t3"], ["pw"])
        for m in range(2, 9):
            cmul(m, m - 1, 1)
        for l in range(1, NLEV):
            cmul(8 + l, 8 + l - 1, 8 + l - 1)
        CO = NPW
        tt(t0[:], are, are, ALU.mult, ["lam"], ["t0"])
        tt(t1[:], aim, aim, ALU.mult, ["lam"], ["t1"])
        tt(t0[:], t0[:], t1[:], ALU.add, ["t0", "t1"], ["t0"])
        V(lambda e: e.reciprocal(out=t0[:], in_=t0[:]), ["t0"], ["t0"])
        ts(t1[:], pw_re[:, 1, :], -1.0, None, ALU.add, None, ["pw"], ["t1"])
        tt(t2[:], t1[:], are, ALU.mult, ["t1", "lam"], ["t2"])
        tt(t3[:], pw_im[:, 1, :], aim, ALU.mult, ["pw", "lam"], ["t3"])
        tt(t2[:], t2[:], t3[:], ALU.add, ["t2", "t3"], ["t2"])
        tt(pw_re[:, CO, :], t2[:], t0[:], ALU.mult, ["t2", "t0"], ["pw"])
        tt(t2[:], pw_im[:, 1, :], are, ALU.mult, ["pw", "lam"], ["t2"])
        tt(t3[:], t1[:], aim, ALU.mult, ["t1", "lam"], ["t3"])
        tt(t2[:], t2[:], t3[:], ALU.subtract, ["t2", "t3"], ["t2"])
        tt(pw_im[:, CO, :], t2[:], t0[:], ALU.mult, ["t2", "t0"], ["pw"])
        V(lambda e: e.tensor_scalar(out=pw_ims[:], in0=pw_im[:], scalar1=sgnR, scalar2=None, op0=ALU.mult), ["pw", "cst"], ["pws"])

        def build_R(out_ap, idx, g, key):
            V(lambda e: e.tensor_scalar(out=out_ap, in0=ident32, scalar1=pw_re[:, idx, g:g + 1], scalar2=None, op0=ALU.mult),
              ["pw", "cst"], [key])
            V(lambda e: e.scalar_tensor_tensor(out=out_ap, in0=Jm, scalar=pw_ims[:, idx, g:g + 1], in1=out_ap,
                                               op0=ALU.mult, op1=ALU.add), ["pws", "cst", key], [key])

        NT3 = [(c, min(342, NCH - c)) for c in range(0, NCH, 342)]
        evk = 0
        for g in range(8):
            for m in range(9):
                build_R(Rb[:, m, :], m, g, ("Rb", m))
            build_R(Rb[:, 9, :], CO, g, ("Rb", 9))
            for l in range(NLEV):
                build_R(Rh[:, l, :], 8 + l, g, ("Rh", l))
            tt(bpad[:], bstk[:], bmask[:, g * 128:(g + 1) * 128], ALU.mult, ["bstk", "cst"], ["bpad"])
            tt(cpad[:], cstk[:], bmask[:, g * 128:(g + 1) * 128], ALU.mult, ["cstk", "cst"], ["cpad"])
            S.add("pe", lambda e: e.matmul(pss[0][:, 0:128], Rb[:, 9, :], bpad[:], start=True, stop=True),
                  r=[("Rb", 9), "bpad"], w=[("ps", 0)])
            S.add("act", lambda e: e.copy(out=bbar[:], in_=pss[0][:, 0:128]), r=[("ps", 0)], w=["bbar"])
            for i in range(8):
                pk = i % 2
                S.add("pe", lambda e, i=i, pk=pk: e.matmul(pss[pk][:, 128:256], bbar[:], Rb[:, 7 - i, :], start=True, stop=True),
                      r=["bbar", ("Rb", 7 - i)], w=[("ps", pk)])
                if i % 2 == 0:
                    S.add("act", lambda e, i=i, pk=pk: e.copy(out=lhsA[:, i, :], in_=pss[pk][:, 128:256]), r=[("ps", pk)], w=[("lhsA", i)])
                else:
                    S.add("dve", lambda e, i=i, pk=pk: e.tensor_copy(out=lhsA[:, i, :], in_=pss[pk][:, 128:256]), r=[("ps", pk)], w=[("lhsA", i)])
            for d in range(9):
                pk = d % 2
                dst = CL0[:] if d == 0 else CLall[:, g, d - 1, :]
                S.add("pe", lambda e, d=d, pk=pk: e.matmul(pss[pk][:, 256:384], Rb[:, d, :], cpad[:], start=True, stop=True),
                      r=[("Rb", d), "cpad"], w=[("ps", pk)])
                S.add("dve", lambda e, dst=dst, pk=pk: e.tensor_scalar(out=dst, in0=pss[pk][:, 256:384], scalar1=sgnR, scalar2=None, op0=ALU.mult),
                      r=[("ps", pk), "cst"], w=[("CL", g, d)])
            for d in range(8):
                bank = 2 + d // 4
                col = (d % 4) * 128
                S.add("pe", lambda e, d=d, bank=bank, col=col, g=g: e.matmul(
                    pss[bank][:, col:col + 128], bbar[:], (CL0[:] if d == 0 else CLall[:, g, d - 1, :]),
                    start=(g == 0), stop=(g == 7)), r=["bbar", ("CL", g, d)], w=[("ps", bank)])
            for (c0, n) in NT3:
                pk = 4 + (evk % 2)
                evk += 1

                def mmA(e, c0=c0, n=n, pk=pk):
                    ins = None
                    for i in range(8):
                        ins = e.matmul(pss[pk][:, 0:n], lhsA[:, i, :], uT[:, c0 * 8 + i:(c0 + n) * 8:8], start=(i == 0), stop=(i == 7))
                    return ins
                S.add("pe", mmA, r=[("lhsA", i) for i in range(8)], w=[("ps", pk)])
                S.add("act", lambda e, c0=c0, n=n, pk=pk: e.copy(out=Ea[:, c0:c0 + n], in_=pss[pk][:, 0:n]), r=[("ps", pk)], w=["Ea"])
            src, dst_ = Ea, Eb
            sk, dk = "Ea", "Eb"
            for l in range(NLEV):
                s = 1 << l
                S.add("act", lambda e, src=src, dst_=dst_, s=s: e.copy(out=dst_[:, 0:s], in_=src[:, 0:s]), r=[sk], w=[dk])
                c = s
                while c < NCH:
                    n = min(512, NCH - c)
                    pk = 4 + (evk % 2)
                    evk += 1
                    S.add("pe", lambda e, l=l, c=c, n=n, s=s, pk=pk, src=src: e.matmul(
                        pss[pk][:, 0:n], Rh[:, l, :], src[:, c - s:c - s + n], start=True, stop=True), r=[("Rh", l), sk], w=[("ps", pk)])
                    S.add("dve", lambda e, c=c, n=n, pk=pk, src=src, dst_=dst_: e.tensor_tensor(
                        out=dst_[:, c:c + n], in0=pss[pk][:, 0:n], in1=src[:, c:c + n], op=ALU.add), r=[("ps", pk), sk], w=[dk])
                    c += n
                src, dst_ = dst_, src
                sk, dk = dk, sk
            S.add("dve", lambda e, g=g: e.memset(Sin[:, g, 0:1], 0.0), w=[("Sin", g)])
            S.add("act", lambda e, g=g, src=src: e.copy(out=Sin[:, g, 1:NCH], in_=src[:, 0:NCH - 1]), r=[sk], w=[("Sin", g)])
        for d in range(8):
            bank = 2 + d // 4
            col = (d % 4) * 128
            if d == 0:
                S.add("dve", lambda e, bank=bank, col=col: e.scalar_tensor_tensor(
                    out=Wb[:, 0, :], in0=ident32, scalar=dvec[:, 0:1], in1=pss[bank][:, col:col + 128], op0=ALU.mult, op1=ALU.add),
                    r=[("ps", bank), "dvec", "cst"], w=["Wb"])
            else:
                S.add("act", lambda e, d=d, bank=bank, col=col: e.copy(out=Wb[:, d, :], in_=pss[bank][:, col:col + 128]),
                      r=[("ps", bank)], w=["Wb"])
        kch = 2
        oi = 0
        while kch < NCH:
            n = min(512, NCH - kch)
            ob = 0
            for j in range(8):
                pk = 4 + (evk % 2)
                evk += 1

                def mmo(e, j=j, kch=kch, n=n, pk=pk):
                    ins = None
                    tot = (j + 1) + 8
                    q = 0
                    for i in range(j + 1):
                        ins = e.matmul(pss[pk][:, 0:n], Wb[:, j - i, :], uT[:, kch * 8 + i:(kch + n) * 8:8], start=(q == 0), stop=(q == tot - 1))
                        q += 1
                    for g in range(8):
                        ins = e.matmul(pss[pk][:, 0:n], CLall[:, g, j, :], Sin[:, g, kch:kch + n], start=(q == 0), stop=(q == tot - 1))
                        q += 1
                    return ins
                S.add("pe", mmo, r=["Wb"] + [("Sin", g) for g in range(8)] + [("CL", g, d) for g in range(8) for d in range(1, 9)],
                      w=[("ps", pk)])
                eng = "act" if j % 2 == 0 else "dve"
                if eng == "act":
                    S.add("act", lambda e, j=j, n=n, pk=pk, ob=ob: e.copy(out=yo[ob][:, j:n * 8:8], in_=pss[pk][:, 0:n]),
                          r=[("ps", pk)], w=[("yo", ob)])
                else:
                    S.add("dve", lambda e, j=j, n=n, pk=pk, ob=ob: e.tensor_copy(out=yo[ob][:, j:n * 8:8], in_=pss[pk][:, 0:n]),
                          r=[("ps", pk)], w=[("yo", ob)])
            t0_ = (kch - 2) * 8
            CH = NX // 4
            for cs in range(0, n * 8, min(CH, n * 8)):
                cn = min(CH, n * 8)
                S.add("sp", lambda e, ob=ob, t0_=t0_, cs=cs, cn=cn: e.dma_start(out=L["ydst"](1, 0, 128, t0_ + cs, cn), in_=yo[ob][:, cs:cs + cn]),
                      r=[("yo", ob)], dma="out")
            kch += n
            oi += 1
        S.emit()


def _consts():
    c = np.zeros((128, 128 * 4 + 8 * 128 + 4), np.float32)
    c[:, 0:128] = np.eye(128, dtype=np.float32)
    J = np.zeros((128, 128), np.float32)
    for p in range(64):
        J[p, 64 + p] = 1.0
        J[64 + p, p] = 1.0
    c[:, 128:256] = J
    s = np.arange(128)[:, None]
    t = np.arange(128)[None, :]
    c[:, 256:384] = (s <= t).astype(np.float32)
    for g in range(8):
        c[:, 512 + g * 128 + g * 16: 512 + g * 128 + (g + 1) * 16] = 1.0
    c[0:64, 1536] = 1.0
    c[64:128, 1536] = -1.0
    return c


def prep_p1(inp, b, r, NX=SEQ):
    hs = [2 * r, 2 * r + 1]
    w_in = inp["w_in"][0]
    cols = []
    for h in hs:
        cols.append(w_in[:, Q_OFF + h * 64:Q_OFF + (h + 1) * 64])
    for h in hs:
        cols.append(w_in[:, K_OFF + h * 64:K_OFF + (h + 1) * 64])
    for h in hs:
        cols.append(w_in[:, V_OFF + h * 64:V_OFF + (h + 1) * 64])
    cols.append(w_in[:, U_OFF + r * 128:U_OFF + (r + 1) * 128])
    cols.append(w_in[:, F_OFF + 2 * r:F_OFF + 2 * r + 2])
    w1 = np.ascontiguousarray(np.concatenate(cols, axis=1), dtype=np.float32)
    lnp = np.concatenate([inp["ln_in_g"].reshape(8, 128).T, inp["ln_in_b"].reshape(8, 128).T], axis=1)
    gs = slice(8 * r, 8 * r + 8)
    a_re = inp["ssm_a_re"][0][gs]
    a_im = inp["ssm_a_im"][0][gs]
    lamp = np.concatenate([np.concatenate([a_re.T, a_re.T], axis=0), np.concatenate([a_im.T, a_im.T], axis=0)], axis=1)
    b_re = inp["ssm_b_re"][0][gs]
    b_im = inp["ssm_b_im"][0][gs]
    bstk = np.concatenate([b_re.transpose(1, 0, 2).reshape(64, 128), b_im.transpose(1, 0, 2).reshape(64, 128)], axis=0)
    c_re = inp["ssm_c_re"][0][gs]
    c_im = inp["ssm_c_im"][0][gs]
    cstk = np.concatenate([c_re.transpose(2, 0, 1).reshape(64, 128), c_im.transpose(2, 0, 1).reshape(64, 128)], axis=0)
    f32 = lambda a: np.ascontiguousarray(a, dtype=np.float32)
    return dict(x=f32(inp["x"][b][:NX]), meta=f32(inp["meta"]), lnp=f32(lnp), w1=w1,
                lnrow=f32(np.stack([inp["ln_in_g"], inp["ln_in_b"]])),
                bf=f32(inp["b_f"][0][2 * r:2 * r + 2].reshape(2, 1)), lamp=f32(lamp),
                logdt=f32(inp["ssm_log_dt"][0][gs].reshape(1, 8)), bstk=f32(bstk), bstks=f32(bstk), cstk=f32(cstk),
                dvec=f32(inp["ssm_d"][0][gs].reshape(128, 1)), cst=_consts())


def prep_p2_shared(inp):
    f32 = lambda a: np.ascontiguousarray(a, dtype=np.float32)
    w_in = inp["w_in"][0]
    w_ga = w_in[:, GA_OFF:GB_OFF]
    w_gb = w_in[:, GB_OFF:GB_OFF + 1024]
    wg = np.zeros((1024, 2048), np.float32)
    for j in range(4):
        for dcl in range(2):
            dc = 2 * j + dcl
            o = j * 512 + dcl * 256
            wg[:, o:o + 128] = w_ga[:, dc * 128:(dc + 1) * 128]
            wg[:, o + 128:o + 256] = w_gb[:, dc * 128:(dc + 1) * 128]
    lnp = np.stack([inp["ln_in_g"], inp["ln_in_b"], inp["ln1_g"][0], inp["ln1_b"][0], inp["ln2_g"][0], inp["ln2_b"][0]])
    bgu = np.zeros((128, 2, N_EXPERTS, 8), np.float32)
    bgu[:, 0] = inp["b_gate"][0].reshape(N_EXPERTS, 8, 128).transpose(2, 0, 1)
    bgu[:, 1] = inp["b_up"][0].reshape(N_EXPERTS, 8, 128).transpose(2, 0, 1)
    return dict(lnp=f32(lnp), wg=wg, wo=f32(inp["w_o"][0]), wua=f32(inp["w_up_a"][0]), wub=f32(inp["w_up_b"][0]),
                wglu=f32(inp["w_glu"][0]), bglu=f32(inp["b_glu"][0].reshape(4, 128).T), wr=f32(inp["w_router"][0]),
                br=f32(inp["b_router"][0].reshape(1, 32)), wgate=f32(inp["w_gate"][0]), wup=f32(inp["w_up"][0]),
                wdown=f32(inp["w_down"][0]), bgu=f32(bgu.reshape(128, -1)), bdown=f32(inp["b_down"][0]),
                ident=np.eye(128, dtype=np.float32))


_NC_CACHE = {}


def build_fused(NX=SEQ, NE=N_EXPERTS):
    nc = bass.Bass("TRN2", target_bir_lowering=False)
    with ExitStack() as top:
        S = Sched(nc, top)
        CH = NX // 4
        ybuf = [nc.dram_tensor(f"ybuf_in{k}", [256, CH], BF16) for k in range(4)]
        ygat = [nc.dram_tensor(f"ybuf_out{k}", [4 * 256, CH], BF16) for k in range(4)]
        build_p1(NX, ctx=dict(nc=nc, S=S, ybuf=[t.ap() for t in ybuf]))
        for k in range(4):
            S.add("pool", lambda e, k=k: e.collective_compute("AllGather", ALU.bypass, replica_groups=[[0, 1, 2, 3], [4, 5, 6, 7]],
                                                              ins=[ybuf[k].ap().opt()], outs=[ygat[k].ap().opt()]), w=[("ygat", k)])
            S.add("pool", lambda e: e.memset(S.fz["pool"][:, 1:2], 0.0), r=[("ygat", k)], w=["fzp"])
        S.emit()
        build_p2(NX // 4, NE, ctx=dict(nc=nc, S=S, ygat=[t.ap() for t in ygat]))
    return nc


def make_maps(inp, NX=SEQ, NE=N_EXPERTS):
    shared = prep_p2_shared(inp)
    shared["lnp2"] = shared.pop("lnp")
    for k in ("wgate", "wup", "wdown"):
        shared[k] = shared[k][:NE]
    shared["bgu"] = np.ascontiguousarray(shared["bgu"].reshape(128, 2, N_EXPERTS, 8)[:, :, :NE].reshape(128, -1))
    NTOK = NX // 4
    maps = []
    for c in range(8):
        b, r = c // 4, c % 4
        m = dict(shared)
        m.update(prep_p1(inp, b, r, NX))
        m.pop("bstks")
        m["x2"] = np.ascontiguousarray(inp["x"][b, r * NTOK:(r + 1) * NTOK], dtype=np.float32)
        oh = np.zeros((128, 4), np.float32)
        oh[:, r] = 1.0
        m["onehot"] = oh
        maps.append(m)
    return maps


def kernel(**inputs):
    inp = {k: np.asarray(v) for k, v in inputs.items()}
    B = inp["x"].shape[0]
    if "f" not in _NC_CACHE:
        _NC_CACHE["f"] = build_fused()
    maps = make_maps(inp)
    if False:
        m = None
    res = run_bass_kernel_spmd(_NC_CACHE["f"], maps, core_ids=list(range(8)))
    out = np.zeros((B, SEQ, D_MODEL), np.float32)
    for c in range(8):
        b, r = c // 4, c % 4
        out[b, r * 2048:(r + 1) * 2048] = res.results[c]["out"]
    return out
```

```python
import math
from contextlib import ExitStack

import numpy as np
import concourse.bass as bass
import concourse.mybir as mybir
from concourse.bass_utils import run_bass_kernel_spmd

F32 = mybir.dt.float32
BF16 = mybir.dt.bfloat16
AF = mybir.ActivationFunctionType
ALU = mybir.AluOpType

D_MODEL = 1024
SEQ = 8192
N_META = 16
N_EXPERTS = 32
LN_EPS = 1e-5
DN_ALPHA = 2.0 ** 0.25
SW_LIMIT = 7.0
SW_ALPHA = 1.702
Q_OFF, K_OFF, V_OFF, F_OFF, U_OFF, GA_OFF, GB_OFF = 0, 512, 1024, 1536, 1544, 2056, 3080


class Sched:
    ENGS = ("pe", "act", "dve", "pool", "sp")
    SEM_ROLL = 1 << 30

    def __init__(self, nc, stack):
        self.nc = nc
        self.stack = stack
        self.eng_obj = {"pe": nc.tensor, "act": nc.scalar, "dve": nc.vector,
                        "pool": nc.gpsimd, "sp": nc.sync}
        self.eng_sems = {e: [stack.enter_context(nc.semaphore(f"s_{e}0"))] for e in self.ENGS}
        self.eng_cnt = {e: 0 for e in self.ENGS}
        self.streams = {}
        self.nsem = 0
        self.fz = {e: stack.enter_context(nc.sbuf_tensor(f"fz_{e}", [128, 2], F32)) for e in ("act", "dve", "pool")}
        self.reset()

    def reset(self):
        self.ops = []
        self.lastw = {}
        self.readers = {}

    def stream(self, name, depth):
        if name not in self.streams:
            sems = [self.stack.enter_context(self.nc.semaphore(f"d_{name}{i}")) for i in range(depth)]
            self.streams[name] = dict(sems=sems, cnt=[0] * depth, k=0)
        return name

    def add(self, eng, fn, r=(), w=(), dma=None, fence=False):
        idx = len(self.ops)
        deps = set()
        for k in r:
            if k in self.lastw:
                deps.add(self.lastw[k])
        for k in w:
            if k in self.lastw:
                deps.add(self.lastw[k])
            for x in self.readers.get(k, ()):
                deps.add(x)
        deps.discard(idx)
        for k in r:
            self.readers.setdefault(k, []).append(idx)
        for k in w:
            self.lastw[k] = idx
            self.readers[k] = []
        self.ops.append(dict(eng=eng, fn=fn, deps=deps, dma=dma, used=False, sig=None, fence=fence))
        for d in deps:
            self.ops[d]["used"] = True
        return idx

    def emit(self, final_wait_dma=True):
        nc = self.nc
        ops = self.ops
        for op in ops:
            if op["dma"] is not None and len(self.streams[op["dma"]]["sems"]) == 0:
                self.nsem += 1
                sem = self.stack.enter_context(nc.semaphore(f"d_one{self.nsem}"))
                op["sig"] = (sem, 16, 16)
            elif op["dma"] is not None:
                st = self.streams[op["dma"]]
                k = st["k"] % len(st["sems"])
                st["k"] += 1
                st["cnt"][k] += 16
                op["sig"] = (st["sems"][k], st["cnt"][k], 16)
            elif op["used"]:
                e = op["eng"]
                if self.eng_cnt[e] >= self.SEM_ROLL:
                    self.eng_sems[e].append(self.stack.enter_context(nc.semaphore(f"s_{e}{len(self.eng_sems[e])}")))
                    self.eng_cnt[e] = 0
                self.eng_cnt[e] += 1
                op["sig"] = (self.eng_sems[e][-1], self.eng_cnt[e], 1)
        per_eng = {e: [] for e in self.ENGS}
        for i, op in enumerate(ops):
            waits = []
            for d in sorted(op["deps"]):
                dop = ops[d]
                if dop["dma"] is None and dop["eng"] == "pe" and op["eng"] == "pe":
                    continue
                waits.append(dop["sig"])
            per_eng[op["eng"]].append((waits, op))
        pending = [op["sig"] for op in ops if op["dma"] is not None]
        with nc.Block() as blk:
            def make(ename):
                lst = per_eng[ename]

                def body(eng):
                    known = {}
                    for waits, op in lst:
                        for (sem, val, _inc) in waits:
                            key = id(sem)
                            if known.get(key, (None, 0))[1] >= val:
                                continue
                            eng.wait_ge(sem, val)
                            known[key] = (sem, val)
                        ins = op["fn"](eng)
                        if op["sig"] is not None:
                            if op["fence"] and ename in self.fz:
                                fz = self.fz[ename]
                                if ename == "act":
                                    ins = eng.memzero(fz[:, 0:1])
                                else:
                                    ins = eng.memset(fz[:, 0:1], 0.0)
                            ins.then_inc(op["sig"][0], op["sig"][2])
                    if ename == "sp" and final_wait_dma:
                        best = {}
                        for (sem, val, _inc) in pending:
                            if best.get(id(sem), (None, 0))[1] < val:
                                best[id(sem)] = (sem, val)
                        for sem, val in best.values():
                            if known.get(id(sem), (None, 0))[1] < val:
                                eng.wait_ge(sem, val)
                return body
            blk.tensor(make("pe"))
            blk.scalar(make("act"))
            blk.vector(make("dve"))
            blk.gpsimd(make("pool"))
            blk.sync(make("sp"))
        self.reset()


def _bc(ap, shape):
    return ap.to_broadcast(shape)


def _ln_tile(S, tile_ap, st6, mv, rstd, gB, bB, key, tag):
    S.add("dve", lambda e: e.bn_stats(out=st6[:, 0, :], in_=tile_ap[:, 0:512]), r=[key], w=[tag + "st0"])
    S.add("dve", lambda e: e.bn_stats(out=st6[:, 1, :], in_=tile_ap[:, 512:1024]), r=[key], w=[tag + "st1"])
    S.add("dve", lambda e: e.bn_aggr(out=mv[:], in_=st6[:]), r=[tag + "st0", tag + "st1"], w=[tag + "mv"], fence=True)
    S.add("dve", lambda e: e.tensor_scalar(out=rstd[:], in0=mv[:, 1:2], scalar1=LN_EPS, scalar2=None,
                                           op0=ALU.add), r=[tag + "mv"], w=[tag + "rs"], fence=True)
    S.add("act", lambda e: e.sqrt(out=rstd[:], in_=rstd[:]), r=[tag + "rs"], w=[tag + "rs"], fence=True)
    S.add("dve", lambda e: e.reciprocal(out=rstd[:], in_=rstd[:]), r=[tag + "rs"], w=[tag + "rs"], fence=True)
    S.add("dve", lambda e: e.tensor_scalar(out=tile_ap, in0=tile_ap, scalar1=mv[:, 0:1], scalar2=rstd[:, 0:1],
                                           op0=ALU.subtract, op1=ALU.mult), r=[key, tag + "mv", tag + "rs"], w=[key])
    S.add("dve", lambda e: e.tensor_tensor(out=tile_ap, in0=tile_ap, in1=gB, op=ALU.mult), r=[key, "lnB"], w=[key])
    S.add("dve", lambda e: e.tensor_tensor(out=tile_ap, in0=tile_ap, in1=bB, op=ALU.add), r=[key, "lnB"], w=[key])


def build_p2(NTOK=2048, NE=32, ctx=None):
    NT = NTOK // 128
    NG = NTOK // 512
    fused = ctx is not None
    nc = ctx["nc"] if fused else bass.Bass("TRN2", target_bir_lowering=False)

    def din(name, shape, dtype=F32):
        return nc.dram_tensor(name, shape, dtype, kind="ExternalInput").ap()
    x = din("x2", [NTOK, 1024])
    if fused:
        ygat = ctx["ygat"]
        onehot = din("onehot", [128, 4])
    else:
        yaT = din("yaT", [512, NTOK])
        ysT = din("ysT", [512, NTOK])
    lnp = din("lnp2", [6, 1024])
    wg = din("wg", [1024, 2048])
    wo = din("wo", [1024, 1024])
    wua = din("wua", [512, 1024])
    wub = din("wub", [512, 1024])
    wglu = din("wglu", [512, 512])
    bglu = din("bglu", [128, 4])
    wr = din("wr", [1024, 32])
    br = din("br", [1, 32])
    wgate = din("wgate", [NE, 1024, 1024])
    wup = din("wup", [NE, 1024, 1024])
    wdown = din("wdown", [NE, 1024, 1024])
    bgu = din("bgu", [128, 2 * NE * 8])
    bdown = din("bdown", [N_EXPERTS, 1024])
    ident = din("ident", [128, 128])
    out = nc.dram_tensor("out", [NTOK, 1024], F32, kind="ExternalOutput").ap()

    with ExitStack() as stack:
        S = ctx["S"] if fused else Sched(nc, stack)

        def sb(name, shape, dtype):
            return stack.enter_context(nc.sbuf_tensor("b_" + name, shape, dtype))

        def psum(name, shape, dtype):
            return stack.enter_context(nc.psum_tensor("b_" + name, shape, dtype))
        hres = sb("hres", [128, NT, 1024], F32)
        identb = sb("identb", [128, 128], BF16)
        ident32 = sb("ident32", [128, 128], F32)
        lnB = sb("lnB", [128, 4, 1024], F32)
        comb = sb("comb", [128, NT, 32], F32)
        st6 = sb("st6", [128, 2, 6], F32)
        mv = sb("mv", [128, 2], F32)
        rstd = sb("rstd", [128, 1], F32)
        pss = [psum(f"ps{i}", [128, 512], F32) for i in range(6)]
        pT = [psum(f"pT{i}", [128, 1024], BF16) for i in range(2)]
        S.stream("x", 2)
        S.stream("misc", 0)
        S.stream("gw", 3)
        S.stream("yin", 0)
        S.stream("wr", 8)
        S.stream("out", 2)

        with ExitStack() as st2:
            def sb2(name, shape, dtype):
                return st2.enter_context(nc.sbuf_tensor("b_" + name, shape, dtype))
            xb = [sb2(f"xb{i}", [128, 1024], BF16) for i in range(2)]
            hT = sb2("hT", [128, 8, 512], BF16)
            ya = sb2("ya", [128, 4, 512], BF16)
            ys32 = sb2("ys32", [128, 4, 512], F32)
            gtmp = sb2("gtmp", [128, 512], F32)
            sgm = sb2("sgm", [128, 512], F32)
            yb = sb2("yb", [128, 4, 512], BF16)
            yb2 = sb2("yb2", [128, 4, 512], BF16)
            merged = sb2("merged", [128, 8, 512], BF16)
            gw = [sb2(f"gw{i}", [128, 8, 512], BF16) for i in range(3)]
            wuat = sb2("wuat", [128, 4, 1024], BF16)
            wubt = sb2("wubt", [128, 4, 1024], BF16)
            wglut = sb2("wglut", [128, 4, 512], BF16)
            bglut = sb2("bglut", [128, 4], F32)
            if fused:
                cand = [sb2(f"cand{i}", [128, 4, 512], BF16) for i in range(2)]
                oht = sb2("oht", [128, 4], F32)
                S.stream("cand", 2)
                S.add("sp", lambda e: e.dma_start(out=oht[:], in_=onehot[:, :]), w=["oht"], dma="misc")
            gat = [sb2(f"gat{i}", [128, 512], F32) for i in range(2)]
            gbt = [sb2(f"gbt{i}", [128, 512], F32) for i in range(2)]

            S.add("sp", lambda e: e.dma_start(out=ident32[:], in_=ident[:, :]), w=["ident32"], dma="misc")
            S.add("dve", lambda e: e.tensor_copy(out=identb[:], in_=ident32[:]), r=["ident32"], w=["identb"])
            for i in range(4):
                S.add("sp", lambda e, i=i: e.dma_start(out=lnB[:, i, :], in_=lnp[i:i + 1, :].partition_broadcast(128)),
                      w=["lnB"], dma="misc")
            S.add("sp", lambda e: e.dma_start(out=bglut[:], in_=bglu[:, :]), w=["bglut"], dma="misc")
            S.add("pool", lambda e: e.dma_start(out=wuat[:], in_=wua.rearrange("(c p) n -> p c n", p=128)), w=["wuat"], dma="misc")
            S.add("pool", lambda e: e.dma_start(out=wubt[:], in_=wub.rearrange("(c p) n -> p c n", p=128)), w=["wubt"], dma="misc")
            S.add("pool", lambda e: e.dma_start(out=wglut[:], in_=wglu.rearrange("(c p) n -> p c n", p=128)), w=["wglut"], dma="misc")
            gwk = 0
            for tg in range(NG):
                c0 = tg * 512
                for ti in range(4):
                    tt = tg * 4 + ti
                    hk = ("hres", tt)
                    S.add("sp", lambda e, tt=tt: e.dma_start(out=hres[:, tt, :], in_=x[tt * 128:(tt + 1) * 128, :]),
                          w=[hk], dma="x")
                    _ln_tile(S, hres[:, tt, :], st6, mv, rstd, lnB[:, 0, :], lnB[:, 1, :], hk, "ln")
                    b = tt % 2
                    S.add("act", lambda e, tt=tt, b=b: e.copy(out=xb[b][:], in_=hres[:, tt, :]), r=[hk], w=[("xb", b)])

                    def tr(e, b=b):
                        ins = None
                        for kc in range(8):
                            ins = e.transpose(out=pT[b][:, kc * 128:(kc + 1) * 128], in_=xb[b][:, kc * 128:(kc + 1) * 128],
                                              identity=identb[:])
                        return ins
                    S.add("pe", tr, r=[("xb", b), "identb"], w=[("pT", b)])
                    S.add("act", lambda e, b=b, ti=ti: e.copy(out=hT[:, :, ti * 128:(ti + 1) * 128],
                                                              in_=pT[b][:, :].rearrange("p (k t) -> p k t", k=8)),
                          r=[("pT", b)], w=["hT"])
                if fused:
                    for which, dst, dkey in ((0, ya, "ya"), (1, ys32, "ys32")):
                        for q in range(4):
                            cb = (2 * tg + which + q) % 2
                            src = ygat[q].rearrange("(r w p) t -> p w r t", r=4, w=2, p=128)[:, which, :, c0:c0 + 512]
                            S.add("sp", lambda e, cb=cb, src=src: e.dma_start(out=cand[cb][:], in_=src), w=[("cand", cb)], dma="cand")
                            if q == 0:
                                S.add("dve", lambda e, cb=cb, dst=dst: e.tensor_scalar(out=dst[:], in0=cand[cb][:], scalar1=oht[:, 0:1], scalar2=None,
                                                                                       op0=ALU.mult), r=[("cand", cb), "oht"], w=[dkey])
                            else:
                                S.add("dve", lambda e, cb=cb, dst=dst, q=q: e.scalar_tensor_tensor(
                                    out=dst[:], in0=cand[cb][:], scalar=oht[:, q:q + 1], in1=dst[:], op0=ALU.mult, op1=ALU.add),
                                    r=[("cand", cb), "oht", dkey], w=[dkey])
                else:
                    S.add("pool", lambda e, c0=c0: e.dma_start(out=ya[:], in_=yaT.rearrange("(c p) t -> p c t", p=128)[:, :, c0:c0 + 512]),
                          w=["ya"], dma="yin")
                    S.add("sp", lambda e, c0=c0: e.dma_start(out=ys32[:], in_=ysT.rearrange("(c p) t -> p c t", p=128)[:, :, c0:c0 + 512]),
                          w=["ys32"], dma="yin")
                for c in range(4):
                    S.add("dve", lambda e, c=c: e.tensor_tensor(out=gtmp[:], in0=ys32[:, c, :], in1=ys32[:, c, :], op=ALU.mult), r=["ys32"], w=["gtmp"])
                    S.add("dve", lambda e: e.tensor_scalar(out=gtmp[:], in0=gtmp[:], scalar1=0.044715, scalar2=1.0,
                                                           op0=ALU.mult, op1=ALU.add), r=["gtmp"], w=["gtmp"])
                    S.add("dve", lambda e, c=c: e.tensor_tensor(out=gtmp[:], in0=gtmp[:], in1=ys32[:, c, :], op=ALU.mult), r=["gtmp", "ys32"], w=["gtmp"])
                    S.add("act", lambda e: e.activation(out=sgm[:], in_=gtmp[:], func=AF.Sigmoid, scale=1.5957691216), r=["gtmp"], w=["sgm"])
                    S.add("dve", lambda e, c=c: e.tensor_tensor(out=yb[:, c, :], in0=ys32[:, c, :], in1=sgm[:], op=ALU.mult), r=["ys32", "sgm"], w=["yb"])
                for cc in range(4):
                    pk = cc % 2

                    def mm(e, cc=cc, pk=pk):
                        ins = None
                        for c in range(4):
                            ins = e.matmul(pss[pk][:, :], wglut[:, c, cc * 128:(cc + 1) * 128], yb[:, c, :],
                                           start=(c == 0), stop=(c == 3))
                        return ins
                    S.add("pe", mm, r=["wglut", "yb"], w=[("ps", pk)])
                    S.add("act", lambda e, cc=cc, pk=pk: e.activation(out=sgm[:], in_=pss[pk][:, :], func=AF.Sigmoid,
                                                                      bias=bglut[:, cc:cc + 1], scale=1.0),
                          r=[("ps", pk), "bglut"], w=["sgm"])
                    S.add("dve", lambda e, cc=cc: e.tensor_tensor(out=yb2[:, cc, :], in0=yb[:, cc, :], in1=sgm[:], op=ALU.mult),
                          r=["yb", "sgm"], w=["yb2"])
                for j in range(4):
                    slot = gwk % 3
                    gwk += 1
                    S.add("pool", lambda e, j=j, slot=slot: e.dma_start(
                        out=gw[slot][:], in_=wg.rearrange("(k p) n -> p k n", p=128)[:, :, j * 512:(j + 1) * 512]),
                        w=[("gw", slot)], dma="gw")
                    for dcl in range(2):
                        dc = 2 * j + dcl
                        tb = dc % 2

                        def mmg(e, slot=slot, dcl=dcl, off=0, pk=0):
                            ins = None
                            for kc in range(8):
                                ins = e.matmul(pss[pk][:, :], gw[slot][:, kc, dcl * 256 + off:dcl * 256 + off + 128], hT[:, kc, :],
                                               start=(kc == 0), stop=(kc == 7))
                            return ins
                        S.add("pe", lambda e, f=mmg: f(e, off=0, pk=0), r=[("gw", slot), "hT"], w=[("ps", 0)])
                        S.add("pe", lambda e, f=mmg: f(e, off=128, pk=1), r=[("gw", slot), "hT"], w=[("ps", 1)])

                        def mmu(e, dc=dc, wt=None, src=None, pk=2):
                            ins = None
                            for c in range(4):
                                ins = e.matmul(pss[pk][:, :], wt[:, c, dc * 128:(dc + 1) * 128], src[:, c, :],
                                               start=(c == 0), stop=(c == 3))
                            return ins
                        S.add("pe", lambda e, f=mmu: f(e, wt=wuat, src=ya, pk=2), r=["wuat", "ya"], w=[("ps", 2)])
                        S.add("pe", lambda e, f=mmu: f(e, wt=wubt, src=yb2, pk=3), r=["wubt", "yb2"], w=[("ps", 3)])
                        S.add("act", lambda e, tb=tb: e.activation(out=gat[tb][:], in_=pss[0][:, :], func=AF.Sigmoid),
                              r=[("ps", 0)], w=[("gat", tb)])
                        S.add("act", lambda e, tb=tb: e.activation(out=gbt[tb][:], in_=pss[1][:, :], func=AF.Sigmoid),
                              r=[("ps", 1)], w=[("gbt", tb)])
                        S.add("dve", lambda e, tb=tb: e.tensor_tensor(out=gat[tb][:], in0=gat[tb][:], in1=pss[2][:, :], op=ALU.mult),
                              r=[("gat", tb), ("ps", 2)], w=[("gat", tb)])
                        S.add("dve", lambda e, tb=tb: e.tensor_tensor(out=gbt[tb][:], in0=gbt[tb][:], in1=pss[3][:, :], op=ALU.mult),
                              r=[("gbt", tb), ("ps", 3)], w=[("gbt", tb)])
                        S.add("dve", lambda e, tb=tb, dc=dc: e.tensor_tensor(out=merged[:, dc, :], in0=gat[tb][:], in1=gbt[tb][:], op=ALU.add),
                              r=[("gat", tb), ("gbt", tb)], w=["merged"])
                for half in range(2):
                    slot = gwk % 3
                    gwk += 1
                    S.add("pool", lambda e, half=half, slot=slot: e.dma_start(
                        out=gw[slot][:], in_=wo.rearrange("(k p) n -> p k n", p=128)[:, :, half * 512:(half + 1) * 512]),
                        w=[("gw", slot)], dma="gw")
                    for ti in range(4):
                        tt = tg * 4 + ti
                        pk = 4 + (ti % 2)

                        def mmo(e, slot=slot, ti=ti, pk=pk):
                            ins = None
                            for kc in range(8):
                                ins = e.matmul(pss[pk][:, :], merged[:, kc, ti * 128:(ti + 1) * 128], gw[slot][:, kc, :],
                                               start=(kc == 0), stop=(kc == 7))
                            return ins
                        S.add("pe", mmo, r=["merged", ("gw", slot)], w=[("ps", pk)])
                        S.add("dve", lambda e, tt=tt, half=half, pk=pk: e.scalar_tensor_tensor(
                            out=hres[:, tt, half * 512:(half + 1) * 512], in0=hres[:, tt, half * 512:(half + 1) * 512],
                            scalar=DN_ALPHA, in1=pss[pk][:, :], op0=ALU.mult, op1=ALU.add),
                            r=[("hres", tt), ("ps", pk)], w=[("hres", tt)])
                for ti in range(4):
                    tt = tg * 4 + ti
                    _ln_tile(S, hres[:, tt, :], st6, mv, rstd, lnB[:, 2, :], lnB[:, 3, :], ("hres", tt), "ln")
            S.emit()

        x1T = sb("x1T", [128, 8, NTOK], BF16)
        with ExitStack() as st2:
            def sb2(name, shape, dtype):
                return st2.enter_context(nc.sbuf_tensor("b_" + name, shape, dtype))
            xb = [sb2(f"xc{i}", [128, 1024], BF16) for i in range(2)]
            wrt = sb2("wrt", [128, 8, 32], BF16)
            brB = sb2("brB", [128, 32], F32)
            bdn = sb2("bdn", [N_EXPERTS, 1024], BF16)
            lg = sb2("lg", [128, 32], F32)
            ex = sb2("ex", [128, 32], F32)
            msk = sb2("msk", [128, 32], F32)
            m8 = sb2("m8", [128, 8], F32)
            negm = sb2("negm", [128, 1], F32)
            ssum = sb2("ssum", [128, 1], F32)
            combTb = sb2("combTb", [32, 128], BF16)
            S.add("pool", lambda e: e.dma_start(out=wrt[:], in_=wr.rearrange("(k p) n -> p k n", p=128)), w=["wrt"], dma="misc")
            S.add("pool", lambda e: e.dma_start(out=bdn[:], in_=bdown[:, :]), w=["bdn"], dma="misc")
            S.add("sp", lambda e: e.dma_start(out=brB[:], in_=br[0:1, :].partition_broadcast(128)), w=["brB"], dma="misc")
            for tt in range(NT):
                b = tt % 2
                hk = ("hres", tt)
                S.add("act", lambda e, tt=tt, b=b: e.copy(out=xb[b][:], in_=hres[:, tt, :]), r=[hk], w=[("xb", b)])

                def tr(e, b=b):
                    ins = None
                    for kc in range(8):
                        ins = e.transpose(out=pT[b][:, kc * 128:(kc + 1) * 128], in_=xb[b][:, kc * 128:(kc + 1) * 128],
                                          identity=identb[:])
                    return ins
                S.add("pe", tr, r=[("xb", b)], w=[("pT", b)])
                S.add("act", lambda e, b=b, tt=tt: e.copy(out=x1T[:, :, tt * 128:(tt + 1) * 128],
                                                          in_=pT[b][:, :].rearrange("p (k t) -> p k t", k=8)),
                      r=[("pT", b)], w=[("x1T", tt)])

                def mmr(e, tt=tt):
                    ins = None
                    for kc in range(8):
                        ins = e.matmul(pss[0][:, 0:32], x1T[:, kc, tt * 128:(tt + 1) * 128], wrt[:, kc, :],
                                       start=(kc == 0), stop=(kc == 7))
                    return ins
                S.add("pe", mmr, r=[("x1T", tt), "wrt"], w=[("ps", 0)])
                S.add("dve", lambda e: e.tensor_tensor(out=lg[:], in0=pss[0][:, 0:32], in1=brB[:], op=ALU.add),
                      r=[("ps", 0), "brB"], w=["lg"])
                S.add("dve", lambda e: e.max(out=m8[:], in_=lg[:]), r=["lg"], w=["m8"])
                S.add("dve", lambda e: e.tensor_scalar(out=msk[:], in0=lg[:], scalar1=m8[:, 3:4], scalar2=None, op0=ALU.is_ge),
                      r=["lg", "m8"], w=["msk"])
                S.add("dve", lambda e: e.tensor_scalar(out=negm[:], in0=m8[:, 0:1], scalar1=-1.0, scalar2=None, op0=ALU.mult),
                      r=["m8"], w=["negm"])
                S.add("act", lambda e: e.activation(out=ex[:], in_=lg[:], func=AF.Exp, bias=negm[:, 0:1], scale=1.0),
                      r=["lg", "negm"], w=["ex"])
                S.add("dve", lambda e: e.tensor_tensor(out=ex[:], in0=ex[:], in1=msk[:], op=ALU.mult), r=["ex", "msk"], w=["ex"])
                S.add("dve", lambda e: e.reduce_sum(out=ssum[:], in_=ex[:], axis=mybir.AxisListType.X), r=["ex"], w=["ssum"])
                S.add("dve", lambda e: e.reciprocal(out=ssum[:], in_=ssum[:]), r=["ssum"], w=["ssum"])
                S.add("dve", lambda e, tt=tt: e.tensor_scalar(out=comb[:, tt, :], in0=ex[:], scalar1=ssum[:, 0:1], scalar2=None, op0=ALU.mult),
                      r=["ex", "ssum"], w=[("comb", tt)])
                S.add("pe", lambda e, tt=tt: e.transpose(out=pss[1][0:32, 0:128], in_=comb[:, tt, :], identity=ident32[:]),
                      r=[("comb", tt)], w=[("ps", 1)])
                S.add("act", lambda e: e.copy(out=combTb[:], in_=pss[1][0:32, 0:128]), r=[("ps", 1)], w=["combTb"])
                for half in range(2):
                    pk = 2 + half
                    S.add("pe", lambda e, half=half, pk=pk: e.matmul(pss[pk][:, :], combTb[:, :], bdn[:, half * 512:(half + 1) * 512],
                                                                     start=True, stop=True),
                          r=["combTb", "bdn"], w=[("ps", pk)])
                    S.add("dve", lambda e, tt=tt, half=half, pk=pk: e.scalar_tensor_tensor(
                        out=hres[:, tt, half * 512:(half + 1) * 512], in0=hres[:, tt, half * 512:(half + 1) * 512],
                        scalar=DN_ALPHA, in1=pss[pk][:, :], op0=ALU.mult, op1=ALU.add),
                        r=[hk, ("ps", pk)], w=[hk])
            S.emit()

        with ExitStack() as st2:
            def sb2(name, shape, dtype):
                return st2.enter_context(nc.sbuf_tensor("b_" + name, shape, dtype))
            R = 8
            ring = [sb2(f"ring{i}", [128, 8, 256], BF16) for i in range(R)]
            actT = sb2("actT", [128, 8, NTOK], BF16)
            bgut = sb2("bgut", [128, 2 * NE * 8], F32)
            gs = [sb2(f"gs{i}", [128, 512], F32) for i in range(2)]
            sg = [sb2(f"sg{i}", [128, 512], F32) for i in range(2)]
            uu = [sb2(f"uu{i}", [128, 512], F32) for i in range(2)]
            S.add("sp", lambda e: e.dma_start(out=bgut[:], in_=bgu[:, :]), w=["bgut"], dma="misc")
            rk = 0

            def load(wsrc, e_, q):
                nonlocal rk
                slot = rk % R
                rk += 1
                S.add("pool", lambda e, slot=slot: e.dma_start(
                    out=ring[slot][:], in_=wsrc[e_].rearrange("(k p) n -> p k n", p=128)[:, :, q * 256:(q + 1) * 256]),
                    w=[("ring", slot)], dma="wr")
                return slot
            tcount = 0
            for ex_ in range(NE):
                for q in range(4):
                    sg_ = load(wgate, ex_, q)
                    su_ = load(wup, ex_, q)
                    for fcl in range(2):
                        fc = 2 * q + fcl
                        bgcol = (0 * NE + ex_) * 8 + fc
                        bucol = (1 * NE + ex_) * 8 + fc
                        for tg in range(NG):
                            tb = tcount % 2
                            tcount += 1
                            pg, pu = tb, 2 + tb

                            def mmgu(e, slot=None, pk=None, fcl=fcl, tg=tg):
                                ins = None
                                for kc in range(8):
                                    ins = e.matmul(pss[pk][:, :], ring[slot][:, kc, fcl * 128:(fcl + 1) * 128],
                                                   x1T[:, kc, tg * 512:(tg + 1) * 512], start=(kc == 0), stop=(kc == 7))
                                return ins
                            S.add("pe", lambda e, f=mmgu, s=sg_, pk=pg: f(e, slot=s, pk=pk), r=[("ring", sg_)], w=[("ps", pg)])
                            S.add("pe", lambda e, f=mmgu, s=su_, pk=pu: f(e, slot=s, pk=pk), r=[("ring", su_)], w=[("ps", pu)])
                            S.add("dve", lambda e, tb=tb, pg=pg, c=bgcol: e.tensor_scalar(
                                out=gs[tb][:], in0=pss[pg][:, :], scalar1=bgut[:, c:c + 1], scalar2=SW_LIMIT, op0=ALU.add, op1=ALU.min),
                                r=[("ps", pg), "bgut"], w=[("gs", tb)])
                            S.add("act", lambda e, tb=tb: e.activation(out=sg[tb][:], in_=gs[tb][:], func=AF.Sigmoid, scale=SW_ALPHA),
                                  r=[("gs", tb)], w=[("sg", tb)])
                            S.add("dve", lambda e, tb=tb, pu=pu, c=bucol: e.tensor_scalar(
                                out=uu[tb][:], in0=pss[pu][:, :], scalar1=bgut[:, c:c + 1], scalar2=SW_LIMIT, op0=ALU.add, op1=ALU.min),
                                r=[("ps", pu), "bgut"], w=[("uu", tb)])
                            S.add("dve", lambda e, tb=tb: e.tensor_scalar(
                                out=uu[tb][:], in0=uu[tb][:], scalar1=-SW_LIMIT, scalar2=1.0, op0=ALU.max, op1=ALU.add),
                                r=[("uu", tb)], w=[("uu", tb)])
                            S.add("dve", lambda e, tb=tb: e.tensor_tensor(out=gs[tb][:], in0=gs[tb][:], in1=sg[tb][:], op=ALU.mult),
                                  r=[("gs", tb), ("sg", tb)], w=[("gs", tb)])
                            S.add("dve", lambda e, tb=tb, fc=fc, tg=tg: e.tensor_tensor(
                                out=actT[:, fc, tg * 512:(tg + 1) * 512], in0=gs[tb][:], in1=uu[tb][:], op=ALU.mult),
                                r=[("gs", tb), ("uu", tb)], w=[("actT", tg)])
                for q in range(4):
                    sd_ = load(wdown, ex_, q)
                    for tt in range(NT):
                        pk = 4 + (tt % 2)

                        def mmd(e, slot=sd_, tt=tt, pk=pk):
                            ins = None
                            for fc in range(8):
                                ins = e.matmul(pss[pk][:, 0:256], actT[:, fc, tt * 128:(tt + 1) * 128], ring[slot][:, fc, :],
                                               start=(fc == 0), stop=(fc == 7))
                            return ins
                        S.add("pe", mmd, r=[("ring", sd_), ("actT", tt // 4)], w=[("ps", pk)])
                        S.add("dve", lambda e, tt=tt, q=q, pk=pk, ex_=ex_: e.scalar_tensor_tensor(
                            out=hres[:, tt, q * 256:(q + 1) * 256], in0=pss[pk][:, 0:256], scalar=comb[:, tt, ex_:ex_ + 1],
                            in1=hres[:, tt, q * 256:(q + 1) * 256], op0=ALU.mult, op1=ALU.add),
                            r=[("ps", pk), ("hres", tt)], w=[("hres", tt)])
            S.emit()

        for i in range(2):
            S.add("sp", lambda e, i=i: e.dma_start(out=lnB[:, i, :], in_=lnp[4 + i:5 + i, :].partition_broadcast(128)),
                  w=["lnB"], dma="misc")
        for tt in range(NT):
            _ln_tile(S, hres[:, tt, :], st6, mv, rstd, lnB[:, 0, :], lnB[:, 1, :], ("hres", tt), "ln")
            S.add("sp", lambda e, tt=tt: e.dma_start(out=out[tt * 128:(tt + 1) * 128, :], in_=hres[:, tt, :]),
                  r=[("hres", tt)], dma="out")
        S.emit()
    return nc


def build_p1(NX=8192, do_attn=True, do_ssm=True, dbg=(), ctx=None):
    NXT = NX // 128
    NQG = NX // 512
    NTOKS = N_META + NX
    NCH = NTOKS // 8
    NKT = NXT + 1
    fused = ctx is not None
    nc = ctx["nc"] if fused else bass.Bass("TRN2", target_bir_lowering=False)
    ODT = BF16 if fused else F32
    dbgs = set(dbg) if isinstance(dbg, (tuple, list, set)) else {dbg}

    def din(name, shape, dtype=F32):
        return nc.dram_tensor(name, shape, dtype, kind="ExternalInput").ap()
    x = din("x", [NX, 1024])
    meta = din("meta", [N_META, 1024])
    lnp = din("lnp", [128, 16])
    lnrow = din("lnrow", [2, 1024])
    w1 = din("w1", [1024, 514])
    bfn = din("bf", [2, 1])
    lamp = din("lamp", [128, 16])
    logdt = din("logdt", [1, 8])
    bstk_d = din("bstk", [128, 128])
    cstk_d = din("cstk", [128, 128])
    dvec_d = din("dvec", [128, 1])
    cst = din("cst", [128, 128 * 4 + 8 * 128 + 4])
    if fused:
        yatt = yssm = None
    else:
        yatt = nc.dram_tensor("yatt", [128, NX], F32, kind="ExternalOutput").ap()
        yssm = nc.dram_tensor("yssm", [128, NX], F32, kind="ExternalOutput").ap()
    NC_ = 128 * 4 + 8 * 128 + 4
    CH = NX // 4

    def ydst(which, r0, r1, t0, n):
        if fused:
            k = t0 // CH
            assert (t0 + n - 1) // CH == k
            return ctx["ybuf"][k][which * 128 + r0:which * 128 + r1, t0 - k * CH:t0 - k * CH + n]
        return (yatt if which == 0 else yssm)[r0:r1, t0:t0 + n]

    with ExitStack() as stack:
        S = ctx["S"] if fused else Sched(nc, stack)

        def sb(name, shape, dtype):
            return stack.enter_context(nc.sbuf_tensor(name, shape, dtype))

        def psum(name, shape, dtype):
            return stack.enter_context(nc.psum_tensor(name, shape, dtype))
        QT = [sb(f"QT{h}", [65, NX], BF16) for h in range(2)]
        KT = [sb(f"KT{h}", [65, NTOKS], BF16) for h in range(2)]
        VV = [sb(f"VV{h}", [128, NKT, 128], BF16) for h in range(2)]
        uT = sb("uT", [128, NTOKS], BF16)
        NFcol = sb("NFcol", [128, NKT, 2], F32)
        cst32 = sb("cst32", [128, NC_], F32)
        identb = sb("identb", [128, 128], BF16)
        trib = sb("trib", [128, 128], BF16)
        ident32 = cst32[:, 0:128]
        Jm = cst32[:, 128:256]
        tri32 = cst32[:, 256:384]
        bmask = cst32[:, 512:512 + 1024]
        sgnR = cst32[:, 1536:1537]
        pss = [psum(f"ps{i}", [128, 512], F32) for i in range(6)]
        pT = [psum(f"pT{i}", [128, 1024], BF16) for i in range(2)]
        S.stream("x", 2)
        S.stream("misc", 0)
        S.stream("row", 4)
        S.stream("out", 2)
        S.stream("rl", 2)

        with ExitStack() as st2:
            def sb2(name, shape, dtype):
                return st2.enter_context(nc.sbuf_tensor(name, shape, dtype))
            w1t = sb2("w1t", [128, 8, 576], BF16)
            lnpt = sb2("lnpt", [128, 16], F32)
            lnB = sb2("lnB1", [128, 2, 1024], F32)
            xt = [sb2(f"xt{i}", [128, 1024], F32) for i in range(2)]
            xn = [sb2(f"xn{i}", [128, 1024], BF16) for i in range(2)]
            hT = sb2("hT", [128, 8, 512], BF16)
            st6 = sb2("st6", [128, 2, 6], F32)
            mv = sb2("mv", [128, 2], F32)
            rstd = sb2("rstd", [128, 1], F32)
            negbf = sb2("negbf", [2, 1], F32)
            e1 = sb2("e1", [2, 512], F32)
            nfg = sb2("nfg", [2, 512], F32)
            carry = sb2("carry", [2, 1], F32)
            ones2 = sb2("ones2", [2, 512], F32)
            fb = sb2("fb", [2, 512], BF16)

            S.add("sp", lambda e: e.dma_start(out=cst32[:], in_=cst[:, :]), w=["cst"], dma="misc")
            S.add("sp", lambda e: e.dma_start(out=lnpt[:], in_=lnp[:, :]), w=["lnpt"], dma="misc")
            for i in range(2):
                S.add("sp", lambda e, i=i: e.dma_start(out=lnB[:, i, :], in_=lnrow[i:i + 1, :].partition_broadcast(128)), w=["lnB"], dma="misc")
            S.add("sp", lambda e: e.dma_start(out=negbf[:], in_=bfn[:, :]), w=["negbf"], dma="misc")
            S.add("dve", lambda e: e.memset(w1t[:, :, 512:576], 0.0), w=["w1t"])
            S.add("pool", lambda e: e.dma_start(out=w1t[:, :, 0:514], in_=w1.rearrange("(k p) n -> p k n", p=128)), w=["w1t"], dma="misc")
            S.add("dve", lambda e: e.tensor_copy(out=identb[:], in_=ident32), r=["cst"], w=["identb"])
            S.add("dve", lambda e: e.tensor_copy(out=trib[:], in_=tri32), r=["cst"], w=["trib"])
            S.add("dve", lambda e: e.tensor_scalar(out=negbf[:], in0=negbf[:], scalar1=-1.0, scalar2=None, op0=ALU.mult),
                  r=["negbf"], w=["negbf"])
            S.add("dve", lambda e: e.memset(ones2[:], 1.0), w=["ones2"])
            S.add("dve", lambda e: e.memset(carry[:], 0.0), w=["carry"])
            for h in range(2):
                S.add("dve", lambda e, h=h: e.memset(KT[h][64:65, :], 1.0), w=[("KTa", h)])
                S.add("pool", lambda e, h=h: e.memset(VV[h][:, :, 64:128], 1.0), w=[("VVo", h)])

            for g in range(NQG + 1):
                n = N_META if g == 0 else 512
                ntile = 1 if g == 0 else 4
                kc0 = 0 if g == 0 else N_META + (g - 1) * 512
                qc0 = (g - 1) * 512
                for ti in range(ntile):
                    np_ = N_META if g == 0 else 128
                    b = (g * 4 + ti) % 2
                    if g == 0:
                        S.add("sp", lambda e, b=b: e.dma_start(out=xt[b][0:N_META, :], in_=meta[:, :]), w=[("xt", b)], dma="x")
                    else:
                        r0 = (g - 1) * 512 + ti * 128
                        S.add("sp", lambda e, b=b, r0=r0: e.dma_start(out=xt[b][:, :], in_=x[r0:r0 + 128, :]), w=[("xt", b)], dma="x")
                    xa = xt[b][0:np_, :]
                    S.add("dve", lambda e, xa=xa, np_=np_: e.bn_stats(out=st6[0:np_, 0, :], in_=xa[:, 0:512]), r=[("xt", b)], w=["st0"])
                    S.add("dve", lambda e, xa=xa, np_=np_: e.bn_stats(out=st6[0:np_, 1, :], in_=xa[:, 512:1024]), r=[("xt", b)], w=["st1"])
                    S.add("dve", lambda e, np_=np_: e.bn_aggr(out=mv[0:np_, :], in_=st6[0:np_, :, :]), r=["st0", "st1"], w=["mv"], fence=True)
                    S.add("dve", lambda e, np_=np_: e.tensor_scalar(out=rstd[0:np_, :], in0=mv[0:np_, 1:2], scalar1=LN_EPS, scalar2=None,
                                                                    op0=ALU.add), r=["mv"], w=["rs"], fence=True)
                    S.add("act", lambda e, np_=np_: e.sqrt(out=rstd[0:np_, :], in_=rstd[0:np_, :]), r=["rs"], w=["rs"], fence=True)
                    S.add("dve", lambda e, np_=np_: e.reciprocal(out=rstd[0:np_, :], in_=rstd[0:np_, :]), r=["rs"], w=["rs"], fence=True)
                    S.add("dve", lambda e, xa=xa, b=b, np_=np_: e.tensor_scalar(
                        out=xa, in0=xa, scalar1=mv[0:np_, 0:1], scalar2=rstd[0:np_, 0:1],
                        op0=ALU.subtract, op1=ALU.mult), r=[("xt", b), "mv", "rs"], w=[("xt", b)])
                    S.add("dve", lambda e, xa=xa, np_=np_: e.tensor_tensor(out=xa, in0=xa, in1=lnB[0:np_, 0, :], op=ALU.mult),
                          r=[("xt", b), "lnB"], w=[("xt", b)])
                    S.add("dve", lambda e, xa=xa, np_=np_: e.tensor_tensor(out=xa, in0=xa, in1=lnB[0:np_, 1, :], op=ALU.add),
                          r=[("xt", b), "lnB"], w=[("xt", b)])
                    S.add("act", lambda e, xa=xa, b=b, np_=np_: e.copy(out=xn[b][0:np_, :], in_=xa), r=[("xt", b)], w=[("xn", b)])

                    def tr(e, b=b, np_=np_):
                        ins = None
                        for kc in range(8):
                            ins = e.transpose(out=pT[b][:, kc * 128:kc * 128 + np_], in_=xn[b][0:np_, kc * 128:(kc + 1) * 128],
                                              identity=identb[0:np_, 0:np_])
                        return ins
                    S.add("pe", tr, r=[("xn", b), "identb"], w=[("pT", b)])
                    S.add("act", lambda e, b=b, ti=ti, np_=np_: e.copy(
                        out=hT[:, :, ti * 128:ti * 128 + np_], in_=pT[b][:, :].rearrange("p (k t) -> p k t", k=8)[:, :, 0:np_]),
                        r=[("pT", b)], w=["hT"])

                if 8 in dbgs:
                    S.emit()
                def proj(e, c_lo, c_hi, pk, n=n):
                    ins = None
                    for kc in range(8):
                        ins = e.matmul(pss[pk][0:c_hi - c_lo, 0:n], w1t[:, kc, c_lo:c_hi], hT[:, kc, 0:n],
                                       start=(kc == 0), stop=(kc == 7))
                    return ins
                for h in range(2):
                    if g > 0:
                        S.add("pe", lambda e, f=proj, h=h: f(e, h * 64, h * 64 + 64, h), r=["w1t", "hT"], w=[("ps", h)])
                        S.add("act", lambda e, h=h, qc0=qc0: e.mul(out=QT[h][0:64, qc0:qc0 + 512], in_=pss[h][0:64, :], mul=0.125),
                              r=[("ps", h)], w=[("QT", h)])
                    S.add("pe", lambda e, f=proj, h=h: f(e, 128 + h * 64, 128 + h * 64 + 64, 2 + h), r=["w1t", "hT"], w=[("ps", 2 + h)])
                    S.add("dve", lambda e, h=h, kc0=kc0, n=n: e.tensor_copy(out=KT[h][0:64, kc0:kc0 + n], in_=pss[2 + h][0:64, 0:n]),
                          r=[("ps", 2 + h)], w=[("KT", h)])
                S.add("pe", lambda e, f=proj: f(e, 384, 512, 4), r=["w1t", "hT"], w=[("ps", 4)])
                S.add("act", lambda e, kc0=kc0, n=n: e.copy(out=uT[:, kc0:kc0 + n], in_=pss[4][:, 0:n]), r=[("ps", 4)], w=["uT"])
                if 1 in dbgs:
                    continue
                S.add("pe", lambda e, f=proj: f(e, 512, 576, 5), r=["w1t", "hT"], w=[("ps", 5)])
                S.add("act", lambda e, n=n: e.activation(out=e1[:, 0:n], in_=pss[5][0:2, 0:n], func=AF.Exp, bias=negbf[:, 0:1], scale=-1.0),
                      r=[("ps", 5), "negbf"], w=["e1"])
                if 11 in dbgs:
                    continue
                S.add("dve", lambda e, n=n: e.tensor_scalar(out=e1[:, 0:n], in0=e1[:, 0:n], scalar1=1.0, scalar2=None, op0=ALU.add),
                      r=["e1"], w=["e1"])
                S.add("act", lambda e, n=n: e.activation(out=e1[:, 0:n], in_=e1[:, 0:n], func=AF.Ln), r=["e1"], w=["e1"])
                if 12 in dbgs:
                    continue
                S.add("dve", lambda e, n=n: e.tensor_tensor_scan(out=nfg[:, 0:n], data0=ones2[:, 0:n], data1=e1[:, 0:n], initial=carry[:, 0:1],
                                                                 op0=ALU.mult, op1=ALU.add), r=["e1", "carry", "ones2"], w=["nfg"])
                if 13 in dbgs:
                    continue
                S.add("dve", lambda e, n=n: e.tensor_copy(out=carry[:], in_=nfg[:, n - 1:n]), r=["nfg"], w=["carry"])
                if 2 in dbgs:
                    continue
                if g > 0:
                    S.add("dve", lambda e: e.tensor_scalar(out=fb[:], in0=nfg[:], scalar1=-1.0, scalar2=None, op0=ALU.mult),
                          r=["nfg"], w=["fb"])
                    for h in range(2):
                        S.add("sp", lambda e, h=h, qc0=qc0: e.dma_start(out=QT[h][64:65, qc0:qc0 + 512], in_=fb[h:h + 1, :]),
                              r=["fb"], w=[("QTa", h)], dma="row")
                if 3 in dbgs:
                    continue
                for ti in range(ntile):
                    np_ = N_META if g == 0 else 128
                    kt = 0 if g == 0 else 1 + (g - 1) * 4 + ti
                    S.add("pe", lambda e, ti=ti, np_=np_: e.transpose(out=pss[5][0:np_, 256 + 2 * ti:258 + 2 * ti],
                                                                      in_=nfg[0:2, ti * 128:ti * 128 + np_], identity=ident32[0:2, 0:2]),
                          r=["nfg"], w=[("ps", 5)])
                    S.add("dve", lambda e, ti=ti, np_=np_, kt=kt: e.tensor_copy(out=NFcol[0:np_, kt, :], in_=pss[5][0:np_, 256 + 2 * ti:258 + 2 * ti]),
                          r=[("ps", 5)], w=["NFcol"])
                if 4 in dbgs:
                    continue
                for ti in range(ntile):
                    np_ = N_META if g == 0 else 128
                    kt = 0 if g == 0 else 1 + (g - 1) * 4 + ti
                    pk = ti % 2

                    def mmv(e, ti=ti, np_=np_, pk=pk):
                        ins = None
                        for kc in range(8):
                            ins = e.matmul(pss[pk][0:np_, 0:128], hT[:, kc, ti * 128:ti * 128 + np_], w1t[:, kc, 256:384],
                                           start=(kc == 0), stop=(kc == 7))
                        return ins
                    S.add("pe", mmv, r=["w1t", "hT"], w=[("ps", pk)])
                    if 5 in dbgs:
                        continue
                    S.add("act", lambda e, np_=np_, kt=kt, pk=pk: e.copy(out=VV[0][0:np_, kt, 0:64], in_=pss[pk][0:np_, 0:64]),
                          r=[("ps", pk), ("VVo", 0)], w=[("VV", 0)])
                    if 6 in dbgs:
                        continue
                    S.add("act", lambda e, np_=np_, kt=kt, pk=pk: e.copy(out=VV[1][0:np_, kt, 0:64], in_=pss[pk][0:np_, 64:128]),
                          r=[("ps", pk), ("VVo", 1)], w=[("VV", 1)])
            if 9 in dbgs:
                d1 = nc.dram_tensor("d_nf", [128, NKT * 2], F32, kind="ExternalOutput").ap()
                d2 = nc.dram_tensor("d_qt", [65, NX], BF16, kind="ExternalOutput").ap()
                d3 = nc.dram_tensor("d_kt", [65, NTOKS], BF16, kind="ExternalOutput").ap()
                d4 = nc.dram_tensor("d_vv", [128, NKT * 128], BF16, kind="ExternalOutput").ap()
                d5 = nc.dram_tensor("d_ut", [128, NTOKS], BF16, kind="ExternalOutput").ap()
                S.add("sp", lambda e: e.dma_start(out=d1[:, :], in_=NFcol[:, :, :].rearrange("p a b -> p (a b)")), r=["NFcol"], dma="out")
                S.add("sp", lambda e: e.dma_start(out=d2[:, :], in_=QT[0][:, :]), r=[("QT", 0), ("QTa", 0)], dma="out")
                S.add("sp", lambda e: e.dma_start(out=d3[:, :], in_=KT[0][:, :]), r=[("KT", 0), ("KTa", 0)], dma="out")
                S.add("sp", lambda e: e.dma_start(out=d4[:, :], in_=VV[0][:, :, :].rearrange("p a b -> p (a b)")), r=[("VV", 0), ("VVo", 0)], dma="out")
                S.add("sp", lambda e: e.dma_start(out=d5[:, :], in_=uT[:, :]), r=["uT"], dma="out")
                d6 = nc.dram_tensor("d_xt", [128, 1024], F32, kind="ExternalOutput").ap()
                d7 = nc.dram_tensor("d_xn", [128, 1024], BF16, kind="ExternalOutput").ap()
                d8 = nc.dram_tensor("d_ht", [128, 8 * 512], BF16, kind="ExternalOutput").ap()
                d9 = nc.dram_tensor("d_mv", [128, 3], F32, kind="ExternalOutput").ap()
                S.add("sp", lambda e: e.dma_start(out=d6[:, :], in_=xt[1][:, :]), r=[("xt", 1)], dma="out")
                S.add("sp", lambda e: e.dma_start(out=d7[:, :], in_=xn[1][:, :]), r=[("xn", 1)], dma="out")
                S.add("sp", lambda e: e.dma_start(out=d8[:, :], in_=hT[:, :, :].rearrange("p a b -> p (a b)")), r=["hT"], dma="out")
                S.add("sp", lambda e: e.dma_start(out=d9[:, 0:2], in_=mv[:, :], allow_slow_non_contiguous=True), r=["mv"], dma="out")
                S.add("sp", lambda e: e.dma_start(out=d9[:, 2:3], in_=rstd[:, :], allow_slow_non_contiguous=True), r=["rs"], dma="out")
            S.emit()

        if do_ssm:
            _ssm_block(nc, S, stack, locals())
        if do_attn:
            with ExitStack() as st2:
                def sb2(name, shape, dtype):
                    return st2.enter_context(nc.sbuf_tensor(name, shape, dtype))
                NPT = 3
                pt = [sb2(f"pt{i}", [128, 512], BF16) for i in range(NPT)]
                rl = [sb2(f"rl{i}", [128, 512], F32) for i in range(2)]
                rl2 = [sb2(f"rlb{i}", [64, 512], F32) for i in range(2)]
                ot = [sb2(f"ot{i}", [64, 512], ODT) for i in range(2)]
                it = 0
                gi = 0
                for i in range(NQG):
                    for h in range(2):
                        ab = 4 + (gi % 2)
                        ob = gi % 2
                        gi += 1
                        nblk = 4 * i + 5
                        blks = []
                        for jb in range(nblk):
                            nk = N_META if jb == 0 else 128
                            kcol = 0 if jb == 0 else N_META + (jb - 1) * 128
                            jt = jb - 1
                            diag = jb > 0 and jt >= 4 * i
                            qs = (jt - 4 * i) * 128 if diag else 0
                            blks.append(dict(jb=jb, nk=nk, kcol=kcol, qs=qs, diag=diag, sbk=it % 2, pb=it % NPT))
                            it += 1

                        def emit_s(bk, h=h, i=i):
                            nk, kcol, qs, sbk, pb, jb = bk["nk"], bk["kcol"], bk["qs"], bk["sbk"], bk["pb"], bk["jb"]
                            S.add("pe", lambda e: e.matmul(
                                pss[sbk][0:nk, qs:512], KT[h][0:65, kcol:kcol + nk], QT[h][0:65, i * 512 + qs:(i + 1) * 512],
                                start=True, stop=True), w=[("ps", sbk)])
                            S.add("act", lambda e: e.activation(
                                out=pt[pb][0:nk, qs:512], in_=pss[sbk][0:nk, qs:512], func=AF.Exp,
                                bias=NFcol[0:nk, jb, h:h + 1], scale=1.0), r=[("ps", sbk)], w=[("pt", pb)])
                            if bk["diag"]:
                                S.add("dve", lambda e: e.tensor_tensor(
                                    out=pt[pb][:, qs:qs + 128], in0=pt[pb][:, qs:qs + 128], in1=trib[:], op=ALU.mult),
                                    r=[("pt", pb)], w=[("pt", pb)])

                        def emit_pv(bk, h=h, ab=ab, nblk=nblk):
                            nk, qs, pb, jb = bk["nk"], bk["qs"], bk["pb"], bk["jb"]
                            S.add("pe", lambda e: e.matmul(
                                pss[ab][:, qs:512], VV[h][0:nk, jb, :], pt[pb][0:nk, qs:512],
                                start=(jb == 0), stop=(jb == nblk - 1)), r=[("pt", pb)], w=[("ps", ab)])
                        emit_s(blks[0])
                        for jb in range(nblk):
                            if jb + 1 < nblk:
                                emit_s(blks[jb + 1])
                            emit_pv(blks[jb])
                        S.add("dve", lambda e, ab=ab, ob=ob: e.reciprocal(out=rl[ob][64:128, :], in_=pss[ab][64:128, :]),
                              r=[("ps", ab)], w=[("rl", ob)])
                        S.add("sp", lambda e, ob=ob: e.dma_start(out=rl2[ob][:, :], in_=rl[ob][64:128, :]),
                              r=[("rl", ob)], w=[("rl2", ob)], dma="rl")
                        S.add("dve", lambda e, ab=ab, ob=ob: e.tensor_tensor(out=ot[ob][:, :], in0=pss[ab][0:64, :], in1=rl2[ob][:, :], op=ALU.mult),
                              r=[("ps", ab), ("rl2", ob)], w=[("ot", ob)])
                        kch_ = (i * 512) // CH
                        S.add("sp", lambda e, h=h, i=i, ob=ob: e.dma_start(out=ydst(0, h * 64, (h + 1) * 64, i * 512, 512), in_=ot[ob][:, :]),
                              r=[("ot", ob)], w=[("ych", kch_, h, i)], dma="out")
                        if fused and h == 1 and ((i + 1) * 512) % CH == 0:
                            deps = [("ych", kch_, hh, ii) for hh in range(2) for ii in range(NQG) if (ii * 512) // CH == kch_]
                            S.add("pool", lambda e, k=kch_: e.collective_compute(
                                "AllGather", ALU.bypass, replica_groups=[[0, 1, 2, 3], [4, 5, 6, 7]],
                                ins=[ctx["ybuf_t"][k].ap().opt()], outs=[ctx["ygat_t"][k].ap().opt()]), r=deps, w=[("ygat", kch_)])
                            S.add("pool", lambda e: e.memset(S.fz["pool"][:, 1:2], 0.0), r=[("ygat", kch_)], w=["fzp"])
                S.emit()
    return nc


def _ssm_block(nc, S, stack, L):
    uT, pss, cst32 = L["uT"], L["pss"], L["cst32"]
    lamp, logdt, bstk_d, cstk_d, dvec_d, yssm = L["lamp"], L["logdt"], L["bstk_d"], L["cstk_d"], L["dvec_d"], L["yssm"]
    NX, NCH, NTOKS = L["NX"], L["NCH"], L["NTOKS"]
    ident32 = cst32[:, 0:128]
    Jm = cst32[:, 128:256]
    bmask = cst32[:, 512:512 + 1024]
    sgnR = cst32[:, 1536:1537]
    NLEV = max(1, (NCH - 1).bit_length())
    POW_SEQ = list(range(9))
    NPW = 9 + NLEV
    PI = math.pi
    with ExitStack() as st2:
        def sb2(name, shape, dtype):
            return st2.enter_context(nc.sbuf_tensor(name, shape, dtype))
        lam = sb2("lam", [128, 16], F32)
        dtb = sb2("dtb", [128, 8], F32)
        t0 = sb2("t0", [128, 8], F32)
        t1 = sb2("t1", [128, 8], F32)
        t2 = sb2("t2", [128, 8], F32)
        t3 = sb2("t3", [128, 8], F32)
        pw_re = sb2("pw_re", [128, NPW + 1, 8], F32)
        pw_im = sb2("pw_im", [128, NPW + 1, 8], F32)
        pw_ims = sb2("pw_ims", [128, NPW + 1, 8], F32)
        bstk = sb2("bstk_sb", [128, 128], F32)
        cstk = sb2("cstk_sb", [128, 128], F32)
        dvec = sb2("dvec_sb", [128, 1], F32)
        bpad = sb2("bpad", [128, 128], BF16)
        cpad = sb2("cpad", [128, 128], BF16)
        Rb = sb2("Rb", [128, 10, 128], BF16)
        Rh = sb2("Rh", [128, NLEV, 128], F32)
        bbar = sb2("bbar", [128, 128], BF16)
        lhsA = sb2("lhsA", [128, 8, 128], BF16)
        CL0 = sb2("CL0", [128, 128], BF16)
        CLall = sb2("CLall", [128, 8, 8, 128], BF16)
        Wb = sb2("Wb", [128, 8, 128], BF16)
        Ea = sb2("Ea", [128, NCH], F32)
        Eb = sb2("Eb", [128, NCH], F32)
        Sin = sb2("Sin", [128, 8, NCH], BF16)
        yo = [sb2("yo0", [128, 4096], L["ODT"])]
        S.stream("ssm", 0)
        S.add("sp", lambda e: e.dma_start(out=lam[:], in_=lamp[:, :]), w=["lam"], dma="ssm")
        S.add("sp", lambda e: e.dma_start(out=dtb[:], in_=logdt[0:1, :].partition_broadcast(128)), w=["dtb"], dma="ssm")
        S.add("sp", lambda e: e.dma_start(out=bstk[:], in_=bstk_d[:, :]), w=["bstk"], dma="ssm")
        S.add("sp", lambda e: e.dma_start(out=cstk[:], in_=cstk_d[:, :]), w=["cstk"], dma="ssm")
        S.add("sp", lambda e: e.dma_start(out=dvec[:], in_=dvec_d[:, :]), w=["dvec"], dma="ssm")
        are, aim = lam[:, 0:8], lam[:, 8:16]

        def V(fn, r, w):
            S.add("dve", fn, r=r, w=w)

        def tt(out, a, b, op, r, w):
            V(lambda e: e.tensor_tensor(out=out, in0=a, in1=b, op=op), r, w)

        def ts(out, a, s1, s2, op0, op1, r, w):
            if op1 is None:
                V(lambda e: e.tensor_scalar(out=out, in0=a, scalar1=s1, scalar2=None, op0=op0), r, w)
            else:
                V(lambda e: e.tensor_scalar(out=out, in0=a, scalar1=s1, scalar2=s2, op0=op0, op1=op1), r, w)
        S.add("act", lambda e: e.activation(out=dtb[:], in_=dtb[:], func=AF.Exp), r=["dtb"], w=["dtb"])
        tt(t0[:], are, dtb[:], ALU.mult, ["lam", "dtb"], ["t0"])
        S.add("act", lambda e: e.activation(out=t0[:], in_=t0[:], func=AF.Exp), r=["t0"], w=["t0"])
        tt(t1[:], aim, dtb[:], ALU.mult, ["lam", "dtb"], ["t1"])
        MAGIC = 12582912.0
        ts(t1[:], t1[:], 1.0 / (2.0 * PI), None, ALU.mult, None, ["t1"], ["t1"])
        ts(t3[:], t1[:], MAGIC, None, ALU.add, None, ["t1"], ["t3"])
        ts(t3[:], t3[:], -MAGIC, None, ALU.add, None, ["t3"], ["t3"])
        tt(t2[:], t1[:], t3[:], ALU.subtract, ["t1", "t3"], ["t2"])
        S.add("act", lambda e: e.activation(out=t2[:], in_=t2[:], func=AF.Sin, scale=2.0 * PI), r=["t2"], w=["t2"])
        ts(t1[:], t1[:], 0.25, None, ALU.add, None, ["t1"], ["t1"])
        ts(t3[:], t1[:], MAGIC, None, ALU.add, None, ["t1"], ["t3"])
        ts(t3[:], t3[:], -MAGIC, None, ALU.add, None, ["t3"], ["t3"])
        tt(t3[:], t1[:], t3[:], ALU.subtract, ["t1", "t3"], ["t3"])
        S.add("act", lambda e: e.activation(out=t3[:], in_=t3[:], func=AF.Sin, scale=2.0 * PI), r=["t3"], w=["t3"])
        tt(pw_re[:, 1, :], t0[:], t3[:], ALU.mult, ["t0", "t3"], ["pw"])
        tt(pw_im[:, 1, :], t0[:], t2[:], ALU.mult, ["t0", "t2"], ["pw"])
        V(lambda e: e.memset(pw_re[:, 0, :], 1.0), [], ["pw"])
        V(lambda e: e.memset(pw_im[:, 0, :], 0.0), [], ["pw"])

        def cmul(o, a, b):
            tt(t0[:], pw_re[:, a, :], pw_re[:, b, :], ALU.mult, ["pw"], ["t0"])
            tt(t1[:], pw_im[:, a, :], pw_im[:, b, :], ALU.mult, ["pw"], ["t1"])
            tt(t2[:], pw_re[:, a, :], pw_im[:, b, :], ALU.mult, ["pw"], ["t2"])
            tt(t3[:], pw_im[:, a, :], pw_re[:, b, :], ALU.mult, ["pw"], ["t3"])
            tt(pw_re[:, o, :], t0[:], t1[:], ALU.subtract, ["t0", "t1"], ["pw"])
            tt(pw_im[:, o, :], t2[:], t3[:], ALU.add, ["t2", "t3"], ["pw"])
        for m in range(2, 9):
            cmul(m, m - 1, 1)
        for l in range(1, NLEV):
            cmul(8 + l, 8 + l - 1, 8 + l - 1)
        CO = NPW
        tt(t0[:], are, are, ALU.mult, ["lam"], ["t0"])
        tt(t1[:], aim, aim, ALU.mult, ["lam"], ["t1"])
        tt(t0[:], t0[:], t1[:], ALU.add, ["t0", "t1"], ["t0"])
        V(lambda e: e.reciprocal(out=t0[:], in_=t0[:]), ["t0"], ["t0"])
        ts(t1[:], pw_re[:, 1, :], -1.0, None, ALU.add, None, ["pw"], ["t1"])
        tt(t2[:], t1[:], are, ALU.mult, ["t1", "lam"], ["t2"])
        tt(t3[:], pw_im[:, 1, :], aim, ALU.mult, ["pw", "lam"], ["t3"])
        tt(t2[:], t2[:], t3[:], ALU.add, ["t2", "t3"], ["t2"])
        tt(pw_re[:, CO, :], t2[:], t0[:], ALU.mult, ["t2", "t0"], ["pw"])
        tt(t2[:], pw_im[:, 1, :], are, ALU.mult, ["pw", "lam"], ["t2"])
        tt(t3[:], t1[:], aim, ALU.mult, ["t1", "lam"], ["t3"])
        tt(t2[:], t2[:], t3[:], ALU.subtract, ["t2", "t3"], ["t2"])
        tt(pw_im[:, CO, :], t2[:], t0[:], ALU.mult, ["t2", "t0"], ["pw"])
        V(lambda e: e.tensor_scalar(out=pw_ims[:], in0=pw_im[:], scalar1=sgnR, scalar2=None, op0=ALU.mult), ["pw", "cst"], ["pws"])

        def build_R(out_ap, idx, g, key):
            V(lambda e: e.tensor_scalar(out=out_ap, in0=ident32, scalar1=pw_re[:, idx, g:g + 1], scalar2=None, op0=ALU.mult),
              ["pw", "cst"], [key])
            V(lambda e: e.scalar_tensor_tensor(out=out_ap, in0=Jm, scalar=pw_ims[:, idx, g:g + 1], in1=out_ap,
                                               op0=ALU.mult, op1=ALU.add), ["pws", "cst", key], [key])

        NT3 = [(c, min(342, NCH - c)) for c in range(0, NCH, 342)]
        evk = 0
        for g in range(8):
            for m in range(9):
                build_R(Rb[:, m, :], m, g, ("Rb", m))
            build_R(Rb[:, 9, :], CO, g, ("Rb", 9))
            for l in range(NLEV):
                build_R(Rh[:, l, :], 8 + l, g, ("Rh", l))
            tt(bpad[:], bstk[:], bmask[:, g * 128:(g + 1) * 128], ALU.mult, ["bstk", "cst"], ["bpad"])
            tt(cpad[:], cstk[:], bmask[:, g * 128:(g + 1) * 128], ALU.mult, ["cstk", "cst"], ["cpad"])
            S.add("pe", lambda e: e.matmul(pss[0][:, 0:128], Rb[:, 9, :], bpad[:], start=True, stop=True),
                  r=[("Rb", 9), "bpad"], w=[("ps", 0)])
            S.add("act", lambda e: e.copy(out=bbar[:], in_=pss[0][:, 0:128]), r=[("ps", 0)], w=["bbar"])
            for i in range(8):
                pk = i % 2
                S.add("pe", lambda e, i=i, pk=pk: e.matmul(pss[pk][:, 128:256], bbar[:], Rb[:, 7 - i, :], start=True, stop=True),
                      r=["bbar", ("Rb", 7 - i)], w=[("ps", pk)])
                if i % 2 == 0:
                    S.add("act", lambda e, i=i, pk=pk: e.copy(out=lhsA[:, i, :], in_=pss[pk][:, 128:256]), r=[("ps", pk)], w=[("lhsA", i)])
                else:
                    S.add("dve", lambda e, i=i, pk=pk: e.tensor_copy(out=lhsA[:, i, :], in_=pss[pk][:, 128:256]), r=[("ps", pk)], w=[("lhsA", i)])
            for d in range(9):
                pk = d % 2
                dst = CL0[:] if d == 0 else CLall[:, g, d - 1, :]
                S.add("pe", lambda e, d=d, pk=pk: e.matmul(pss[pk][:, 256:384], Rb[:, d, :], cpad[:], start=True, stop=True),
                      r=[("Rb", d), "cpad"], w=[("ps", pk)])
                S.add("dve", lambda e, dst=dst, pk=pk: e.tensor_scalar(out=dst, in0=pss[pk][:, 256:384], scalar1=sgnR, scalar2=None, op0=ALU.mult),
                      r=[("ps", pk), "cst"], w=[("CL", g, d)])
            for d in range(8):
                bank = 2 + d // 4
                col = (d % 4) * 128
                S.add("pe", lambda e, d=d, bank=bank, col=col, g=g: e.matmul(
                    pss[bank][:, col:col + 128], bbar[:], (CL0[:] if d == 0 else CLall[:, g, d - 1, :]),
                    start=(g == 0), stop=(g == 7)), r=["bbar", ("CL", g, d)], w=[("ps", bank)])
            for (c0, n) in NT3:
                pk = 4 + (evk % 2)
                evk += 1

                def mmA(e, c0=c0, n=n, pk=pk):
                    ins = None
                    for i in range(8):
                        ins = e.matmul(pss[pk][:, 0:n], lhsA[:, i, :], uT[:, c0 * 8 + i:(c0 + n) * 8:8], start=(i == 0), stop=(i == 7))
                    return ins
                S.add("pe", mmA, r=[("lhsA", i) for i in range(8)], w=[("ps", pk)])
                S.add("act", lambda e, c0=c0, n=n, pk=pk: e.copy(out=Ea[:, c0:c0 + n], in_=pss[pk][:, 0:n]), r=[("ps", pk)], w=["Ea"])
            src, dst_ = Ea, Eb
            sk, dk = "Ea", "Eb"
            for l in range(NLEV):
                s = 1 << l
                S.add("act", lambda e, src=src, dst_=dst_, s=s: e.copy(out=dst_[:, 0:s], in_=src[:, 0:s]), r=[sk], w=[dk])
                c = s
                while c < NCH:
                    n = min(512, NCH - c)
                    pk = 4 + (evk % 2)
                    evk += 1
                    S.add("pe", lambda e, l=l, c=c, n=n, s=s, pk=pk, src=src: e.matmul(
                        pss[pk][:, 0:n], Rh[:, l, :], src[:, c - s:c - s + n], start=True, stop=True), r=[("Rh", l), sk], w=[("ps", pk)])
                    S.add("dve", lambda e, c=c, n=n, pk=pk, src=src, dst_=dst_: e.tensor_tensor(
                        out=dst_[:, c:c + n], in0=pss[pk][:, 0:n], in1=src[:, c:c + n], op=ALU.add), r=[("ps", pk), sk], w=[dk])
                    c += n
                src, dst_ = dst_, src
                sk, dk = dk, sk
            S.add("dve", lambda e, g=g: e.memset(Sin[:, g, 0:1], 0.0), w=[("Sin", g)])
            S.add("act", lambda e, g=g, src=src: e.copy(out=Sin[:, g, 1:NCH], in_=src[:, 0:NCH - 1]), r=[sk], w=[("Sin", g)])
        for d in range(8):
            bank = 2 + d // 4
            col = (d % 4) * 128
            if d == 0:
                S.add("dve", lambda e, bank=bank, col=col: e.scalar_tensor_tensor(
                    out=Wb[:, 0, :], in0=ident32, scalar=dvec[:, 0:1], in1=pss[bank][:, col:col + 128], op0=ALU.mult, op1=ALU.add),
                    r=[("ps", bank), "dvec", "cst"], w=["Wb"])
            else:
                S.add("act", lambda e, d=d, bank=bank, col=col: e.copy(out=Wb[:, d, :], in_=pss[bank][:, col:col + 128]),
                      r=[("ps", bank)], w=["Wb"])
        kch = 2
        oi = 0
        while kch < NCH:
            n = min(512, NCH - kch)
            ob = 0
            for j in range(8):
                pk = 4 + (evk % 2)
                evk += 1

                def mmo(e, j=j, kch=kch, n=n, pk=pk):
                    ins = None
                    tot = (j + 1) + 8
                    q = 0
                    for i in range(j + 1):
                        ins = e.matmul(pss[pk][:, 0:n], Wb[:, j - i, :], uT[:, kch * 8 + i:(kch + n) * 8:8], start=(q == 0), stop=(q == tot - 1))
                        q += 1
                    for g in range(8):
                        ins = e.matmul(pss[pk][:, 0:n], CLall[:, g, j, :], Sin[:, g, kch:kch + n], start=(q == 0), stop=(q == tot - 1))
                        q += 1
                    return ins
                S.add("pe", mmo, r=["Wb"] + [("Sin", g) for g in range(8)] + [("CL", g, d) for g in range(8) for d in range(1, 9)],
                      w=[("ps", pk)])
                eng = "act" if j % 2 == 0 else "dve"
                if eng == "act":
                    S.add("act", lambda e, j=j, n=n, pk=pk, ob=ob: e.copy(out=yo[ob][:, j:n * 8:8], in_=pss[pk][:, 0:n]),
                          r=[("ps", pk)], w=[("yo", ob)])
                else:
                    S.add("dve", lambda e, j=j, n=n, pk=pk, ob=ob: e.tensor_copy(out=yo[ob][:, j:n * 8:8], in_=pss[pk][:, 0:n]),
                          r=[("ps", pk)], w=[("yo", ob)])
            t0_ = (kch - 2) * 8
            CH = NX // 4
            for cs in range(0, n * 8, min(CH, n * 8)):
                cn = min(CH, n * 8)
                S.add("sp", lambda e, ob=ob, t0_=t0_, cs=cs, cn=cn: e.dma_start(out=L["ydst"](1, 0, 128, t0_ + cs, cn), in_=yo[ob][:, cs:cs + cn]),
                      r=[("yo", ob)], dma="out")
            kch += n
            oi += 1
        S.emit()


def _consts():
    c = np.zeros((128, 128 * 4 + 8 * 128 + 4), np.float32)
    c[:, 0:128] = np.eye(128, dtype=np.float32)
    J = np.zeros((128, 128), np.float32)
    for p in range(64):
        J[p, 64 + p] = 1.0
        J[64 + p, p] = 1.0
    c[:, 128:256] = J
    s = np.arange(128)[:, None]
    t = np.arange(128)[None, :]
    c[:, 256:384] = (s <= t).astype(np.float32)
    for g in range(8):
        c[:, 512 + g * 128 + g * 16: 512 + g * 128 + (g + 1) * 16] = 1.0
    c[0:64, 1536] = 1.0
    c[64:128, 1536] = -1.0
    return c


def prep_p1(inp, b, r, NX=SEQ):
    hs = [2 * r, 2 * r + 1]
    w_in = inp["w_in"][0]
    cols = []
    for h in hs:
        cols.append(w_in[:, Q_OFF + h * 64:Q_OFF + (h + 1) * 64])
    for h in hs:
        cols.append(w_in[:, K_OFF + h * 64:K_OFF + (h + 1) * 64])
    for h in hs:
        cols.append(w_in[:, V_OFF + h * 64:V_OFF + (h + 1) * 64])
    cols.append(w_in[:, U_OFF + r * 128:U_OFF + (r + 1) * 128])
    cols.append(w_in[:, F_OFF + 2 * r:F_OFF + 2 * r + 2])
    w1 = np.ascontiguousarray(np.concatenate(cols, axis=1), dtype=np.float32)
    lnp = np.concatenate([inp["ln_in_g"].reshape(8, 128).T, inp["ln_in_b"].reshape(8, 128).T], axis=1)
    gs = slice(8 * r, 8 * r + 8)
    a_re = inp["ssm_a_re"][0][gs]
    a_im = inp["ssm_a_im"][0][gs]
    lamp = np.concatenate([np.concatenate([a_re.T, a_re.T], axis=0), np.concatenate([a_im.T, a_im.T], axis=0)], axis=1)
    b_re = inp["ssm_b_re"][0][gs]
    b_im = inp["ssm_b_im"][0][gs]
    bstk = np.concatenate([b_re.transpose(1, 0, 2).reshape(64, 128), b_im.transpose(1, 0, 2).reshape(64, 128)], axis=0)
    c_re = inp["ssm_c_re"][0][gs]
    c_im = inp["ssm_c_im"][0][gs]
    cstk = np.concatenate([c_re.transpose(2, 0, 1).reshape(64, 128), c_im.transpose(2, 0, 1).reshape(64, 128)], axis=0)
    f32 = lambda a: np.ascontiguousarray(a, dtype=np.float32)
    return dict(x=f32(inp["x"][b][:NX]), meta=f32(inp["meta"]), lnp=f32(lnp), w1=w1,
                lnrow=f32(np.stack([inp["ln_in_g"], inp["ln_in_b"]])),
                bf=f32(inp["b_f"][0][2 * r:2 * r + 2].reshape(2, 1)), lamp=f32(lamp),
                logdt=f32(inp["ssm_log_dt"][0][gs].reshape(1, 8)), bstk=f32(bstk), bstks=f32(bstk), cstk=f32(cstk),
                dvec=f32(inp["ssm_d"][0][gs].reshape(128, 1)), cst=_consts())


def prep_p2_shared(inp):
    f32 = lambda a: np.ascontiguousarray(a, dtype=np.float32)
    w_in = inp["w_in"][0]
    w_ga = w_in[:, GA_OFF:GB_OFF]
    w_gb = w_in[:, GB_OFF:GB_OFF + 1024]
    wg = np.zeros((1024, 2048), np.float32)
    for j in range(4):
        for dcl in range(2):
            dc = 2 * j + dcl
            o = j * 512 + dcl * 256
            wg[:, o:o + 128] = w_ga[:, dc * 128:(dc + 1) * 128]
            wg[:, o + 128:o + 256] = w_gb[:, dc * 128:(dc + 1) * 128]
    lnp = np.stack([inp["ln_in_g"], inp["ln_in_b"], inp["ln1_g"][0], inp["ln1_b"][0], inp["ln2_g"][0], inp["ln2_b"][0]])
    bgu = np.zeros((128, 2, N_EXPERTS, 8), np.float32)
    bgu[:, 0] = inp["b_gate"][0].reshape(N_EXPERTS, 8, 128).transpose(2, 0, 1)
    bgu[:, 1] = inp["b_up"][0].reshape(N_EXPERTS, 8, 128).transpose(2, 0, 1)
    return dict(lnp=f32(lnp), wg=wg, wo=f32(inp["w_o"][0]), wua=f32(inp["w_up_a"][0]), wub=f32(inp["w_up_b"][0]),
                wglu=f32(inp["w_glu"][0]), bglu=f32(inp["b_glu"][0].reshape(4, 128).T), wr=f32(inp["w_router"][0]),
                br=f32(inp["b_router"][0].reshape(1, 32)), wgate=f32(inp["w_gate"][0]), wup=f32(inp["w_up"][0]),
                wdown=f32(inp["w_down"][0]), bgu=f32(bgu.reshape(128, -1)), bdown=f32(inp["b_down"][0]),
                ident=np.eye(128, dtype=np.float32))


_NC_CACHE = {}


def build_fused(NX=SEQ, NE=N_EXPERTS):
    nc = bass.Bass("TRN2", target_bir_lowering=False)
    with ExitStack() as top:
        S = Sched(nc, top)
        CH = NX // 4
        ybuf = [nc.dram_tensor(f"ybuf_in{k}", [256, CH], BF16) for k in range(4)]
        ygat = [nc.dram_tensor(f"ybuf_out{k}", [4 * 256, CH], BF16) for k in range(4)]
        build_p1(NX, ctx=dict(nc=nc, S=S, ybuf=[t.ap() for t in ybuf], ybuf_t=ybuf, ygat_t=ygat))
        build_p2(NX // 4, NE, ctx=dict(nc=nc, S=S, ygat=[t.ap() for t in ygat]))
    return nc


def make_maps(inp, NX=SEQ, NE=N_EXPERTS):
    shared = prep_p2_shared(inp)
    shared["lnp2"] = shared.pop("lnp")
    for k in ("wgate", "wup", "wdown"):
        shared[k] = shared[k][:NE]
    shared["bgu"] = np.ascontiguousarray(shared["bgu"].reshape(128, 2, N_EXPERTS, 8)[:, :, :NE].reshape(128, -1))
    NTOK = NX // 4
    maps = []
    for c in range(8):
        b, r = c // 4, c % 4
        m = dict(shared)
        m.update(prep_p1(inp, b, r, NX))
        m.pop("bstks")
        m["x2"] = np.ascontiguousarray(inp["x"][b, r * NTOK:(r + 1) * NTOK], dtype=np.float32)
        oh = np.zeros((128, 4), np.float32)
        oh[:, r] = 1.0
        m["onehot"] = oh
        maps.append(m)
    return maps


def kernel(**inputs):
    inp = {k: np.asarray(v) for k, v in inputs.items()}
    B = inp["x"].shape[0]
    if "f" not in _NC_CACHE:
        _NC_CACHE["f"] = build_fused()
    maps = make_maps(inp)
    if False:
        m = None
    res = run_bass_kernel_spmd(_NC_CACHE["f"], maps, core_ids=list(range(8)))
    out = np.zeros((B, SEQ, D_MODEL), np.float32)
    for c in range(8):
        b, r = c // 4, c % 4
        out[b, r * 2048:(r + 1) * 2048] = res.results[c]["out"]
    return out
```

```python
import math
from contextlib import ExitStack

import numpy as np
import concourse.bass as bass
import concourse.mybir as mybir
from concourse.bass_utils import run_bass_kernel_spmd

F32 = mybir.dt.float32
BF16 = mybir.dt.bfloat16
AF = mybir.ActivationFunctionType
ALU = mybir.AluOpType

D_MODEL = 1024
SEQ = 8192
N_META = 16
N_EXPERTS = 32
LN_EPS = 1e-5
DN_ALPHA = 2.0 ** 0.25
SW_LIMIT = 7.0
SW_ALPHA = 1.702
Q_OFF, K_OFF, V_OFF, F_OFF, U_OFF, GA_OFF, GB_OFF = 0, 512, 1024, 1536, 1544, 2056, 3080
MERGE_SSM_ATTN = True


class Sched:
    ENGS = ("pe", "act", "dve", "pool", "sp")
    SEM_ROLL = 1 << 30

    def __init__(self, nc, stack):
        self.nc = nc
        self.stack = stack
        self.eng_obj = {"pe": nc.tensor, "act": nc.scalar, "dve": nc.vector,
                        "pool": nc.gpsimd, "sp": nc.sync}
        self.eng_sems = {e: [stack.enter_context(nc.semaphore(f"s_{e}0"))] for e in self.ENGS}
        self.eng_cnt = {e: 0 for e in self.ENGS}
        self.streams = {}
        self.nsem = 0
        self.fz = {e: stack.enter_context(nc.sbuf_tensor(f"fz_{e}", [128, 2], F32)) for e in ("act", "dve", "pool")}
        self.reset()

    def reset(self):
        self.ops = []
        self.lastw = {}
        self.readers = {}
        self._rec = None

    def rec_begin(self):
        self._rec = []

    def rec_end(self):
        r, self._rec = self._rec, None
        return r

    def replay(self, a, b):
        ia = ib = 0
        while ia < len(a) or ib < len(b):
            fa = ia / max(1, len(a))
            fb = ib / max(1, len(b))
            if ib >= len(b) or (ia < len(a) and fa <= fb):
                self.add(*a[ia])
                ia += 1
            else:
                self.add(*b[ib])
                ib += 1

    def stream(self, name, depth):
        if name not in self.streams:
            sems = [self.stack.enter_context(self.nc.semaphore(f"d_{name}{i}")) for i in range(depth)]
            self.streams[name] = dict(sems=sems, cnt=[0] * depth, k=0)
        return name

    def add(self, eng, fn, r=(), w=(), dma=None, fence=False):
        if self._rec is not None:
            self._rec.append((eng, fn, tuple(r), tuple(w), dma, fence))
            return -1
        idx = len(self.ops)
        deps = set()
        for k in r:
            if k in self.lastw:
                deps.add(self.lastw[k])
        for k in w:
            if k in self.lastw:
                deps.add(self.lastw[k])
            for x in self.readers.get(k, ()):
                deps.add(x)
        deps.discard(idx)
        for k in r:
            self.readers.setdefault(k, []).append(idx)
        for k in w:
            self.lastw[k] = idx
            self.readers[k] = []
        self.ops.append(dict(eng=eng, fn=fn, deps=deps, dma=dma, used=False, sig=None, fence=fence))
        for d in deps:
            self.ops[d]["used"] = True
        return idx

    def emit(self, final_wait_dma=True):
        nc = self.nc
        ops = self.ops
        for op in ops:
            if op["dma"] is not None and len(self.streams[op["dma"]]["sems"]) == 0:
                self.nsem += 1
                sem = self.stack.enter_context(nc.semaphore(f"d_one{self.nsem}"))
                op["sig"] = (sem, 16, 16)
            elif op["dma"] is not None:
                st = self.streams[op["dma"]]
                k = st["k"] % len(st["sems"])
                st["k"] += 1
                st["cnt"][k] += 16
                op["sig"] = (st["sems"][k], st["cnt"][k], 16)
            elif op["used"]:
                e = op["eng"]
                if self.eng_cnt[e] >= self.SEM_ROLL:
                    self.eng_sems[e].append(self.stack.enter_context(nc.semaphore(f"s_{e}{len(self.eng_sems[e])}")))
                    self.eng_cnt[e] = 0
                self.eng_cnt[e] += 1
                op["sig"] = (self.eng_sems[e][-1], self.eng_cnt[e], 1)
        per_eng = {e: [] for e in self.ENGS}
        for i, op in enumerate(ops):
            waits = []
            for d in sorted(op["deps"]):
                dop = ops[d]
                if dop["dma"] is None and dop["eng"] == "pe" and op["eng"] == "pe":
                    continue
                waits.append(dop["sig"])
            per_eng[op["eng"]].append((waits, op))
        pending = [op["sig"] for op in ops if op["dma"] is not None]
        with nc.Block() as blk:
            def make(ename):
                lst = per_eng[ename]

                def body(eng):
                    known = {}
                    for waits, op in lst:
                        for (sem, val, _inc) in waits:
                            key = id(sem)
                            if known.get(key, (None, 0))[1] >= val:
                                continue
                            eng.wait_ge(sem, val)
                            known[key] = (sem, val)
                        ins = op["fn"](eng)
                        if op["sig"] is not None:
                            if op["fence"] and ename in self.fz:
                                fz = self.fz[ename]
                                if ename == "act":
                                    ins = eng.memzero(fz[:, 0:1])
                                else:
                                    ins = eng.memset(fz[:, 0:1], 0.0)
                            ins.then_inc(op["sig"][0], op["sig"][2])
                    if ename == "sp" and final_wait_dma:
                        best = {}
                        for (sem, val, _inc) in pending:
                            if best.get(id(sem), (None, 0))[1] < val:
                                best[id(sem)] = (sem, val)
                        for sem, val in best.values():
                            if known.get(id(sem), (None, 0))[1] < val:
                                eng.wait_ge(sem, val)
                return body
            blk.tensor(make("pe"))
            blk.scalar(make("act"))
            blk.vector(make("dve"))
            blk.gpsimd(make("pool"))
            blk.sync(make("sp"))
        self.reset()


def _bc(ap, shape):
    return ap.to_broadcast(shape)


def _ln_tile(S, tile_ap, st6, mv, rstd, gB, bB, key, tag):
    S.add("dve", lambda e: e.bn_stats(out=st6[:, 0, :], in_=tile_ap[:, 0:512]), r=[key], w=[tag + "st0"])
    S.add("dve", lambda e: e.bn_stats(out=st6[:, 1, :], in_=tile_ap[:, 512:1024]), r=[key], w=[tag + "st1"])
    S.add("dve", lambda e: e.bn_aggr(out=mv[:], in_=st6[:]), r=[tag + "st0", tag + "st1"], w=[tag + "mv"], fence=True)
    S.add("dve", lambda e: e.tensor_scalar(out=rstd[:], in0=mv[:, 1:2], scalar1=LN_EPS, scalar2=None,
                                           op0=ALU.add), r=[tag + "mv"], w=[tag + "rs"], fence=True)
    S.add("act", lambda e: e.sqrt(out=rstd[:], in_=rstd[:]), r=[tag + "rs"], w=[tag + "rs"], fence=True)
    S.add("dve", lambda e: e.reciprocal(out=rstd[:], in_=rstd[:]), r=[tag + "rs"], w=[tag + "rs"], fence=True)
    S.add("dve", lambda e: e.scalar_tensor_tensor(out=tile_ap, in0=tile_ap, scalar=mv[:, 0:1], in1=gB,
                                                  op0=ALU.subtract, op1=ALU.mult), r=[key, tag + "mv", "lnB"], w=[key])
    S.add("dve", lambda e: e.scalar_tensor_tensor(out=tile_ap, in0=tile_ap, scalar=rstd[:, 0:1], in1=bB,
                                                  op0=ALU.mult, op1=ALU.add), r=[key, tag + "rs", "lnB"], w=[key])


def build_p2(NTOK=2048, NE=32, ctx=None):
    NT = NTOK // 128
    NG = NTOK // 512
    fused = ctx is not None
    nc = ctx["nc"] if fused else bass.Bass("TRN2", target_bir_lowering=False)

    def din(name, shape, dtype=F32):
        return nc.dram_tensor(name, shape, dtype, kind="ExternalInput").ap()
    x = din("x2", [NTOK, 1024])
    if fused:
        ygat = ctx["ygat"]
        onehot = din("onehot", [128, 4])
    else:
        yaT = din("yaT", [512, NTOK])
        ysT = din("ysT", [512, NTOK])
    lnp = din("lnp2", [6, 1024])
    wg = din("wg", [1024, 2048])
    wo = din("wo", [1024, 1024])
    wua = din("wua", [512, 1024])
    wub = din("wub", [512, 1024])
    wglu = din("wglu", [512, 512])
    bglu = din("bglu", [128, 4])
    wr = din("wr", [1024, 32])
    br = din("br", [1, 32])
    wgate = din("wgate", [NE, 1024, 1024])
    wup = din("wup", [NE, 1024, 1024])
    wdown = din("wdown", [NE, 1024, 1024])
    bgu = din("bgu", [128, 2 * NE * 8])
    bdown = din("bdown", [N_EXPERTS, 1024])
    ident = din("ident", [128, 128])
    out = nc.dram_tensor("out", [NTOK, 1024], F32, kind="ExternalOutput").ap()

    with ExitStack() as stack:
        S = ctx["S"] if fused else Sched(nc, stack)

        def sb(name, shape, dtype):
            return stack.enter_context(nc.sbuf_tensor("b_" + name, shape, dtype))

        def psum(name, shape, dtype):
            return stack.enter_context(nc.psum_tensor("b_" + name, shape, dtype))
        hres = sb("hres", [128, NT, 1024], F32)
        identb = sb("identb", [128, 128], BF16)
        ident32 = sb("ident32", [128, 128], F32)
        lnB = sb("lnB", [128, 4, 1024], F32)
        comb = sb("comb", [128, NT, 32], F32)
        st6 = sb("st6", [128, 2, 6], F32)
        mv = sb("mv", [128, 2], F32)
        rstd = sb("rstd", [128, 1], F32)
        pss = [psum(f"ps{i}", [128, 512], F32) for i in range(6)]
        pT = [psum(f"pT{i}", [128, 1024], BF16) for i in range(2)]
        S.stream("x", 2)
        S.stream("misc", 0)
        S.stream("gw", 3)
        S.stream("yin", 0)
        S.stream("wr", 8)
        S.stream("out", 2)

        with ExitStack() as st2:
            def sb2(name, shape, dtype):
                return st2.enter_context(nc.sbuf_tensor("b_" + name, shape, dtype))
            xb = [sb2(f"xb{i}", [128, 1024], BF16) for i in range(2)]
            hT = sb2("hT", [128, 8, 512], BF16)
            ya = sb2("ya", [128, 4, 512], BF16)
            ys32 = sb2("ys32", [128, 4, 512], F32)
            gtmp = sb2("gtmp", [128, 512], F32)
            sgm = sb2("sgm", [128, 512], F32)
            yb = sb2("yb", [128, 4, 512], BF16)
            yb2 = sb2("yb2", [128, 4, 512], BF16)
            merged = sb2("merged", [128, 8, 512], BF16)
            gw = [sb2(f"gw{i}", [128, 8, 512], BF16) for i in range(3)]
            wuat = sb2("wuat", [128, 4, 1024], BF16)
            wubt = sb2("wubt", [128, 4, 1024], BF16)
            wglut = sb2("wglut", [128, 4, 512], BF16)
            bglut = sb2("bglut", [128, 4], F32)
            if fused:
                cand = [sb2(f"cand{i}", [128, 4, 512], BF16) for i in range(2)]
                oht = sb2("oht", [128, 4], F32)
                S.stream("cand", 2)
                S.add("sp", lambda e: e.dma_start(out=oht[:], in_=onehot[:, :]), w=["oht"], dma="misc")
            gat = [sb2(f"gat{i}", [128, 512], F32) for i in range(2)]
            gbt = [sb2(f"gbt{i}", [128, 512], F32) for i in range(2)]

            S.add("sp", lambda e: e.dma_start(out=ident32[:], in_=ident[:, :]), w=["ident32"], dma="misc")
            S.add("dve", lambda e: e.tensor_copy(out=identb[:], in_=ident32[:]), r=["ident32"], w=["identb"])
            for i in range(4):
                S.add("sp", lambda e, i=i: e.dma_start(out=lnB[:, i, :], in_=lnp[i:i + 1, :].partition_broadcast(128)),
                      w=["lnB"], dma="misc")
            S.add("sp", lambda e: e.dma_start(out=bglut[:], in_=bglu[:, :]), w=["bglut"], dma="misc")
            S.add("pool", lambda e: e.dma_start(out=wuat[:], in_=wua.rearrange("(c p) n -> p c n", p=128)), w=["wuat"], dma="misc")
            S.add("pool", lambda e: e.dma_start(out=wubt[:], in_=wub.rearrange("(c p) n -> p c n", p=128)), w=["wubt"], dma="misc")
            S.add("pool", lambda e: e.dma_start(out=wglut[:], in_=wglu.rearrange("(c p) n -> p c n", p=128)), w=["wglut"], dma="misc")
            gwk = 0
            for tg in range(NG):
                c0 = tg * 512
                for ti in range(4):
                    tt = tg * 4 + ti
                    hk = ("hres", tt)
                    S.add("sp", lambda e, tt=tt: e.dma_start(out=hres[:, tt, :], in_=x[tt * 128:(tt + 1) * 128, :]),
                          w=[hk], dma="x")
                    _ln_tile(S, hres[:, tt, :], st6, mv, rstd, lnB[:, 0, :], lnB[:, 1, :], hk, "ln")
                    b = tt % 2
                    S.add("act", lambda e, tt=tt, b=b: e.copy(out=xb[b][:], in_=hres[:, tt, :]), r=[hk], w=[("xb", b)])

                    def tr(e, b=b):
                        ins = None
                        for kc in range(8):
                            ins = e.transpose(out=pT[b][:, kc * 128:(kc + 1) * 128], in_=xb[b][:, kc * 128:(kc + 1) * 128],
                                              identity=identb[:])
                        return ins
                    S.add("pe", tr, r=[("xb", b), "identb"], w=[("pT", b)])
                    S.add("act", lambda e, b=b, ti=ti: e.copy(out=hT[:, :, ti * 128:(ti + 1) * 128],
                                                              in_=pT[b][:, :].rearrange("p (k t) -> p k t", k=8)),
                          r=[("pT", b)], w=["hT"])
                if fused:
                    for which, dst, dkey in ((0, ya, "ya"), (1, ys32, "ys32")):
                        for q in range(4):
                            cb = (2 * tg + which + q) % 2
                            src = ygat[q].rearrange("(r w p) t -> p w r t", r=4, w=2, p=128)[:, which, :, c0:c0 + 512]
                            S.add("sp", lambda e, cb=cb, src=src: e.dma_start(out=cand[cb][:], in_=src), w=[("cand", cb)], dma="cand")
                            if q == 0:
                                S.add("dve", lambda e, cb=cb, dst=dst: e.tensor_scalar(out=dst[:], in0=cand[cb][:], scalar1=oht[:, 0:1], scalar2=None,
                                                                                       op0=ALU.mult), r=[("cand", cb), "oht"], w=[dkey])
                            else:
                                S.add("dve", lambda e, cb=cb, dst=dst, q=q: e.scalar_tensor_tensor(
                                    out=dst[:], in0=cand[cb][:], scalar=oht[:, q:q + 1], in1=dst[:], op0=ALU.mult, op1=ALU.add),
                                    r=[("cand", cb), "oht", dkey], w=[dkey])
                else:
                    S.add("pool", lambda e, c0=c0: e.dma_start(out=ya[:], in_=yaT.rearrange("(c p) t -> p c t", p=128)[:, :, c0:c0 + 512]),
                          w=["ya"], dma="yin")
                    S.add("sp", lambda e, c0=c0: e.dma_start(out=ys32[:], in_=ysT.rearrange("(c p) t -> p c t", p=128)[:, :, c0:c0 + 512]),
                          w=["ys32"], dma="yin")
                for c in range(4):
                    S.add("dve", lambda e, c=c: e.tensor_tensor(out=gtmp[:], in0=ys32[:, c, :], in1=ys32[:, c, :], op=ALU.mult), r=["ys32"], w=["gtmp"])
                    S.add("dve", lambda e: e.tensor_scalar(out=gtmp[:], in0=gtmp[:], scalar1=0.044715, scalar2=1.0,
                                                           op0=ALU.mult, op1=ALU.add), r=["gtmp"], w=["gtmp"])
                    S.add("dve", lambda e, c=c: e.tensor_tensor(out=gtmp[:], in0=gtmp[:], in1=ys32[:, c, :], op=ALU.mult), r=["gtmp", "ys32"], w=["gtmp"])
                    S.add("act", lambda e: e.activation(out=sgm[:], in_=gtmp[:], func=AF.Sigmoid, scale=1.5957691216), r=["gtmp"], w=["sgm"])
                    S.add("dve", lambda e, c=c: e.tensor_tensor(out=yb[:, c, :], in0=ys32[:, c, :], in1=sgm[:], op=ALU.mult), r=["ys32", "sgm"], w=["yb"])
                for cc in range(4):
                    pk = cc % 2

                    def mm(e, cc=cc, pk=pk):
                        ins = None
                        for c in range(4):
                            ins = e.matmul(pss[pk][:, :], wglut[:, c, cc * 128:(cc + 1) * 128], yb[:, c, :],
                                           start=(c == 0), stop=(c == 3))
                        return ins
                    S.add("pe", mm, r=["wglut", "yb"], w=[("ps", pk)])
                    S.add("act", lambda e, cc=cc, pk=pk: e.activation(out=sgm[:], in_=pss[pk][:, :], func=AF.Sigmoid,
                                                                      bias=bglut[:, cc:cc + 1], scale=1.0),
                          r=[("ps", pk), "bglut"], w=["sgm"])
                    S.add("dve", lambda e, cc=cc: e.tensor_tensor(out=yb2[:, cc, :], in0=yb[:, cc, :], in1=sgm[:], op=ALU.mult),
                          r=["yb", "sgm"], w=["yb2"])
                for j in range(4):
                    slot = gwk % 3
                    gwk += 1
                    S.add("pool", lambda e, j=j, slot=slot: e.dma_start(
                        out=gw[slot][:], in_=wg.rearrange("(k p) n -> p k n", p=128)[:, :, j * 512:(j + 1) * 512]),
                        w=[("gw", slot)], dma="gw")
                    for dcl in range(2):
                        dc = 2 * j + dcl
                        tb = dc % 2

                        def mmg(e, slot=slot, dcl=dcl, off=0, pk=0):
                            ins = None
                            for kc in range(8):
                                ins = e.matmul(pss[pk][:, :], gw[slot][:, kc, dcl * 256 + off:dcl * 256 + off + 128], hT[:, kc, :],
                                               start=(kc == 0), stop=(kc == 7))
                            return ins
                        S.add("pe", lambda e, f=mmg: f(e, off=0, pk=0), r=[("gw", slot), "hT"], w=[("ps", 0)])
                        S.add("pe", lambda e, f=mmg: f(e, off=128, pk=1), r=[("gw", slot), "hT"], w=[("ps", 1)])

                        def mmu(e, dc=dc, wt=None, src=None, pk=2):
                            ins = None
                            for c in range(4):
                                ins = e.matmul(pss[pk][:, :], wt[:, c, dc * 128:(dc + 1) * 128], src[:, c, :],
                                               start=(c == 0), stop=(c == 3))
                            return ins
                        S.add("pe", lambda e, f=mmu: f(e, wt=wuat, src=ya, pk=2), r=["wuat", "ya"], w=[("ps", 2)])
                        S.add("pe", lambda e, f=mmu: f(e, wt=wubt, src=yb2, pk=3), r=["wubt", "yb2"], w=[("ps", 3)])
                        S.add("act", lambda e, tb=tb: e.activation(out=gat[tb][:], in_=pss[0][:, :], func=AF.Sigmoid),
                              r=[("ps", 0)], w=[("gat", tb)])
                        S.add("act", lambda e, tb=tb: e.activation(out=gbt[tb][:], in_=pss[1][:, :], func=AF.Sigmoid),
                              r=[("ps", 1)], w=[("gbt", tb)])
                        S.add("dve", lambda e, tb=tb: e.tensor_tensor(out=gat[tb][:], in0=gat[tb][:], in1=pss[2][:, :], op=ALU.mult),
                              r=[("gat", tb), ("ps", 2)], w=[("gat", tb)])
                        S.add("dve", lambda e, tb=tb: e.tensor_tensor(out=gbt[tb][:], in0=gbt[tb][:], in1=pss[3][:, :], op=ALU.mult),
                              r=[("gbt", tb), ("ps", 3)], w=[("gbt", tb)])
                        S.add("dve", lambda e, tb=tb, dc=dc: e.tensor_tensor(out=merged[:, dc, :], in0=gat[tb][:], in1=gbt[tb][:], op=ALU.add),
                              r=[("gat", tb), ("gbt", tb)], w=["merged"])
                for half in range(2):
                    slot = gwk % 3
                    gwk += 1
                    S.add("pool", lambda e, half=half, slot=slot: e.dma_start(
                        out=gw[slot][:], in_=wo.rearrange("(k p) n -> p k n", p=128)[:, :, half * 512:(half + 1) * 512]),
                        w=[("gw", slot)], dma="gw")
                    for ti in range(4):
                        tt = tg * 4 + ti
                        pk = 4 + (ti % 2)

                        def mmo(e, slot=slot, ti=ti, pk=pk):
                            ins = None
                            for kc in range(8):
                                ins = e.matmul(pss[pk][:, :], merged[:, kc, ti * 128:(ti + 1) * 128], gw[slot][:, kc, :],
                                               start=(kc == 0), stop=(kc == 7))
                            return ins
                        S.add("pe", mmo, r=["merged", ("gw", slot)], w=[("ps", pk)])
                        S.add("dve", lambda e, tt=tt, half=half, pk=pk: e.scalar_tensor_tensor(
                            out=hres[:, tt, half * 512:(half + 1) * 512], in0=hres[:, tt, half * 512:(half + 1) * 512],
                            scalar=DN_ALPHA, in1=pss[pk][:, :], op0=ALU.mult, op1=ALU.add),
                            r=[("hres", tt), ("ps", pk)], w=[("hres", tt)])
                for ti in range(4):
                    tt = tg * 4 + ti
                    _ln_tile(S, hres[:, tt, :], st6, mv, rstd, lnB[:, 2, :], lnB[:, 3, :], ("hres", tt), "ln")
            S.emit()

        x1T = sb("x1T", [128, 8, NTOK], BF16)
        with ExitStack() as st2:
            def sb2(name, shape, dtype):
                return st2.enter_context(nc.sbuf_tensor("b_" + name, shape, dtype))
            xb = [sb2(f"xc{i}", [128, 1024], BF16) for i in range(2)]
            wrt = sb2("wrt", [128, 8, 32], BF16)
            brB = sb2("brB", [128, 32], F32)
            bdn = sb2("bdn", [N_EXPERTS, 1024], BF16)
            lg = sb2("lg", [128, 32], F32)
            ex = sb2("ex", [128, 32], F32)
            msk = sb2("msk", [128, 32], F32)
            m8 = sb2("m8", [128, 8], F32)
            negm = sb2("negm", [128, 1], F32)
            ssum = sb2("ssum", [128, 1], F32)
            combTb = sb2("combTb", [32, 128], BF16)
            S.add("pool", lambda e: e.dma_start(out=wrt[:], in_=wr.rearrange("(k p) n -> p k n", p=128)), w=["wrt"], dma="misc")
            S.add("pool", lambda e: e.dma_start(out=bdn[:], in_=bdown[:, :]), w=["bdn"], dma="misc")
            S.add("sp", lambda e: e.dma_start(out=brB[:], in_=br[0:1, :].partition_broadcast(128)), w=["brB"], dma="misc")
            for tt in range(NT):
                b = tt % 2
                hk = ("hres", tt)
                S.add("act", lambda e, tt=tt, b=b: e.copy(out=xb[b][:], in_=hres[:, tt, :]), r=[hk], w=[("xb", b)])

                def tr(e, b=b):
                    ins = None
                    for kc in range(8):
                        ins = e.transpose(out=pT[b][:, kc * 128:(kc + 1) * 128], in_=xb[b][:, kc * 128:(kc + 1) * 128],
                                          identity=identb[:])
                    return ins
                S.add("pe", tr, r=[("xb", b)], w=[("pT", b)])
                S.add("act", lambda e, b=b, tt=tt: e.copy(out=x1T[:, :, tt * 128:(tt + 1) * 128],
                                                          in_=pT[b][:, :].rearrange("p (k t) -> p k t", k=8)),
                      r=[("pT", b)], w=[("x1T", tt)])

                def mmr(e, tt=tt):
                    ins = None
                    for kc in range(8):
                        ins = e.matmul(pss[0][:, 0:32], x1T[:, kc, tt * 128:(tt + 1) * 128], wrt[:, kc, :],
                                       start=(kc == 0), stop=(kc == 7))
                    return ins
                S.add("pe", mmr, r=[("x1T", tt), "wrt"], w=[("ps", 0)])
                S.add("dve", lambda e: e.tensor_tensor(out=lg[:], in0=pss[0][:, 0:32], in1=brB[:], op=ALU.add),
                      r=[("ps", 0), "brB"], w=["lg"])
                S.add("dve", lambda e: e.max(out=m8[:], in_=lg[:]), r=["lg"], w=["m8"])
                S.add("dve", lambda e: e.tensor_scalar(out=msk[:], in0=lg[:], scalar1=m8[:, 3:4], scalar2=None, op0=ALU.is_ge),
                      r=["lg", "m8"], w=["msk"])
                S.add("dve", lambda e: e.tensor_scalar(out=negm[:], in0=m8[:, 0:1], scalar1=-1.0, scalar2=None, op0=ALU.mult),
                      r=["m8"], w=["negm"])
                S.add("act", lambda e: e.activation(out=ex[:], in_=lg[:], func=AF.Exp, bias=negm[:, 0:1], scale=1.0),
                      r=["lg", "negm"], w=["ex"])
                S.add("dve", lambda e: e.tensor_tensor(out=ex[:], in0=ex[:], in1=msk[:], op=ALU.mult), r=["ex", "msk"], w=["ex"])
                S.add("dve", lambda e: e.reduce_sum(out=ssum[:], in_=ex[:], axis=mybir.AxisListType.X), r=["ex"], w=["ssum"])
                S.add("dve", lambda e: e.reciprocal(out=ssum[:], in_=ssum[:]), r=["ssum"], w=["ssum"])
                S.add("dve", lambda e, tt=tt: e.tensor_scalar(out=comb[:, tt, :], in0=ex[:], scalar1=ssum[:, 0:1], scalar2=None, op0=ALU.mult),
                      r=["ex", "ssum"], w=[("comb", tt)])
                S.add("pe", lambda e, tt=tt: e.transpose(out=pss[1][0:32, 0:128], in_=comb[:, tt, :], identity=ident32[:]),
                      r=[("comb", tt)], w=[("ps", 1)])
                S.add("act", lambda e: e.copy(out=combTb[:], in_=pss[1][0:32, 0:128]), r=[("ps", 1)], w=["combTb"])
                for half in range(2):
                    pk = 2 + half
                    S.add("pe", lambda e, half=half, pk=pk: e.matmul(pss[pk][:, :], combTb[:, :], bdn[:, half * 512:(half + 1) * 512],
                                                                     start=True, stop=True),
                          r=["combTb", "bdn"], w=[("ps", pk)])
                    S.add("dve", lambda e, tt=tt, half=half, pk=pk: e.scalar_tensor_tensor(
                        out=hres[:, tt, half * 512:(half + 1) * 512], in0=hres[:, tt, half * 512:(half + 1) * 512],
                        scalar=DN_ALPHA, in1=pss[pk][:, :], op0=ALU.mult, op1=ALU.add),
                        r=[hk, ("ps", pk)], w=[hk])
            S.emit()

        with ExitStack() as st2:
            def sb2(name, shape, dtype):
                return st2.enter_context(nc.sbuf_tensor("b_" + name, shape, dtype))
            R = 8
            ring = [sb2(f"ring{i}", [128, 8, 256], BF16) for i in range(R)]
            actT = sb2("actT", [128, 8, NTOK], BF16)
            bgut = sb2("bgut", [128, 2 * NE * 8], F32)
            gs = [sb2(f"gs{i}", [128, 512], F32) for i in range(2)]
            sg = [sb2(f"sg{i}", [128, 512], F32) for i in range(2)]
            uu = [sb2(f"uu{i}", [128, 512], F32) for i in range(2)]
            S.add("sp", lambda e: e.dma_start(out=bgut[:], in_=bgu[:, :]), w=["bgut"], dma="misc")
            rk = 0

            def load(wsrc, e_, q):
                nonlocal rk
                slot = rk % R
                rk += 1
                S.add("pool", lambda e, slot=slot: e.dma_start(
                    out=ring[slot][:], in_=wsrc[e_].rearrange("(k p) n -> p k n", p=128)[:, :, q * 256:(q + 1) * 256]),
                    w=[("ring", slot)], dma="wr")
                return slot
            tcount = 0
            for ex_ in range(NE):
                for q in range(4):
                    sg_ = load(wgate, ex_, q)
                    su_ = load(wup, ex_, q)
                    for fcl in range(2):
                        fc = 2 * q + fcl
                        bgcol = (0 * NE + ex_) * 8 + fc
                        bucol = (1 * NE + ex_) * 8 + fc
                        for tg in range(NG):
                            tb = tcount % 2
                            tcount += 1
                            pg, pu = tb, 2 + tb

                            def mmgu(e, slot=None, pk=None, fcl=fcl, tg=tg):
                                ins = None
                                for kc in range(8):
                                    ins = e.matmul(pss[pk][:, :], ring[slot][:, kc, fcl * 128:(fcl + 1) * 128],
                                                   x1T[:, kc, tg * 512:(tg + 1) * 512], start=(kc == 0), stop=(kc == 7))
                                return ins
                            S.add("pe", lambda e, f=mmgu, s=sg_, pk=pg: f(e, slot=s, pk=pk), r=[("ring", sg_)], w=[("ps", pg)])
                            S.add("pe", lambda e, f=mmgu, s=su_, pk=pu: f(e, slot=s, pk=pk), r=[("ring", su_)], w=[("ps", pu)])
                            S.add("dve", lambda e, tb=tb, pg=pg, c=bgcol: e.tensor_scalar(
                                out=gs[tb][:], in0=pss[pg][:, :], scalar1=bgut[:, c:c + 1], scalar2=SW_LIMIT, op0=ALU.add, op1=ALU.min),
                                r=[("ps", pg), "bgut"], w=[("gs", tb)])
                            S.add("act", lambda e, tb=tb: e.activation(out=sg[tb][:], in_=gs[tb][:], func=AF.Sigmoid, scale=SW_ALPHA),
                                  r=[("gs", tb)], w=[("sg", tb)])
                            S.add("dve", lambda e, tb=tb, pu=pu, c=bucol: e.tensor_scalar(
                                out=uu[tb][:], in0=pss[pu][:, :], scalar1=bgut[:, c:c + 1], scalar2=SW_LIMIT, op0=ALU.add, op1=ALU.min),
                                r=[("ps", pu), "bgut"], w=[("uu", tb)])
                            S.add("dve", lambda e, tb=tb: e.tensor_scalar(
                                out=uu[tb][:], in0=uu[tb][:], scalar1=-SW_LIMIT, scalar2=1.0, op0=ALU.max, op1=ALU.add),
                                r=[("uu", tb)], w=[("uu", tb)])
                            S.add("dve", lambda e, tb=tb: e.tensor_tensor(out=gs[tb][:], in0=gs[tb][:], in1=sg[tb][:], op=ALU.mult),
                                  r=[("gs", tb), ("sg", tb)], w=[("gs", tb)])
                            S.add("dve", lambda e, tb=tb, fc=fc, tg=tg: e.tensor_tensor(
                                out=actT[:, fc, tg * 512:(tg + 1) * 512], in0=gs[tb][:], in1=uu[tb][:], op=ALU.mult),
                                r=[("gs", tb), ("uu", tb)], w=[("actT", tg)])
                for q in range(4):
                    sd_ = load(wdown, ex_, q)
                    for tt in range(NT):
                        pk = 4 + (tt % 2)

                        def mmd(e, slot=sd_, tt=tt, pk=pk):
                            ins = None
                            for fc in range(8):
                                ins = e.matmul(pss[pk][:, 0:256], actT[:, fc, tt * 128:(tt + 1) * 128], ring[slot][:, fc, :],
                                               start=(fc == 0), stop=(fc == 7))
                            return ins
                        S.add("pe", mmd, r=[("ring", sd_), ("actT", tt // 4)], w=[("ps", pk)])
                        S.add("dve", lambda e, tt=tt, q=q, pk=pk, ex_=ex_: e.scalar_tensor_tensor(
                            out=hres[:, tt, q * 256:(q + 1) * 256], in0=pss[pk][:, 0:256], scalar=comb[:, tt, ex_:ex_ + 1],
                            in1=hres[:, tt, q * 256:(q + 1) * 256], op0=ALU.mult, op1=ALU.add),
                            r=[("ps", pk), ("hres", tt)], w=[("hres", tt)])
            S.emit()

        for i in range(2):
            S.add("sp", lambda e, i=i: e.dma_start(out=lnB[:, i, :], in_=lnp[4 + i:5 + i, :].partition_broadcast(128)),
                  w=["lnB"], dma="misc")
        for tt in range(NT):
            _ln_tile(S, hres[:, tt, :], st6, mv, rstd, lnB[:, 0, :], lnB[:, 1, :], ("hres", tt), "ln")
            S.add("sp", lambda e, tt=tt: e.dma_start(out=out[tt * 128:(tt + 1) * 128, :], in_=hres[:, tt, :]),
                  r=[("hres", tt)], dma="out")
        S.emit()
    return nc


def build_p1(NX=8192, do_attn=True, do_ssm=True, dbg=(), ctx=None):
    NXT = NX // 128
    NQG = NX // 512
    NTOKS = N_META + NX
    NCH = NTOKS // 8
    NKT = NXT + 1
    fused = ctx is not None
    nc = ctx["nc"] if fused else bass.Bass("TRN2", target_bir_lowering=False)
    ODT = BF16 if fused else F32
    dbgs = set(dbg) if isinstance(dbg, (tuple, list, set)) else {dbg}

    def din(name, shape, dtype=F32):
        return nc.dram_tensor(name, shape, dtype, kind="ExternalInput").ap()
    x = din("x", [NX, 1024])
    meta = din("meta", [N_META, 1024])
    lnp = din("lnp", [128, 16])
    lnrow = din("lnrow", [2, 1024])
    w1 = din("w1", [1024, 514])
    bfn = din("bf", [2, 1])
    lamp = din("lamp", [128, 16])
    logdt = din("logdt", [1, 8])
    bstk_d = din("bstk", [128, 128])
    cstk_d = din("cstk", [128, 128])
    dvec_d = din("dvec", [128, 1])
    cst = din("cst", [128, 128 * 4 + 8 * 128 + 4])
    if fused:
        yatt = yssm = None
    else:
        yatt = nc.dram_tensor("yatt", [128, NX], F32, kind="ExternalOutput").ap()
        yssm = nc.dram_tensor("yssm", [128, NX], F32, kind="ExternalOutput").ap()
    NC_ = 128 * 4 + 8 * 128 + 4
    CH = NX // 4

    def ydst(which, r0, r1, t0, n):
        if fused:
            k = t0 // CH
            assert (t0 + n - 1) // CH == k
            return ctx["ybuf"][k][which * 128 + r0:which * 128 + r1, t0 - k * CH:t0 - k * CH + n]
        return (yatt if which == 0 else yssm)[r0:r1, t0:t0 + n]

    with ExitStack() as stack:
        S = ctx["S"] if fused else Sched(nc, stack)

        def sb(name, shape, dtype):
            return stack.enter_context(nc.sbuf_tensor(name, shape, dtype))

        def psum(name, shape, dtype):
            return stack.enter_context(nc.psum_tensor(name, shape, dtype))
        QT = [sb(f"QT{h}", [65, NX], BF16) for h in range(2)]
        KT = [sb(f"KT{h}", [65, NTOKS], BF16) for h in range(2)]
        VV = [sb(f"VV{h}", [128, NKT, 128], BF16) for h in range(2)]
        uT = sb("uT", [128, NTOKS], BF16)
        NFcol = sb("NFcol", [128, NKT, 2], F32)
        cst32 = sb("cst32", [128, NC_], F32)
        identb = sb("identb", [128, 128], BF16)
        trib = sb("trib", [128, 128], BF16)
        ident32 = cst32[:, 0:128]
        Jm = cst32[:, 128:256]
        tri32 = cst32[:, 256:384]
        bmask = cst32[:, 512:512 + 1024]
        sgnR = cst32[:, 1536:1537]
        pss = [psum(f"ps{i}", [128, 512], F32) for i in range(6)]
        S.stream("x", 2)
        S.stream("misc", 0)
        S.stream("row", 4)
        S.stream("out_s", 1)
        S.stream("out", 2)
        S.stream("rl", 2)

        with ExitStack() as st2:
            def sb2(name, shape, dtype):
                return st2.enter_context(nc.sbuf_tensor(name, shape, dtype))
            pT = [st2.enter_context(nc.psum_tensor(f"pT{i}", [128, 1024], BF16)) for i in range(2)]
            w1t = sb2("w1t", [128, 8, 576], BF16)
            lnpt = sb2("lnpt", [128, 16], F32)
            lnB = sb2("lnB1", [128, 2, 1024], F32)
            xt = [sb2(f"xt{i}", [128, 1024], F32) for i in range(2)]
            xn = [sb2(f"xn{i}", [128, 1024], BF16) for i in range(2)]
            hT = sb2("hT", [128, 8, 512], BF16)
            st6 = sb2("st6", [128, 2, 6], F32)
            mv = sb2("mv", [128, 2], F32)
            rstd = sb2("rstd", [128, 1], F32)
            negbf = sb2("negbf", [2, 1], F32)
            e1 = sb2("e1", [2, 512], F32)
            nfg = sb2("nfg", [2, 512], F32)
            carry = sb2("carry", [2, 1], F32)
            ones2 = sb2("ones2", [2, 512], F32)
            fb = sb2("fb", [2, 512], BF16)

            S.add("sp", lambda e: e.dma_start(out=cst32[:], in_=cst[:, :]), w=["cst"], dma="misc")
            S.add("sp", lambda e: e.dma_start(out=lnpt[:], in_=lnp[:, :]), w=["lnpt"], dma="misc")
            for i in range(2):
                S.add("sp", lambda e, i=i: e.dma_start(out=lnB[:, i, :], in_=lnrow[i:i + 1, :].partition_broadcast(128)), w=["lnB"], dma="misc")
            S.add("sp", lambda e: e.dma_start(out=negbf[:], in_=bfn[:, :]), w=["negbf"], dma="misc")
            S.add("dve", lambda e: e.memset(w1t[:, :, 512:576], 0.0), w=["w1t"])
            S.add("pool", lambda e: e.dma_start(out=w1t[:, :, 0:514], in_=w1.rearrange("(k p) n -> p k n", p=128)), w=["w1t"], dma="misc")
            S.add("dve", lambda e: e.tensor_copy(out=identb[:], in_=ident32), r=["cst"], w=["identb"])
            S.add("dve", lambda e: e.tensor_copy(out=trib[:], in_=tri32), r=["cst"], w=["trib"])
            S.add("dve", lambda e: e.tensor_scalar(out=negbf[:], in0=negbf[:], scalar1=-1.0, scalar2=None, op0=ALU.mult),
                  r=["negbf"], w=["negbf"])
            S.add("dve", lambda e: e.memset(ones2[:], 1.0), w=["ones2"])
            S.add("dve", lambda e: e.memset(carry[:], 0.0), w=["carry"])
            for h in range(2):
                S.add("dve", lambda e, h=h: e.memset(KT[h][64:65, :], 1.0), w=[("KTa", h)])
                S.add("pool", lambda e, h=h: e.memset(VV[h][:, :, 64:128], 1.0), w=[("VVo", h)])

            for g in range(NQG + 1):
                n = N_META if g == 0 else 512
                ntile = 1 if g == 0 else 4
                kc0 = 0 if g == 0 else N_META + (g - 1) * 512
                qc0 = (g - 1) * 512
                for ti in range(ntile):
                    np_ = N_META if g == 0 else 128
                    b = (g * 4 + ti) % 2
                    if g == 0:
                        S.add("sp", lambda e, b=b: e.dma_start(out=xt[b][0:N_META, :], in_=meta[:, :]), w=[("xt", b)], dma="x")
                    else:
                        r0 = (g - 1) * 512 + ti * 128
                        S.add("sp", lambda e, b=b, r0=r0: e.dma_start(out=xt[b][:, :], in_=x[r0:r0 + 128, :]), w=[("xt", b)], dma="x")
                    xa = xt[b][0:np_, :]
                    S.add("dve", lambda e, xa=xa, np_=np_: e.bn_stats(out=st6[0:np_, 0, :], in_=xa[:, 0:512]), r=[("xt", b)], w=["st0"])
                    S.add("dve", lambda e, xa=xa, np_=np_: e.bn_stats(out=st6[0:np_, 1, :], in_=xa[:, 512:1024]), r=[("xt", b)], w=["st1"])
                    S.add("dve", lambda e, np_=np_: e.bn_aggr(out=mv[0:np_, :], in_=st6[0:np_, :, :]), r=["st0", "st1"], w=["mv"], fence=True)
                    S.add("dve", lambda e, np_=np_: e.tensor_scalar(out=rstd[0:np_, :], in0=mv[0:np_, 1:2], scalar1=LN_EPS, scalar2=None,
                                                                    op0=ALU.add), r=["mv"], w=["rs"], fence=True)
                    S.add("act", lambda e, np_=np_: e.sqrt(out=rstd[0:np_, :], in_=rstd[0:np_, :]), r=["rs"], w=["rs"], fence=True)
                    S.add("dve", lambda e, np_=np_: e.reciprocal(out=rstd[0:np_, :], in_=rstd[0:np_, :]), r=["rs"], w=["rs"], fence=True)
                    S.add("dve", lambda e, xa=xa, np_=np_: e.scalar_tensor_tensor(
                        out=xa, in0=xa, scalar=mv[0:np_, 0:1], in1=lnB[0:np_, 0, :], op0=ALU.subtract, op1=ALU.mult),
                        r=[("xt", b), "mv", "lnB"], w=[("xt", b)])
                    S.add("dve", lambda e, xa=xa, b=b, np_=np_: e.scalar_tensor_tensor(
                        out=xn[b][0:np_, :], in0=xa, scalar=rstd[0:np_, 0:1], in1=lnB[0:np_, 1, :], op0=ALU.mult, op1=ALU.add),
                        r=[("xt", b), "rs", "lnB"], w=[("xn", b)])

                    def tr(e, b=b, np_=np_):
                        ins = None
                        for kc in range(8):
                            ins = e.transpose(out=pT[b][:, kc * 128:kc * 128 + np_], in_=xn[b][0:np_, kc * 128:(kc + 1) * 128],
                                              identity=identb[0:np_, 0:np_])
                        return ins
                    S.add("pe", tr, r=[("xn", b), "identb"], w=[("pT", b)])
                    S.add("act", lambda e, b=b, ti=ti, np_=np_: e.copy(
                        out=hT[:, :, ti * 128:ti * 128 + np_], in_=pT[b][:, :].rearrange("p (k t) -> p k t", k=8)[:, :, 0:np_]),
                        r=[("pT", b)], w=["hT"])

                if 8 in dbgs:
                    S.emit()
                def proj(e, c_lo, c_hi, pk, n=n):
                    ins = None
                    for kc in range(8):
                        ins = e.matmul(pss[pk][0:c_hi - c_lo, 0:n], w1t[:, kc, c_lo:c_hi], hT[:, kc, 0:n],
                                       start=(kc == 0), stop=(kc == 7))
                    return ins
                for h in range(2):
                    if g > 0:
                        S.add("pe", lambda e, f=proj, h=h: f(e, h * 64, h * 64 + 64, h), r=["w1t", "hT"], w=[("ps", h)])
                        S.add("act", lambda e, h=h, qc0=qc0: e.mul(out=QT[h][0:64, qc0:qc0 + 512], in_=pss[h][0:64, :], mul=0.125),
                              r=[("ps", h)], w=[("QT", h)])
                    S.add("pe", lambda e, f=proj, h=h: f(e, 128 + h * 64, 128 + h * 64 + 64, 2 + h), r=["w1t", "hT"], w=[("ps", 2 + h)])
                    S.add("dve", lambda e, h=h, kc0=kc0, n=n: e.tensor_copy(out=KT[h][0:64, kc0:kc0 + n], in_=pss[2 + h][0:64, 0:n]),
                          r=[("ps", 2 + h)], w=[("KT", h)])
                S.add("pe", lambda e, f=proj: f(e, 384, 512, 4), r=["w1t", "hT"], w=[("ps", 4)])
                S.add("act", lambda e, kc0=kc0, n=n: e.copy(out=uT[:, kc0:kc0 + n], in_=pss[4][:, 0:n]), r=[("ps", 4)], w=["uT"])
                if 1 in dbgs:
                    continue
                S.add("pe", lambda e, f=proj: f(e, 512, 576, 5), r=["w1t", "hT"], w=[("ps", 5)])
                S.add("act", lambda e, n=n: e.activation(out=e1[:, 0:n], in_=pss[5][0:2, 0:n], func=AF.Exp, bias=negbf[:, 0:1], scale=-1.0),
                      r=[("ps", 5), "negbf"], w=["e1"])
                if 11 in dbgs:
                    continue
                S.add("dve", lambda e, n=n: e.tensor_scalar(out=e1[:, 0:n], in0=e1[:, 0:n], scalar1=1.0, scalar2=None, op0=ALU.add),
                      r=["e1"], w=["e1"])
                S.add("act", lambda e, n=n: e.activation(out=e1[:, 0:n], in_=e1[:, 0:n], func=AF.Ln), r=["e1"], w=["e1"])
                if 12 in dbgs:
                    continue
                S.add("dve", lambda e, n=n: e.tensor_tensor_scan(out=nfg[:, 0:n], data0=ones2[:, 0:n], data1=e1[:, 0:n], initial=carry[:, 0:1],
                                                                 op0=ALU.mult, op1=ALU.add), r=["e1", "carry", "ones2"], w=["nfg"])
                if 13 in dbgs:
                    continue
                S.add("dve", lambda e, n=n: e.tensor_copy(out=carry[:], in_=nfg[:, n - 1:n]), r=["nfg"], w=["carry"])
                if 2 in dbgs:
                    continue
                if g > 0:
                    S.add("dve", lambda e: e.tensor_scalar(out=fb[:], in0=nfg[:], scalar1=-1.0, scalar2=None, op0=ALU.mult),
                          r=["nfg"], w=["fb"])
                    for h in range(2):
                        S.add("sp", lambda e, h=h, qc0=qc0: e.dma_start(out=QT[h][64:65, qc0:qc0 + 512], in_=fb[h:h + 1, :]),
                              r=["fb"], w=[("QTa", h)], dma="row")
                if 3 in dbgs:
                    continue
                for ti in range(ntile):
                    np_ = N_META if g == 0 else 128
                    kt = 0 if g == 0 else 1 + (g - 1) * 4 + ti
                    S.add("pe", lambda e, ti=ti, np_=np_: e.transpose(out=pss[5][0:np_, 256 + 2 * ti:258 + 2 * ti],
                                                                      in_=nfg[0:2, ti * 128:ti * 128 + np_], identity=ident32[0:2, 0:2]),
                          r=["nfg"], w=[("ps", 5)])
                    S.add("dve", lambda e, ti=ti, np_=np_, kt=kt: e.tensor_copy(out=NFcol[0:np_, kt, :], in_=pss[5][0:np_, 256 + 2 * ti:258 + 2 * ti]),
                          r=[("ps", 5)], w=["NFcol"])
                if 4 in dbgs:
                    continue
                for ti in range(ntile):
                    np_ = N_META if g == 0 else 128
                    kt = 0 if g == 0 else 1 + (g - 1) * 4 + ti
                    pk = ti % 2

                    def mmv(e, ti=ti, np_=np_, pk=pk):
                        ins = None
                        for kc in range(8):
                            ins = e.matmul(pss[pk][0:np_, 0:128], hT[:, kc, ti * 128:ti * 128 + np_], w1t[:, kc, 256:384],
                                           start=(kc == 0), stop=(kc == 7))
                        return ins
                    S.add("pe", mmv, r=["w1t", "hT"], w=[("ps", pk)])
                    if 5 in dbgs:
                        continue
                    S.add("act", lambda e, np_=np_, kt=kt, pk=pk: e.copy(out=VV[0][0:np_, kt, 0:64], in_=pss[pk][0:np_, 0:64]),
                          r=[("ps", pk), ("VVo", 0)], w=[("VV", 0)])
                    if 6 in dbgs:
                        continue
                    S.add("act", lambda e, np_=np_, kt=kt, pk=pk: e.copy(out=VV[1][0:np_, kt, 0:64], in_=pss[pk][0:np_, 64:128]),
                          r=[("ps", pk), ("VVo", 1)], w=[("VV", 1)])
            if 9 in dbgs:
                d1 = nc.dram_tensor("d_nf", [128, NKT * 2], F32, kind="ExternalOutput").ap()
                d2 = nc.dram_tensor("d_qt", [65, NX], BF16, kind="ExternalOutput").ap()
                d3 = nc.dram_tensor("d_kt", [65, NTOKS], BF16, kind="ExternalOutput").ap()
                d4 = nc.dram_tensor("d_vv", [128, NKT * 128], BF16, kind="ExternalOutput").ap()
                d5 = nc.dram_tensor("d_ut", [128, NTOKS], BF16, kind="ExternalOutput").ap()
                S.add("sp", lambda e: e.dma_start(out=d1[:, :], in_=NFcol[:, :, :].rearrange("p a b -> p (a b)")), r=["NFcol"], dma="out")
                S.add("sp", lambda e: e.dma_start(out=d2[:, :], in_=QT[0][:, :]), r=[("QT", 0), ("QTa", 0)], dma="out")
                S.add("sp", lambda e: e.dma_start(out=d3[:, :], in_=KT[0][:, :]), r=[("KT", 0), ("KTa", 0)], dma="out")
                S.add("sp", lambda e: e.dma_start(out=d4[:, :], in_=VV[0][:, :, :].rearrange("p a b -> p (a b)")), r=[("VV", 0), ("VVo", 0)], dma="out")
                S.add("sp", lambda e: e.dma_start(out=d5[:, :], in_=uT[:, :]), r=["uT"], dma="out")
                d6 = nc.dram_tensor("d_xt", [128, 1024], F32, kind="ExternalOutput").ap()
                d7 = nc.dram_tensor("d_xn", [128, 1024], BF16, kind="ExternalOutput").ap()
                d8 = nc.dram_tensor("d_ht", [128, 8 * 512], BF16, kind="ExternalOutput").ap()
                d9 = nc.dram_tensor("d_mv", [128, 3], F32, kind="ExternalOutput").ap()
                S.add("sp", lambda e: e.dma_start(out=d6[:, :], in_=xt[1][:, :]), r=[("xt", 1)], dma="out")
                S.add("sp", lambda e: e.dma_start(out=d7[:, :], in_=xn[1][:, :]), r=[("xn", 1)], dma="out")
                S.add("sp", lambda e: e.dma_start(out=d8[:, :], in_=hT[:, :, :].rearrange("p a b -> p (a b)")), r=["hT"], dma="out")
                S.add("sp", lambda e: e.dma_start(out=d9[:, 0:2], in_=mv[:, :], allow_slow_non_contiguous=True), r=["mv"], dma="out")
                S.add("sp", lambda e: e.dma_start(out=d9[:, 2:3], in_=rstd[:, :], allow_slow_non_contiguous=True), r=["rs"], dma="out")
            S.emit()

        merged = bool(fused and do_ssm and do_attn and MERGE_SSM_ATTN)
        if do_ssm and not merged:
            _ssm_block(nc, S, stack, locals())
        if do_attn:
            with ExitStack() as st2:
                def sb2(name, shape, dtype):
                    return st2.enter_context(nc.sbuf_tensor(name, shape, dtype))
                if merged:
                    psx = [st2.enter_context(nc.psum_tensor(f"psx{i}", [128, 512], F32)) for i in range(2)]
                    S.rec_begin()
                    _ssm_block(nc, S, stack, dict(locals(), st_ext=st2, ssm_merged=True))
                    recA = S.rec_end()
                    S.rec_begin()
                    SB = [(pss[1], ("ps", 1)), (psx[0], ("psx", 0))]
                    AB = [(psx[1], ("psx", 1))]
                else:
                    SB = [(pss[0], ("ps", 0)), (pss[1], ("ps", 1))]
                    AB = [(pss[4], ("ps", 4)), (pss[5], ("ps", 5))]
                coll_deferred = []
                NPT = 3
                pt = [sb2(f"pt{i}", [128, 512], BF16) for i in range(NPT)]
                rl = [sb2(f"rl{i}", [128, 512], F32) for i in range(2)]
                rl2 = [sb2(f"rlb{i}", [64, 512], F32) for i in range(2)]
                ot = [sb2(f"ot{i}", [64, 512], ODT) for i in range(2)]
                it = 0
                gi = 0
                for i in range(NQG):
                    for h in range(2):
                        ab = gi % len(AB)
                        ob = gi % 2
                        gi += 1
                        nblk = 4 * i + 5
                        blks = []
                        for jb in range(nblk):
                            nk = N_META if jb == 0 else 128
                            kcol = 0 if jb == 0 else N_META + (jb - 1) * 128
                            jt = jb - 1
                            diag = jb > 0 and jt >= 4 * i
                            qs = (jt - 4 * i) * 128 if diag else 0
                            blks.append(dict(jb=jb, nk=nk, kcol=kcol, qs=qs, diag=diag, sbk=it % 2, pb=it % NPT))
                            it += 1

                        def emit_s(bk, h=h, i=i):
                            nk, kcol, qs, sbk, pb, jb = bk["nk"], bk["kcol"], bk["qs"], bk["sbk"], bk["pb"], bk["jb"]
                            S.add("pe", lambda e: e.matmul(
                                SB[sbk][0][0:nk, qs:512], KT[h][0:65, kcol:kcol + nk], QT[h][0:65, i * 512 + qs:(i + 1) * 512],
                                start=True, stop=True), w=[SB[sbk][1]])
                            S.add("act", lambda e: e.activation(
                                out=pt[pb][0:nk, qs:512], in_=SB[sbk][0][0:nk, qs:512], func=AF.Exp,
                                bias=NFcol[0:nk, jb, h:h + 1], scale=1.0), r=[SB[sbk][1]], w=[("pt", pb)])
                            if bk["diag"]:
                                S.add("dve", lambda e: e.tensor_tensor(
                                    out=pt[pb][:, qs:qs + 128], in0=pt[pb][:, qs:qs + 128], in1=trib[:], op=ALU.mult),
                                    r=[("pt", pb)], w=[("pt", pb)])

                        def emit_pv(bk, h=h, ab=ab, nblk=nblk):
                            nk, qs, pb, jb = bk["nk"], bk["qs"], bk["pb"], bk["jb"]
                            S.add("pe", lambda e: e.matmul(
                                AB[ab][0][:, qs:512], VV[h][0:nk, jb, :], pt[pb][0:nk, qs:512],
                                start=(jb == 0), stop=(jb == nblk - 1)), r=[("pt", pb)], w=[AB[ab][1]])
                        emit_s(blks[0])
                        for jb in range(nblk):
                            if jb + 1 < nblk:
                                emit_s(blks[jb + 1])
                            emit_pv(blks[jb])
                        S.add("dve", lambda e, ab=ab, ob=ob: e.reciprocal(out=rl[ob][64:128, :], in_=AB[ab][0][64:128, :]),
                              r=[AB[ab][1]], w=[("rl", ob)])
                        S.add("sp", lambda e, ob=ob: e.dma_start(out=rl2[ob][:, :], in_=rl[ob][64:128, :]),
                              r=[("rl", ob)], w=[("rl2", ob)], dma="rl")
                        S.add("dve", lambda e, ab=ab, ob=ob: e.tensor_tensor(out=ot[ob][:, :], in0=AB[ab][0][0:64, :], in1=rl2[ob][:, :], op=ALU.mult),
                              r=[AB[ab][1], ("rl2", ob)], w=[("ot", ob)])
                        kch_ = (i * 512) // CH
                        S.add("sp", lambda e, h=h, i=i, ob=ob: e.dma_start(out=ydst(0, h * 64, (h + 1) * 64, i * 512, 512), in_=ot[ob][:, :]),
                              r=[("ot", ob)], w=[("ych", kch_, h, i)], dma="out")
                        if fused and h == 1 and ((i + 1) * 512) % CH == 0:
                            deps = [("ych", kch_, hh, ii) for hh in range(2) for ii in range(NQG) if (ii * 512) // CH == kch_]
                            deps.append(("ychs", kch_))

                            def coll(k=kch_, deps=deps):
                                S.add("pool", lambda e: e.collective_compute(
                                    "AllGather", ALU.bypass, replica_groups=[[0, 1, 2, 3], [4, 5, 6, 7]],
                                    ins=[ctx["ybuf_t"][k].ap().opt()], outs=[ctx["ygat_t"][k].ap().opt()]), r=deps, w=[("ygat", k)])
                                S.add("pool", lambda e: e.memset(S.fz["pool"][:, 1:2], 0.0), r=[("ygat", k)], w=["fzp"])
                            if merged:
                                coll_deferred.append(coll)
                            else:
                                coll()
                if merged:
                    recB = S.rec_end()
                    S.replay(recA, recB)
                    for c_ in coll_deferred:
                        c_()
                S.emit()
    return nc


def _ssm_block(nc, S, stack, L):
    uT, pss, cst32 = L["uT"], L["pss"], L["cst32"]
    lamp, logdt, bstk_d, cstk_d, dvec_d, yssm = L["lamp"], L["logdt"], L["bstk_d"], L["cstk_d"], L["dvec_d"], L["yssm"]
    NX, NCH, NTOKS = L["NX"], L["NCH"], L["NTOKS"]
    ident32 = cst32[:, 0:128]
    Jm = cst32[:, 128:256]
    bmask = cst32[:, 512:512 + 1024]
    sgnR = cst32[:, 1536:1537]
    NLEV = max(1, (NCH - 1).bit_length())
    POW_SEQ = list(range(9))
    NPW = 9 + NLEV
    PI = math.pi
    ssm_merged = bool(L.get("ssm_merged"))
    with ExitStack() as st2:
        def sb2(name, shape, dtype):
            return (L.get("st_ext") or st2).enter_context(nc.sbuf_tensor(name, shape, dtype))
        lam = sb2("lam", [128, 16], F32)
        dtb = sb2("dtb", [128, 8], F32)
        t0 = sb2("t0", [128, 8], F32)
        t1 = sb2("t1", [128, 8], F32)
        t2 = sb2("t2", [128, 8], F32)
        t3 = sb2("t3", [128, 8], F32)
        pw_re = sb2("pw_re", [128, NPW + 1, 8], F32)
        pw_im = sb2("pw_im", [128, NPW + 1, 8], F32)
        pw_ims = sb2("pw_ims", [128, NPW + 1, 8], F32)
        bstk = sb2("bstk_sb", [128, 128], F32)
        cstk = sb2("cstk_sb", [128, 128], F32)
        dvec = sb2("dvec_sb", [128, 1], F32)
        bpad = sb2("bpad", [128, 128], BF16)
        cpad = sb2("cpad", [128, 128], BF16)
        Rb = sb2("Rb", [128, 10, 128], BF16)
        Rh = sb2("Rh", [128, NLEV, 128], F32)
        bbar = sb2("bbar", [128, 128], BF16)
        lhsA = sb2("lhsA", [128, 8, 128], BF16)
        CL0 = sb2("CL0", [128, 128], BF16)
        CLall = sb2("CLall", [128, 8, 8, 128], BF16)
        Wb = sb2("Wb", [128, 8, 128], BF16)
        Ea = sb2("Ea", [128, NCH], F32)
        Eb = sb2("Eb", [128, NCH], F32)
        Sin = sb2("Sin", [128, 8, NCH], BF16)
        yo = [sb2("yo0", [128, 4096], L["ODT"])]
        S.stream("ssm", 0)
        S.add("sp", lambda e: e.dma_start(out=lam[:], in_=lamp[:, :]), w=["lam"], dma="ssm")
        S.add("sp", lambda e: e.dma_start(out=dtb[:], in_=logdt[0:1, :].partition_broadcast(128)), w=["dtb"], dma="ssm")
        S.add("sp", lambda e: e.dma_start(out=bstk[:], in_=bstk_d[:, :]), w=["bstk"], dma="ssm")
        S.add("sp", lambda e: e.dma_start(out=cstk[:], in_=cstk_d[:, :]), w=["cstk"], dma="ssm")
        S.add("sp", lambda e: e.dma_start(out=dvec[:], in_=dvec_d[:, :]), w=["dvec"], dma="ssm")
        are, aim = lam[:, 0:8], lam[:, 8:16]

        def V(fn, r, w):
            S.add("dve", fn, r=r, w=w)

        def tt(out, a, b, op, r, w):
            V(lambda e: e.tensor_tensor(out=out, in0=a, in1=b, op=op), r, w)

        def ts(out, a, s1, s2, op0, op1, r, w):
            if op1 is None:
                V(lambda e: e.tensor_scalar(out=out, in0=a, scalar1=s1, scalar2=None, op0=op0), r, w)
            else:
                V(lambda e: e.tensor_scalar(out=out, in0=a, scalar1=s1, scalar2=s2, op0=op0, op1=op1), r, w)
        S.add("act", lambda e: e.activation(out=dtb[:], in_=dtb[:], func=AF.Exp), r=["dtb"], w=["dtb"])
        tt(t0[:], are, dtb[:], ALU.mult, ["lam", "dtb"], ["t0"])
        S.add("act", lambda e: e.activation(out=t0[:], in_=t0[:], func=AF.Exp), r=["t0"], w=["t0"])
        tt(t1[:], aim, dtb[:], ALU.mult, ["lam", "dtb"], ["t1"])
        MAGIC = 12582912.0
        ts(t1[:], t1[:], 1.0 / (2.0 * PI), None, ALU.mult, None, ["t1"], ["t1"])
        ts(t3[:], t1[:], MAGIC, None, ALU.add, None, ["t1"], ["t3"])
        ts(t3[:], t3[:], -MAGIC, None, ALU.add, None, ["t3"], ["t3"])
        tt(t2[:], t1[:], t3[:], ALU.subtract, ["t1", "t3"], ["t2"])
        S.add("act", lambda e: e.activation(out=t2[:], in_=t2[:], func=AF.Sin, scale=2.0 * PI), r=["t2"], w=["t2"])
        ts(t1[:], t1[:], 0.25, None, ALU.add, None, ["t1"], ["t1"])
        ts(t3[:], t1[:], MAGIC, None, ALU.add, None, ["t1"], ["t3"])
        ts(t3[:], t3[:], -MAGIC, None, ALU.add, None, ["t3"], ["t3"])
        tt(t3[:], t1[:], t3[:], ALU.subtract, ["t1", "t3"], ["t3"])
        S.add("act", lambda e: e.activation(out=t3[:], in_=t3[:], func=AF.Sin, scale=2.0 * PI), r=["t3"], w=["t3"])
        tt(pw_re[:, 1, :], t0[:], t3[:], ALU.mult, ["t0", "t3"], ["pw"])
        tt(pw_im[:, 1, :], t0[:], t2[:], ALU.mult, ["t0", "t2"], ["pw"])
        V(lambda e: e.memset(pw_re[:, 0, :], 1.0), [], ["pw"])
        V(lambda e: e.memset(pw_im[:, 0, :], 0.0), [], ["pw"])

        def cmul(o, a, b):
            tt(t0[:], pw_re[:, a, :], pw_re[:, b, :], ALU.mult, ["pw"], ["t0"])
            tt(t1[:], pw_im[:, a, :], pw_im[:, b, :], ALU.mult, ["pw"], ["t1"])
            tt(t2[:], pw_re[:, a, :], pw_im[:, b, :], ALU.mult, ["pw"], ["t2"])
            tt(t3[:], pw_im[:, a, :], pw_re[:, b, :], ALU.mult, ["pw"], ["t3"])
            tt(pw_re[:, o, :], t0[:], t1[:], ALU.subtract, ["t0", "t1"], ["pw"])
            tt(pw_im[:, o, :], t2[:], t3[:], ALU.add, ["t2", "t3"], ["pw"])
        for m in range(2, 9):
            cmul(m, m - 1, 1)
        for l in range(1, NLEV):
            cmul(8 + l, 8 + l - 1, 8 + l - 1)
        CO = NPW
        tt(t0[:], are, are, ALU.mult, ["lam"], ["t0"])
        tt(t1[:], aim, aim, ALU.mult, ["lam"], ["t1"])
        tt(t0[:], t0[:], t1[:], ALU.add, ["t0", "t1"], ["t0"])
        V(lambda e: e.reciprocal(out=t0[:], in_=t0[:]), ["t0"], ["t0"])
        ts(t1[:], pw_re[:, 1, :], -1.0, None, ALU.add, None, ["pw"], ["t1"])
        tt(t2[:], t1[:], are, ALU.mult, ["t1", "lam"], ["t2"])
        tt(t3[:], pw_im[:, 1, :], aim, ALU.mult, ["pw", "lam"], ["t3"])
        tt(t2[:], t2[:], t3[:], ALU.add, ["t2", "t3"], ["t2"])
        tt(pw_re[:, CO, :], t2[:], t0[:], ALU.mult, ["t2", "t0"], ["pw"])
        tt(t2[:], pw_im[:, 1, :], are, ALU.mult, ["pw", "lam"], ["t2"])
        tt(t3[:], t1[:], aim, ALU.mult, ["t1", "lam"], ["t3"])
        tt(t2[:], t2[:], t3[:], ALU.subtract, ["t2", "t3"], ["t2"])
        tt(pw_im[:, CO, :], t2[:], t0[:], ALU.mult, ["t2", "t0"], ["pw"])
        V(lambda e: e.tensor_scalar(out=pw_ims[:], in0=pw_im[:], scalar1=sgnR, scalar2=None, op0=ALU.mult), ["pw", "cst"], ["pws"])

        def build_R(out_ap, idx, g, key):
            V(lambda e: e.tensor_scalar(out=out_ap, in0=ident32, scalar1=pw_re[:, idx, g:g + 1], scalar2=None, op0=ALU.mult),
              ["pw", "cst"], [key])
            V(lambda e: e.scalar_tensor_tensor(out=out_ap, in0=Jm, scalar=pw_ims[:, idx, g:g + 1], in1=out_ap,
                                               op0=ALU.mult, op1=ALU.add), ["pws", "cst", key], [key])

        NT3 = [(c, min(342, NCH - c)) for c in range(0, NCH, 342)]
        evk = 0
        for g in range(8):
            for m in range(9):
                build_R(Rb[:, m, :], m, g, ("Rb", m))
            build_R(Rb[:, 9, :], CO, g, ("Rb", 9))
            for l in range(NLEV):
                build_R(Rh[:, l, :], 8 + l, g, ("Rh", l))
            tt(bpad[:], bstk[:], bmask[:, g * 128:(g + 1) * 128], ALU.mult, ["bstk", "cst"], ["bpad"])
            tt(cpad[:], cstk[:], bmask[:, g * 128:(g + 1) * 128], ALU.mult, ["cstk", "cst"], ["cpad"])
            S.add("pe", lambda e: e.matmul(pss[0][:, 0:128], Rb[:, 9, :], bpad[:], start=True, stop=True),
                  r=[("Rb", 9), "bpad"], w=[("ps", 0)])
            S.add("act", lambda e: e.copy(out=bbar[:], in_=pss[0][:, 0:128]), r=[("ps", 0)], w=["bbar"])
            for i in range(8):
                pk = 0 if ssm_merged else i % 2
                S.add("pe", lambda e, i=i, pk=pk: e.matmul(pss[pk][:, 128:256], bbar[:], Rb[:, 7 - i, :], start=True, stop=True),
                      r=["bbar", ("Rb", 7 - i)], w=[("ps", pk)])
                if i % 2 == 0:
                    S.add("act", lambda e, i=i, pk=pk: e.copy(out=lhsA[:, i, :], in_=pss[pk][:, 128:256]), r=[("ps", pk)], w=[("lhsA", i)])
                else:
                    S.add("dve", lambda e, i=i, pk=pk: e.tensor_copy(out=lhsA[:, i, :], in_=pss[pk][:, 128:256]), r=[("ps", pk)], w=[("lhsA", i)])
            for d in range(9):
                pk = 0 if ssm_merged else d % 2
                dst = CL0[:] if d == 0 else CLall[:, g, d - 1, :]
                S.add("pe", lambda e, d=d, pk=pk: e.matmul(pss[pk][:, 256:384], Rb[:, d, :], cpad[:], start=True, stop=True),
                      r=[("Rb", d), "cpad"], w=[("ps", pk)])
                S.add("dve", lambda e, dst=dst, pk=pk: e.tensor_scalar(out=dst, in0=pss[pk][:, 256:384], scalar1=sgnR, scalar2=None, op0=ALU.mult),
                      r=[("ps", pk), "cst"], w=[("CL", g, d)])
            for d in range(8):
                bank = 2 + d // 4
                col = (d % 4) * 128
                S.add("pe", lambda e, d=d, bank=bank, col=col, g=g: e.matmul(
                    pss[bank][:, col:col + 128], bbar[:], (CL0[:] if d == 0 else CLall[:, g, d - 1, :]),
                    start=(g == 0), stop=(g == 7)), r=["bbar", ("CL", g, d)], w=[("ps", bank)])
            for (c0, n) in NT3:
                pk = 4 + (evk % 2)
                evk += 1

                def mmA(e, c0=c0, n=n, pk=pk):
                    ins = None
                    for i in range(8):
                        ins = e.matmul(pss[pk][:, 0:n], lhsA[:, i, :], uT[:, c0 * 8 + i:(c0 + n) * 8:8], start=(i == 0), stop=(i == 7))
                    return ins
                S.add("pe", mmA, r=[("lhsA", i) for i in range(8)], w=[("ps", pk)])
                S.add("act", lambda e, c0=c0, n=n, pk=pk: e.copy(out=Ea[:, c0:c0 + n], in_=pss[pk][:, 0:n]), r=[("ps", pk)], w=["Ea"])
            src, dst_ = Ea, Eb
            sk, dk = "Ea", "Eb"
            for l in range(NLEV):
                s = 1 << l
                S.add("act", lambda e, src=src, dst_=dst_, s=s: e.copy(out=dst_[:, 0:s], in_=src[:, 0:s]), r=[sk], w=[dk])
                c = s
                while c < NCH:
                    n = min(512, NCH - c)
                    pk = 4 + (evk % 2)
                    evk += 1
                    S.add("pe", lambda e, l=l, c=c, n=n, s=s, pk=pk, src=src: e.matmul(
                        pss[pk][:, 0:n], Rh[:, l, :], src[:, c - s:c - s + n], start=True, stop=True), r=[("Rh", l), sk], w=[("ps", pk)])
                    S.add("dve", lambda e, c=c, n=n, pk=pk, src=src, dst_=dst_: e.tensor_tensor(
                        out=dst_[:, c:c + n], in0=pss[pk][:, 0:n], in1=src[:, c:c + n], op=ALU.add), r=[("ps", pk), sk], w=[dk])
                    c += n
                src, dst_ = dst_, src
                sk, dk = dk, sk
            S.add("dve", lambda e, g=g: e.memset(Sin[:, g, 0:1], 0.0), w=[("Sin", g)])
            S.add("act", lambda e, g=g, src=src: e.copy(out=Sin[:, g, 1:NCH], in_=src[:, 0:NCH - 1]), r=[sk], w=[("Sin", g)])
        for d in range(8):
            bank = 2 + d // 4
            col = (d % 4) * 128
            if d == 0:
                S.add("dve", lambda e, bank=bank, col=col: e.scalar_tensor_tensor(
                    out=Wb[:, 0, :], in0=ident32, scalar=dvec[:, 0:1], in1=pss[bank][:, col:col + 128], op0=ALU.mult, op1=ALU.add),
                    r=[("ps", bank), "dvec", "cst"], w=["Wb"])
            else:
                S.add("act", lambda e, d=d, bank=bank, col=col: e.copy(out=Wb[:, d, :], in_=pss[bank][:, col:col + 128]),
                      r=[("ps", bank)], w=["Wb"])
        kch = 2
        oi = 0
        while kch < NCH:
            n = min(512, NCH - kch)
            ob = 0
            for j in range(8):
                pk = 4 + (evk % 2)
                evk += 1

                def mmo(e, j=j, kch=kch, n=n, pk=pk):
                    ins = None
                    tot = (j + 1) + 8
                    q = 0
                    for i in range(j + 1):
                        ins = e.matmul(pss[pk][:, 0:n], Wb[:, j - i, :], uT[:, kch * 8 + i:(kch + n) * 8:8], start=(q == 0), stop=(q == tot - 1))
                        q += 1
                    for g in range(8):
                        ins = e.matmul(pss[pk][:, 0:n], CLall[:, g, j, :], Sin[:, g, kch:kch + n], start=(q == 0), stop=(q == tot - 1))
                        q += 1
                    return ins
                S.add("pe", mmo, r=["Wb"] + [("Sin", g) for g in range(8)] + [("CL", g, d) for g in range(8) for d in range(1, 9)],
                      w=[("ps", pk)])
                eng = "act" if j % 2 == 0 else "dve"
                if eng == "act":
                    S.add("act", lambda e, j=j, n=n, pk=pk, ob=ob: e.copy(out=yo[ob][:, j:n * 8:8], in_=pss[pk][:, 0:n]),
                          r=[("ps", pk)], w=[("yo", ob)])
                else:
                    S.add("dve", lambda e, j=j, n=n, pk=pk, ob=ob: e.tensor_copy(out=yo[ob][:, j:n * 8:8], in_=pss[pk][:, 0:n]),
                          r=[("ps", pk)], w=[("yo", ob)])
            t0_ = (kch - 2) * 8
            CH = NX // 4
            for cs in range(0, n * 8, min(CH, n * 8)):
                cn = min(CH, n * 8)
                S.add("sp", lambda e, ob=ob, t0_=t0_, cs=cs, cn=cn: e.dma_start(out=L["ydst"](1, 0, 128, t0_ + cs, cn), in_=yo[ob][:, cs:cs + cn]),
                      r=[("yo", ob)], w=[("ychs", (t0_ + cs) // CH)], dma="out_s")
            kch += n
            oi += 1
        if not ssm_merged:
            S.emit()


def _consts():
    c = np.zeros((128, 128 * 4 + 8 * 128 + 4), np.float32)
    c[:, 0:128] = np.eye(128, dtype=np.float32)
    J = np.zeros((128, 128), np.float32)
    for p in range(64):
        J[p, 64 + p] = 1.0
        J[64 + p, p] = 1.0
    c[:, 128:256] = J
    s = np.arange(128)[:, None]
    t = np.arange(128)[None, :]
    c[:, 256:384] = (s <= t).astype(np.float32)
    for g in range(8):
        c[:, 512 + g * 128 + g * 16: 512 + g * 128 + (g + 1) * 16] = 1.0
    c[0:64, 1536] = 1.0
    c[64:128, 1536] = -1.0
    return c


def prep_p1(inp, b, r, NX=SEQ):
    hs = [2 * r, 2 * r + 1]
    w_in = inp["w_in"][0]
    cols = []
    for h in hs:
        cols.append(w_in[:, Q_OFF + h * 64:Q_OFF + (h + 1) * 64])
    for h in hs:
        cols.append(w_in[:, K_OFF + h * 64:K_OFF + (h + 1) * 64])
    for h in hs:
        cols.append(w_in[:, V_OFF + h * 64:V_OFF + (h + 1) * 64])
    cols.append(w_in[:, U_OFF + r * 128:U_OFF + (r + 1) * 128])
    cols.append(w_in[:, F_OFF + 2 * r:F_OFF + 2 * r + 2])
    w1 = np.ascontiguousarray(np.concatenate(cols, axis=1), dtype=np.float32)
    lnp = np.concatenate([inp["ln_in_g"].reshape(8, 128).T, inp["ln_in_b"].reshape(8, 128).T], axis=1)
    gs = slice(8 * r, 8 * r + 8)
    a_re = inp["ssm_a_re"][0][gs]
    a_im = inp["ssm_a_im"][0][gs]
    lamp = np.concatenate([np.concatenate([a_re.T, a_re.T], axis=0), np.concatenate([a_im.T, a_im.T], axis=0)], axis=1)
    b_re = inp["ssm_b_re"][0][gs]
    b_im = inp["ssm_b_im"][0][gs]
    bstk = np.concatenate([b_re.transpose(1, 0, 2).reshape(64, 128), b_im.transpose(1, 0, 2).reshape(64, 128)], axis=0)
    c_re = inp["ssm_c_re"][0][gs]
    c_im = inp["ssm_c_im"][0][gs]
    cstk = np.concatenate([c_re.transpose(2, 0, 1).reshape(64, 128), c_im.transpose(2, 0, 1).reshape(64, 128)], axis=0)
    f32 = lambda a: np.ascontiguousarray(a, dtype=np.float32)
    return dict(x=f32(inp["x"][b][:NX]), meta=f32(inp["meta"]), lnp=f32(lnp), w1=w1,
                lnrow=f32(np.stack([inp["ln_in_g"], inp["ln_in_b"]])),
                bf=f32(inp["b_f"][0][2 * r:2 * r + 2].reshape(2, 1)), lamp=f32(lamp),
                logdt=f32(inp["ssm_log_dt"][0][gs].reshape(1, 8)), bstk=f32(bstk), bstks=f32(bstk), cstk=f32(cstk),
                dvec=f32(inp["ssm_d"][0][gs].reshape(128, 1)), cst=_consts())


def prep_p2_shared(inp):
    f32 = lambda a: np.ascontiguousarray(a, dtype=np.float32)
    w_in = inp["w_in"][0]
    w_ga = w_in[:, GA_OFF:GB_OFF]
    w_gb = w_in[:, GB_OFF:GB_OFF + 1024]
    wg = np.zeros((1024, 2048), np.float32)
    for j in range(4):
        for dcl in range(2):
            dc = 2 * j + dcl
            o = j * 512 + dcl * 256
            wg[:, o:o + 128] = w_ga[:, dc * 128:(dc + 1) * 128]
            wg[:, o + 128:o + 256] = w_gb[:, dc * 128:(dc + 1) * 128]
    lnp = np.stack([inp["ln_in_g"], inp["ln_in_b"], inp["ln1_g"][0], inp["ln1_b"][0], inp["ln2_g"][0], inp["ln2_b"][0]])
    bgu = np.zeros((128, 2, N_EXPERTS, 8), np.float32)
    bgu[:, 0] = inp["b_gate"][0].reshape(N_EXPERTS, 8, 128).transpose(2, 0, 1)
    bgu[:, 1] = inp["b_up"][0].reshape(N_EXPERTS, 8, 128).transpose(2, 0, 1)
    return dict(lnp=f32(lnp), wg=wg, wo=f32(inp["w_o"][0]), wua=f32(inp["w_up_a"][0]), wub=f32(inp["w_up_b"][0]),
                wglu=f32(inp["w_glu"][0]), bglu=f32(inp["b_glu"][0].reshape(4, 128).T), wr=f32(inp["w_router"][0]),
                br=f32(inp["b_router"][0].reshape(1, 32)), wgate=f32(inp["w_gate"][0]), wup=f32(inp["w_up"][0]),
                wdown=f32(inp["w_down"][0]), bgu=f32(bgu.reshape(128, -1)), bdown=f32(inp["b_down"][0]),
                ident=np.eye(128, dtype=np.float32))


_NC_CACHE = {}


def build_fused(NX=SEQ, NE=N_EXPERTS):
    nc = bass.Bass("TRN2", target_bir_lowering=False)
    with ExitStack() as top:
        S = Sched(nc, top)
        CH = NX // 4
        ybuf = [nc.dram_tensor(f"ybuf_in{k}", [256, CH], BF16) for k in range(4)]
        ygat = [nc.dram_tensor(f"ybuf_out{k}", [4 * 256, CH], BF16) for k in range(4)]
        build_p1(NX, ctx=dict(nc=nc, S=S, ybuf=[t.ap() for t in ybuf], ybuf_t=ybuf, ygat_t=ygat))
        build_p2(NX // 4, NE, ctx=dict(nc=nc, S=S, ygat=[t.ap() for t in ygat]))
    return nc


def make_maps(inp, NX=SEQ, NE=N_EXPERTS):
    shared = prep_p2_shared(inp)
    shared["lnp2"] = shared.pop("lnp")
    for k in ("wgate", "wup", "wdown"):
        shared[k] = shared[k][:NE]
    shared["bgu"] = np.ascontiguousarray(shared["bgu"].reshape(128, 2, N_EXPERTS, 8)[:, :, :NE].reshape(128, -1))
    NTOK = NX // 4
    maps = []
    for c in range(8):
        b, r = c // 4, c % 4
        m = dict(shared)
        m.update(prep_p1(inp, b, r, NX))
        m.pop("bstks")
        m["x2"] = np.ascontiguousarray(inp["x"][b, r * NTOK:(r + 1) * NTOK], dtype=np.float32)
        oh = np.zeros((128, 4), np.float32)
        oh[:, r] = 1.0
        m["onehot"] = oh
        maps.append(m)
    return maps


def kernel(**inputs):
    inp = {k: np.asarray(v) for k, v in inputs.items()}
    B = inp["x"].shape[0]
    if "f" not in _NC_CACHE:
        _NC_CACHE["f"] = build_fused()
    maps = make_maps(inp)
    if False:
        m = None
    res = run_bass_kernel_spmd(_NC_CACHE["f"], maps, core_ids=list(range(8)))
    out = np.zeros((B, SEQ, D_MODEL), np.float32)
    for c in range(8):
        b, r = c // 4, c % 4
        out[b, r * 2048:(r + 1) * 2048] = res.results[c]["out"]
    return out
```
